# Optimizing a Trainium2 kernel written in Bass

```python
import math
import jax, jax.numpy as jnp
from jax import lax
import numpy as np

D_MODEL = 1024
BATCH = 32
SEQ = 2048
DEPTH = 2

N_META = 16
DN_ALPHA = (2 * DEPTH) ** 0.25
DN_BETA = (8 * DEPTH) ** -0.25

A_HEADS = 4
A_HEAD_DIM = D_MODEL // 8
A_OUT = A_HEADS * A_HEAD_DIM
A_KV_RANK = D_MODEL // 4
IDX_HEADS = 8
IDX_DIM = D_MODEL // 16
IDX_TOPK_CAP = 256
Q_BLOCK = 128

RG_WIDTH = D_MODEL // 2
RG_BLOCKS = 8
RG_C = 8.0
CONV_WIDTH = 4

GDN_K_HEADS = 8
GDN_V_HEADS = 16
GDN_HEAD_DIM = D_MODEL // 8
GDN_QK_WIDTH = GDN_K_HEADS * GDN_HEAD_DIM
GDN_V_WIDTH = GDN_V_HEADS * GDN_HEAD_DIM
GDN_CONV_CH = 2 * GDN_QK_WIDTH + GDN_V_WIDTH
CHUNK = 64

MOE_GROUPS = 4
MOE_PER_GROUP = 8
N_EXPERTS = MOE_GROUPS * MOE_PER_GROUP
MOE_TOPK = 2
EXPERT_FF = D_MODEL // 2
MOE_BLOCK = 256

EVEN_PARTS = (A_OUT, A_KV_RANK, IDX_HEADS * IDX_DIM, IDX_DIM, IDX_HEADS, RG_WIDTH, RG_WIDTH)
EVEN_IN_WIDTH = sum(EVEN_PARTS)
EVEN_MIX_WIDTH = A_OUT + RG_WIDTH
ODD_PARTS = (GDN_CONV_CH, GDN_V_WIDTH, GDN_V_HEADS, GDN_V_HEADS)
ODD_IN_WIDTH = sum(ODD_PARTS)

kernel_name = 'hybrid_dsa_rglru_gdn_hiermoe_deepnorm'

F32 = jnp.float32


def _offsets(parts):
    out, s = [], 0
    for p in parts[:-1]:
        s += p
        out.append(s)
    return out


def layer_norm(x, g, b, eps=1e-5):
    xf = x.astype(F32)
    mu = jnp.mean(xf, -1, keepdims=True)
    var = jnp.mean(jnp.square(xf - mu), -1, keepdims=True)
    return ((xf - mu) * lax.rsqrt(var + eps) * g.astype(F32) + b.astype(F32)).astype(x.dtype)


def rms_norm(x, g, eps=1e-6):
    xf = x.astype(F32)
    y = xf * lax.rsqrt(jnp.mean(jnp.square(xf), -1, keepdims=True) + eps)
    return (y * g.astype(F32)).astype(x.dtype)


def l2_normalize(x, eps=1e-6):
    xf = x.astype(F32)
    return xf * lax.rsqrt(jnp.sum(jnp.square(xf), -1, keepdims=True) + eps)


def causal_depthwise_conv(x, w):
    c = x.shape[-1]
    return lax.conv_general_dilated(
        x, w[:, None, :].astype(x.dtype), window_strides=(1,),
        padding=[(w.shape[0] - 1, 0)], dimension_numbers=('NWC', 'WIO', 'NWC'),
        feature_group_count=c)


def dsa_attention(q, k, v, q_idx, k_idx, w_idx):
    B, T, H, dh = q.shape
    n_sel = min(IDX_TOPK_CAP, T // 4)
    nb = -(-T // Q_BLOCK)
    tp = nb * Q_BLOCK

    def to_blocks(a):
        a = jnp.pad(a, [(0, 0), (0, tp - T)] + [(0, 0)] * (a.ndim - 2))
        return a.reshape(B, nb, Q_BLOCK, *a.shape[2:]).swapaxes(0, 1)

    q_pos = jnp.arange(tp, dtype=jnp.int32).reshape(nb, Q_BLOCK)
    key_pos = jnp.arange(T, dtype=jnp.int32)
    k_idx_f = k_idx.astype(F32)

    def block(args):
        qb, qib, wb, pb = args
        s = jnp.einsum('bqhd,bsd->bqhs', qib.astype(F32), k_idx_f) * IDX_DIM ** -0.5
        score = jnp.einsum('bqh,bqhs->bqs', wb.astype(F32), jax.nn.relu(s))
        visible = key_pos[None, :] <= pb[:, None]
        score = jnp.where(visible[None], score, -jnp.inf)
        _, sel = lax.top_k(score, n_sel)
        k_sel = jax.vmap(lambda kk, ii: kk[ii])(k, sel)
        v_sel = jax.vmap(lambda vv, ii: vv[ii])(v, sel)
        logits = jnp.einsum('bqhd,bqkd->bqhk', qb, k_sel).astype(F32) * dh ** -0.5
        valid = sel <= pb[None, :, None]
        logits = jnp.where(valid[:, :, None, :], logits, -jnp.inf)
        p = jax.nn.softmax(logits, axis=-1).astype(v.dtype)
        return jnp.einsum('bqhk,bqkd->bqhd', p, v_sel)

    out = lax.map(block, (to_blocks(q), to_blocks(q_idx), to_blocks(w_idx), q_pos))
    return out.swapaxes(0, 1).reshape(B, tp, H, dh)[:, :T]


def rg_lru(x, w_a, b_a, w_x, b_x, lam):
    B, T, R = x.shape
    xb = x.reshape(B, T, RG_BLOCKS, R // RG_BLOCKS)
    r = jax.nn.sigmoid(jnp.einsum('btni,nij->btnj', xb, w_a).reshape(B, T, R) + b_a)
    i = jax.nn.sigmoid(jnp.einsum('btni,nij->btnj', xb, w_x).reshape(B, T, R) + b_x)
    log_a = -RG_C * r.astype(F32) * jax.nn.softplus(-lam.astype(F32))
    a = jnp.exp(log_a)
    u = jnp.sqrt(-jnp.expm1(2.0 * log_a)) * (i * x).astype(F32)

    def combine(c1, c2):
        a1, b1 = c1
        a2, b2 = c2
        return a1 * a2, a2 * b1 + b2

    _, h = lax.associative_scan(combine, (a, u), axis=1)
    return h.astype(x.dtype)


def chunked_gated_delta_rule(q, k, v, g, beta):
    B, T, H, dk = q.shape
    dv = v.shape[-1]
    n = T // CHUNK

    def blocks(a):
        a = a.astype(F32).reshape(B, n, CHUNK, H, *a.shape[3:])
        return jnp.moveaxis(a, 3, 1)

    q = blocks(q) * dk ** -0.5
    k, v, g, beta = blocks(k), blocks(v), blocks(g), blocks(beta)
    gc = jnp.cumsum(g, axis=-1)
    causal = jnp.tril(jnp.ones((CHUNK, CHUNK), bool))
    eye = jnp.eye(CHUNK, dtype=F32)
    decay = jnp.exp(jnp.where(causal, gc[..., :, None] - gc[..., None, :], -jnp.inf))
    kb = k * beta[..., None]
    m = jnp.tril(jnp.einsum('bhnid,bhnjd->bhnij', kb, k) * decay, -1)
    rhs = jnp.concatenate([v * beta[..., None], kb * jnp.exp(gc)[..., None]], axis=-1)
    sol = lax.linalg.triangular_solve(m + eye, rhs, left_side=True, lower=True)
    u, w = sol[..., :dv], sol[..., dv:]
    attn = jnp.einsum('bhnid,bhnjd->bhnij', q, k) * decay

    def step(S, xs):
        q_c, k_c, u_c, w_c, g_c, a_c = xs
        v_new = u_c - jnp.einsum('bhck,bhkv->bhcv', w_c, S)
        o = (jnp.einsum('bhck,bhkv->bhcv', q_c * jnp.exp(g_c)[..., None], S)
             + jnp.einsum('bhij,bhjv->bhiv', a_c, v_new))
        g_last = g_c[..., -1:]
        S = (S * jnp.exp(g_last)[..., None]
             + jnp.einsum('bhck,bhcv->bhkv', k_c * jnp.exp(g_last - g_c)[..., None], v_new))
        return S, o

    xs = tuple(jnp.moveaxis(a, 2, 0) for a in (q, k, u, w, gc, attn))
    S0 = jnp.zeros((B, H, dk, dv), F32)
    _, o = lax.scan(step, S0, xs)
    o = jnp.moveaxis(o, 0, 2).reshape(B, H, T, dv)
    return jnp.swapaxes(o, 1, 2)


def even_mixer(x, w_in, kv_norm, w_uk, w_uv, conv_w, conv_b, rg_wa, rg_ba, rg_wx, rg_bx, rg_lambda, w_out):
    B, T, _ = x.shape
    q, c_kv, q_idx, k_idx, w_idx, gate_b, x_b = jnp.split(x @ w_in, _offsets(EVEN_PARTS), axis=-1)
    latent = rms_norm(c_kv, kv_norm)
    k = latent @ w_uk
    v = latent @ w_uv
    attn = dsa_attention(q.reshape(B, T, A_HEADS, A_HEAD_DIM), k, v,
                         q_idx.reshape(B, T, IDX_HEADS, IDX_DIM), k_idx, w_idx * IDX_HEADS ** -0.5)
    xr = causal_depthwise_conv(x_b, conv_w) + conv_b
    rec = rg_lru(xr, rg_wa, rg_ba, rg_wx, rg_bx, rg_lambda) * jax.nn.gelu(gate_b)
    mixed = jnp.concatenate([attn.reshape(B, T, A_OUT), rec], axis=-1)
    return mixed @ w_out


def odd_mixer(x, w_in, conv_w, a_log, dt_bias, o_norm, w_out):
    B, T, _ = x.shape
    qkv, z, a, b = jnp.split(x @ w_in, _offsets(ODD_PARTS), axis=-1)
    qkv = jax.nn.silu(causal_depthwise_conv(qkv, conv_w))
    q, k, v = jnp.split(qkv, [GDN_QK_WIDTH, 2 * GDN_QK_WIDTH], axis=-1)
    rep = GDN_V_HEADS // GDN_K_HEADS
    q = jnp.repeat(l2_normalize(q.reshape(B, T, GDN_K_HEADS, GDN_HEAD_DIM)), rep, axis=2)
    k = jnp.repeat(l2_normalize(k.reshape(B, T, GDN_K_HEADS, GDN_HEAD_DIM)), rep, axis=2)
    v = v.reshape(B, T, GDN_V_HEADS, GDN_HEAD_DIM)
    g = -jnp.exp(a_log.astype(F32)) * jax.nn.softplus(a.astype(F32) + dt_bias.astype(F32))
    beta = jax.nn.sigmoid(b.astype(F32))
    front = (-N_META) % CHUNK
    back = (-(T + front)) % CHUNK

    def padt(t):
        return jnp.pad(t, [(0, 0), (front, back)] + [(0, 0)] * (t.ndim - 2))

    o = chunked_gated_delta_rule(padt(q), padt(k), padt(v), padt(g), padt(beta))[:, front:front + T]
    o = rms_norm(o.astype(x.dtype), o_norm) * jax.nn.silu(z.reshape(B, T, GDN_V_HEADS, GDN_HEAD_DIM))
    return o.reshape(B, T, GDN_V_WIDTH) @ w_out


def routed_expert_ffn(xf, expert_idx, gates, w_gate, w_up, w_down):
    N, D = xf.shape
    A = N * MOE_TOPK
    flat_e = expert_idx.reshape(A).astype(jnp.int32)
    order = jnp.argsort(flat_e)
    sorted_e = flat_e[order]
    counts = jnp.bincount(flat_e, length=N_EXPERTS)
    padded = (counts + MOE_BLOCK - 1) // MOE_BLOCK * MOE_BLOCK
    start = jnp.cumsum(counts) - counts
    pend = jnp.cumsum(padded)
    pstart = pend - padded
    slot = pstart[sorted_e] + jnp.arange(A, dtype=jnp.int32) - start[sorted_e]
    n_blocks = -(-A // MOE_BLOCK) + N_EXPERTS
    P = n_blocks * MOE_BLOCK
    src = jnp.full((P,), N, jnp.int32).at[slot].set((order // MOE_TOPK).astype(jnp.int32))
    x_pad = jnp.concatenate([xf, jnp.zeros((1, D), xf.dtype)], axis=0)
    xs = x_pad[src].reshape(n_blocks, MOE_BLOCK, D)
    block_start = jnp.arange(n_blocks, dtype=jnp.int32) * MOE_BLOCK
    block_expert = jnp.minimum(jnp.searchsorted(pend, block_start, side='right'), N_EXPERTS - 1)

    def expert_block(args):
        xb, e = args
        hid = jax.nn.silu(xb @ w_gate[e]) * (xb @ w_up[e])
        return hid @ w_down[e]

    ys = lax.map(expert_block, (xs, block_expert)).reshape(P, D)
    y_assign = jnp.zeros((A, D), ys.dtype).at[order].set(ys[slot])
    return jnp.einsum('nk,nkd->nd', gates.astype(ys.dtype), y_assign.reshape(N, MOE_TOPK, D))


def hierarchical_moe(x, group_w, group_b, expert_w, expert_b, w_gate, w_up, w_down):
    B, T, D = x.shape
    xf = x.reshape(B * T, D)
    n = xf.shape[0]
    grp_logits = (xf @ group_w + group_b).astype(F32)
    grp = jnp.argmax(grp_logits, axis=-1)
    grp_gate = jnp.take_along_axis(jax.nn.softmax(grp_logits, axis=-1), grp[:, None], axis=1)
    exp_logits = (xf @ expert_w + expert_b).astype(F32).reshape(n, MOE_GROUPS, MOE_PER_GROUP)
    exp_logits = jnp.take_along_axis(exp_logits, grp[:, None, None], axis=1)[:, 0]
    top_val, top_local = lax.top_k(exp_logits, MOE_TOPK)
    gates = jax.nn.softmax(top_val, axis=-1) * grp_gate
    expert_idx = grp[:, None].astype(jnp.int32) * MOE_PER_GROUP + top_local
    y = routed_expert_ffn(xf, expert_idx, gates, w_gate, w_up, w_down)
    return y.reshape(B, T, D)


def setup_inputs(seed: int = 0) -> dict:
    key = jax.random.key(seed)
    ks = iter(jax.random.split(key, 40))
    NE = (DEPTH + 1) // 2
    NO = DEPTH // 2

    def nrm(shape, scale):
        return jax.random.normal(next(ks), shape, F32) * scale

    def unif(shape, lo, hi):
        return jax.random.uniform(next(ks), shape, F32, lo, hi)

    rg_a = unif((NE, RG_WIDTH), 0.9, 0.999) ** (1.0 / RG_C)
    dt = jnp.exp(unif((NO, GDN_V_HEADS), math.log(1e-3), math.log(1e-1)))
    bs = RG_WIDTH // RG_BLOCKS
    return {
        'x': nrm((BATCH, SEQ, D_MODEL), 1.0),
        'meta_tokens': nrm((N_META, D_MODEL), 1.0),
        'even_w_in': nrm((NE, D_MODEL, EVEN_IN_WIDTH), D_MODEL ** -0.5),
        'even_kv_norm': 1.0 + nrm((NE, A_KV_RANK), 0.02),
        'even_w_uk': nrm((NE, A_KV_RANK, A_HEAD_DIM), A_KV_RANK ** -0.5),
        'even_w_uv': nrm((NE, A_KV_RANK, A_HEAD_DIM), A_KV_RANK ** -0.5),
        'even_conv_w': nrm((NE, CONV_WIDTH, RG_WIDTH), CONV_WIDTH ** -0.5),
        'even_conv_b': nrm((NE, RG_WIDTH), 0.02),
        'even_rg_wa': nrm((NE, RG_BLOCKS, bs, bs), bs ** -0.5),
        'even_rg_ba': nrm((NE, RG_WIDTH), 0.02),
        'even_rg_wx': nrm((NE, RG_BLOCKS, bs, bs), bs ** -0.5),
        'even_rg_bx': nrm((NE, RG_WIDTH), 0.02),
        'even_rg_lambda': jnp.log(rg_a) - jnp.log1p(-rg_a),
        'even_w_out': nrm((NE, EVEN_MIX_WIDTH, D_MODEL), EVEN_MIX_WIDTH ** -0.5 * DN_BETA),
        'odd_w_in': nrm((NO, D_MODEL, ODD_IN_WIDTH), D_MODEL ** -0.5),
        'odd_conv_w': nrm((NO, CONV_WIDTH, GDN_CONV_CH), CONV_WIDTH ** -0.5),
        'odd_a_log': jnp.log(unif((NO, GDN_V_HEADS), 1.0, 16.0)),
        'odd_dt_bias': dt + jnp.log(-jnp.expm1(-dt)),
        'odd_o_norm': 1.0 + nrm((NO, GDN_HEAD_DIM), 0.02),
        'odd_w_out': nrm((NO, GDN_V_WIDTH, D_MODEL), GDN_V_WIDTH ** -0.5 * DN_BETA),
        'ln_g': 1.0 + nrm((DEPTH, 2, D_MODEL), 0.02),
        'ln_b': nrm((DEPTH, 2, D_MODEL), 0.02),
        'moe_group_w': nrm((DEPTH, D_MODEL, MOE_GROUPS), D_MODEL ** -0.5),
        'moe_group_b': nrm((DEPTH, MOE_GROUPS), 0.01),
        'moe_expert_w': nrm((DEPTH, D_MODEL, N_EXPERTS), D_MODEL ** -0.5),
        'moe_expert_b': nrm((DEPTH, N_EXPERTS), 0.01),
        'moe_w_gate': nrm((DEPTH, N_EXPERTS, D_MODEL, EXPERT_FF), D_MODEL ** -0.5),
        'moe_w_up': nrm((DEPTH, N_EXPERTS, D_MODEL, EXPERT_FF), D_MODEL ** -0.5),
        'moe_w_down': nrm((DEPTH, N_EXPERTS, EXPERT_FF, D_MODEL), EXPERT_FF ** -0.5 * DN_BETA),
    }


def reference(x, meta_tokens, even_w_in, even_kv_norm, even_w_uk, even_w_uv, even_conv_w, even_conv_b,
              even_rg_wa, even_rg_ba, even_rg_wx, even_rg_bx, even_rg_lambda, even_w_out,
              odd_w_in, odd_conv_w, odd_a_log, odd_dt_bias, odd_o_norm, odd_w_out,
              ln_g, ln_b, moe_group_w, moe_group_b, moe_expert_w, moe_expert_b,
              moe_w_gate, moe_w_up, moe_w_down):
    B = x.shape[0]
    meta = jnp.broadcast_to(meta_tokens.astype(x.dtype)[None], (B, N_META, D_MODEL))
    h = jnp.concatenate([meta, x], axis=1)
    for layer in range(DEPTH):
        i = layer // 2
        if layer % 2 == 0:
            mix = even_mixer(h, even_w_in[i], even_kv_norm[i], even_w_uk[i], even_w_uv[i],
                             even_conv_w[i], even_conv_b[i], even_rg_wa[i], even_rg_ba[i],
                             even_rg_wx[i], even_rg_bx[i], even_rg_lambda[i], even_w_out[i])
        else:
            mix = odd_mixer(h, odd_w_in[i], odd_conv_w[i], odd_a_log[i], odd_dt_bias[i],
                            odd_o_norm[i], odd_w_out[i])
        h = layer_norm(DN_ALPHA * h + mix, ln_g[layer, 0], ln_b[layer, 0])
        ffn = hierarchical_moe(h, moe_group_w[layer], moe_group_b[layer], moe_expert_w[layer],
                               moe_expert_b[layer], moe_w_gate[layer], moe_w_up[layer], moe_w_down[layer])
        h = layer_norm(DN_ALPHA * h + ffn, ln_g[layer, 1], ln_b[layer, 1])
    return h[:, N_META:]
```

```python
import contextlib
import numpy as np
import concourse.bass as bass
import concourse.mybir as mybir
from concourse.bass_utils import run_bass_kernel_spmd

F32 = mybir.dt.float32
BF16 = mybir.dt.bfloat16
I32 = mybir.dt.int32
AF = mybir.ActivationFunctionType
ALU = mybir.AluOpType
AX = mybir.AxisListType

NCORES = 8
D = 1024
SEQ = 2048
NMETA = 16
T = SEQ + NMETA
TP = 17 * 128
NT = TP // 128
SPC = 4
NTOK = SPC * TP
NTILE = NTOK // 128
ALPHA = 4.0 ** 0.25
NEG = -1.0e30


class Res:
    __slots__ = ("w", "r", "dsem")

    def __init__(self):
        self.w = None
        self.r = {}
        self.dsem = None


class KB:
    def __init__(self, nc):
        self.nc = nc
        self.es = contextlib.ExitStack()
        self.stack = [self.es]
        self.eng = {"pe": nc.tensor, "act": nc.scalar, "dve": nc.vector, "pool": nc.gpsimd, "sp": nc.sync}
        self.sems = {}
        self.cnt = {}
        for k in self.eng:
            self.sems[k] = self.es.enter_context(nc.semaphore("sem_" + k))
            self.cnt[k] = 0
        self.seen = {k: {} for k in self.eng}
        self.res = {}
        self.dcount = {}
        self.free_dsems = []
        self.scope_res = [[]]
        self.nd = 0
        self.n_inst = 0
        self.uid = 0
        self.psum_names = set()

    def sb(self, name, shape, dtype=F32):
        self.uid += 1
        return self.stack[-1].enter_context(self.nc.sbuf_tensor("%s_%d" % (name, self.uid), list(shape), dtype))

    def ps(self, name, shape, dtype=F32):
        self.uid += 1
        nm = "%s_%d" % (name, self.uid)
        self.psum_names.add(nm)
        return self.stack[-1].enter_context(self.nc.psum_tensor(nm, list(shape), dtype))

    def dram(self, name, shape, dtype=F32, kind="Internal"):
        return self.nc.dram_tensor(name, list(shape), dtype, kind=kind).ap()

    @contextlib.contextmanager
    def scope(self):
        st = contextlib.ExitStack()
        self.stack.append(st)
        self.scope_res.append([])
        try:
            yield
        finally:
            self.barrier()
            for key in self.scope_res.pop():
                r = self.res.pop(key, None)
                if r is not None and r.dsem is not None:
                    self.free_dsems.append(r.dsem)
            self.stack.pop()
            st.close()

    def _res(self, ap):
        key = ap if isinstance(ap, str) else ap.name
        r = self.res.get(key)
        if r is None:
            r = self.res[key] = Res()
            self.scope_res[-1].append(key)
        return r

    def _wait(self, e, semkey, val):
        if semkey in self.dcount:
            val = max(val, self.dcount[semkey])
        if self.seen[e].get(semkey, 0) >= val:
            return
        if semkey == e and val > self.cnt[e]:
            return
        self.seen[e][semkey] = val
        self.eng[e].wait_ge(self.sems[semkey], val)

    def _deps(self, e, reads, writes):
        for a in reads:
            r = self._res(a)
            if r.w is not None:
                self._wait(e, *r.w)
        for a in writes:
            r = self._res(a)
            if r.w is not None:
                self._wait(e, *r.w)
            for sk, v in r.r.items():
                self._wait(e, sk, v)

    def _done(self, ev, reads, writes):
        for a in reads:
            r = self._res(a)
            if r.r.get(ev[0], 0) < ev[1]:
                r.r[ev[0]] = ev[1]
        for a in writes:
            r = self._res(a)
            r.w = ev
            r.r = {}

    def I(self, e, fn, reads, writes, *args, inc=True, **kw):
        writes = list(writes) + [a for a in reads if (not isinstance(a, str)) and a.name in self.psum_names]
        self._deps(e, reads, writes)
        ins = getattr(self.eng[e], fn)(*args, **kw)
        if inc:
            self.cnt[e] += 1
            ins.then_inc(self.sems[e], 1)
            self._done((e, self.cnt[e]), reads, writes)
        else:
            self._done((e, self.cnt[e] + 1), reads, writes)
        self.n_inst += 1
        return ins

    def dma(self, q, out, in_, reads=None, writes=None, group=None, indirect=None, **kw):
        reads = [in_] if reads is None else reads
        writes = [out] if writes is None else writes
        dst = self._res(group if group is not None else writes[0])
        if dst.dsem is None:
            if self.free_dsems:
                dst.dsem = self.free_dsems.pop()
            else:
                self.nd += 1
                dst.dsem = "d%d" % self.nd
                self.sems[dst.dsem] = self.es.enter_context(self.nc.semaphore(dst.dsem))
                self.dcount[dst.dsem] = 0
        if group is None:
            self._deps(q, reads, writes)
        else:
            self._deps(q, reads, [])
            for a in writes:
                r = self._res(a)
                if r.w is not None and r.w[0] != dst.dsem:
                    self._wait(q, *r.w)
                for sk, v in r.r.items():
                    self._wait(q, sk, v)
        if indirect is None:
            ins = self.eng[q].dma_start(out=out, in_=in_, **kw)
        else:
            ins = self.eng[q].indirect_dma_start(out=out, in_=in_, **indirect)
        self.dcount[dst.dsem] += 16
        ins.then_inc(self.sems[dst.dsem], 16)
        self._done((dst.dsem, self.dcount[dst.dsem]), reads, writes)
        self.n_inst += 1
        return ins

    def copy(self, e, reads, writes, out, in_):
        return self.I(e, "copy" if e == "act" else "tensor_copy", reads, writes, out=out, in_=in_)

    def barrier(self):
        for e in self.eng:
            for e2 in ("pe", "act", "dve", "pool"):
                if self.cnt[e2]:
                    self._wait(e, e2, self.cnt[e2])
            for sk, c in self.dcount.items():
                if c:
                    self._wait(e, sk, c)

    def finish(self):
        self.barrier()
        self.es.close()


def load_consts(kb, cd):
    c = {}
    for name, shape, dt in (("ident", [128, 128], F32), ("identb", [128, 128], BF16),
                            ("ones", [128, 128], F32), ("sut", [128, 128], F32),
                            ("ramp", [128, 128], F32), ("pidx", [128, 128], F32), ("cmask", [128, 128], F32),
                            ("onesb", [128, 128], BF16)):
        t = kb.sb("c_" + name, shape, dt)
        kb.dma("sp", t[:], cd[name])
        c[name] = t
    return c


def layer_norm_tile(kb, acc, g_bc, b_bc, outt, st6, mv, eng2="pool"):
    for j in range(2):
        kb.I("dve", "bn_stats", [acc], [st6], out=st6[:, j, :], in_=acc[:, j * 512:(j + 1) * 512])
    kb.I("dve", "bn_aggr", [st6], [mv], out=mv[:, 0:2], in_=st6[:].rearrange("p a b -> p (a b)"))
    kb.I("dve", "tensor_scalar", [mv], [mv], out=mv[:, 2:3], in0=mv[:, 1:2], scalar1=1e-5, scalar2=None, op0=ALU.add)
    kb.I("act", "sqrt", [mv], [mv], out=mv[:, 2:3], in_=mv[:, 2:3])
    kb.I("dve", "reciprocal", [mv], [mv], out=mv[:, 2:3], in_=mv[:, 2:3])
    kb.I("dve", "tensor_scalar", [acc, mv], [acc], out=acc[:], in0=acc[:], scalar1=mv[:, 0:1], scalar2=mv[:, 2:3],
         op0=ALU.subtract, op1=ALU.mult)
    kb.I(eng2, "tensor_tensor", [acc, g_bc], [acc], out=acc[:], in0=acc[:], in1=g_bc[:], op=ALU.mult)
    kb.I(eng2, "tensor_tensor", [acc, b_bc], [outt], out=outt[:], in0=acc[:], in1=b_bc[:], op=ALU.add)


NBLK = NTOK * 2 // 256 + 32
NSLOT = NBLK * 256


def build_moe(kb, c, h_d, wr_d, br_d, wg_d, wu_d, wd_d, lng_d, lnb_d, out_d, xs_d, ys_d, ntile=NTILE):
    nc = kb.nc
    nblk = ntile * 128 * 2 // 256 + 32
    with kb.scope():
        s1i = kb.sb("s1i", [128, ntile], I32)
        s2i = kb.sb("s2i", [128, ntile], I32)
        g1 = kb.sb("g1", [128, ntile])
        g2 = kb.sb("g2", [128, ntile])
        idxe = kb.sb("idxe", [128, nblk], I32)
        g_bc = kb.sb("g_bc", [128, D])
        b_bc = kb.sb("b_bc", [128, D])
        kb.dma("sp", g_bc[:], lng_d.partition_broadcast(128))
        kb.dma("sp", b_bc[:], lnb_d.partition_broadcast(128))
        hts = [kb.sb("ht%d" % i, [128, D]) for i in range(2)]

        with kb.scope():
            wr = kb.sb("wr", [128, 8, 36])
            kb.dma("sp", wr[:], wr_d.rearrange("(k p) e -> p k e", p=128))
            br = kb.sb("br", [128, 36])
            kb.dma("sp", br[:], br_d.partition_broadcast(128))
            L = kb.sb("L", [128, ntile, 36])
            hT = [kb.sb("hT%d" % i, [128, 8, 128]) for i in range(2)]
            tp = [kb.ps("tp%d" % i, [128, 8, 128]) for i in range(2)]
            lg = [kb.ps("lg%d" % i, [128, 36]) for i in range(2)]
            for t in range(ntile):
                ht = hts[t % 2]
                kb.dma("sp", ht[:], h_d[t * 128:(t + 1) * 128, :])
                tpp = tp[t % 2]
                for k in range(8):
                    kb.I("pe", "transpose", [ht, c["ident"]], [tpp], out=tpp[:, k, :], in_=ht[:, k * 128:(k + 1) * 128],
                         identity=c["ident"][:], inc=(k == 7))
                hTt = hT[t % 2]
                kb.I("act", "copy", [tpp], [hTt], out=hTt[:], in_=tpp[:])
                lgp = lg[t % 2]
                for k in range(8):
                    kb.I("pe", "matmul", [hTt, wr], [lgp], lgp[:], lhsT=hTt[:, k, :], rhs=wr[:, k, :],
                         start=(k == 0), stop=(k == 7), inc=(k == 7))
                kb.I("dve", "tensor_tensor", [lgp, br], [L], out=L[:, t, :], in0=lgp[:], in1=br[:], op=ALU.add)

            NE = ntile * 32
            GL = L[:, :, 0:4]
            EL = L[:, :, 4:36]
            gmax = kb.sb("gmax", [128, ntile])
            t4 = kb.sb("t4", [128, ntile, 4])
            goh = kb.sb("goh", [128, ntile, 4])
            gg = kb.sb("gg", [128, ntile])
            ELm = kb.sb("ELm", [128, ntile, 32])
            EL2 = kb.sb("EL2", [128, ntile, 32])
            oh1 = kb.sb("oh1", [128, ntile, 32])
            oh2 = kb.sb("oh2", [128, ntile, 32])
            m1 = kb.sb("m1", [128, ntile])
            m2 = kb.sb("m2", [128, ntile])
            tmp = kb.sb("tmp", [128, ntile])
            V = "dve"
            kb.I(V, "tensor_reduce", [L], [gmax], out=gmax[:], in_=GL, axis=AX.X, op=ALU.max)
            bc4 = lambda a: a[:].unsqueeze(2).to_broadcast([128, ntile, 4])
            bc32 = lambda a: a[:].unsqueeze(2).to_broadcast([128, ntile, 32])
            kb.I(V, "tensor_tensor", [L, gmax], [goh], out=goh[:], in0=GL, in1=bc4(gmax), op=ALU.is_equal)
            kb.I(V, "tensor_tensor", [L, gmax], [t4], out=t4[:], in0=GL, in1=bc4(gmax), op=ALU.subtract)
            kb.I("act", "activation", [t4], [t4], out=t4[:], in_=t4[:], func=AF.Exp)
            kb.I(V, "tensor_reduce", [t4], [gg], out=gg[:], in_=t4[:], axis=AX.X, op=ALU.add)
            kb.I(V, "reciprocal", [gg], [gg], out=gg[:], in_=gg[:])
            kb.I(V, "tensor_scalar", [goh], [t4], out=t4[:], in0=goh[:], scalar1=-NEG, scalar2=NEG,
                 op0=ALU.mult, op1=ALU.add)
            kb.I(V, "tensor_tensor", [L, t4], [ELm], out=ELm[:].rearrange("p t (g e) -> p t g e", g=4),
                 in0=EL.rearrange("p t (g e) -> p t g e", g=4),
                 in1=t4[:].unsqueeze(3).to_broadcast([128, ntile, 4, 8]), op=ALU.add)
            kb.I(V, "tensor_reduce", [ELm], [m1], out=m1[:], in_=ELm[:], axis=AX.X, op=ALU.max)
            kb.I(V, "tensor_tensor", [ELm, m1], [oh1], out=oh1[:], in0=ELm[:], in1=bc32(m1), op=ALU.is_equal)
            kb.I(V, "scalar_tensor_tensor", [oh1, ELm], [EL2], out=EL2[:], in0=oh1[:], scalar=NEG, in1=ELm[:],
                 op0=ALU.mult, op1=ALU.add)
            kb.I(V, "tensor_reduce", [EL2], [m2], out=m2[:], in_=EL2[:], axis=AX.X, op=ALU.max)
            kb.I(V, "tensor_tensor", [EL2, m2], [oh2], out=oh2[:], in0=EL2[:], in1=bc32(m2), op=ALU.is_equal)
            kb.I(V, "tensor_tensor", [m1, m2], [tmp], out=tmp[:], in0=m2[:], in1=m1[:], op=ALU.subtract)
            kb.I("act", "activation", [tmp], [tmp], out=tmp[:], in_=tmp[:], func=AF.Exp)
            kb.I(V, "tensor_scalar", [tmp], [m1], out=m1[:], in0=tmp[:], scalar1=1.0, scalar2=None, op0=ALU.add)
            kb.I(V, "reciprocal", [m1], [m1], out=m1[:], in_=m1[:])
            kb.I(V, "tensor_tensor", [tmp, m1], [m2], out=m2[:], in0=tmp[:], in1=m1[:], op=ALU.mult)
            kb.I(V, "tensor_tensor", [m1, gg], [g1], out=g1[:], in0=m1[:], in1=gg[:], op=ALU.mult)
            kb.I(V, "tensor_tensor", [m2, gg], [g2], out=g2[:], in0=m2[:], in1=gg[:], op=ALU.mult)
            OH = ELm
            kb.I(V, "tensor_tensor", [oh1, oh2], [OH], out=OH[:], in0=oh1[:], in1=oh2[:], op=ALU.add)
            RK = kb.sb("RK", [128, NE])
            CT = kb.sb("CT", [128, ntile, 32])
            OHf = OH[:].rearrange("p t e -> p (t e)")
            CTf = CT[:].rearrange("p t e -> p (t e)")
            pr = [kb.ps("pr%d" % i, [128, 512]) for i in range(2)]
            ci = 0
            for dst, lhs in ((RK[:], c["sut"]), (CTf, c["ones"])):
                for o in range(0, NE, 512):
                    n = min(512, NE - o)
                    p = pr[ci % 2]
                    ci += 1
                    kb.I("pe", "matmul", [lhs, OH], [p], p[:, 0:n], lhsT=lhs[:], rhs=OHf[:, o:o + n],
                         start=True, stop=True)
                    kb.I("act", "copy", [p], [RK if dst is not CTf else CT], out=dst[:, o:o + n], in_=p[:, 0:n])
            base = kb.sb("base", [128, ntile, 32])
            kb.I(V, "memset", [], [base], base[:, 0, :], 0.0)
            for t in range(1, ntile):
                kb.I(V, "tensor_tensor", [base, CT], [base], out=base[:, t, :], in0=base[:, t - 1, :],
                     in1=CT[:, t - 1, :], op=ALU.add)
            tot = kb.sb("tot", [128, 32])
            pad = kb.sb("pad", [128, 32])
            pend = kb.sb("pend", [128, 32])
            one32 = kb.sb("one32", [128, 32])
            kb.I(V, "memset", [], [one32], one32[:], 1.0)
            kb.I(V, "tensor_tensor", [base, CT], [tot], out=tot[:], in0=base[:, ntile - 1, :], in1=CT[:, ntile - 1, :],
                 op=ALU.add)
            thr = kb.sb("thr", [128, nblk])
            kb.I(V, "tensor_scalar", [c["ramp"]], [thr], out=thr[:], in0=c["ramp"][:, 0:nblk], scalar1=256.0,
                 scalar2=None, op0=ALU.mult)
            cmp0 = kb.sb("cmp0", [128, 32, nblk])
            kb.I(V, "tensor_tensor", [tot, thr], [cmp0], out=cmp0[:],
                 in0=tot[:].unsqueeze(2).to_broadcast([128, 32, nblk]),
                 in1=thr[:].unsqueeze(1).to_broadcast([128, 32, nblk]), op=ALU.is_gt)
            kb.I(V, "tensor_reduce", [cmp0], [pad], out=pad[:], in_=cmp0[:], axis=AX.X, op=ALU.add)
            kb.I(V, "tensor_scalar", [pad], [pad], out=pad[:], in0=pad[:], scalar1=256.0, scalar2=None, op0=ALU.mult)
            kb.I(V, "tensor_tensor_scan", [one32, pad], [pend], out=pend[:], data0=one32[:], data1=pad[:], initial=0.0,
                 op0=ALU.mult, op1=ALU.add)
            kb.I(V, "tensor_tensor", [pend, pad], [pad], out=pad[:], in0=pend[:], in1=pad[:], op=ALU.subtract)
            cmp = kb.sb("cmp", [128, nblk, 32])
            bef = kb.sb("bef", [128, nblk])
            kb.I(V, "tensor_scalar", [c["ramp"]], [bef], out=bef[:], in0=c["ramp"][:, 0:nblk], scalar1=256.0,
                 scalar2=None, op0=ALU.mult)
            kb.I(V, "tensor_tensor", [pend, bef], [cmp], out=cmp[:],
                 in0=pend[:].unsqueeze(1).to_broadcast([128, nblk, 32]),
                 in1=bef[:].unsqueeze(2).to_broadcast([128, nblk, 32]), op=ALU.is_le)
            kb.I(V, "tensor_reduce", [cmp], [bef], out=bef[:], in_=cmp[:], axis=AX.X, op=ALU.add)
            kb.I(V, "tensor_scalar", [bef], [bef], out=bef[:], in0=bef[:], scalar1=31.0, scalar2=None, op0=ALU.min)
            ig = kb.sb("ig", [128, nblk])
            kb.I(V, "tensor_scalar", [bef, c["pidx"]], [ig], out=ig[:], in0=bef[:], scalar1=128.0, scalar2=c["pidx"][:, 0:1],
                 op0=ALU.mult, op1=ALU.add)
            usedf = kb.sb("usedf", [128, nblk])
            kb.I(V, "tensor_scalar", [thr, pend], [usedf], out=usedf[:], in0=thr[:], scalar1=pend[:, 31:32], scalar2=None, op0=ALU.is_lt)
            kb.I(V, "tensor_tensor", [ig, usedf], [ig], out=ig[:], in0=ig[:], in1=usedf[:], op=ALU.mult)
            kb.I(V, "tensor_scalar", [usedf], [usedf], out=usedf[:], in0=usedf[:], scalar1=-8192.0, scalar2=8192.0, op0=ALU.mult, op1=ALU.add)
            kb.I(V, "tensor_tensor", [ig, usedf], [ig], out=ig[:], in0=ig[:], in1=usedf[:], op=ALU.add)
            kb.I(V, "tensor_copy", [ig], [idxe], out=idxe[:], in_=ig[:])
            SL = EL2
            RK3 = RK[:].rearrange("p (t e) -> p t e", e=32)
            kb.I(V, "tensor_tensor", [RK, base], [SL], out=SL[:], in0=RK3, in1=base[:], op=ALU.add)
            kb.I(V, "tensor_tensor", [SL, pad], [SL], out=SL[:], in0=SL[:],
                 in1=pad[:].unsqueeze(1).to_broadcast([128, ntile, 32]), op=ALU.add)
            for oh, si in ((oh1, s1i), (oh2, s2i)):
                kb.I(V, "tensor_tensor", [SL, oh], [oh], out=oh[:], in0=SL[:], in1=oh[:], op=ALU.mult)
                kb.I(V, "tensor_reduce", [oh], [tmp], out=tmp[:], in_=oh[:], axis=AX.X, op=ALU.add)
                kb.I(V, "tensor_copy", [tmp], [si], out=si[:], in_=tmp[:])

        with kb.scope():
            hbs = [kb.sb("hb%d" % i, [128, D], BF16) for i in range(2)]
            for t in range(ntile):
                ht = hts[t % 2]
                hb = hbs[t % 2]
                kb.dma("sp", ht[:], h_d[t * 128:(t + 1) * 128, :])
                kb.I("act", "copy", [ht], [hb], out=hb[:], in_=ht[:])
                for si in (s1i, s2i):
                    kb.dma("pool", xs_d, hb[:], reads=[hb, si], writes=[xs_d], indirect=dict(
                        out_offset=bass.IndirectOffsetOnAxis(ap=si[:, t:t + 1], axis=0), in_offset=None))

        with kb.scope():
            xin = [[kb.sb("xin%d_%d" % (i, s), [128, D], BF16) for s in range(2)] for i in range(2)]
            xT = [kb.sb("xT%d" % i, [128, 8, 256], BF16) for i in range(2)]
            tps = [kb.ps("tps%d" % i, [128, 8, 128], BF16) for i in range(2)]
            stg = [kb.sb("stg%d" % i, [128, 4096]) for i in range(3)]
            wgb = [kb.sb("wgb%d" % i, [128, 8, 512], BF16) for i in range(2)]
            wub = [kb.sb("wub%d" % i, [128, 8, 512], BF16) for i in range(2)]
            wdb = [kb.sb("wdb%d" % i, [128, 4, 1024], BF16) for i in range(2)]
            hgp = [kb.ps("hgp%d" % i, [128, 256]) for i in range(2)]
            hup = [kb.ps("hup%d" % i, [128, 256]) for i in range(2)]
            yp = [kb.ps("yp%d" % i, [128, 512]) for i in range(2)]
            sg = [kb.sb("sg%d" % i, [128, 256]) for i in range(2)]
            hTb = [kb.sb("hTb%d" % i, [128, 4, 256], BF16) for i in range(2)]
            yb = [kb.sb("yb%d" % i, [128, D]) for i in range(2)]
            sti = 0
            cast_eng = ["dve", "act"]
            bc_reg = nc.gpsimd.alloc_register("moe_bc_%d" % kb.uid)
            nc.gpsimd.reg_mov(bc_reg, 4095)

            def load_weights(b):
                nonlocal sti
                par = b % 2
                for src, dstt in ((wg_d, wgb[par]), (wu_d, wub[par]), (wd_d, wdb[par])):
                    st = stg[sti % 3]
                    kb.dma("pool", st[:], src, reads=[idxe], writes=[st],
                           indirect=dict(out_offset=None, in_offset=bass.IndirectOffsetOnAxis(ap=idxe[:, b:b + 1], axis=0),
                                         bounds_check=bc_reg, oob_is_err=False))
                    dflat = dstt[:].rearrange("p a b -> p (a b)")
                    for half in range(2):
                        ce = cast_eng[(2 * sti + half) % 2]
                        kb.copy(ce, [st], [dstt], dflat[:, half * 2048:(half + 1) * 2048], st[:, half * 2048:(half + 1) * 2048])
                    sti += 1

            def load_tokens(b):
                par = b % 2
                for s in range(2):
                    xi = xin[par][s]
                    kb.dma("act", xi[:], xs_d[b * 256 + s * 128: b * 256 + (s + 1) * 128, :])
                    tpp = tps[s]
                    for k in range(8):
                        kb.I("pe", "transpose", [xi, c["identb"]], [tpp], out=tpp[:, k, :], in_=xi[:, k * 128:(k + 1) * 128],
                             identity=c["identb"][:], inc=(k == 7))
                    kb.I("dve", "tensor_copy", [tpp], [xT[par]], out=xT[par][:, :, s * 128:(s + 1) * 128], in_=tpp[:])

            def gate_up(b):
                par = b % 2
                for f in range(4):
                    hg = hgp[f % 2]
                    hu = hup[f % 2]
                    for k in range(8):
                        kb.I("pe", "matmul", [wgb[par], xT[par]], [hg], hg[:], lhsT=wgb[par][:, k, f * 128:(f + 1) * 128],
                             rhs=xT[par][:, k, :], start=(k == 0), stop=(k == 7), inc=(k == 7))
                    for k in range(8):
                        kb.I("pe", "matmul", [wub[par], xT[par]], [hu], hu[:], lhsT=wub[par][:, k, f * 128:(f + 1) * 128],
                             rhs=xT[par][:, k, :], start=(k == 0), stop=(k == 7), inc=(k == 7))
                    sgt = sg[f % 2]
                    kb.I("act", "activation", [hg], [sgt], out=sgt[:], in_=hg[:], func=AF.Silu)
                    kb.I("dve", "tensor_tensor", [sgt, hu], [hTb[par]], out=hTb[par][:, f, :], in0=sgt[:], in1=hu[:], op=ALU.mult)

            def down(b):
                par = b % 2
                for s in range(2):
                    ybt = yb[s]
                    for dh in range(2):
                        y = yp[dh]
                        for f in range(4):
                            kb.I("pe", "matmul", [hTb[par], wdb[par]], [y], y[:], lhsT=hTb[par][:, f, s * 128:(s + 1) * 128],
                                 rhs=wdb[par][:, f, dh * 512:(dh + 1) * 512], start=(f == 0), stop=(f == 3), inc=(f == 3))
                        kb.I("act" if dh == 0 else "dve", "tensor_copy" if dh else "copy", [y], [ybt],
                             out=ybt[:, dh * 512:(dh + 1) * 512], in_=y[:])
                    kb.dma("sp", ys_d[b * 256 + s * 128: b * 256 + (s + 1) * 128, :], ybt[:])

            load_weights(0)
            load_tokens(0)
            for b in range(nblk):
                if b + 1 < nblk:
                    load_weights(b + 1)
                gate_up(b)
                if b + 1 < nblk:
                    load_tokens(b + 1)
                down(b)

        with kb.scope():
            NS = 3
            y1 = [kb.sb("y1_%d" % i, [128, D]) for i in range(NS)]
            y2 = [kb.sb("y2_%d" % i, [128, D]) for i in range(NS)]
            hts5 = [kb.sb("ht5_%d" % i, [128, D]) for i in range(NS)]
            acc = [kb.sb("acc%d" % i, [128, D]) for i in range(2)]
            outt = [kb.sb("outt%d" % i, [128, D]) for i in range(2)]
            st6 = kb.sb("st6", [128, 2, 6])
            mv = kb.sb("mv", [128, 4])

            def issue(t):
                p = t % NS
                kb.dma("sp", hts5[p][:], h_d[t * 128:(t + 1) * 128, :])
                for yy, si in ((y1[p], s1i), (y2[p], s2i)):
                    kb.dma("pool", yy[:], ys_d, reads=[ys_d, si], writes=[yy], indirect=dict(
                        out_offset=None, in_offset=bass.IndirectOffsetOnAxis(ap=si[:, t:t + 1], axis=0)))
            for t in range(min(NS - 1, ntile)):
                issue(t)
            for t in range(ntile):
                if t + NS - 1 < ntile:
                    issue(t + NS - 1)
                p = t % NS
                ht = hts5[p]
                a = acc[t % 2]
                kb.I("dve", "tensor_scalar", [y1[p], g1], [a], out=a[:], in0=y1[p][:], scalar1=g1[:, t:t + 1], scalar2=None,
                     op0=ALU.mult)
                kb.I("dve", "scalar_tensor_tensor", [y2[p], g2, a], [a], out=a[:], in0=y2[p][:], scalar=g2[:, t:t + 1],
                     in1=a[:], op0=ALU.mult, op1=ALU.add)
                kb.I("dve", "scalar_tensor_tensor", [ht, a], [a], out=a[:], in0=ht[:], scalar=ALPHA, in1=a[:],
                     op0=ALU.mult, op1=ALU.add)
                layer_norm_tile(kb, a, g_bc, b_bc, outt[t % 2], st6, mv)
                kb.dma("sp", out_d[t * 128:(t + 1) * 128, :], outt[t % 2][:])

def make_consts():
    import ml_dtypes
    i = np.arange(128)
    return {
        "ident": np.eye(128, dtype=np.float32),
        "identb": np.eye(128, dtype=np.float32).astype(ml_dtypes.bfloat16),
        "ones": np.ones((128, 128), np.float32),
        "sut": (i[:, None] < i[None, :]).astype(np.float32),
        "ramp": np.broadcast_to(i[None, :].astype(np.float32), (128, 128)).copy(),
        "pidx": np.broadcast_to(i[:, None].astype(np.float32), (128, 128)).copy(),
        "cmask": np.where(i[None, :] > i[:, None], NEG, 0.0).astype(np.float32),
        "onesb": np.ones((128, 128), np.float32).astype(ml_dtypes.bfloat16),
    }


CHUNKS = [(0, 512), (512, 512), (1024, 512), (1536, 512), (2048, 128)]
EV_Q, EV_CKV, EV_QI, EV_KI, EV_WI, EV_GATE, EV_XB = 0, 512, 768, 1280, 1344, 1352, 1864


def proj_fm(kb, ps2, cnt, wb, col0, hT, dst_fn, evac):
    for (t0, n) in CHUNKS:
        p = ps2[cnt[0] % 2]
        for k in range(8):
            kb.I("pe", "matmul", [wb, hT], [p], p[:, 0:n], lhsT=wb[:, k, col0:col0 + 128], rhs=hT[:, k, t0:t0 + n],
                 start=(k == 0), stop=(k == 7), inc=(k == 7))
        evac(cnt[0], p[:, 0:n], t0, n)
        cnt[0] += 1


def build_even(kb, c, h_d, win_d, wout_d, wuk_d, wuv_d, rgw_d, vec_d, lng_d, lnb_d, out_d, nseq=SPC):
    with kb.scope():
        winb = kb.sb("winb", [128, 8, 2376], BF16)
        wk2 = kb.sb("wk2", [128, 8, 128], BF16)
        wukb = kb.sb("wukb", [128, 2, 128], BF16)
        wuvb = kb.sb("wuvb", [128, 2, 128], BF16)
        rgwb = kb.sb("rgwb", [128, 8, 128], BF16)
        vec = kb.sb("vec", [128, 40])
        cc = kb.sb("cc", [128, 4])
        g_bc = kb.sb("g_bc", [128, D])
        b_bc = kb.sb("b_bc", [128, D])
        kb.dma("sp", g_bc[:], lng_d.partition_broadcast(128))
        kb.dma("sp", b_bc[:], lnb_d.partition_broadcast(128))
        kb.dma("sp", vec[:], vec_d)
        with kb.scope():
            stg = [kb.sb("wstg%d" % i, [128, 2376]) for i in range(2)]
            for k in range(8):
                st = stg[k % 2]
                kb.dma("sp", st[:], win_d[k * 128:(k + 1) * 128, :])
                kb.copy("act" if k % 2 else "pool", [st], [winb], winb[:, k, :], st[:])
            for j in range(2):
                kb.I("dve", "tensor_copy", [winb], [wk2], out=wk2[:, :, j * 64:(j + 1) * 64], in_=winb[:, :, EV_KI:EV_KI + 64])
            st = stg[0]
            kb.dma("sp", st[:, 0:256], wuk_d.rearrange("(c p) d -> p c d", p=128))
            kb.I("dve", "tensor_copy", [st], [wukb], out=wukb[:], in_=st[:, 0:256].rearrange("p (c d) -> p c d", c=2))
            st = stg[1]
            kb.dma("sp", st[:, 0:256], wuv_d.rearrange("(c p) d -> p c d", p=128))
            kb.I("dve", "tensor_copy", [st], [wuvb], out=wuvb[:], in_=st[:, 0:256].rearrange("p (c d) -> p c d", c=2))
            st = stg[0]
            kb.dma("sp", st[:, 0:1024], rgw_d)
            kb.I("dve", "tensor_copy", [st], [rgwb], out=rgwb[:], in_=st[:, 0:1024].rearrange("p (c d) -> p c d", c=8))
            kb.I("act", "activation", [vec], [cc], out=cc[:], in_=vec[:, 28:32], func=AF.Exp, scale=-1.0)
            kb.I("act", "activation", [cc], [cc], out=cc[:], in_=cc[:], func=AF.Ln, bias=1.0)
            kb.I("dve", "tensor_scalar", [cc], [cc], out=cc[:], in0=cc[:], scalar1=-8.0, scalar2=None, op0=ALU.mult)
        cw = lambda j, ch: vec[:, j * 4 + ch: j * 4 + ch + 1]
        cb = lambda ch: vec[:, 16 + ch:17 + ch]
        ba = lambda ch: vec[:, 20 + ch:21 + ch]
        bx = lambda ch: vec[:, 24 + ch:25 + ch]
        kvn = lambda ch: vec[:, 32 + ch:33 + ch]

        for s in range(nseq):
            r0 = s * TP
            with kb.scope():
                hT = kb.sb("hT", [128, 8, TP], BF16)
                qT = kb.sb("qT", [128, 4, TP], BF16)
                qiT = kb.sb("qiT", [128, 4, TP], BF16)
                kiT2 = kb.sb("kiT2", [128, TP], BF16)
                kT = kb.sb("kT", [128, TP], BF16)
                vtok = kb.sb("vtok", [128, NT, 130], BF16)
                wq = kb.sb("wq", [128, NT, 8])
                kb.I("pool", "memset", [], [vtok], vtok[:, :, 128:130], 1.0)
                with kb.scope():
                    gateT = kb.sb("gateT", [128, 4, TP], BF16)
                    xbT = kb.sb("xbT", [128, 4, TP], BF16)
                    with kb.scope():
                        hts = [kb.sb("eht%d" % i, [128, D]) for i in range(2)]
                        tp = [kb.ps("etp%d" % i, [128, 8, 128]) for i in range(2)]
                        for t in range(NT):
                            ht = hts[t % 2]
                            kb.dma("sp", ht[:], h_d[r0 + t * 128: r0 + (t + 1) * 128, :])
                            tpp = tp[t % 2]
                            for k in range(8):
                                kb.I("pe", "transpose", [ht, c["ident"]], [tpp], out=tpp[:, k, :],
                                     in_=ht[:, k * 128:(k + 1) * 128], identity=c["ident"][:], inc=(k == 7))
                            kb.copy("act" if t % 2 else "dve", [tpp], [hT], hT[:, :, t * 128:(t + 1) * 128], tpp[:])
                    with kb.scope():
                        ckvT = kb.sb("ckvT", [128, 2, TP], BF16)
                        latT = kb.sb("latT", [128, 2, TP], BF16)
                        ps2 = [kb.ps("pp%d" % i, [128, 512]) for i in range(2)]
                        cnt = [0]

                        def mk_evac(dst, ci):
                            def ev(n_, p, t0, n):
                                kb.copy("act" if n_ % 2 else "dve", [p], [dst], dst[:, ci, t0:t0 + n] if ci is not None else dst[:, t0:t0 + n], p)
                            return ev
                        for ci in range(4):
                            proj_fm(kb, ps2, cnt, winb, EV_Q + ci * 128, hT, None, mk_evac(qT, ci))
                            proj_fm(kb, ps2, cnt, winb, EV_QI + ci * 128, hT, None, mk_evac(qiT, ci))
                            proj_fm(kb, ps2, cnt, winb, EV_GATE + ci * 128, hT, None, mk_evac(gateT, ci))
                            proj_fm(kb, ps2, cnt, winb, EV_XB + ci * 128, hT, None, mk_evac(xbT, ci))
                        for ci in range(2):
                            proj_fm(kb, ps2, cnt, winb, EV_CKV + ci * 128, hT, None, mk_evac(ckvT, ci))
                        proj_fm(kb, ps2, cnt, wk2, 0, hT, None, mk_evac(kiT2, None))
                        wps = [kb.ps("wps%d" % i, [128, 8]) for i in range(2)]
                        for t in range(NT):
                            p = wps[t % 2]
                            for k in range(8):
                                kb.I("pe", "matmul", [hT, winb], [p], p[:], lhsT=hT[:, k, t * 128:(t + 1) * 128],
                                     rhs=winb[:, k, EV_WI:EV_WI + 8], start=(k == 0), stop=(k == 7), inc=(k == 7))
                            kb.copy("act", [p], [wq], wq[:, t, :], p[:])
                        sq = kb.sb("sq", [128, 2, 512], BF16)
                        rstd = kb.sb("rstd", [128, 512])
                        for (t0, n) in CHUNKS:
                            kb.I("act", "activation", [ckvT], [sq], out=sq[:, :, 0:n], in_=ckvT[:, :, t0:t0 + n], func=AF.Square)
                            p = ps2[cnt[0] % 2]
                            cnt[0] += 1
                            for ci in range(2):
                                kb.I("pe", "matmul", [c["onesb"], sq], [p], p[:, 0:n], lhsT=c["onesb"][:], rhs=sq[:, ci, 0:n],
                                     start=(ci == 0), stop=(ci == 1), inc=(ci == 1))
                            kb.I("dve", "tensor_scalar", [p], [rstd], out=rstd[:, 0:n], in0=p[:, 0:n], scalar1=1.0 / 256, scalar2=1e-6,
                                 op0=ALU.mult, op1=ALU.add)
                            kb.I("act", "sqrt", [rstd], [rstd], out=rstd[:, 0:n], in_=rstd[:, 0:n])
                            kb.I("dve", "reciprocal", [rstd], [rstd], out=rstd[:, 0:n], in_=rstd[:, 0:n])
                            for ci in range(2):
                                kb.I("dve", "scalar_tensor_tensor", [ckvT, vec, rstd], [latT], out=latT[:, ci, t0:t0 + n],
                                     in0=ckvT[:, ci, t0:t0 + n], scalar=kvn(ci), in1=rstd[:, 0:n], op0=ALU.mult, op1=ALU.mult)
                            p = ps2[cnt[0] % 2]
                            cnt[0] += 1
                            for ci in range(2):
                                kb.I("pe", "matmul", [wukb, latT], [p], p[:, 0:n], lhsT=wukb[:, ci, :], rhs=latT[:, ci, t0:t0 + n],
                                     start=(ci == 0), stop=(ci == 1), inc=(ci == 1))
                            kb.copy("act", [p], [kT], kT[:, t0:t0 + n], p[:, 0:n])
                        for t in range(NT):
                            p = ps2[cnt[0] % 2]
                            cnt[0] += 1
                            for ci in range(2):
                                kb.I("pe", "matmul", [latT, wuvb], [p], p[:, 0:128], lhsT=latT[:, ci, t * 128:(t + 1) * 128],
                                     rhs=wuvb[:, ci, :], start=(ci == 0), stop=(ci == 1), inc=(ci == 1))
                            kb.copy("dve", [p], [vtok], vtok[:, t, 0:128], p[:, 0:128])
                    mixT = hT
                    with kb.scope():
                        HS = TP // 2
                        F = lambda nm: kb.sb(nm, [128, HS])
                        xr, rr, ii, aa, uu, hh, gl = F("xr"), F("rr"), F("ii"), F("aa"), F("uu"), F("hh"), F("gl")
                        xrb = kb.sb("xrb", [128, HS], BF16)
                        carry = kb.sb("carry", [128, 4])
                        gps = [kb.ps("gps%d" % i, [128, 512]) for i in range(4)]
                        gi = 0
                        for ch in range(4):
                            for hf in range(2):
                                o = hf * HS
                                x = xbT[:, ch, :]
                                kb.I("dve", "tensor_scalar", [xbT, vec], [xr], out=xr[:], in0=x[:, o:o + HS], scalar1=cw(3, ch), scalar2=cb(ch),
                                     op0=ALU.mult, op1=ALU.add)
                                for d in (1, 2, 3):
                                    lo = d if hf == 0 else 0
                                    kb.I("dve", "scalar_tensor_tensor", [xbT, vec, xr], [xr], out=xr[:, lo:HS], in0=x[:, o + lo - d:o + HS - d],
                                         scalar=cw(3 - d, ch), in1=xr[:, lo:HS], op0=ALU.mult, op1=ALU.add)
                                kb.copy("pool", [xr], [xrb], xrb[:], xr[:])
                                for (t0, n) in ((0, 512), (512, 512), (1024, 64)):
                                    pa = gps[gi % 4]
                                    px = gps[(gi + 1) % 4]
                                    gi += 2
                                    kb.I("pe", "matmul", [rgwb, xrb], [pa], pa[:, 0:n], lhsT=rgwb[:, ch, :], rhs=xrb[:, t0:t0 + n], start=True, stop=True)
                                    kb.I("pe", "matmul", [rgwb, xrb], [px], px[:, 0:n], lhsT=rgwb[:, 4 + ch, :], rhs=xrb[:, t0:t0 + n], start=True, stop=True)
                                    kb.I("act", "activation", [pa, vec], [rr], out=rr[:, t0:t0 + n], in_=pa[:, 0:n], func=AF.Sigmoid, bias=ba(ch))
                                    kb.I("act", "activation", [px, vec], [ii], out=ii[:, t0:t0 + n], in_=px[:, 0:n], func=AF.Sigmoid, bias=bx(ch))
                                kb.I("act", "activation", [rr, cc], [aa], out=aa[:], in_=rr[:], func=AF.Exp, scale=cc[:, ch:ch + 1])
                                kb.I("pool", "tensor_tensor", [aa], [uu], out=uu[:], in0=aa[:], in1=aa[:], op=ALU.mult)
                                kb.I("act", "activation", [uu], [uu], out=uu[:], in_=uu[:], func=AF.Sqrt, scale=-1.0, bias=1.0)
                                kb.I("pool", "tensor_tensor", [ii, xr], [ii], out=ii[:], in0=ii[:], in1=xr[:], op=ALU.mult)
                                kb.I("pool", "tensor_tensor", [uu, ii], [uu], out=uu[:], in0=uu[:], in1=ii[:], op=ALU.mult)
                                kb.I("dve", "tensor_tensor_scan", [aa, uu, carry], [hh], out=hh[:], data0=aa[:], data1=uu[:],
                                     initial=(0.0 if hf == 0 else carry[:, ch:ch + 1]), op0=ALU.mult, op1=ALU.add)
                                if hf == 0:
                                    kb.I("dve", "tensor_copy", [hh], [carry], out=carry[:, ch:ch + 1], in_=hh[:, HS - 1:HS])
                                g = gateT[:, ch, o:o + HS]
                                kb.I("act", "activation", [gateT], [gl], out=gl[:], in_=g, func=AF.Square)
                                kb.I("pool", "tensor_scalar", [gl], [gl], out=gl[:], in0=gl[:], scalar1=0.044715, scalar2=1.0, op0=ALU.mult, op1=ALU.add)
                                kb.I("pool", "tensor_tensor", [gl, gateT], [gl], out=gl[:], in0=gl[:], in1=g, op=ALU.mult)
                                kb.I("act", "activation", [gl], [gl], out=gl[:], in_=gl[:], func=AF.Sigmoid, scale=1.5957691216)
                                kb.I("pool", "tensor_tensor", [gl, gateT], [gl], out=gl[:], in0=gl[:], in1=g, op=ALU.mult)
                                kb.I("dve", "tensor_tensor", [hh, gl], [mixT], out=mixT[:, 4 + ch, o:o + HS], in0=hh[:], in1=gl[:], op=ALU.mult)
                with kb.scope():
                    score = [kb.sb("score%d" % i, [128, TP]) for i in range(2)]
                    mask = [kb.sb("mask%d" % i, [128, TP], BF16) for i in range(2)]
                    m8 = [kb.sb("m8_%d" % i, [128, 8]) for i in range(2)]
                    work = kb.sb("work", [128, TP])
                    thrc = kb.sb("thrc", [128, 1])
                    rl = [kb.sb("rl%d" % i, [128, 512]) for i in range(2)]
                    lgt = [kb.sb("lgt%d" % i, [128, TP]) for i in range(2)]
                    ee = [kb.sb("ee%d" % i, [128, TP], BF16) for i in range(2)]
                    pT = [kb.sb("pT%d" % i, [128, NT, 128], BF16) for i in range(2)]
                    sm = [kb.sb("sm%d" % i, [128, 4]) for i in range(2)]
                    on = [kb.sb("on%d" % i, [128, 128], BF16) for i in range(2)]
                    sps = [kb.ps("sps%d" % i, [128, 512]) for i in range(2)]
                    lps = [kb.ps("lps%d" % i, [128, 512]) for i in range(2)]
                    tps = [kb.ps("atp%d" % i, [128, 8, 128], BF16) for i in range(2)]
                    ops = kb.ps("ops", [128, 132])
                    otp = kb.ps("otp", [128, 128], BF16)
                    kb.I("dve", "memset", [], [thrc], thrc[:], -1.0e29)
                    cnts = {"si": 0, "ti": 0, "li": 0}

                    def indexer(i):
                        sc = score[i % 2]
                        nk = (i + 1) * 128
                        qs = slice(i * 128, (i + 1) * 128)
                        for t0 in range(0, nk, 512):
                            n = min(512, nk - t0)
                            for h in range(8):
                                p = sps[cnts["si"] % 2]
                                r = rl[cnts["si"] % 2]
                                cnts["si"] += 1
                                pr = slice((h % 2) * 64, (h % 2) * 64 + 64)
                                kb.I("pe", "matmul", [qiT, kiT2], [p], p[:, 0:n], lhsT=qiT[pr, h // 2, qs], rhs=kiT2[pr, t0:t0 + n],
                                     start=True, stop=True)
                                kb.I("act", "activation", [p], [r], out=r[:, 0:n], in_=p[:, 0:n], func=AF.Relu)
                                if h == 0:
                                    kb.I("dve", "tensor_scalar", [r, wq], [sc], out=sc[:, t0:t0 + n], in0=r[:, 0:n],
                                         scalar1=wq[:, i, 0:1], scalar2=None, op0=ALU.mult)
                                else:
                                    kb.I("dve", "scalar_tensor_tensor", [r, wq, sc], [sc], out=sc[:, t0:t0 + n], in0=r[:, 0:n],
                                         scalar=wq[:, i, h:h + 1], in1=sc[:, t0:t0 + n], op0=ALU.mult, op1=ALU.add)
                        kb.I("dve", "tensor_tensor", [sc, c["cmask"]], [sc], out=sc[:, qs], in0=sc[:, qs], in1=c["cmask"][:], op=ALU.add)

                    def topk(i):
                        sc, mk, m = score[i % 2], mask[i % 2], m8[i % 2]
                        nk = (i + 1) * 128
                        if i >= 2:
                            for it in range(32):
                                src = sc if it == 0 else work
                                kb.I("dve", "max", [src], [m], out=m[:], in_=src[:, 0:nk])
                                if it < 31:
                                    kb.I("dve", "match_replace", [m, src], [work], out=work[:, 0:nk], in_to_replace=m[:],
                                         in_values=src[:, 0:nk], imm_value=NEG)
                            kb.I("dve", "tensor_scalar", [sc, m], [mk], out=mk[:, 0:nk], in0=sc[:, 0:nk], scalar1=m[:, 7:8],
                                 scalar2=None, op0=ALU.is_ge)
                        else:
                            kb.I("dve", "tensor_scalar", [sc, thrc], [mk], out=mk[:, 0:nk], in0=sc[:, 0:nk], scalar1=thrc[:, 0:1],
                                 scalar2=None, op0=ALU.is_ge)

                    def qk(i, h):
                        nk = (i + 1) * 128
                        qs = slice(i * 128, (i + 1) * 128)
                        lg = lgt[h % 2]
                        for t0 in range(0, nk, 512):
                            n = min(512, nk - t0)
                            p = lps[cnts["li"] % 2]
                            cnts["li"] += 1
                            kb.I("pe", "matmul", [qT, kT], [p], p[:, 0:n], lhsT=qT[:, h, qs], rhs=kT[:, t0:t0 + n], start=True, stop=True)
                            kb.I("act", "mul", [p], [lg], out=lg[:, t0:t0 + n], in_=p[:, 0:n], mul=128.0 ** -0.5)

                    def attention(i):
                        mk = mask[i % 2]
                        nk = (i + 1) * 128
                        qs = slice(i * 128, (i + 1) * 128)
                        qk(i, 0)
                        for h in range(4):
                            lg, e_, s_, pT_, on_ = lgt[h % 2], ee[h % 2], sm[h % 2], pT[h % 2], on[h % 2]
                            kb.I("dve", "tensor_reduce", [lg], [s_], out=s_[:, 0:1], in_=lg[:, 0:nk], axis=AX.X, op=ALU.max)
                            kb.I("dve", "tensor_scalar", [s_], [s_], out=s_[:, 1:2], in0=s_[:, 0:1], scalar1=-1.0, scalar2=None, op0=ALU.mult)
                            kb.I("act", "activation", [lg, s_], [e_], out=e_[:, 0:nk], in_=lg[:, 0:nk], func=AF.Exp, bias=s_[:, 1:2])
                            if h < 3:
                                qk(i, h + 1)
                            kb.I("pool", "tensor_tensor", [e_, mk], [e_], out=e_[:, 0:nk], in0=e_[:, 0:nk], in1=mk[:, 0:nk], op=ALU.mult)
                            for j0 in range(0, i + 1, 8):
                                j1 = min(j0 + 8, i + 1)
                                tpp = tps[cnts["ti"] % 2]
                                cnts["ti"] += 1
                                for j in range(j0, j1):
                                    kb.I("pe", "transpose", [e_, c["identb"]], [tpp], out=tpp[:, j - j0, :], in_=e_[:, j * 128:(j + 1) * 128],
                                         identity=c["identb"][:], inc=(j == j1 - 1))
                                kb.copy("act" if cnts["ti"] % 2 else "dve", [tpp], [pT_], pT_[:, j0:j1, :], tpp[:, 0:j1 - j0, :])
                            for j in range(i + 1):
                                kb.I("pe", "matmul", [pT_, vtok], [ops], ops[:, 0:129], lhsT=pT_[:, j, :], rhs=vtok[:, j, 0:129], start=(j == 0), stop=(j == i),
                                     inc=(j == i))
                            kb.I("dve", "reciprocal", [ops], [s_], out=s_[:, 3:4], in_=ops[:, 128:129])
                            kb.I("dve", "tensor_scalar", [ops, s_], [on_], out=on_[:], in0=ops[:, 0:128], scalar1=s_[:, 3:4], scalar2=None, op0=ALU.mult)
                            kb.I("pe", "transpose", [on_, c["identb"]], [otp], out=otp[:], in_=on_[:], identity=c["identb"][:])
                            kb.copy("act", [otp], [mixT], mixT[:, h, qs], otp[:])

                    indexer(0)
                    topk(0)
                    for i in range(NT):
                        if i + 1 < NT:
                            indexer(i + 1)
                        attention(i)
                        if i + 1 < NT:
                            topk(i + 1)
                with kb.scope():
                    woutb = kb.sb("woutb", [128, 8, D], BF16)
                    stg = [kb.sb("ostg%d" % i, [128, D]) for i in range(2)]
                    for k in range(8):
                        kb.dma("sp", stg[k % 2][:], wout_d[k * 128:(k + 1) * 128, :])
                        kb.copy("act" if k % 2 else "pool", [stg[k % 2]], [woutb], woutb[:, k, :], stg[k % 2][:])
                    hts = [kb.sb("oht%d" % i, [128, D]) for i in range(2)]
                    acc = [kb.sb("oacc%d" % i, [128, D]) for i in range(2)]
                    outt = [kb.sb("oout%d" % i, [128, D]) for i in range(2)]
                    st6 = kb.sb("st6", [128, 2, 6])
                    mv = kb.sb("mv", [128, 4])
                    ops2 = [kb.ps("opp%d" % i, [128, 512]) for i in range(4)]
                    for t in range(NT):
                        ht = hts[t % 2]
                        a = acc[t % 2]
                        kb.dma("sp", ht[:], h_d[r0 + t * 128: r0 + (t + 1) * 128, :])
                        for hf in range(2):
                            p = ops2[(2 * t + hf) % 4]
                            for k in range(8):
                                kb.I("pe", "matmul", [mixT, woutb], [p], p[:], lhsT=mixT[:, k, t * 128:(t + 1) * 128],
                                     rhs=woutb[:, k, hf * 512:(hf + 1) * 512], start=(k == 0), stop=(k == 7), inc=(k == 7))
                            kb.I("dve", "scalar_tensor_tensor", [ht, p], [a], out=a[:, hf * 512:(hf + 1) * 512], in0=ht[:, hf * 512:(hf + 1) * 512],
                                 scalar=ALPHA, in1=p[:], op0=ALU.mult, op1=ALU.add)
                        layer_norm_tile(kb, a, g_bc, b_bc, outt[t % 2], st6, mv)
                        kb.dma("sp", out_d[r0 + t * 128: r0 + (t + 1) * 128, :], outt[t % 2][:])


def prep_even(inp, i):
    f = lambda a: np.ascontiguousarray(a, dtype=np.float32)
    pc = lambda v, n: f(v.reshape(n, 128).T)
    vec = np.zeros((128, 40), np.float32)
    cwt = inp["even_conv_w"][i]
    for j in range(4):
        vec[:, j * 4:(j + 1) * 4] = pc(cwt[j], 4)
    vec[:, 16:20] = pc(inp["even_conv_b"][i], 4)
    vec[:, 20:24] = pc(inp["even_rg_ba"][i], 4)
    vec[:, 24:28] = pc(inp["even_rg_bx"][i], 4)
    vec[:, 28:32] = pc(inp["even_rg_lambda"][i], 4)
    vec[:, 32:34] = pc(inp["even_kv_norm"][i], 2)
    rgw = np.zeros((128, 8, 128), np.float32)
    for g, nm in enumerate(("even_rg_wa", "even_rg_wx")):
        w = inp[nm][i]
        for n in range(8):
            o = (n % 2) * 64
            rgw[o:o + 64, g * 4 + n // 2, o:o + 64] = w[n]
    return {
        "ev_win": f(inp["even_w_in"][i]), "ev_wout": f(inp["even_w_out"][i]),
        "ev_wuk": f(inp["even_w_uk"][i]), "ev_wuv": f(inp["even_w_uv"][i]),
        "ev_rgw": f(rgw.reshape(128, 1024)), "ev_vec": vec,
        "ev_lng": f(inp["ln_g"][2 * i, 0]), "ev_lnb": f(inp["ln_b"][2 * i, 0]),
    }


def build_gdn(kb, c, h_d, gw_d, wab_d, gconv_d, alog_d, dtb_d, onorm_d, wout_d, lng_d, lnb_d, out_d, o_d, nseq=SPC, stop=0):
    with kb.scope():
        g_bc = kb.sb("g_bc", [128, D])
        b_bc = kb.sb("b_bc", [128, D])
        kb.dma("sp", g_bc[:], lng_d.partition_broadcast(128))
        kb.dma("sp", b_bc[:], lnb_d.partition_broadcast(128))
        wabb = kb.sb("wabb", [128, 8, 32], BF16)
        gconv = kb.sb("gconv", [128, 8, 4, 4])
        nA = kb.sb("nA", [128, 16])
        dtb = kb.sb("dtb", [128, 16])
        onorm = kb.sb("onorm", [128, 1])
        eps6 = kb.sb("eps6", [128, 1])
        kb.I("pool", "memset", [], [eps6], eps6[:], 1e-6)
        kb.dma("sp", gconv[:], gconv_d)
        kb.dma("sp", nA[:], alog_d.partition_broadcast(128))
        kb.dma("sp", dtb[:], dtb_d.partition_broadcast(128))
        kb.dma("sp", onorm[:], onorm_d.rearrange("(p o) -> p o", o=1))
        kb.I("act", "activation", [nA], [nA], out=nA[:], in_=nA[:], func=AF.Exp)
        kb.I("dve", "tensor_scalar", [nA], [nA], out=nA[:], in0=nA[:], scalar1=-1.0, scalar2=None, op0=ALU.mult)
        ut = kb.sb("ut", [128, 128])
        slt = kb.sb("slt", [128, 128])
        lmask = kb.sb("lmask", [128, 128])
        smask = kb.sb("smask", [128, 128])
        kb.I("dve", "tensor_tensor", [c["sut"], c["ident"]], [ut], out=ut[:], in0=c["sut"][:], in1=c["ident"][:], op=ALU.add)
        kb.I("dve", "tensor_scalar", [ut], [slt], out=slt[:], in0=ut[:], scalar1=-1.0, scalar2=1.0, op0=ALU.mult, op1=ALU.add)
        kb.I("dve", "tensor_copy", [slt], [smask], out=smask[:], in_=slt[:])
        kb.I("dve", "tensor_tensor", [slt, c["ident"]], [lmask], out=lmask[:], in0=slt[:], in1=c["ident"][:], op=ALU.add)
        with kb.scope():
            st = kb.sb("abstg", [128, 8, 32])
            kb.dma("sp", st[:], wab_d.rearrange("(k p) e -> p k e", p=128))
            kb.I("dve", "tensor_copy", [st], [wabb], out=wabb[:], in_=st[:])

        for s in range(nseq):
            r0 = s * TP
            with kb.scope():
                hT = kb.sb("hT", [128, 8, TP], BF16)
                with kb.scope():
                    hts = [kb.sb("ght%d" % i, [128, D]) for i in range(2)]
                    tp = [kb.ps("gtp%d" % i, [128, 8, 128]) for i in range(2)]
                    for t in range(NT):
                        ht = hts[t % 2]
                        kb.dma("sp", ht[:], h_d[r0 + t * 128: r0 + (t + 1) * 128, :])
                        tpp = tp[t % 2]
                        for k in range(8):
                            kb.I("pe", "transpose", [ht, c["ident"]], [tpp], out=tpp[:, k, :],
                                 in_=ht[:, k * 128:(k + 1) * 128], identity=c["ident"][:], inc=(k == 7))
                        kb.copy("act" if t % 2 else "dve", [tpp], [hT], hT[:, :, t * 128:(t + 1) * 128], tpp[:])
                if stop == 1:
                    return
                S3 = lambda nm: kb.sb(nm, [128, NT, 16])
                gg, beta, gc, egc, ekd, egl, bege = S3("gg"), S3("beta"), S3("gc"), S3("egc"), S3("ekd"), S3("egl"), S3("bege")
                with kb.scope():
                    abp = [kb.ps("abp%d" % i, [128, 32]) for i in range(2)]
                    ab = kb.sb("ab", [128, NT, 32])
                    tmp = S3("tmpa")
                    for t in range(NT):
                        p = abp[t % 2]
                        for k in range(8):
                            kb.I("pe", "matmul", [hT, wabb], [p], p[:], lhsT=hT[:, k, t * 128:(t + 1) * 128], rhs=wabb[:, k, :],
                                 start=(k == 0), stop=(k == 7), inc=(k == 7))
                        kb.copy("act", [p], [ab], ab[:, t, :], p[:])
                    bcT = lambda a: a[:].unsqueeze(1).to_broadcast([128, NT, 16])
                    kb.I("dve", "tensor_tensor", [ab, dtb], [gg], out=gg[:], in0=ab[:, :, 0:16], in1=bcT(dtb), op=ALU.add)
                    kb.I("dve", "tensor_scalar", [gg], [tmp], out=tmp[:], in0=gg[:], scalar1=-1.0, scalar2=None, op0=ALU.mult)
                    kb.I("dve", "tensor_tensor", [gg, tmp], [tmp], out=tmp[:], in0=gg[:], in1=tmp[:], op=ALU.max)
                    kb.I("act", "activation", [tmp], [tmp], out=tmp[:], in_=tmp[:], func=AF.Exp, scale=-1.0)
                    kb.I("act", "activation", [tmp], [tmp], out=tmp[:], in_=tmp[:], func=AF.Ln, bias=1.0)
                    kb.I("dve", "tensor_scalar", [gg], [gg], out=gg[:], in0=gg[:], scalar1=0.0, scalar2=None, op0=ALU.max)
                    kb.I("dve", "tensor_tensor", [gg, tmp], [gg], out=gg[:], in0=gg[:], in1=tmp[:], op=ALU.add)
                    kb.I("dve", "tensor_tensor", [gg, nA], [gg], out=gg[:], in0=gg[:], in1=bcT(nA), op=ALU.mult)
                    kb.I("act", "activation", [ab], [beta], out=beta[:], in_=ab[:, :, 16:32], func=AF.Sigmoid)
                    for t in range(NT):
                        p = abp[t % 2]
                        kb.I("pe", "matmul", [ut, gg], [p], p[:, 0:16], lhsT=ut[:], rhs=gg[:, t, :], start=True, stop=True, inc=False)
                        kb.I("pe", "matmul", [c["ones"], gg], [p], p[:, 16:32], lhsT=c["ones"][:], rhs=gg[:, t, :], start=True, stop=True)
                        kb.copy("act", [p], [gc], gc[:, t, :], p[:, 0:16])
                        kb.copy("dve", [p], [egl], egl[:, t, :], p[:, 16:32])
                    kb.I("dve", "tensor_tensor", [egl, gc], [ekd], out=ekd[:], in0=egl[:], in1=gc[:], op=ALU.subtract)
                    kb.I("act", "activation", [ekd], [ekd], out=ekd[:], in_=ekd[:], func=AF.Exp)
                    kb.I("act", "activation", [egl], [egl], out=egl[:], in_=egl[:], func=AF.Exp)
                    kb.I("act", "activation", [gc], [egc], out=egc[:], in_=gc[:], func=AF.Exp)
                    kb.I("dve", "tensor_tensor", [beta, egc], [bege], out=bege[:], in0=beta[:], in1=egc[:], op=ALU.mult)

                if stop == 2:
                    return
                for kh in range(8):
                    with kb.scope():
                        wsl = kb.sb("wsl", [128, 8, 768], BF16)
                        with kb.scope():
                            stg = [kb.sb("gstg%d" % i, [128, 768]) for i in range(2)]
                            for k in range(8):
                                kb.dma("sp", stg[k % 2][:], gw_d[kh, k * 128:(k + 1) * 128, :])
                                kb.copy("act" if k % 2 else "pool", [stg[k % 2]], [wsl], wsl[:, k, :], stg[k % 2][:])
                        qkT = kb.sb("qkT", [128, 2, TP], BF16)
                        vT = kb.sb("vT", [128, 2, TP], BF16)
                        zsT = kb.sb("zsT", [128, 2, TP], BF16)
                        ktok = kb.sb("ktok", [128, NT, 128], BF16)
                        vtok = kb.sb("vtok", [128, NT, 256], BF16)
                        oTk = kb.sb("oTk", [128, 2, TP], BF16)
                        with kb.scope():
                            raws = [kb.sb("raw%d" % i, [128, TP]) for i in range(2)]
                            cvs = [kb.sb("cv%d" % i, [128, TP]) for i in range(2)]
                            sq = kb.sb("sq", [128, 512], BF16)
                            rstd = kb.sb("rstd", [128, 512])
                            ps2 = [kb.ps("gpp%d" % i, [128, 512]) for i in range(2)]
                            tps = [kb.ps("gtq%d" % i, [128, 8, 128], BF16) for i in range(2)]
                            cnt = [0]
                            ti = 0
                            for fi in range(6):
                                raw, cv = raws[fi % 2], cvs[fi % 2]
                                def ev(n_, p, t0, n, fi=fi, raw=raw):
                                    if fi < 4:
                                        kb.copy("act" if n_ % 2 else "dve", [p], [raw], raw[:, t0:t0 + n], p)
                                    else:
                                        kb.I("act", "activation", [p], [zsT], out=zsT[:, fi - 4, t0:t0 + n], in_=p, func=AF.Silu)
                                proj_fm(kb, ps2, cnt, wsl, fi * 128, hT, None, ev)
                                if fi >= 4:
                                    continue
                                cwc = lambda j: gconv[:, kh, fi, j:j + 1]
                                kb.I("dve", "tensor_scalar", [raw, gconv], [cv], out=cv[:], in0=raw[:], scalar1=cwc(3), scalar2=None, op0=ALU.mult)
                                for d in (1, 2, 3):
                                    kb.I("dve", "scalar_tensor_tensor", [raw, gconv, cv], [cv], out=cv[:, d:TP], in0=raw[:, 0:TP - d],
                                         scalar=cwc(3 - d), in1=cv[:, d:TP], op0=ALU.mult, op1=ALU.add)
                                if fi >= 2:
                                    kb.I("act", "activation", [cv], [vT], out=vT[:, fi - 2, :], in_=cv[:], func=AF.Silu)
                                    for j0 in range(0, NT, 8):
                                        j1 = min(j0 + 8, NT)
                                        tpp = tps[ti % 2]
                                        ti += 1
                                        for j in range(j0, j1):
                                            kb.I("pe", "transpose", [vT, c["identb"]], [tpp], out=tpp[:, j - j0, :],
                                                 in_=vT[:, fi - 2, j * 128:(j + 1) * 128], identity=c["identb"][:], inc=(j == j1 - 1))
                                        kb.copy("dve", [tpp], [vtok], vtok[:, j0:j1, (fi - 2) * 128:(fi - 1) * 128], tpp[:, 0:j1 - j0, :])
                                    continue
                                kb.I("act", "activation", [cv], [cv], out=cv[:], in_=cv[:], func=AF.Silu)
                                for (t0, n) in CHUNKS:
                                    kb.I("pool", "tensor_tensor", [cv], [sq], out=sq[:, 0:n], in0=cv[:, t0:t0 + n], in1=cv[:, t0:t0 + n], op=ALU.mult)
                                    p = ps2[cnt[0] % 2]
                                    cnt[0] += 1
                                    kb.I("pe", "matmul", [c["onesb"], sq], [p], p[:, 0:n], lhsT=c["onesb"][:], rhs=sq[:, 0:n], start=True, stop=True)
                                    kb.I("act", "activation", [p], [rstd], out=rstd[:, 0:n], in_=p[:, 0:n], func=AF.Ln, bias=eps6[:, 0:1])
                                    kb.I("act", "activation", [rstd], [rstd], out=rstd[:, 0:n], in_=rstd[:, 0:n], func=AF.Exp, scale=-0.5)
                                    kb.I("dve", "scalar_tensor_tensor", [cv, rstd], [qkT], out=qkT[:, fi, t0:t0 + n], in0=cv[:, t0:t0 + n],
                                         scalar=(128.0 ** -0.5 if fi == 0 else 1.0), in1=rstd[:, 0:n], op0=ALU.mult, op1=ALU.mult)
                                if fi == 1:
                                    for j0 in range(0, NT, 8):
                                        j1 = min(j0 + 8, NT)
                                        tpp = tps[ti % 2]
                                        ti += 1
                                        for j in range(j0, j1):
                                            kb.I("pe", "transpose", [qkT, c["identb"]], [tpp], out=tpp[:, j - j0, :],
                                                 in_=qkT[:, 1, j * 128:(j + 1) * 128], identity=c["identb"][:], inc=(j == j1 - 1))
                                        kb.copy("dve", [tpp], [ktok], ktok[:, j0:j1, :], tpp[:, 0:j1 - j0, :])
                        if stop == 3:
                            return
                        with kb.scope():
                            B = lambda nm: kb.sb(nm, [128, 128], BF16)
                            Fp = lambda nm: kb.sb(nm, [128, 128])
                            otok = [kb.sb("otok%d" % j, [128, NT, 128]) for j in range(2)]
                            us_all = [kb.sb("us_all%d" % j, [128, NT, 128]) for j in range(2)]
                            wT_all = [kb.sb("wT_all%d" % j, [128, NT, 128], BF16) for j in range(2)]
                            aT_all = [kb.sb("aT_all%d" % j, [128, NT, 128], BF16) for j in range(2)]
                            kd_all = [kb.sb("kd_all%d" % j, [128, NT, 128], BF16) for j in range(2)]
                            pGA = kb.ps("pGA", [128, 2, 128])
                            X = [kb.ps("pX%d" % i, [128, 4, 128]) for i in range(4)]
                            TPb = kb.ps("TPb", [128, 8, 128], BF16)
                            CH = []
                            for ci in range(4):
                                CH.append(dict(
                                    Rm=Fp("Rm%d" % ci), Dm=Fp("Dm%d" % ci), attn=B("attn%d" % ci),
                                    Mb=[B("Mb%d_0" % ci), B("Mb%d_1" % ci)],
                                    NA=[kb.sb("NA%d_0" % ci, [128, 2, 128], BF16), kb.sb("NA%d_1" % ci, [128, 2, 128], BF16)],
                                    vb=B("vb%d" % ci), kbg=B("kbg%d" % ci), X=X[ci]))
                            GsAs = [(Fp("Gs0"), Fp("As0")), (Fp("Gs1"), Fp("As1"))]
                            for j in range(2):
                                hd = 2 * kh + j
                                kb.I("pool", "tensor_tensor", [ktok, ekd], [kd_all[j]], out=kd_all[j][:], in0=ktok[:],
                                     in1=ekd[:, :, hd:hd + 1].to_broadcast([128, NT, 128]), op=ALU.mult)
                            Ssb = [Fp("S0"), Fp("S1")]
                            Sbf = [B("Sb0"), B("Sb1")]
                            vnew = [B("vnew0"), B("vnew1")]
                            o1 = [Fp("o1_0"), Fp("o1_1")]
                            P2 = [kb.ps("pP2_%d" % j, [128, 4, 128]) for j in range(2)]
                            for j in range(2):
                                kb.I("pool", "memset", [], [Ssb[j]], Ssb[j][:], 0.0)
                                kb.I("pool", "memset", [], [Sbf[j]], Sbf[j][:], 0.0)

                            def phase2(tlist):
                                for t in tlist:
                                    ts_ = slice(t * 128, (t + 1) * 128)
                                    for j in range(2):
                                        W = P2[j]
                                        kb.I("pe", "matmul", [wT_all[j], Sbf[j]], [W], W[:, 0, :], lhsT=wT_all[j][:, t, :], rhs=Sbf[j][:], start=True, stop=True, inc=False)
                                        kb.I("pe", "matmul", [qkT, Sbf[j]], [W], W[:, 1, :], lhsT=qkT[:, 0, ts_], rhs=Sbf[j][:], start=True, stop=True)
                                    yield
                                    for j in range(2):
                                        W = P2[j]
                                        hd = 2 * kh + j
                                        kb.I("dve", "tensor_tensor", [us_all[j], W], [vnew[j]], out=vnew[j][:], in0=us_all[j][:, t, :], in1=W[:, 0, :], op=ALU.subtract)
                                        kb.I("dve", "tensor_scalar", [W, egc], [o1[j]], out=o1[j][:], in0=W[:, 1, :], scalar1=egc[:, t, hd:hd + 1], scalar2=None, op0=ALU.mult)
                                    yield
                                    for j in range(2):
                                        V_ = P2[j]
                                        kb.I("pe", "matmul", [aT_all[j], vnew[j]], [V_], V_[:, 2, :], lhsT=aT_all[j][:, t, :], rhs=vnew[j][:], start=True, stop=True, inc=False)
                                        kb.I("pe", "matmul", [kd_all[j], vnew[j]], [V_], V_[:, 3, :], lhsT=kd_all[j][:, t, :], rhs=vnew[j][:], start=True, stop=True)
                                    yield
                                    for j in range(2):
                                        V_ = P2[j]
                                        hd = 2 * kh + j
                                        kb.I("dve", "scalar_tensor_tensor", [Ssb[j], egl, V_], [Sbf[j]], out=Sbf[j][:], in0=Ssb[j][:], scalar=egl[:, t, hd:hd + 1],
                                             in1=V_[:, 3, :], op0=ALU.mult, op1=ALU.add)
                                        kb.I("dve", "scalar_tensor_tensor", [Ssb[j], egl, V_], [Ssb[j]], out=Ssb[j][:], in0=Ssb[j][:], scalar=egl[:, t, hd:hd + 1],
                                             in1=V_[:, 3, :], op0=ALU.mult, op1=ALU.add)
                                        kb.I("dve", "tensor_tensor", [o1[j], V_], [otok[j]], out=otok[j][:, t, :], in0=o1[j][:], in1=V_[:, 2, :], op=ALU.add)
                                    yield
                            gen2 = iter(())
                            for t0 in range(0, NT, 2):
                                tl = [t for t in (t0, t0 + 1) if t < NT]
                                chains = []
                                for ti_, t in enumerate(tl):
                                    ts_ = slice(t * 128, (t + 1) * 128)
                                    Gs, As = GsAs[ti_]
                                    kb.I("pe", "matmul", [qkT], [pGA], pGA[:, 0, :], lhsT=qkT[:, 1, ts_], rhs=qkT[:, 1, ts_], start=True, stop=True, inc=False)
                                    kb.I("pe", "matmul", [qkT], [pGA], pGA[:, 1, :], lhsT=qkT[:, 0, ts_], rhs=qkT[:, 1, ts_], start=True, stop=True)
                                    kb.I("dve", "tensor_tensor", [pGA, smask], [Gs], out=Gs[:], in0=pGA[:, 0, :], in1=smask[:], op=ALU.mult)
                                    kb.I("dve", "tensor_tensor", [pGA, lmask], [As], out=As[:], in0=pGA[:, 1, :], in1=lmask[:], op=ALU.mult)
                                    for j in range(2):
                                        chn = dict(CH[ti_ * 2 + j])
                                        chn.update(t=t, j=j, hd=2 * kh + j, Gs=Gs, As=As, slot=ti_ * 2 + j)
                                        chains.append(chn)
                                col = lambda a, q: a[:, q["t"], q["hd"]:q["hd"] + 1]
                                for q in chains:
                                    kb.I("act", "mul", [ut, gg], [q["Rm"]], out=q["Rm"][:], in_=ut[:], mul=col(gg, q))
                                for q in chains:
                                    kb.I("pe", "matmul", [q["Rm"], slt], [q["X"]], q["X"][:, 0, :], lhsT=q["Rm"][:], rhs=slt[:], start=True, stop=True)
                                for q in chains:
                                    kb.I("act", "activation", [q["X"]], [q["Dm"]], out=q["Dm"][:], in_=q["X"][:, 0, :], func=AF.Exp)
                                for q in chains:
                                    kb.I("dve", "scalar_tensor_tensor", [q["Gs"], beta, q["Dm"]], [q["Mb"][0]], out=q["Mb"][0][:], in0=q["Gs"][:],
                                         scalar=col(beta, q), in1=q["Dm"][:], op0=ALU.mult, op1=ALU.mult)
                                    kb.I("pool", "tensor_tensor", [q["As"], q["Dm"]], [q["attn"]], out=q["attn"][:], in0=q["As"][:], in1=q["Dm"][:], op=ALU.mult)
                                    kb.copy("act", [c["identb"]], [q["NA"][0]], q["NA"][0][:, 1, :], c["identb"][:])
                                for q in chains:
                                    sl = q["slot"]
                                    kb.I("pe", "transpose", [q["Mb"][0], c["identb"]], [TPb], out=TPb[:, 2 * sl, :], in_=q["Mb"][0][:], identity=c["identb"][:], inc=False)
                                    kb.I("pe", "transpose", [q["attn"], c["identb"]], [TPb], out=TPb[:, 2 * sl + 1, :], in_=q["attn"][:], identity=c["identb"][:])
                                for q in chains:
                                    sl = q["slot"]
                                    kb.copy("act", [TPb], [q["NA"][0]], q["NA"][0][:, 0, :], TPb[:, 2 * sl, :])
                                    kb.copy("dve", [TPb], [aT_all[q["j"]]], aT_all[q["j"]][:, q["t"], :], TPb[:, 2 * sl + 1, :])
                                cur = 0
                                for st_ in range(7):
                                    nxt = 1 - cur
                                    next(gen2, None)
                                    for q in chains:
                                        Xq, M, NA = q["X"], q["Mb"][cur], q["NA"][cur]
                                        if st_ < 6:
                                            kb.I("pe", "matmul", [NA, M], [Xq], Xq[:, 0, :], lhsT=NA[:, 0, :], rhs=M[:], start=True, stop=True, inc=False)
                                            kb.I("pe", "matmul", [M, NA], [Xq], Xq[:, 1:3, :], lhsT=M[:], rhs=NA[:], start=True, stop=True)
                                        else:
                                            kb.I("pe", "matmul", [M, NA], [Xq], Xq[:, 2, :], lhsT=M[:], rhs=NA[:, 1, :], start=True, stop=True)
                                    next(gen2, None)
                                    for q in chains:
                                        Xq, NA, NAn = q["X"], q["NA"][cur], q["NA"][nxt]
                                        if st_ < 6:
                                            kb.copy("act", [Xq], [q["Mb"][nxt]], q["Mb"][nxt][:], Xq[:, 0, :])
                                            if st_ < 5:
                                                kb.copy("act", [Xq], [NAn], NAn[:, 0, :], Xq[:, 1, :])
                                        kb.I("dve", "tensor_tensor", [NA, Xq], [NAn], out=NAn[:, 1, :], in0=NA[:, 1, :], in1=Xq[:, 2, :],
                                             op=(ALU.subtract if st_ == 0 else ALU.add))
                                    cur = nxt
                                for q in chains:
                                    j, t = q["j"], q["t"]
                                    kb.I("act", "mul", [vtok, beta], [q["vb"]], out=q["vb"][:], in_=vtok[:, t, j * 128:(j + 1) * 128], mul=col(beta, q))
                                    kb.I("pool", "tensor_scalar", [ktok, bege], [q["kbg"]], out=q["kbg"][:], in0=ktok[:, t, :], scalar1=col(bege, q),
                                         scalar2=None, op0=ALU.mult)
                                for q in chains:
                                    TT = q["NA"][cur][:, 1, :]
                                    kb.I("pe", "matmul", [q["NA"][cur], q["vb"]], [q["X"]], q["X"][:, 0, :], lhsT=TT, rhs=q["vb"][:], start=True, stop=True, inc=False)
                                    kb.I("pe", "matmul", [q["kbg"], q["NA"][cur]], [q["X"]], q["X"][:, 1, :], lhsT=q["kbg"][:], rhs=TT, start=True, stop=True)
                                for q in chains:
                                    j, t = q["j"], q["t"]
                                    kb.copy("act", [q["X"]], [us_all[j]], us_all[j][:, t, :], q["X"][:, 0, :])
                                    kb.copy("dve", [q["X"]], [wT_all[j]], wT_all[j][:, t, :], q["X"][:, 1, :])
                                for _ in gen2:
                                    pass
                                gen2 = phase2(tl)
                            for _ in gen2:
                                pass
                            if stop == 4:
                                return
                            with kb.scope():
                                sqo = kb.sb("sqo", [128, NT, 128])
                                ssq = kb.sb("ssq", [128, NT])
                                onb = kb.sb("onb", [128, NT, 128], BF16)
                                tpo = [TPb, TPb]
                                ti = 0
                                for j in range(2):
                                    kb.I("pool", "tensor_tensor", [otok[j]], [sqo], out=sqo[:], in0=otok[j][:], in1=otok[j][:], op=ALU.mult)
                                    kb.I("dve", "tensor_reduce", [sqo], [ssq], out=ssq[:], in_=sqo[:], axis=AX.X, op=ALU.add)
                                    kb.I("dve", "tensor_scalar", [ssq], [ssq], out=ssq[:], in0=ssq[:], scalar1=1.0 / 128, scalar2=1e-6, op0=ALU.mult, op1=ALU.add)
                                    kb.I("act", "sqrt", [ssq], [ssq], out=ssq[:], in_=ssq[:])
                                    kb.I("dve", "reciprocal", [ssq], [ssq], out=ssq[:], in_=ssq[:])
                                    kb.I("dve", "tensor_tensor", [otok[j], ssq], [onb], out=onb[:], in0=otok[j][:],
                                         in1=ssq[:].unsqueeze(2).to_broadcast([128, NT, 128]), op=ALU.mult)
                                    for j0 in range(0, NT, 8):
                                        j1 = min(j0 + 8, NT)
                                        tpp = tpo[ti % 2]
                                        ti += 1
                                        for jj in range(j0, j1):
                                            kb.I("pe", "transpose", [onb, c["identb"]], [tpp], out=tpp[:, jj - j0, :], in_=onb[:, jj, :], identity=c["identb"][:], inc=(jj == j1 - 1))
                                        kb.I("dve", "scalar_tensor_tensor", [tpp, onorm, zsT], [oTk], out=oTk[:, j, j0 * 128:j1 * 128],
                                             in0=tpp[:, 0:j1 - j0, :].rearrange("p a b -> p (a b)"), scalar=onorm[:, 0:1], in1=zsT[:, j, j0 * 128:j1 * 128],
                                             op0=ALU.mult, op1=ALU.mult)
                                for j in range(2):
                                    kb.dma("sp", o_d[s, 2 * kh + j, :, :], oTk[:, j, :])
                if stop == 5:
                    return
                with kb.scope():
                    woutb = kb.sb("gwoutb", [128, 16, D], BF16)
                    stg = [kb.sb("gostg%d" % i, [128, D]) for i in range(2)]
                    for k in range(16):
                        kb.dma("sp", stg[k % 2][:], wout_d[k * 128:(k + 1) * 128, :])
                        kb.copy("act" if k % 2 else "pool", [stg[k % 2]], [woutb], woutb[:, k, :], stg[k % 2][:])
                    oTt = [kb.sb("oTt%d" % i, [128, 16, 128], BF16) for i in range(2)]
                    hts = [kb.sb("goht%d" % i, [128, D]) for i in range(2)]
                    acc = [kb.sb("goacc%d" % i, [128, D]) for i in range(2)]
                    outt = [kb.sb("goout%d" % i, [128, D]) for i in range(2)]
                    st6 = kb.sb("st6", [128, 2, 6])
                    mv = kb.sb("mv", [128, 4])
                    ops2 = [kb.ps("gopp%d" % i, [128, 512]) for i in range(4)]
                    for t in range(NT):
                        ht = hts[t % 2]
                        a = acc[t % 2]
                        ot = oTt[t % 2]
                        kb.dma("sp", ht[:], h_d[r0 + t * 128: r0 + (t + 1) * 128, :])
                        for hq in range(4):
                            kb.dma("act", ot[:, hq * 4:(hq + 1) * 4, :], o_d[s, hq * 4:(hq + 1) * 4, :, t * 128:(t + 1) * 128].rearrange("h p t -> p h t"),
                                   writes=[ot], group="oTt%d" % (t % 2))
                        for hf in range(2):
                            p = ops2[(2 * t + hf) % 4]
                            for k in range(16):
                                kb.I("pe", "matmul", [ot, woutb], [p], p[:], lhsT=ot[:, k, :], rhs=woutb[:, k, hf * 512:(hf + 1) * 512],
                                     start=(k == 0), stop=(k == 15), inc=(k == 15))
                            kb.I("dve", "scalar_tensor_tensor", [ht, p], [a], out=a[:, hf * 512:(hf + 1) * 512], in0=ht[:, hf * 512:(hf + 1) * 512],
                                 scalar=ALPHA, in1=p[:], op0=ALU.mult, op1=ALU.add)
                        layer_norm_tile(kb, a, g_bc, b_bc, outt[t % 2], st6, mv)
                        kb.dma("sp", out_d[r0 + t * 128: r0 + (t + 1) * 128, :], outt[t % 2][:])


def prep_gdn(inp, i):
    f = lambda a: np.ascontiguousarray(a, dtype=np.float32)
    w = inp["odd_w_in"][i]
    cwt = inp["odd_conv_w"][i]
    gw = np.zeros((8, 1024, 768), np.float32)
    gconv = np.zeros((128, 8, 4, 4), np.float32)
    for kh in range(8):
        cols = [np.arange(kh * 128, kh * 128 + 128), 1024 + np.arange(kh * 128, kh * 128 + 128),
                2048 + np.arange(2 * kh * 128, 2 * kh * 128 + 256)]
        zc = 4096 + np.arange(2 * kh * 128, 2 * kh * 128 + 256)
        gw[kh] = w[:, np.concatenate(cols + [zc])]
        cc_ = np.concatenate(cols)
        for fi in range(4):
            gconv[:, kh, fi, :] = cwt[:, cc_[fi * 128:(fi + 1) * 128]].T
    return {
        "gd_w": gw, "gd_wab": f(w[:, 6144:6176]), "gd_conv": gconv.reshape(128, 128).reshape(128, 8, 4, 4),
        "gd_alog": f(inp["odd_a_log"][i]), "gd_dtb": f(inp["odd_dt_bias"][i]), "gd_onorm": f(inp["odd_o_norm"][i]),
        "gd_wout": f(inp["odd_w_out"][i]), "gd_lng": f(inp["ln_g"][2 * i + 1, 0]), "gd_lnb": f(inp["ln_b"][2 * i + 1, 0]),
    }


def perm_expert(w, nk):
    E, R, F_ = w.shape
    return np.ascontiguousarray(w.reshape(E, nk, 128, F_).transpose(0, 2, 1, 3).reshape(E * 128, nk * F_), dtype=np.float32)


def prep_moe(inp, layer, pfx):
    f = lambda a: np.ascontiguousarray(a, dtype=np.float32)
    return {
        pfx + "wr": f(np.concatenate([inp["moe_group_w"][layer], inp["moe_expert_w"][layer]], axis=1)),
        pfx + "br": f(np.concatenate([inp["moe_group_b"][layer], inp["moe_expert_b"][layer]], axis=0)),
        pfx + "wg": perm_expert(inp["moe_w_gate"][layer], 8), pfx + "wu": perm_expert(inp["moe_w_up"][layer], 8),
        pfx + "wd": perm_expert(inp["moe_w_down"][layer], 4),
        pfx + "lng": f(inp["ln_g"][layer, 1]), pfx + "lnb": f(inp["ln_b"][layer, 1]),
    }


def build_program(shapes):
    nc = bass.Bass("TRN2", target_bir_lowering=False)
    kb = KB(nc)
    dd = {}
    for n, (shp, dt) in shapes.items():
        dd[n] = nc.dram_tensor(n, list(shp), dt, kind="ExternalInput").ap()
    out_d = nc.dram_tensor("out", [NTOK, D], F32, kind="ExternalOutput").ap()
    h1_d = kb.dram("h1", [NTOK, D])
    h2_d = kb.dram("h2", [NTOK, D])
    h3_d = kb.dram("h3", [NTOK, D])
    xs_d = kb.dram("xs", [NBLK * 256, D], BF16)
    ys_d = kb.dram("ys", [NBLK * 256, D], F32)
    o_d = kb.dram("o_scr", [SPC, 16, 128, TP], BF16)
    c = load_consts(kb, {k[2:]: v for k, v in dd.items() if k.startswith("c_")})
    build_even(kb, c, dd["h0"], dd["ev_win"], dd["ev_wout"], dd["ev_wuk"], dd["ev_wuv"], dd["ev_rgw"], dd["ev_vec"],
               dd["ev_lng"], dd["ev_lnb"], h1_d)
    build_moe(kb, c, h1_d, dd["m0_wr"], dd["m0_br"], dd["m0_wg"], dd["m0_wu"], dd["m0_wd"], dd["m0_lng"], dd["m0_lnb"],
              h2_d, xs_d, ys_d)
    build_gdn(kb, c, h2_d, dd["gd_w"], dd["gd_wab"], dd["gd_conv"], dd["gd_alog"], dd["gd_dtb"], dd["gd_onorm"],
              dd["gd_wout"], dd["gd_lng"], dd["gd_lnb"], h3_d, o_d)
    build_moe(kb, c, h3_d, dd["m1_wr"], dd["m1_br"], dd["m1_wg"], dd["m1_wu"], dd["m1_wd"], dd["m1_lng"], dd["m1_lnb"],
              out_d, xs_d, ys_d)
    kb.finish()
    return nc


def kernel(**inputs):
    import ml_dtypes
    inp = {k: np.asarray(v) for k, v in inputs.items()}
    x = inp["x"].astype(np.float32, copy=False)
    B = x.shape[0]
    hp = np.zeros((B, TP, D), np.float32)
    hp[:, :NMETA] = inp["meta_tokens"][None]
    hp[:, NMETA:T] = x
    shared = {}
    for k, v in make_consts().items():
        shared["c_" + k] = v
    shared.update(prep_even(inp, 0))
    shared.update(prep_gdn(inp, 0))
    shared.update(prep_moe(inp, 0, "m0_"))
    shared.update(prep_moe(inp, 1, "m1_"))
    shapes = {n: (v.shape, BF16 if v.dtype == ml_dtypes.bfloat16 else F32) for n, v in shared.items()}
    shapes["h0"] = ((NTOK, D), F32)
    nc = build_program(shapes)
    in_maps = []
    for ci in range(NCORES):
        m = dict(shared)
        m["h0"] = np.ascontiguousarray(hp[ci * SPC:(ci + 1) * SPC].reshape(NTOK, D))
        in_maps.append(m)
    res = run_bass_kernel_spmd(nc, in_maps, core_ids=list(range(NCORES)))
    outs = [r["out"].reshape(SPC, TP, D)[:, NMETA:T] for r in res.results]
    return np.ascontiguousarray(np.concatenate(outs, axis=0).astype(np.float32))
```

```python
import contextlib
import numpy as np
import concourse.bass as bass
import concourse.mybir as mybir
from concourse.bass_utils import run_bass_kernel_spmd

F32 = mybir.dt.float32
BF16 = mybir.dt.bfloat16
I32 = mybir.dt.int32
AF = mybir.ActivationFunctionType
ALU = mybir.AluOpType
AX = mybir.AxisListType

NCORES = 8
D = 1024
SEQ = 2048
NMETA = 16
T = SEQ + NMETA
TP = 17 * 128
NT = TP // 128
SPC = 4
NTOK = SPC * TP
NTILE = NTOK // 128
ALPHA = 4.0 ** 0.25
NEG = -1.0e30


class Res:
    __slots__ = ("w", "r", "dsem")

    def __init__(self):
        self.w = None
        self.r = {}
        self.dsem = None


class KB:
    def __init__(self, nc):
        self.nc = nc
        self.es = contextlib.ExitStack()
        self.stack = [self.es]
        self.eng = {"pe": nc.tensor, "act": nc.scalar, "dve": nc.vector, "pool": nc.gpsimd, "sp": nc.sync}
        self.sems = {}
        self.cnt = {}
        for k in self.eng:
            self.sems[k] = self.es.enter_context(nc.semaphore("sem_" + k))
            self.cnt[k] = 0
        self.seen = {k: {} for k in self.eng}
        self.res = {}
        self.dcount = {}
        self.free_dsems = []
        self.scope_res = [[]]
        self.nd = 0
        self.n_inst = 0
        self.uid = 0
        self.psum_names = set()

    def sb(self, name, shape, dtype=F32):
        self.uid += 1
        return self.stack[-1].enter_context(self.nc.sbuf_tensor("%s_%d" % (name, self.uid), list(shape), dtype))

    def ps(self, name, shape, dtype=F32):
        self.uid += 1
        nm = "%s_%d" % (name, self.uid)
        self.psum_names.add(nm)
        return self.stack[-1].enter_context(self.nc.psum_tensor(nm, list(shape), dtype))

    def dram(self, name, shape, dtype=F32, kind="Internal"):
        return self.nc.dram_tensor(name, list(shape), dtype, kind=kind).ap()

    @contextlib.contextmanager
    def scope(self):
        st = contextlib.ExitStack()
        self.stack.append(st)
        self.scope_res.append([])
        try:
            yield
        finally:
            self.barrier()
            for key in self.scope_res.pop():
                r = self.res.pop(key, None)
                if r is not None and r.dsem is not None:
                    self.free_dsems.append(r.dsem)
            self.stack.pop()
            st.close()

    def _res(self, ap):
        key = ap if isinstance(ap, str) else ap.name
        r = self.res.get(key)
        if r is None:
            r = self.res[key] = Res()
            self.scope_res[-1].append(key)
        return r

    def _wait(self, e, semkey, val):
        if semkey in self.dcount:
            val = max(val, self.dcount[semkey])
        if self.seen[e].get(semkey, 0) >= val:
            return
        if semkey == e and val > self.cnt[e]:
            return
        self.seen[e][semkey] = val
        self.eng[e].wait_ge(self.sems[semkey], val)

    def _deps(self, e, reads, writes):
        for a in reads:
            r = self._res(a)
            if r.w is not None:
                self._wait(e, *r.w)
        for a in writes:
            r = self._res(a)
            if r.w is not None:
                self._wait(e, *r.w)
            for sk, v in r.r.items():
                self._wait(e, sk, v)

    def _done(self, ev, reads, writes):
        for a in reads:
            r = self._res(a)
            if r.r.get(ev[0], 0) < ev[1]:
                r.r[ev[0]] = ev[1]
        for a in writes:
            r = self._res(a)
            r.w = ev
            r.r = {}

    def I(self, e, fn, reads, writes, *args, inc=True, **kw):
        writes = list(writes) + [a for a in reads if (not isinstance(a, str)) and a.name in self.psum_names]
        self._deps(e, reads, writes)
        ins = getattr(self.eng[e], fn)(*args, **kw)
        if inc:
            self.cnt[e] += 1
            ins.then_inc(self.sems[e], 1)
            self._done((e, self.cnt[e]), reads, writes)
        else:
            self._done((e, self.cnt[e] + 1), reads, writes)
        self.n_inst += 1
        return ins

    def dma(self, q, out, in_, reads=None, writes=None, group=None, indirect=None, **kw):
        reads = [in_] if reads is None else reads
        writes = [out] if writes is None else writes
        dst = self._res(group if group is not None else writes[0])
        if dst.dsem is None:
            if self.free_dsems:
                dst.dsem = self.free_dsems.pop()
            else:
                self.nd += 1
                dst.dsem = "d%d" % self.nd
                self.sems[dst.dsem] = self.es.enter_context(self.nc.semaphore(dst.dsem))
                self.dcount[dst.dsem] = 0
        if group is None:
            self._deps(q, reads, writes)
        else:
            self._deps(q, reads, [])
            for a in writes:
                r = self._res(a)
                if r.w is not None and r.w[0] != dst.dsem:
                    self._wait(q, *r.w)
                for sk, v in r.r.items():
                    self._wait(q, sk, v)
        if indirect is None:
            ins = self.eng[q].dma_start(out=out, in_=in_, **kw)
        else:
            ins = self.eng[q].indirect_dma_start(out=out, in_=in_, **indirect)
        self.dcount[dst.dsem] += 16
        ins.then_inc(self.sems[dst.dsem], 16)
        self._done((dst.dsem, self.dcount[dst.dsem]), reads, writes)
        self.n_inst += 1
        return ins

    def copy(self, e, reads, writes, out, in_):
        return self.I(e, "copy" if e == "act" else "tensor_copy", reads, writes, out=out, in_=in_)

    def barrier(self):
        for e in self.eng:
            for e2 in ("pe", "act", "dve", "pool"):
                if self.cnt[e2]:
                    self._wait(e, e2, self.cnt[e2])
            for sk, c in self.dcount.items():
                if c:
                    self._wait(e, sk, c)

    def finish(self):
        self.barrier()
        self.es.close()


def load_consts(kb, cd):
    c = {}
    for name, shape, dt in (("ident", [128, 128], F32), ("identb", [128, 128], BF16),
                            ("ones", [128, 128], F32), ("sut", [128, 128], F32),
                            ("ramp", [128, 128], F32), ("pidx", [128, 128], F32), ("cmask", [128, 128], F32),
                            ("onesb", [128, 128], BF16)):
        t = kb.sb("c_" + name, shape, dt)
        kb.dma("sp", t[:], cd[name])
        c[name] = t
    return c


def layer_norm_tile(kb, acc, g_bc, b_bc, outt, st6, mv, eng2="pool"):
    for j in range(2):
        kb.I("dve", "bn_stats", [acc], [st6], out=st6[:, j, :], in_=acc[:, j * 512:(j + 1) * 512])
    kb.I("dve", "bn_aggr", [st6], [mv], out=mv[:, 0:2], in_=st6[:].rearrange("p a b -> p (a b)"))
    kb.I("dve", "tensor_scalar", [mv], [mv], out=mv[:, 2:3], in0=mv[:, 1:2], scalar1=1e-5, scalar2=None, op0=ALU.add)
    kb.I("act", "sqrt", [mv], [mv], out=mv[:, 2:3], in_=mv[:, 2:3])
    kb.I("dve", "reciprocal", [mv], [mv], out=mv[:, 2:3], in_=mv[:, 2:3])
    kb.I("dve", "scalar_tensor_tensor", [mv], [mv], out=mv[:, 3:4], in0=mv[:, 0:1], scalar=-1.0, in1=mv[:, 2:3], op0=ALU.mult, op1=ALU.mult)
    kb.I("act", "activation", [acc, mv], [acc], out=acc[:], in_=acc[:], func=AF.Identity, scale=mv[:, 2:3], bias=mv[:, 3:4])
    kb.I("dve", "tensor_tensor", [acc, g_bc], [acc], out=acc[:], in0=acc[:], in1=g_bc[:], op=ALU.mult)
    kb.I(eng2, "tensor_tensor", [acc, b_bc], [outt], out=outt[:], in0=acc[:], in1=b_bc[:], op=ALU.add)


MOE_BS = 4
MOE_BLK = MOE_BS * 128
NBLK = -(-NTOK * 2 // MOE_BLK) + 32
NSLOT = NBLK * MOE_BLK


def build_moe(kb, c, h_d, wr_d, br_d, wg_d, wu_d, wd_d, lng_d, lnb_d, out_d, xs_d, ys_d, ntile=NTILE):
    nc = kb.nc
    nblk = -(-ntile * 128 * 2 // MOE_BLK) + 32
    BS, BLK = MOE_BS, MOE_BLK
    with kb.scope():
        s1i = kb.sb("s1i", [128, ntile], I32)
        s2i = kb.sb("s2i", [128, ntile], I32)
        g1 = kb.sb("g1", [128, ntile])
        g2 = kb.sb("g2", [128, ntile])
        idxe = kb.sb("idxe", [128, nblk], I32)
        g_bc = kb.sb("g_bc", [128, D])
        b_bc = kb.sb("b_bc", [128, D])
        kb.dma("sp", g_bc[:], lng_d.partition_broadcast(128))
        kb.dma("sp", b_bc[:], lnb_d.partition_broadcast(128))
        hts = [kb.sb("ht%d" % i, [128, D]) for i in range(2)]

        with kb.scope():
            wr = kb.sb("wr", [128, 8, 36])
            kb.dma("sp", wr[:], wr_d.rearrange("(k p) e -> p k e", p=128))
            br = kb.sb("br", [128, 36])
            kb.dma("sp", br[:], br_d.partition_broadcast(128))
            L = kb.sb("L", [128, ntile, 36])
            hT = [kb.sb("hT%d" % i, [128, 8, 128]) for i in range(2)]
            tp = [kb.ps("tp%d" % i, [128, 8, 128]) for i in range(2)]
            lg = [kb.ps("lg%d" % i, [128, 36]) for i in range(2)]
            for t in range(ntile):
                ht = hts[t % 2]
                kb.dma("sp", ht[:], h_d[t * 128:(t + 1) * 128, :])
                tpp = tp[t % 2]
                for k in range(8):
                    kb.I("pe", "transpose", [ht, c["ident"]], [tpp], out=tpp[:, k, :], in_=ht[:, k * 128:(k + 1) * 128],
                         identity=c["ident"][:], inc=(k == 7))
                hTt = hT[t % 2]
                kb.I("act", "copy", [tpp], [hTt], out=hTt[:], in_=tpp[:])
                lgp = lg[t % 2]
                for k in range(8):
                    kb.I("pe", "matmul", [hTt, wr], [lgp], lgp[:], lhsT=hTt[:, k, :], rhs=wr[:, k, :],
                         start=(k == 0), stop=(k == 7), inc=(k == 7))
                kb.I("dve", "tensor_tensor", [lgp, br], [L], out=L[:, t, :], in0=lgp[:], in1=br[:], op=ALU.add)

            NE = ntile * 32
            GL = L[:, :, 0:4]
            EL = L[:, :, 4:36]
            gmax = kb.sb("gmax", [128, ntile])
            t4 = kb.sb("t4", [128, ntile, 4])
            goh = kb.sb("goh", [128, ntile, 4])
            gg = kb.sb("gg", [128, ntile])
            ELm = kb.sb("ELm", [128, ntile, 32])
            EL2 = kb.sb("EL2", [128, ntile, 32])
            oh1 = kb.sb("oh1", [128, ntile, 32])
            oh2 = kb.sb("oh2", [128, ntile, 32])
            m1 = kb.sb("m1", [128, ntile])
            m2 = kb.sb("m2", [128, ntile])
            tmp = kb.sb("tmp", [128, ntile])
            V = "dve"
            kb.I(V, "tensor_reduce", [L], [gmax], out=gmax[:], in_=GL, axis=AX.X, op=ALU.max)
            bc4 = lambda a: a[:].unsqueeze(2).to_broadcast([128, ntile, 4])
            bc32 = lambda a: a[:].unsqueeze(2).to_broadcast([128, ntile, 32])
            kb.I(V, "tensor_tensor", [L, gmax], [goh], out=goh[:], in0=GL, in1=bc4(gmax), op=ALU.is_equal)
            kb.I(V, "tensor_tensor", [L, gmax], [t4], out=t4[:], in0=GL, in1=bc4(gmax), op=ALU.subtract)
            kb.I("act", "activation", [t4], [t4], out=t4[:], in_=t4[:], func=AF.Exp)
            kb.I(V, "tensor_reduce", [t4], [gg], out=gg[:], in_=t4[:], axis=AX.X, op=ALU.add)
            kb.I(V, "reciprocal", [gg], [gg], out=gg[:], in_=gg[:])
            kb.I(V, "tensor_scalar", [goh], [t4], out=t4[:], in0=goh[:], scalar1=-NEG, scalar2=NEG,
                 op0=ALU.mult, op1=ALU.add)
            kb.I(V, "tensor_tensor", [L, t4], [ELm], out=ELm[:].rearrange("p t (g e) -> p t g e", g=4),
                 in0=EL.rearrange("p t (g e) -> p t g e", g=4),
                 in1=t4[:].unsqueeze(3).to_broadcast([128, ntile, 4, 8]), op=ALU.add)
            kb.I(V, "tensor_reduce", [ELm], [m1], out=m1[:], in_=ELm[:], axis=AX.X, op=ALU.max)
            kb.I(V, "tensor_tensor", [ELm, m1], [oh1], out=oh1[:], in0=ELm[:], in1=bc32(m1), op=ALU.is_equal)
            kb.I(V, "scalar_tensor_tensor", [oh1, ELm], [EL2], out=EL2[:], in0=oh1[:], scalar=NEG, in1=ELm[:],
                 op0=ALU.mult, op1=ALU.add)
            kb.I(V, "tensor_reduce", [EL2], [m2], out=m2[:], in_=EL2[:], axis=AX.X, op=ALU.max)
            kb.I(V, "tensor_tensor", [EL2, m2], [oh2], out=oh2[:], in0=EL2[:], in1=bc32(m2), op=ALU.is_equal)
            kb.I(V, "tensor_tensor", [m1, m2], [tmp], out=tmp[:], in0=m2[:], in1=m1[:], op=ALU.subtract)
            kb.I("act", "activation", [tmp], [tmp], out=tmp[:], in_=tmp[:], func=AF.Exp)
            kb.I(V, "tensor_scalar", [tmp], [m1], out=m1[:], in0=tmp[:], scalar1=1.0, scalar2=None, op0=ALU.add)
            kb.I(V, "reciprocal", [m1], [m1], out=m1[:], in_=m1[:])
            kb.I(V, "tensor_tensor", [tmp, m1], [m2], out=m2[:], in0=tmp[:], in1=m1[:], op=ALU.mult)
            kb.I(V, "tensor_tensor", [m1, gg], [g1], out=g1[:], in0=m1[:], in1=gg[:], op=ALU.mult)
            kb.I(V, "tensor_tensor", [m2, gg], [g2], out=g2[:], in0=m2[:], in1=gg[:], op=ALU.mult)
            OH = ELm
            kb.I(V, "tensor_tensor", [oh1, oh2], [OH], out=OH[:], in0=oh1[:], in1=oh2[:], op=ALU.add)
            RK = kb.sb("RK", [128, NE])
            CT = kb.sb("CT", [128, ntile, 32])
            OHf = OH[:].rearrange("p t e -> p (t e)")
            CTf = CT[:].rearrange("p t e -> p (t e)")
            pr = [kb.ps("pr%d" % i, [128, 512]) for i in range(2)]
            ci = 0
            for dst, lhs in ((RK[:], c["sut"]), (CTf, c["ones"])):
                for o in range(0, NE, 512):
                    n = min(512, NE - o)
                    p = pr[ci % 2]
                    ci += 1
                    kb.I("pe", "matmul", [lhs, OH], [p], p[:, 0:n], lhsT=lhs[:], rhs=OHf[:, o:o + n],
                         start=True, stop=True)
                    kb.I("act", "copy", [p], [RK if dst is not CTf else CT], out=dst[:, o:o + n], in_=p[:, 0:n])
            base = kb.sb("base", [128, ntile, 32])
            kb.I(V, "memset", [], [base], base[:, 0, :], 0.0)
            for t in range(1, ntile):
                kb.I(V, "tensor_tensor", [base, CT], [base], out=base[:, t, :], in0=base[:, t - 1, :],
                     in1=CT[:, t - 1, :], op=ALU.add)
            tot = kb.sb("tot", [128, 32])
            pad = kb.sb("pad", [128, 32])
            pend = kb.sb("pend", [128, 32])
            one32 = kb.sb("one32", [128, 32])
            kb.I(V, "memset", [], [one32], one32[:], 1.0)
            kb.I(V, "tensor_tensor", [base, CT], [tot], out=tot[:], in0=base[:, ntile - 1, :], in1=CT[:, ntile - 1, :],
                 op=ALU.add)
            thr = kb.sb("thr", [128, nblk])
            kb.I(V, "tensor_scalar", [c["ramp"]], [thr], out=thr[:], in0=c["ramp"][:, 0:nblk], scalar1=float(BLK),
                 scalar2=None, op0=ALU.mult)
            cmp0 = kb.sb("cmp0", [128, 32, nblk])
            kb.I(V, "tensor_tensor", [tot, thr], [cmp0], out=cmp0[:],
                 in0=tot[:].unsqueeze(2).to_broadcast([128, 32, nblk]),
                 in1=thr[:].unsqueeze(1).to_broadcast([128, 32, nblk]), op=ALU.is_gt)
            kb.I(V, "tensor_reduce", [cmp0], [pad], out=pad[:], in_=cmp0[:], axis=AX.X, op=ALU.add)
            kb.I(V, "tensor_scalar", [pad], [pad], out=pad[:], in0=pad[:], scalar1=float(BLK), scalar2=None, op0=ALU.mult)
            kb.I(V, "tensor_tensor_scan", [one32, pad], [pend], out=pend[:], data0=one32[:], data1=pad[:], initial=0.0,
                 op0=ALU.mult, op1=ALU.add)
            kb.I(V, "tensor_tensor", [pend, pad], [pad], out=pad[:], in0=pend[:], in1=pad[:], op=ALU.subtract)
            cmp = kb.sb("cmp", [128, nblk, 32])
            bef = kb.sb("bef", [128, nblk])
            kb.I(V, "tensor_scalar", [c["ramp"]], [bef], out=bef[:], in0=c["ramp"][:, 0:nblk], scalar1=float(BLK),
                 scalar2=None, op0=ALU.mult)
            kb.I(V, "tensor_tensor", [pend, bef], [cmp], out=cmp[:],
                 in0=pend[:].unsqueeze(1).to_broadcast([128, nblk, 32]),
                 in1=bef[:].unsqueeze(2).to_broadcast([128, nblk, 32]), op=ALU.is_le)
            kb.I(V, "tensor_reduce", [cmp], [bef], out=bef[:], in_=cmp[:], axis=AX.X, op=ALU.add)
            kb.I(V, "tensor_scalar", [bef], [bef], out=bef[:], in0=bef[:], scalar1=31.0, scalar2=None, op0=ALU.min)
            ig = kb.sb("ig", [128, nblk])
            kb.I(V, "tensor_scalar", [bef, c["pidx"]], [ig], out=ig[:], in0=bef[:], scalar1=128.0, scalar2=c["pidx"][:, 0:1],
                 op0=ALU.mult, op1=ALU.add)
            usedf = kb.sb("usedf", [128, nblk])
            kb.I(V, "tensor_scalar", [thr, pend], [usedf], out=usedf[:], in0=thr[:], scalar1=pend[:, 31:32], scalar2=None, op0=ALU.is_lt)
            kb.I(V, "tensor_tensor", [ig, usedf], [ig], out=ig[:], in0=ig[:], in1=usedf[:], op=ALU.mult)
            kb.I(V, "tensor_scalar", [usedf], [usedf], out=usedf[:], in0=usedf[:], scalar1=-8192.0, scalar2=8192.0, op0=ALU.mult, op1=ALU.add)
            kb.I(V, "tensor_tensor", [ig, usedf], [ig], out=ig[:], in0=ig[:], in1=usedf[:], op=ALU.add)
            kb.I(V, "tensor_copy", [ig], [idxe], out=idxe[:], in_=ig[:])
            SL = EL2
            RK3 = RK[:].rearrange("p (t e) -> p t e", e=32)
            kb.I(V, "tensor_tensor", [RK, base], [SL], out=SL[:], in0=RK3, in1=base[:], op=ALU.add)
            kb.I(V, "tensor_tensor", [SL, pad], [SL], out=SL[:], in0=SL[:],
                 in1=pad[:].unsqueeze(1).to_broadcast([128, ntile, 32]), op=ALU.add)
            for oh, si in ((oh1, s1i), (oh2, s2i)):
                kb.I(V, "tensor_tensor", [SL, oh], [oh], out=oh[:], in0=SL[:], in1=oh[:], op=ALU.mult)
                kb.I(V, "tensor_reduce", [oh], [tmp], out=tmp[:], in_=oh[:], axis=AX.X, op=ALU.add)
                kb.I(V, "tensor_copy", [tmp], [si], out=si[:], in_=tmp[:])

        with kb.scope():
            hbs = [kb.sb("hb%d" % i, [128, D], BF16) for i in range(2)]
            for t in range(ntile):
                ht = hts[t % 2]
                hb = hbs[t % 2]
                kb.dma("sp", ht[:], h_d[t * 128:(t + 1) * 128, :])
                kb.I("act", "copy", [ht], [hb], out=hb[:], in_=ht[:])
                for si in (s1i, s2i):
                    kb.dma("pool", xs_d, hb[:], reads=[hb, si], writes=[xs_d], indirect=dict(
                        out_offset=bass.IndirectOffsetOnAxis(ap=si[:, t:t + 1], axis=0), in_offset=None))

        with kb.scope():
            xin = [[kb.sb("xin%d_%d" % (i, s), [128, D], BF16) for s in range(BS)] for i in range(2)]
            xT = [kb.sb("xT%d" % i, [128, 8, BLK], BF16) for i in range(2)]
            tps = [kb.ps("tps%d" % i, [128, 8, 128], BF16) for i in range(2)]
            stg = [kb.sb("stg%d" % i, [128, 4096]) for i in range(3)]
            wgb = [kb.sb("wgb%d" % i, [128, 8, 512], BF16) for i in range(2)]
            wub = [kb.sb("wub%d" % i, [128, 8, 512], BF16) for i in range(2)]
            wdb = [kb.sb("wdb%d" % i, [128, 4, 1024], BF16) for i in range(2)]
            hgp = [kb.ps("hgp%d" % i, [128, BLK]) for i in range(2)]
            hup = [kb.ps("hup%d" % i, [128, BLK]) for i in range(2)]
            yp = [kb.ps("yp%d" % i, [128, 512]) for i in range(2)]
            sg = [kb.sb("sg%d" % i, [128, BLK]) for i in range(2)]
            hTb = [kb.sb("hTb%d" % i, [128, 4, BLK], BF16) for i in range(2)]
            yb = [kb.sb("yb%d" % i, [128, D]) for i in range(2)]
            sti = 0
            cast_eng = ["dve", "act"]
            bc_reg = nc.gpsimd.alloc_register("moe_bc_%d" % kb.uid)
            nc.gpsimd.reg_mov(bc_reg, 4095)

            def load_weights(b):
                nonlocal sti
                par = b % 2
                for src, dstt in ((wg_d, wgb[par]), (wu_d, wub[par]), (wd_d, wdb[par])):
                    st = stg[sti % 3]
                    kb.dma("pool", st[:], src, reads=[idxe], writes=[st],
                           indirect=dict(out_offset=None, in_offset=bass.IndirectOffsetOnAxis(ap=idxe[:, b:b + 1], axis=0),
                                         bounds_check=bc_reg, oob_is_err=False))
                    dflat = dstt[:].rearrange("p a b -> p (a b)")
                    for half in range(2):
                        ce = cast_eng[(2 * sti + half) % 2]
                        kb.copy(ce, [st], [dstt], dflat[:, half * 2048:(half + 1) * 2048], st[:, half * 2048:(half + 1) * 2048])
                    sti += 1

            def load_tokens(b):
                par = b % 2
                for s in range(BS):
                    xi = xin[par][s]
                    kb.dma("act", xi[:], xs_d[b * BLK + s * 128: b * BLK + (s + 1) * 128, :])
                    tpp = tps[s % 2]
                    for k in range(8):
                        kb.I("pe", "transpose", [xi, c["identb"]], [tpp], out=tpp[:, k, :], in_=xi[:, k * 128:(k + 1) * 128],
                             identity=c["identb"][:], inc=(k == 7))
                    kb.I("dve", "tensor_copy", [tpp], [xT[par]], out=xT[par][:, :, s * 128:(s + 1) * 128], in_=tpp[:])

            def gate_up(b):
                par = b % 2
                for f in range(4):
                    hg = hgp[f % 2]
                    hu = hup[f % 2]
                    for k in range(8):
                        kb.I("pe", "matmul", [wgb[par], xT[par]], [hg], hg[:], lhsT=wgb[par][:, k, f * 128:(f + 1) * 128],
                             rhs=xT[par][:, k, :], start=(k == 0), stop=(k == 7), inc=(k == 7))
                    for k in range(8):
                        kb.I("pe", "matmul", [wub[par], xT[par]], [hu], hu[:], lhsT=wub[par][:, k, f * 128:(f + 1) * 128],
                             rhs=xT[par][:, k, :], start=(k == 0), stop=(k == 7), inc=(k == 7))
                    sgt = sg[f % 2]
                    kb.I("act", "activation", [hg], [sgt], out=sgt[:], in_=hg[:], func=AF.Silu)
                    kb.I("dve", "tensor_tensor", [sgt, hu], [hTb[par]], out=hTb[par][:, f, :], in0=sgt[:], in1=hu[:], op=ALU.mult)

            def down(b):
                par = b % 2
                for s in range(BS):
                    ybt = yb[s % 2]
                    for dh in range(2):
                        y = yp[dh]
                        for f in range(4):
                            kb.I("pe", "matmul", [hTb[par], wdb[par]], [y], y[:], lhsT=hTb[par][:, f, s * 128:(s + 1) * 128],
                                 rhs=wdb[par][:, f, dh * 512:(dh + 1) * 512], start=(f == 0), stop=(f == 3), inc=(f == 3))
                        kb.I("act" if dh == 0 else "dve", "tensor_copy" if dh else "copy", [y], [ybt],
                             out=ybt[:, dh * 512:(dh + 1) * 512], in_=y[:])
                    kb.dma("sp", ys_d[b * BLK + s * 128: b * BLK + (s + 1) * 128, :], ybt[:])

            load_weights(0)
            load_tokens(0)
            for b in range(nblk):
                if b + 1 < nblk:
                    load_weights(b + 1)
                gate_up(b)
                if b + 1 < nblk:
                    load_tokens(b + 1)
                down(b)

        with kb.scope():
            NS = 3
            y1 = [kb.sb("y1_%d" % i, [128, D]) for i in range(NS)]
            y2 = [kb.sb("y2_%d" % i, [128, D]) for i in range(NS)]
            hts5 = [kb.sb("ht5_%d" % i, [128, D]) for i in range(NS)]
            acc = [kb.sb("acc%d" % i, [128, D]) for i in range(2)]
            outt = [kb.sb("outt%d" % i, [128, D]) for i in range(2)]
            st6 = kb.sb("st6", [128, 2, 6])
            mv = kb.sb("mv", [128, 4])

            def issue(t):
                p = t % NS
                kb.dma("sp", hts5[p][:], h_d[t * 128:(t + 1) * 128, :])
                for yy, si in ((y1[p], s1i), (y2[p], s2i)):
                    kb.dma("pool", yy[:], ys_d, reads=[ys_d, si], writes=[yy], indirect=dict(
                        out_offset=None, in_offset=bass.IndirectOffsetOnAxis(ap=si[:, t:t + 1], axis=0)))
            for t in range(min(NS - 1, ntile)):
                issue(t)
            for t in range(ntile):
                if t + NS - 1 < ntile:
                    issue(t + NS - 1)
                p = t % NS
                ht = hts5[p]
                a = acc[t % 2]
                kb.I("act", "mul", [y1[p], g1], [a], out=a[:], in_=y1[p][:], mul=g1[:, t:t + 1])
                kb.I("dve", "scalar_tensor_tensor", [y2[p], g2, a], [a], out=a[:], in0=y2[p][:], scalar=g2[:, t:t + 1],
                     in1=a[:], op0=ALU.mult, op1=ALU.add)
                kb.I("dve", "scalar_tensor_tensor", [ht, a], [a], out=a[:], in0=ht[:], scalar=ALPHA, in1=a[:],
                     op0=ALU.mult, op1=ALU.add)
                layer_norm_tile(kb, a, g_bc, b_bc, outt[t % 2], st6, mv)
                kb.dma("sp", out_d[t * 128:(t + 1) * 128, :], outt[t % 2][:])

def make_consts():
    import ml_dtypes
    i = np.arange(128)
    return {
        "ident": np.eye(128, dtype=np.float32),
        "identb": np.eye(128, dtype=np.float32).astype(ml_dtypes.bfloat16),
        "ones": np.ones((128, 128), np.float32),
        "sut": (i[:, None] < i[None, :]).astype(np.float32),
        "ramp": np.broadcast_to(i[None, :].astype(np.float32), (128, 128)).copy(),
        "pidx": np.broadcast_to(i[:, None].astype(np.float32), (128, 128)).copy(),
        "cmask": np.where(i[None, :] > i[:, None], NEG, 0.0).astype(np.float32),
        "onesb": np.ones((128, 128), np.float32).astype(ml_dtypes.bfloat16),
    }


CHUNKS = [(0, 512), (512, 512), (1024, 512), (1536, 512), (2048, 128)]
EV_Q, EV_CKV, EV_QI, EV_KI, EV_WI, EV_GATE, EV_XB = 0, 512, 768, 1280, 1344, 1352, 1864


def proj_fm(kb, ps2, cnt, wb, col0, hT, dst_fn, evac):
    for (t0, n) in CHUNKS:
        p = ps2[cnt[0] % 2]
        for k in range(8):
            kb.I("pe", "matmul", [wb, hT], [p], p[:, 0:n], lhsT=wb[:, k, col0:col0 + 128], rhs=hT[:, k, t0:t0 + n],
                 start=(k == 0), stop=(k == 7), inc=(k == 7))
        evac(cnt[0], p[:, 0:n], t0, n)
        cnt[0] += 1


def build_even(kb, c, h_d, win_d, wout_d, wuk_d, wuv_d, rgw_d, vec_d, lng_d, lnb_d, out_d, nseq=SPC):
    with kb.scope():
        winb = kb.sb("winb", [128, 8, 2376], BF16)
        wk2 = kb.sb("wk2", [128, 8, 128], BF16)
        wukb = kb.sb("wukb", [128, 2, 128], BF16)
        wuvb = kb.sb("wuvb", [128, 2, 128], BF16)
        rgwb = kb.sb("rgwb", [128, 8, 128], BF16)
        vec = kb.sb("vec", [128, 40])
        cc = kb.sb("cc", [128, 4])
        g_bc = kb.sb("g_bc", [128, D])
        b_bc = kb.sb("b_bc", [128, D])
        kb.dma("sp", g_bc[:], lng_d.partition_broadcast(128))
        kb.dma("sp", b_bc[:], lnb_d.partition_broadcast(128))
        kb.dma("sp", vec[:], vec_d)
        with kb.scope():
            stg = [kb.sb("wstg%d" % i, [128, 2376]) for i in range(2)]
            for k in range(8):
                st = stg[k % 2]
                kb.dma("sp", st[:], win_d[k * 128:(k + 1) * 128, :])
                kb.copy("act" if k % 2 else "pool", [st], [winb], winb[:, k, :], st[:])
            for j in range(2):
                kb.I("dve", "tensor_copy", [winb], [wk2], out=wk2[:, :, j * 64:(j + 1) * 64], in_=winb[:, :, EV_KI:EV_KI + 64])
            st = stg[0]
            kb.dma("sp", st[:, 0:256], wuk_d.rearrange("(c p) d -> p c d", p=128))
            kb.I("dve", "tensor_copy", [st], [wukb], out=wukb[:], in_=st[:, 0:256].rearrange("p (c d) -> p c d", c=2))
            st = stg[1]
            kb.dma("sp", st[:, 0:256], wuv_d.rearrange("(c p) d -> p c d", p=128))
            kb.I("dve", "tensor_copy", [st], [wuvb], out=wuvb[:], in_=st[:, 0:256].rearrange("p (c d) -> p c d", c=2))
            st = stg[0]
            kb.dma("sp", st[:, 0:1024], rgw_d)
            kb.I("dve", "tensor_copy", [st], [rgwb], out=rgwb[:], in_=st[:, 0:1024].rearrange("p (c d) -> p c d", c=8))
            kb.I("act", "activation", [vec], [cc], out=cc[:], in_=vec[:, 28:32], func=AF.Exp, scale=-1.0)
            kb.I("act", "activation", [cc], [cc], out=cc[:], in_=cc[:], func=AF.Ln, bias=1.0)
            kb.I("dve", "tensor_scalar", [cc], [cc], out=cc[:], in0=cc[:], scalar1=-8.0, scalar2=None, op0=ALU.mult)
        cw = lambda j, ch: vec[:, j * 4 + ch: j * 4 + ch + 1]
        cb = lambda ch: vec[:, 16 + ch:17 + ch]
        ba = lambda ch: vec[:, 20 + ch:21 + ch]
        bx = lambda ch: vec[:, 24 + ch:25 + ch]
        kvn = lambda ch: vec[:, 32 + ch:33 + ch]

        for s in range(nseq):
            r0 = s * TP
            with kb.scope():
                hT = kb.sb("hT", [128, 8, TP], BF16)
                qT = kb.sb("qT", [128, 4, TP], BF16)
                qiT = kb.sb("qiT", [128, 4, TP], BF16)
                kiT2 = kb.sb("kiT2", [128, TP], BF16)
                kT = kb.sb("kT", [128, TP], BF16)
                vtok = kb.sb("vtok", [128, NT, 130], BF16)
                wq = kb.sb("wq", [128, NT, 8])
                kb.I("pool", "memset", [], [vtok], vtok[:, :, 128:130], 1.0)
                with kb.scope():
                    gateT = kb.sb("gateT", [128, 4, TP], BF16)
                    xbT = kb.sb("xbT", [128, 4, TP], BF16)
                    with kb.scope():
                        hts = [kb.sb("eht%d" % i, [128, D]) for i in range(2)]
                        tp = [kb.ps("etp%d" % i, [128, 8, 128]) for i in range(2)]
                        for t in range(NT):
                            ht = hts[t % 2]
                            kb.dma("sp", ht[:], h_d[r0 + t * 128: r0 + (t + 1) * 128, :])
                            tpp = tp[t % 2]
                            for k in range(8):
                                kb.I("pe", "transpose", [ht, c["ident"]], [tpp], out=tpp[:, k, :],
                                     in_=ht[:, k * 128:(k + 1) * 128], identity=c["ident"][:], inc=(k == 7))
                            kb.copy("act" if t % 2 else "dve", [tpp], [hT], hT[:, :, t * 128:(t + 1) * 128], tpp[:])
                    with kb.scope():
                        ckvT = kb.sb("ckvT", [128, 2, TP], BF16)
                        latT = kb.sb("latT", [128, 2, TP], BF16)
                        ps2 = [kb.ps("pp%d" % i, [128, 512]) for i in range(2)]
                        cnt = [0]

                        def mk_evac(dst, ci):
                            def ev(n_, p, t0, n):
                                kb.copy("act" if n_ % 2 else "dve", [p], [dst], dst[:, ci, t0:t0 + n] if ci is not None else dst[:, t0:t0 + n], p)
                            return ev
                        for ci in range(4):
                            proj_fm(kb, ps2, cnt, winb, EV_Q + ci * 128, hT, None, mk_evac(qT, ci))
                            proj_fm(kb, ps2, cnt, winb, EV_QI + ci * 128, hT, None, mk_evac(qiT, ci))
                            proj_fm(kb, ps2, cnt, winb, EV_GATE + ci * 128, hT, None, mk_evac(gateT, ci))
                            proj_fm(kb, ps2, cnt, winb, EV_XB + ci * 128, hT, None, mk_evac(xbT, ci))
                        for ci in range(2):
                            proj_fm(kb, ps2, cnt, winb, EV_CKV + ci * 128, hT, None, mk_evac(ckvT, ci))
                        proj_fm(kb, ps2, cnt, wk2, 0, hT, None, mk_evac(kiT2, None))
                        wps = [kb.ps("wps%d" % i, [128, 8]) for i in range(2)]
                        for t in range(NT):
                            p = wps[t % 2]
                            for k in range(8):
                                kb.I("pe", "matmul", [hT, winb], [p], p[:], lhsT=hT[:, k, t * 128:(t + 1) * 128],
                                     rhs=winb[:, k, EV_WI:EV_WI + 8], start=(k == 0), stop=(k == 7), inc=(k == 7))
                            kb.copy("act", [p], [wq], wq[:, t, :], p[:])
                        sq = kb.sb("sq", [128, 2, 512], BF16)
                        rstd = kb.sb("rstd", [128, 512])
                        for (t0, n) in CHUNKS:
                            kb.I("act", "activation", [ckvT], [sq], out=sq[:, :, 0:n], in_=ckvT[:, :, t0:t0 + n], func=AF.Square)
                            p = ps2[cnt[0] % 2]
                            cnt[0] += 1
                            for ci in range(2):
                                kb.I("pe", "matmul", [c["onesb"], sq], [p], p[:, 0:n], lhsT=c["onesb"][:], rhs=sq[:, ci, 0:n],
                                     start=(ci == 0), stop=(ci == 1), inc=(ci == 1))
                            kb.I("dve", "tensor_scalar", [p], [rstd], out=rstd[:, 0:n], in0=p[:, 0:n], scalar1=1.0 / 256, scalar2=1e-6,
                                 op0=ALU.mult, op1=ALU.add)
                            kb.I("act", "sqrt", [rstd], [rstd], out=rstd[:, 0:n], in_=rstd[:, 0:n])
                            kb.I("dve", "reciprocal", [rstd], [rstd], out=rstd[:, 0:n], in_=rstd[:, 0:n])
                            for ci in range(2):
                                kb.I("dve", "scalar_tensor_tensor", [ckvT, vec, rstd], [latT], out=latT[:, ci, t0:t0 + n],
                                     in0=ckvT[:, ci, t0:t0 + n], scalar=kvn(ci), in1=rstd[:, 0:n], op0=ALU.mult, op1=ALU.mult)
                            p = ps2[cnt[0] % 2]
                            cnt[0] += 1
                            for ci in range(2):
                                kb.I("pe", "matmul", [wukb, latT], [p], p[:, 0:n], lhsT=wukb[:, ci, :], rhs=latT[:, ci, t0:t0 + n],
                                     start=(ci == 0), stop=(ci == 1), inc=(ci == 1))
                            kb.copy("act", [p], [kT], kT[:, t0:t0 + n], p[:, 0:n])
                        for t in range(NT):
                            p = ps2[cnt[0] % 2]
                            cnt[0] += 1
                            for ci in range(2):
                                kb.I("pe", "matmul", [latT, wuvb], [p], p[:, 0:128], lhsT=latT[:, ci, t * 128:(t + 1) * 128],
                                     rhs=wuvb[:, ci, :], start=(ci == 0), stop=(ci == 1), inc=(ci == 1))
                            kb.copy("dve", [p], [vtok], vtok[:, t, 0:128], p[:, 0:128])
                    mixT = hT
                    with kb.scope():
                        HS = TP // 2
                        F = lambda nm: kb.sb(nm, [128, HS])
                        xr, rr, ii, aa, uu, hh, gl = F("xr"), F("rr"), F("ii"), F("aa"), F("uu"), F("hh"), F("gl")
                        xrb = kb.sb("xrb", [128, HS], BF16)
                        carry = kb.sb("carry", [128, 4])
                        gps = [kb.ps("gps%d" % i, [128, 512]) for i in range(4)]
                        gi = 0
                        for ch in range(4):
                            for hf in range(2):
                                o = hf * HS
                                x = xbT[:, ch, :]
                                kb.I("dve", "tensor_scalar", [xbT, vec], [xr], out=xr[:], in0=x[:, o:o + HS], scalar1=cw(3, ch), scalar2=cb(ch),
                                     op0=ALU.mult, op1=ALU.add)
                                for d in (1, 2, 3):
                                    lo = d if hf == 0 else 0
                                    kb.I("dve", "scalar_tensor_tensor", [xbT, vec, xr], [xr], out=xr[:, lo:HS], in0=x[:, o + lo - d:o + HS - d],
                                         scalar=cw(3 - d, ch), in1=xr[:, lo:HS], op0=ALU.mult, op1=ALU.add)
                                kb.copy("pool", [xr], [xrb], xrb[:], xr[:])
                                for (t0, n) in ((0, 512), (512, 512), (1024, 64)):
                                    pa = gps[gi % 4]
                                    px = gps[(gi + 1) % 4]
                                    gi += 2
                                    kb.I("pe", "matmul", [rgwb, xrb], [pa], pa[:, 0:n], lhsT=rgwb[:, ch, :], rhs=xrb[:, t0:t0 + n], start=True, stop=True)
                                    kb.I("pe", "matmul", [rgwb, xrb], [px], px[:, 0:n], lhsT=rgwb[:, 4 + ch, :], rhs=xrb[:, t0:t0 + n], start=True, stop=True)
                                    kb.I("act", "activation", [pa, vec], [rr], out=rr[:, t0:t0 + n], in_=pa[:, 0:n], func=AF.Sigmoid, bias=ba(ch))
                                    kb.I("act", "activation", [px, vec], [ii], out=ii[:, t0:t0 + n], in_=px[:, 0:n], func=AF.Sigmoid, bias=bx(ch))
                                kb.I("act", "activation", [rr, cc], [aa], out=aa[:], in_=rr[:], func=AF.Exp, scale=cc[:, ch:ch + 1])
                                kb.I("pool", "tensor_tensor", [aa], [uu], out=uu[:], in0=aa[:], in1=aa[:], op=ALU.mult)
                                kb.I("act", "activation", [uu], [uu], out=uu[:], in_=uu[:], func=AF.Sqrt, scale=-1.0, bias=1.0)
                                kb.I("pool", "tensor_tensor", [ii, xr], [ii], out=ii[:], in0=ii[:], in1=xr[:], op=ALU.mult)
                                kb.I("pool", "tensor_tensor", [uu, ii], [uu], out=uu[:], in0=uu[:], in1=ii[:], op=ALU.mult)
                                kb.I("dve", "tensor_tensor_scan", [aa, uu, carry], [hh], out=hh[:], data0=aa[:], data1=uu[:],
                                     initial=(0.0 if hf == 0 else carry[:, ch:ch + 1]), op0=ALU.mult, op1=ALU.add)
                                if hf == 0:
                                    kb.I("dve", "tensor_copy", [hh], [carry], out=carry[:, ch:ch + 1], in_=hh[:, HS - 1:HS])
                                g = gateT[:, ch, o:o + HS]
                                kb.I("act", "activation", [gateT], [gl], out=gl[:], in_=g, func=AF.Square)
                                kb.I("pool", "tensor_scalar", [gl], [gl], out=gl[:], in0=gl[:], scalar1=0.044715, scalar2=1.0, op0=ALU.mult, op1=ALU.add)
                                kb.I("pool", "tensor_tensor", [gl, gateT], [gl], out=gl[:], in0=gl[:], in1=g, op=ALU.mult)
                                kb.I("act", "activation", [gl], [gl], out=gl[:], in_=gl[:], func=AF.Sigmoid, scale=1.5957691216)
                                kb.I("pool", "tensor_tensor", [gl, gateT], [gl], out=gl[:], in0=gl[:], in1=g, op=ALU.mult)
                                kb.I("dve", "tensor_tensor", [hh, gl], [mixT], out=mixT[:, 4 + ch, o:o + HS], in0=hh[:], in1=gl[:], op=ALU.mult)
                with kb.scope():
                    score = [kb.sb("score%d" % i, [128, TP]) for i in range(2)]
                    mask = [kb.sb("mask%d" % i, [128, TP], BF16) for i in range(2)]
                    m8 = [kb.sb("m8_%d" % i, [128, 8]) for i in range(2)]
                    work = kb.sb("work", [128, TP])
                    thrc = kb.sb("thrc", [128, 1])
                    rl = [kb.sb("rl%d" % i, [128, 512]) for i in range(2)]
                    lgt = [kb.sb("lgt%d" % i, [128, TP]) for i in range(2)]
                    ee = [kb.sb("ee%d" % i, [128, TP], BF16) for i in range(2)]
                    pT = [kb.sb("pT%d" % i, [128, NT, 128], BF16) for i in range(2)]
                    sm = [kb.sb("sm%d" % i, [128, 4]) for i in range(2)]
                    on = [kb.sb("on%d" % i, [128, 128], BF16) for i in range(2)]
                    sps = [kb.ps("sps%d" % i, [128, 512]) for i in range(2)]
                    lps = [kb.ps("lps%d" % i, [128, 512]) for i in range(2)]
                    tps = [kb.ps("atp%d" % i, [128, 8, 128], BF16) for i in range(2)]
                    ops = kb.ps("ops", [128, 132])
                    otp = kb.ps("otp", [128, 128], BF16)
                    kb.I("dve", "memset", [], [thrc], thrc[:], -1.0e29)
                    cnts = {"si": 0, "ti": 0, "li": 0}

                    def indexer(i):
                        sc = score[i % 2]
                        nk = (i + 1) * 128
                        qs = slice(i * 128, (i + 1) * 128)
                        for t0 in range(0, nk, 512):
                            n = min(512, nk - t0)
                            for h in range(8):
                                p = sps[cnts["si"] % 2]
                                r = rl[cnts["si"] % 2]
                                cnts["si"] += 1
                                pr = slice((h % 2) * 64, (h % 2) * 64 + 64)
                                kb.I("pe", "matmul", [qiT, kiT2], [p], p[:, 0:n], lhsT=qiT[pr, h // 2, qs], rhs=kiT2[pr, t0:t0 + n],
                                     start=True, stop=True)
                                kb.I("act", "activation", [p], [r], out=r[:, 0:n], in_=p[:, 0:n], func=AF.Relu)
                                if h == 0:
                                    kb.I("dve", "tensor_scalar", [r, wq], [sc], out=sc[:, t0:t0 + n], in0=r[:, 0:n],
                                         scalar1=wq[:, i, 0:1], scalar2=None, op0=ALU.mult)
                                else:
                                    kb.I("dve", "scalar_tensor_tensor", [r, wq, sc], [sc], out=sc[:, t0:t0 + n], in0=r[:, 0:n],
                                         scalar=wq[:, i, h:h + 1], in1=sc[:, t0:t0 + n], op0=ALU.mult, op1=ALU.add)
                        kb.I("dve", "tensor_tensor", [sc, c["cmask"]], [sc], out=sc[:, qs], in0=sc[:, qs], in1=c["cmask"][:], op=ALU.add)

                    def topk(i):
                        sc, mk, m = score[i % 2], mask[i % 2], m8[i % 2]
                        nk = (i + 1) * 128
                        if i >= 2:
                            for it in range(32):
                                src = sc if it == 0 else work
                                kb.I("dve", "max", [src], [m], out=m[:], in_=src[:, 0:nk])
                                if it < 31:
                                    kb.I("dve", "match_replace", [m, src], [work], out=work[:, 0:nk], in_to_replace=m[:],
                                         in_values=src[:, 0:nk], imm_value=NEG)
                            kb.I("dve", "tensor_scalar", [sc, m], [mk], out=mk[:, 0:nk], in0=sc[:, 0:nk], scalar1=m[:, 7:8],
                                 scalar2=None, op0=ALU.is_ge)
                        else:
                            kb.I("dve", "tensor_scalar", [sc, thrc], [mk], out=mk[:, 0:nk], in0=sc[:, 0:nk], scalar1=thrc[:, 0:1],
                                 scalar2=None, op0=ALU.is_ge)

                    def qk(i, h):
                        nk = (i + 1) * 128
                        qs = slice(i * 128, (i + 1) * 128)
                        lg = lgt[h % 2]
                        for t0 in range(0, nk, 512):
                            n = min(512, nk - t0)
                            p = lps[cnts["li"] % 2]
                            cnts["li"] += 1
                            kb.I("pe", "matmul", [qT, kT], [p], p[:, 0:n], lhsT=qT[:, h, qs], rhs=kT[:, t0:t0 + n], start=True, stop=True)
                            kb.I("act", "mul", [p], [lg], out=lg[:, t0:t0 + n], in_=p[:, 0:n], mul=128.0 ** -0.5)

                    def attention(i):
                        mk = mask[i % 2]
                        nk = (i + 1) * 128
                        qs = slice(i * 128, (i + 1) * 128)
                        qk(i, 0)
                        for h in range(4):
                            lg, e_, s_, pT_, on_ = lgt[h % 2], ee[h % 2], sm[h % 2], pT[h % 2], on[h % 2]
                            kb.I("dve", "tensor_reduce", [lg], [s_], out=s_[:, 0:1], in_=lg[:, 0:nk], axis=AX.X, op=ALU.max)
                            kb.I("dve", "tensor_scalar", [s_], [s_], out=s_[:, 1:2], in0=s_[:, 0:1], scalar1=-1.0, scalar2=None, op0=ALU.mult)
                            kb.I("act", "activation", [lg, s_], [e_], out=e_[:, 0:nk], in_=lg[:, 0:nk], func=AF.Exp, bias=s_[:, 1:2])
                            if h < 3:
                                qk(i, h + 1)
                            kb.I("pool", "tensor_tensor", [e_, mk], [e_], out=e_[:, 0:nk], in0=e_[:, 0:nk], in1=mk[:, 0:nk], op=ALU.mult)
                            for j0 in range(0, i + 1, 8):
                                j1 = min(j0 + 8, i + 1)
                                tpp = tps[cnts["ti"] % 2]
                                cnts["ti"] += 1
                                for j in range(j0, j1):
                                    kb.I("pe", "transpose", [e_, c["identb"]], [tpp], out=tpp[:, j - j0, :], in_=e_[:, j * 128:(j + 1) * 128],
                                         identity=c["identb"][:], inc=(j == j1 - 1))
                                kb.copy("act" if cnts["ti"] % 2 else "dve", [tpp], [pT_], pT_[:, j0:j1, :], tpp[:, 0:j1 - j0, :])
                            for j in range(i + 1):
                                kb.I("pe", "matmul", [pT_, vtok], [ops], ops[:, 0:129], lhsT=pT_[:, j, :], rhs=vtok[:, j, 0:129], start=(j == 0), stop=(j == i),
                                     inc=(j == i))
                            kb.I("dve", "reciprocal", [ops], [s_], out=s_[:, 3:4], in_=ops[:, 128:129])
                            kb.I("dve", "tensor_scalar", [ops, s_], [on_], out=on_[:], in0=ops[:, 0:128], scalar1=s_[:, 3:4], scalar2=None, op0=ALU.mult)
                            kb.I("pe", "transpose", [on_, c["identb"]], [otp], out=otp[:], in_=on_[:], identity=c["identb"][:])
                            kb.copy("act", [otp], [mixT], mixT[:, h, qs], otp[:])

                    indexer(0)
                    topk(0)
                    for i in range(NT):
                        if i + 1 < NT:
                            indexer(i + 1)
                        attention(i)
                        if i + 1 < NT:
                            topk(i + 1)
                with kb.scope():
                    woutb = kb.sb("woutb", [128, 8, D], BF16)
                    stg = [kb.sb("ostg%d" % i, [128, D]) for i in range(2)]
                    for k in range(8):
                        kb.dma("sp", stg[k % 2][:], wout_d[k * 128:(k + 1) * 128, :])
                        kb.copy("act" if k % 2 else "pool", [stg[k % 2]], [woutb], woutb[:, k, :], stg[k % 2][:])
                    hts = [kb.sb("oht%d" % i, [128, D]) for i in range(2)]
                    acc = [kb.sb("oacc%d" % i, [128, D]) for i in range(2)]
                    outt = [kb.sb("oout%d" % i, [128, D]) for i in range(2)]
                    st6 = kb.sb("st6", [128, 2, 6])
                    mv = kb.sb("mv", [128, 4])
                    ops2 = [kb.ps("opp%d" % i, [128, 512]) for i in range(4)]
                    for t in range(NT):
                        ht = hts[t % 2]
                        a = acc[t % 2]
                        kb.dma("sp", ht[:], h_d[r0 + t * 128: r0 + (t + 1) * 128, :])
                        for hf in range(2):
                            p = ops2[(2 * t + hf) % 4]
                            for k in range(8):
                                kb.I("pe", "matmul", [mixT, woutb], [p], p[:], lhsT=mixT[:, k, t * 128:(t + 1) * 128],
                                     rhs=woutb[:, k, hf * 512:(hf + 1) * 512], start=(k == 0), stop=(k == 7), inc=(k == 7))
                            kb.I("dve", "scalar_tensor_tensor", [ht, p], [a], out=a[:, hf * 512:(hf + 1) * 512], in0=ht[:, hf * 512:(hf + 1) * 512],
                                 scalar=ALPHA, in1=p[:], op0=ALU.mult, op1=ALU.add)
                        layer_norm_tile(kb, a, g_bc, b_bc, outt[t % 2], st6, mv)
                        kb.dma("sp", out_d[r0 + t * 128: r0 + (t + 1) * 128, :], outt[t % 2][:])


def prep_even(inp, i):
    f = lambda a: np.ascontiguousarray(a, dtype=np.float32)
    pc = lambda v, n: f(v.reshape(n, 128).T)
    vec = np.zeros((128, 40), np.float32)
    cwt = inp["even_conv_w"][i]
    for j in range(4):
        vec[:, j * 4:(j + 1) * 4] = pc(cwt[j], 4)
    vec[:, 16:20] = pc(inp["even_conv_b"][i], 4)
    vec[:, 20:24] = pc(inp["even_rg_ba"][i], 4)
    vec[:, 24:28] = pc(inp["even_rg_bx"][i], 4)
    vec[:, 28:32] = pc(inp["even_rg_lambda"][i], 4)
    vec[:, 32:34] = pc(inp["even_kv_norm"][i], 2)
    rgw = np.zeros((128, 8, 128), np.float32)
    for g, nm in enumerate(("even_rg_wa", "even_rg_wx")):
        w = inp[nm][i]
        for n in range(8):
            o = (n % 2) * 64
            rgw[o:o + 64, g * 4 + n // 2, o:o + 64] = w[n]
    return {
        "ev_win": f(inp["even_w_in"][i]), "ev_wout": f(inp["even_w_out"][i]),
        "ev_wuk": f(inp["even_w_uk"][i]), "ev_wuv": f(inp["even_w_uv"][i]),
        "ev_rgw": f(rgw.reshape(128, 1024)), "ev_vec": vec,
        "ev_lng": f(inp["ln_g"][2 * i, 0]), "ev_lnb": f(inp["ln_b"][2 * i, 0]),
    }


def build_gdn(kb, c, h_d, gw_d, wab_d, gconv_d, alog_d, dtb_d, onorm_d, wout_d, lng_d, lnb_d, out_d, o_d, nseq=SPC, stop=0):
    with kb.scope():
        g_bc = kb.sb("g_bc", [128, D])
        b_bc = kb.sb("b_bc", [128, D])
        kb.dma("sp", g_bc[:], lng_d.partition_broadcast(128))
        kb.dma("sp", b_bc[:], lnb_d.partition_broadcast(128))
        wabb = kb.sb("wabb", [128, 8, 32], BF16)
        gconv = kb.sb("gconv", [128, 8, 4, 4])
        nA = kb.sb("nA", [128, 16])
        dtb = kb.sb("dtb", [128, 16])
        onorm = kb.sb("onorm", [128, 1])
        eps6 = kb.sb("eps6", [128, 1])
        kb.I("pool", "memset", [], [eps6], eps6[:], 1e-6)
        kb.dma("sp", gconv[:], gconv_d)
        kb.dma("sp", nA[:], alog_d.partition_broadcast(128))
        kb.dma("sp", dtb[:], dtb_d.partition_broadcast(128))
        kb.dma("sp", onorm[:], onorm_d.rearrange("(p o) -> p o", o=1))
        kb.I("act", "activation", [nA], [nA], out=nA[:], in_=nA[:], func=AF.Exp)
        kb.I("dve", "tensor_scalar", [nA], [nA], out=nA[:], in0=nA[:], scalar1=-1.0, scalar2=None, op0=ALU.mult)
        ut = kb.sb("ut", [128, 128])
        slt = kb.sb("slt", [128, 128])
        lmask = kb.sb("lmask", [128, 128])
        smask = kb.sb("smask", [128, 128])
        kb.I("dve", "tensor_tensor", [c["sut"], c["ident"]], [ut], out=ut[:], in0=c["sut"][:], in1=c["ident"][:], op=ALU.add)
        kb.I("dve", "tensor_scalar", [ut], [slt], out=slt[:], in0=ut[:], scalar1=-1.0, scalar2=1.0, op0=ALU.mult, op1=ALU.add)
        kb.I("dve", "tensor_copy", [slt], [smask], out=smask[:], in_=slt[:])
        kb.I("dve", "tensor_tensor", [slt, c["ident"]], [lmask], out=lmask[:], in0=slt[:], in1=c["ident"][:], op=ALU.add)
        with kb.scope():
            st = kb.sb("abstg", [128, 8, 32])
            kb.dma("sp", st[:], wab_d.rearrange("(k p) e -> p k e", p=128))
            kb.I("dve", "tensor_copy", [st], [wabb], out=wabb[:], in_=st[:])

        for s in range(nseq):
            r0 = s * TP
            with kb.scope():
                hT = kb.sb("hT", [128, 8, TP], BF16)
                with kb.scope():
                    hts = [kb.sb("ght%d" % i, [128, D]) for i in range(2)]
                    tp = [kb.ps("gtp%d" % i, [128, 8, 128]) for i in range(2)]
                    for t in range(NT):
                        ht = hts[t % 2]
                        kb.dma("sp", ht[:], h_d[r0 + t * 128: r0 + (t + 1) * 128, :])
                        tpp = tp[t % 2]
                        for k in range(8):
                            kb.I("pe", "transpose", [ht, c["ident"]], [tpp], out=tpp[:, k, :],
                                 in_=ht[:, k * 128:(k + 1) * 128], identity=c["ident"][:], inc=(k == 7))
                        kb.copy("act" if t % 2 else "dve", [tpp], [hT], hT[:, :, t * 128:(t + 1) * 128], tpp[:])
                if stop == 1:
                    return
                S3 = lambda nm: kb.sb(nm, [128, NT, 16])
                gg, beta, gc, egc, ekd, egl, bege = S3("gg"), S3("beta"), S3("gc"), S3("egc"), S3("ekd"), S3("egl"), S3("bege")
                with kb.scope():
                    abp = [kb.ps("abp%d" % i, [128, 32]) for i in range(2)]
                    ab = kb.sb("ab", [128, NT, 32])
                    tmp = S3("tmpa")
                    for t in range(NT):
                        p = abp[t % 2]
                        for k in range(8):
                            kb.I("pe", "matmul", [hT, wabb], [p], p[:], lhsT=hT[:, k, t * 128:(t + 1) * 128], rhs=wabb[:, k, :],
                                 start=(k == 0), stop=(k == 7), inc=(k == 7))
                        kb.copy("act", [p], [ab], ab[:, t, :], p[:])
                    bcT = lambda a: a[:].unsqueeze(1).to_broadcast([128, NT, 16])
                    kb.I("dve", "tensor_tensor", [ab, dtb], [gg], out=gg[:], in0=ab[:, :, 0:16], in1=bcT(dtb), op=ALU.add)
                    kb.I("dve", "tensor_scalar", [gg], [tmp], out=tmp[:], in0=gg[:], scalar1=-1.0, scalar2=None, op0=ALU.mult)
                    kb.I("dve", "tensor_tensor", [gg, tmp], [tmp], out=tmp[:], in0=gg[:], in1=tmp[:], op=ALU.max)
                    kb.I("act", "activation", [tmp], [tmp], out=tmp[:], in_=tmp[:], func=AF.Exp, scale=-1.0)
                    kb.I("act", "activation", [tmp], [tmp], out=tmp[:], in_=tmp[:], func=AF.Ln, bias=1.0)
                    kb.I("dve", "tensor_scalar", [gg], [gg], out=gg[:], in0=gg[:], scalar1=0.0, scalar2=None, op0=ALU.max)
                    kb.I("dve", "tensor_tensor", [gg, tmp], [gg], out=gg[:], in0=gg[:], in1=tmp[:], op=ALU.add)
                    kb.I("dve", "tensor_tensor", [gg, nA], [gg], out=gg[:], in0=gg[:], in1=bcT(nA), op=ALU.mult)
                    kb.I("act", "activation", [ab], [beta], out=beta[:], in_=ab[:, :, 16:32], func=AF.Sigmoid)
                    for t in range(NT):
                        p = abp[t % 2]
                        kb.I("pe", "matmul", [ut, gg], [p], p[:, 0:16], lhsT=ut[:], rhs=gg[:, t, :], start=True, stop=True, inc=False)
                        kb.I("pe", "matmul", [c["ones"], gg], [p], p[:, 16:32], lhsT=c["ones"][:], rhs=gg[:, t, :], start=True, stop=True)
                        kb.copy("act", [p], [gc], gc[:, t, :], p[:, 0:16])
                        kb.copy("dve", [p], [egl], egl[:, t, :], p[:, 16:32])
                    kb.I("dve", "tensor_tensor", [egl, gc], [ekd], out=ekd[:], in0=egl[:], in1=gc[:], op=ALU.subtract)
                    kb.I("act", "activation", [ekd], [ekd], out=ekd[:], in_=ekd[:], func=AF.Exp)
                    kb.I("act", "activation", [egl], [egl], out=egl[:], in_=egl[:], func=AF.Exp)
                    kb.I("act", "activation", [gc], [egc], out=egc[:], in_=gc[:], func=AF.Exp)
                    kb.I("dve", "tensor_tensor", [beta, egc], [bege], out=bege[:], in0=beta[:], in1=egc[:], op=ALU.mult)

                if stop == 2:
                    return
                wsls = [kb.sb("wsl%d" % i, [128, 8, 768], BF16) for i in range(2)]
                gstg = [kb.sb("gstg%d" % i, [128, 768]) for i in range(2)]

                def load_wsl(kh_):
                    w_ = wsls[kh_ % 2]
                    for k in range(8):
                        kb.dma("sp", gstg[k % 2][:], gw_d[kh_, k * 128:(k + 1) * 128, :])
                        kb.copy("act" if k % 2 else "pool", [gstg[k % 2]], [w_], w_[:, k, :], gstg[k % 2][:])
                load_wsl(0)
                for kh in range(8):
                    with kb.scope():
                        wsl = wsls[kh % 2]
                        qkT = kb.sb("qkT", [128, 2, TP], BF16)
                        zsT = kb.sb("zsT", [128, 2, TP], BF16)
                        ktok = kb.sb("ktok", [128, NT, 128], BF16)
                        vtok = kb.sb("vtok", [128, NT, 256], BF16)
                        oTk = kb.sb("oTk", [128, 2, TP], BF16)
                        with kb.scope():
                            vT = kb.sb("vT", [128, 2, TP], BF16)
                            raws = [kb.sb("raw%d" % i, [128, TP]) for i in range(2)]
                            cvs = [kb.sb("cv%d" % i, [128, TP]) for i in range(2)]
                            sqs = [kb.sb("sq%d" % i, [128, 512], BF16) for i in range(3)]
                            rstds = [kb.sb("rstd%d" % i, [128, 512]) for i in range(3)]
                            ps2 = [kb.ps("gpp%d" % i, [128, 512]) for i in range(2)]
                            ps3 = [kb.ps("gpq%d" % i, [128, 512]) for i in range(2)]
                            tps = [kb.ps("gtq%d" % i, [128, 8, 128], BF16) for i in range(2)]
                            cnt = [0]
                            ti = 0
                            def do_proj(fi):
                                raw = raws[fi % 2]
                                def ev(n_, p, t0, n, fi=fi, raw=raw):
                                    if fi < 4:
                                        kb.copy("act" if n_ % 2 else "dve", [p], [raw], raw[:, t0:t0 + n], p)
                                    else:
                                        kb.I("act", "activation", [p], [zsT], out=zsT[:, fi - 4, t0:t0 + n], in_=p, func=AF.Silu)
                                proj_fm(kb, ps2, cnt, wsl, fi * 128, hT, None, ev)

                            def do_post(fi):
                                nonlocal ti
                                raw, cv = raws[fi % 2], cvs[fi % 2]
                                cwc = lambda j: gconv[:, kh, fi, j:j + 1]
                                kb.I("dve", "tensor_scalar", [raw, gconv], [cv], out=cv[:], in0=raw[:], scalar1=cwc(3), scalar2=None, op0=ALU.mult)
                                for d in (1, 2, 3):
                                    kb.I("dve", "scalar_tensor_tensor", [raw, gconv, cv], [cv], out=cv[:, d:TP], in0=raw[:, 0:TP - d],
                                         scalar=cwc(3 - d), in1=cv[:, d:TP], op0=ALU.mult, op1=ALU.add)
                                if fi >= 2:
                                    kb.I("act", "activation", [cv], [vT], out=vT[:, fi - 2, :], in_=cv[:], func=AF.Silu)
                                    for j0 in range(0, NT, 8):
                                        j1 = min(j0 + 8, NT)
                                        tpp = tps[ti % 2]
                                        ti += 1
                                        for j in range(j0, j1):
                                            kb.I("pe", "transpose", [vT, c["identb"]], [tpp], out=tpp[:, j - j0, :],
                                                 in_=vT[:, fi - 2, j * 128:(j + 1) * 128], identity=c["identb"][:], inc=(j == j1 - 1))
                                        kb.copy("dve", [tpp], [vtok], vtok[:, j0:j1, (fi - 2) * 128:(fi - 1) * 128], tpp[:, 0:j1 - j0, :])
                                    return
                                kb.I("act", "activation", [cv], [cv], out=cv[:], in_=cv[:], func=AF.Silu)
                                for ci_, (t0, n) in enumerate(CHUNKS):
                                    sq, rstd = sqs[ci_ % 3], rstds[ci_ % 3]
                                    kb.I("pool", "tensor_tensor", [cv], [sq], out=sq[:, 0:n], in0=cv[:, t0:t0 + n], in1=cv[:, t0:t0 + n], op=ALU.mult)
                                    p = ps3[ci_ % 2]
                                    kb.I("pe", "matmul", [c["onesb"], sq], [p], p[:, 0:n], lhsT=c["onesb"][:], rhs=sq[:, 0:n], start=True, stop=True)
                                    kb.I("act", "activation", [p], [rstd], out=rstd[:, 0:n], in_=p[:, 0:n], func=AF.Ln, bias=eps6[:, 0:1])
                                    kb.I("act", "activation", [rstd], [rstd], out=rstd[:, 0:n], in_=rstd[:, 0:n], func=AF.Exp, scale=-0.5)
                                    kb.I("dve", "scalar_tensor_tensor", [cv, rstd], [qkT], out=qkT[:, fi, t0:t0 + n], in0=cv[:, t0:t0 + n],
                                         scalar=(128.0 ** -0.5 if fi == 0 else 1.0), in1=rstd[:, 0:n], op0=ALU.mult, op1=ALU.mult)
                                if fi == 1:
                                    for j0 in range(0, NT, 8):
                                        j1 = min(j0 + 8, NT)
                                        tpp = tps[ti % 2]
                                        ti += 1
                                        for j in range(j0, j1):
                                            kb.I("pe", "transpose", [qkT, c["identb"]], [tpp], out=tpp[:, j - j0, :],
                                                 in_=qkT[:, 1, j * 128:(j + 1) * 128], identity=c["identb"][:], inc=(j == j1 - 1))
                                        kb.copy("dve", [tpp], [ktok], ktok[:, j0:j1, :], tpp[:, 0:j1 - j0, :])

                            do_proj(0)
                            for fi in range(6):
                                if fi + 1 < 6:
                                    do_proj(fi + 1)
                                if fi < 4:
                                    do_post(fi)
                        if stop == 3:
                            return
                        if kh + 1 < 8:
                            load_wsl(kh + 1)
                        with kb.scope():
                            B = lambda nm: kb.sb(nm, [128, 128], BF16)
                            Fp = lambda nm: kb.sb(nm, [128, 128])
                            otok = [kb.sb("otok%d" % j, [128, NT, 128]) for j in range(2)]
                            us_all = [kb.sb("us_all%d" % j, [128, NT, 128]) for j in range(2)]
                            wT_all = [kb.sb("wT_all%d" % j, [128, NT, 128], BF16) for j in range(2)]
                            aT_all = [kb.sb("aT_all%d" % j, [128, NT, 128], BF16) for j in range(2)]
                            kd_all = [kb.sb("kd_all%d" % j, [128, NT, 128], BF16) for j in range(2)]
                            pGA = kb.ps("pGA", [128, 2, 128])
                            X = [kb.ps("pX%d" % i, [128, 4, 128]) for i in range(4)]
                            TPb = kb.ps("TPb", [128, 8, 128], BF16)
                            CH = []
                            for ci in range(4):
                                CH.append(dict(
                                    Rm=Fp("Rm%d" % ci), Dm=Fp("Dm%d" % ci), attn=B("attn%d" % ci),
                                    Mb=[B("Mb%d_0" % ci), B("Mb%d_1" % ci)],
                                    NA=[kb.sb("NA%d_0" % ci, [128, 2, 128], BF16), kb.sb("NA%d_1" % ci, [128, 2, 128], BF16)],
                                    vb=B("vb%d" % ci), kbg=B("kbg%d" % ci), X=X[ci]))
                            GsAs = [(Fp("Gs0"), Fp("As0")), (Fp("Gs1"), Fp("As1"))]
                            for j in range(2):
                                hd = 2 * kh + j
                                kb.I("pool", "tensor_tensor", [ktok, ekd], [kd_all[j]], out=kd_all[j][:], in0=ktok[:],
                                     in1=ekd[:, :, hd:hd + 1].to_broadcast([128, NT, 128]), op=ALU.mult)
                            Ssb = [Fp("S0"), Fp("S1")]
                            Sbf = [B("Sb0"), B("Sb1")]
                            vnew = [B("vnew0"), B("vnew1")]
                            o1 = [Fp("o1_0"), Fp("o1_1")]
                            P2 = [kb.ps("pP2_%d" % j, [128, 4, 128]) for j in range(2)]
                            for j in range(2):
                                kb.I("pool", "memset", [], [Ssb[j]], Ssb[j][:], 0.0)
                                kb.I("pool", "memset", [], [Sbf[j]], Sbf[j][:], 0.0)

                            def phase2(tlist):
                                for t in tlist:
                                    ts_ = slice(t * 128, (t + 1) * 128)
                                    for j in range(2):
                                        W = P2[j]
                                        kb.I("pe", "matmul", [wT_all[j], Sbf[j]], [W], W[:, 0, :], lhsT=wT_all[j][:, t, :], rhs=Sbf[j][:], start=True, stop=True, inc=False)
                                        kb.I("pe", "matmul", [qkT, Sbf[j]], [W], W[:, 1, :], lhsT=qkT[:, 0, ts_], rhs=Sbf[j][:], start=True, stop=True)
                                    yield
                                    for j in range(2):
                                        W = P2[j]
                                        hd = 2 * kh + j
                                        kb.I("dve", "tensor_tensor", [us_all[j], W], [vnew[j]], out=vnew[j][:], in0=us_all[j][:, t, :], in1=W[:, 0, :], op=ALU.subtract)
                                        kb.I("dve", "tensor_scalar", [W, egc], [o1[j]], out=o1[j][:], in0=W[:, 1, :], scalar1=egc[:, t, hd:hd + 1], scalar2=None, op0=ALU.mult)
                                    yield
                                    for j in range(2):
                                        V_ = P2[j]
                                        kb.I("pe", "matmul", [aT_all[j], vnew[j]], [V_], V_[:, 2, :], lhsT=aT_all[j][:, t, :], rhs=vnew[j][:], start=True, stop=True, inc=False)
                                        kb.I("pe", "matmul", [kd_all[j], vnew[j]], [V_], V_[:, 3, :], lhsT=kd_all[j][:, t, :], rhs=vnew[j][:], start=True, stop=True)
                                    yield
                                    for j in range(2):
                                        V_ = P2[j]
                                        hd = 2 * kh + j
                                        kb.I("dve", "scalar_tensor_tensor", [Ssb[j], egl, V_], [Sbf[j]], out=Sbf[j][:], in0=Ssb[j][:], scalar=egl[:, t, hd:hd + 1],
                                             in1=V_[:, 3, :], op0=ALU.mult, op1=ALU.add)
                                        kb.I("dve", "scalar_tensor_tensor", [Ssb[j], egl, V_], [Ssb[j]], out=Ssb[j][:], in0=Ssb[j][:], scalar=egl[:, t, hd:hd + 1],
                                             in1=V_[:, 3, :], op0=ALU.mult, op1=ALU.add)
                                        kb.I("dve", "tensor_tensor", [o1[j], V_], [otok[j]], out=otok[j][:, t, :], in0=o1[j][:], in1=V_[:, 2, :], op=ALU.add)
                                    yield
                            gen2 = iter(())
                            for t0 in range(0, NT, 2):
                                tl = [t for t in (t0, t0 + 1) if t < NT]
                                chains = []
                                for ti_, t in enumerate(tl):
                                    ts_ = slice(t * 128, (t + 1) * 128)
                                    Gs, As = GsAs[ti_]
                                    kb.I("pe", "matmul", [qkT], [pGA], pGA[:, 0, :], lhsT=qkT[:, 1, ts_], rhs=qkT[:, 1, ts_], start=True, stop=True, inc=False)
                                    kb.I("pe", "matmul", [qkT], [pGA], pGA[:, 1, :], lhsT=qkT[:, 0, ts_], rhs=qkT[:, 1, ts_], start=True, stop=True)
                                    kb.I("dve", "tensor_tensor", [pGA, smask], [Gs], out=Gs[:], in0=pGA[:, 0, :], in1=smask[:], op=ALU.mult)
                                    kb.I("dve", "tensor_tensor", [pGA, lmask], [As], out=As[:], in0=pGA[:, 1, :], in1=lmask[:], op=ALU.mult)
                                    for j in range(2):
                                        chn = dict(CH[ti_ * 2 + j])
                                        chn.update(t=t, j=j, hd=2 * kh + j, Gs=Gs, As=As, slot=ti_ * 2 + j)
                                        chains.append(chn)
                                col = lambda a, q: a[:, q["t"], q["hd"]:q["hd"] + 1]
                                for q in chains:
                                    kb.I("act", "mul", [ut, gg], [q["Rm"]], out=q["Rm"][:], in_=ut[:], mul=col(gg, q))
                                for q in chains:
                                    kb.I("pe", "matmul", [q["Rm"], slt], [q["X"]], q["X"][:, 0, :], lhsT=q["Rm"][:], rhs=slt[:], start=True, stop=True)
                                for q in chains:
                                    kb.I("act", "activation", [q["X"]], [q["Dm"]], out=q["Dm"][:], in_=q["X"][:, 0, :], func=AF.Exp)
                                for q in chains:
                                    kb.I("dve", "scalar_tensor_tensor", [q["Gs"], beta, q["Dm"]], [q["Mb"][0]], out=q["Mb"][0][:], in0=q["Gs"][:],
                                         scalar=col(beta, q), in1=q["Dm"][:], op0=ALU.mult, op1=ALU.mult)
                                    kb.I("pool", "tensor_tensor", [q["As"], q["Dm"]], [q["attn"]], out=q["attn"][:], in0=q["As"][:], in1=q["Dm"][:], op=ALU.mult)
                                    kb.copy("act", [c["identb"]], [q["NA"][0]], q["NA"][0][:, 1, :], c["identb"][:])
                                for q in chains:
                                    sl = q["slot"]
                                    kb.I("pe", "transpose", [q["Mb"][0], c["identb"]], [TPb], out=TPb[:, 2 * sl, :], in_=q["Mb"][0][:], identity=c["identb"][:], inc=False)
                                    kb.I("pe", "transpose", [q["attn"], c["identb"]], [TPb], out=TPb[:, 2 * sl + 1, :], in_=q["attn"][:], identity=c["identb"][:])
                                for q in chains:
                                    sl = q["slot"]
                                    kb.copy("act", [TPb], [q["NA"][0]], q["NA"][0][:, 0, :], TPb[:, 2 * sl, :])
                                    kb.copy("dve", [TPb], [aT_all[q["j"]]], aT_all[q["j"]][:, q["t"], :], TPb[:, 2 * sl + 1, :])
                                cur = 0
                                for st_ in range(7):
                                    nxt = 1 - cur
                                    next(gen2, None)
                                    for q in chains:
                                        Xq, M, NA = q["X"], q["Mb"][cur], q["NA"][cur]
                                        if st_ < 6:
                                            kb.I("pe", "matmul", [NA, M], [Xq], Xq[:, 0, :], lhsT=NA[:, 0, :], rhs=M[:], start=True, stop=True, inc=False)
                                            kb.I("pe", "matmul", [M, NA], [Xq], Xq[:, 1:3, :], lhsT=M[:], rhs=NA[:], start=True, stop=True)
                                        else:
                                            kb.I("pe", "matmul", [M, NA], [Xq], Xq[:, 2, :], lhsT=M[:], rhs=NA[:, 1, :], start=True, stop=True)
                                    next(gen2, None)
                                    for q in chains:
                                        Xq, NA, NAn = q["X"], q["NA"][cur], q["NA"][nxt]
                                        if st_ < 6:
                                            kb.copy("act", [Xq], [q["Mb"][nxt]], q["Mb"][nxt][:], Xq[:, 0, :])
                                            if st_ < 5:
                                                kb.copy("act", [Xq], [NAn], NAn[:, 0, :], Xq[:, 1, :])
                                        kb.I("dve", "tensor_tensor", [NA, Xq], [NAn], out=NAn[:, 1, :], in0=NA[:, 1, :], in1=Xq[:, 2, :],
                                             op=(ALU.subtract if st_ == 0 else ALU.add))
                                    cur = nxt
                                for q in chains:
                                    j, t = q["j"], q["t"]
                                    kb.I("act", "mul", [vtok, beta], [q["vb"]], out=q["vb"][:], in_=vtok[:, t, j * 128:(j + 1) * 128], mul=col(beta, q))
                                    kb.I("pool", "tensor_scalar", [ktok, bege], [q["kbg"]], out=q["kbg"][:], in0=ktok[:, t, :], scalar1=col(bege, q),
                                         scalar2=None, op0=ALU.mult)
                                for q in chains:
                                    TT = q["NA"][cur][:, 1, :]
                                    kb.I("pe", "matmul", [q["NA"][cur], q["vb"]], [q["X"]], q["X"][:, 0, :], lhsT=TT, rhs=q["vb"][:], start=True, stop=True, inc=False)
                                    kb.I("pe", "matmul", [q["kbg"], q["NA"][cur]], [q["X"]], q["X"][:, 1, :], lhsT=q["kbg"][:], rhs=TT, start=True, stop=True)
                                for q in chains:
                                    j, t = q["j"], q["t"]
                                    kb.copy("act", [q["X"]], [us_all[j]], us_all[j][:, t, :], q["X"][:, 0, :])
                                    kb.copy("dve", [q["X"]], [wT_all[j]], wT_all[j][:, t, :], q["X"][:, 1, :])
                                for _ in gen2:
                                    pass
                                gen2 = phase2(tl)
                            for _ in gen2:
                                pass
                            if stop == 4:
                                return
                            with kb.scope():
                                sqo = us_all[0]
                                ssq = kb.sb("ssq", [128, NT])
                                onb = wT_all[0]
                                tpo = [TPb, TPb]
                                ti = 0
                                for j in range(2):
                                    kb.I("pool", "tensor_tensor", [otok[j]], [sqo], out=sqo[:], in0=otok[j][:], in1=otok[j][:], op=ALU.mult)
                                    kb.I("dve", "tensor_reduce", [sqo], [ssq], out=ssq[:], in_=sqo[:], axis=AX.X, op=ALU.add)
                                    kb.I("dve", "tensor_scalar", [ssq], [ssq], out=ssq[:], in0=ssq[:], scalar1=1.0 / 128, scalar2=1e-6, op0=ALU.mult, op1=ALU.add)
                                    kb.I("act", "sqrt", [ssq], [ssq], out=ssq[:], in_=ssq[:])
                                    kb.I("dve", "reciprocal", [ssq], [ssq], out=ssq[:], in_=ssq[:])
                                    kb.I("dve", "tensor_tensor", [otok[j], ssq], [onb], out=onb[:], in0=otok[j][:],
                                         in1=ssq[:].unsqueeze(2).to_broadcast([128, NT, 128]), op=ALU.mult)
                                    for j0 in range(0, NT, 8):
                                        j1 = min(j0 + 8, NT)
                                        tpp = tpo[ti % 2]
                                        ti += 1
                                        for jj in range(j0, j1):
                                            kb.I("pe", "transpose", [onb, c["identb"]], [tpp], out=tpp[:, jj - j0, :], in_=onb[:, jj, :], identity=c["identb"][:], inc=(jj == j1 - 1))
                                        kb.I("dve", "scalar_tensor_tensor", [tpp, onorm, zsT], [oTk], out=oTk[:, j, j0 * 128:j1 * 128],
                                             in0=tpp[:, 0:j1 - j0, :].rearrange("p a b -> p (a b)"), scalar=onorm[:, 0:1], in1=zsT[:, j, j0 * 128:j1 * 128],
                                             op0=ALU.mult, op1=ALU.mult)
                                for j in range(2):
                                    kb.dma("sp", o_d[s, 2 * kh + j, :, :], oTk[:, j, :])
                if stop == 5:
                    return
                with kb.scope():
                    woutb = kb.sb("gwoutb", [128, 16, D], BF16)
                    stg = [kb.sb("gostg%d" % i, [128, D]) for i in range(2)]
                    for k in range(16):
                        kb.dma("sp", stg[k % 2][:], wout_d[k * 128:(k + 1) * 128, :])
                        kb.copy("act" if k % 2 else "pool", [stg[k % 2]], [woutb], woutb[:, k, :], stg[k % 2][:])
                    oTt = [kb.sb("oTt%d" % i, [128, 16, 128], BF16) for i in range(2)]
                    hts = [kb.sb("goht%d" % i, [128, D]) for i in range(2)]
                    acc = [kb.sb("goacc%d" % i, [128, D]) for i in range(2)]
                    outt = [kb.sb("goout%d" % i, [128, D]) for i in range(2)]
                    st6 = kb.sb("st6", [128, 2, 6])
                    mv = kb.sb("mv", [128, 4])
                    ops2 = [kb.ps("gopp%d" % i, [128, 512]) for i in range(4)]
                    for t in range(NT):
                        ht = hts[t % 2]
                        a = acc[t % 2]
                        ot = oTt[t % 2]
                        kb.dma("sp", ht[:], h_d[r0 + t * 128: r0 + (t + 1) * 128, :])
                        for hq in range(4):
                            kb.dma("act", ot[:, hq * 4:(hq + 1) * 4, :], o_d[s, hq * 4:(hq + 1) * 4, :, t * 128:(t + 1) * 128].rearrange("h p t -> p h t"),
                                   writes=[ot], group="oTt%d" % (t % 2))
                        for hf in range(2):
                            p = ops2[(2 * t + hf) % 4]
                            for k in range(16):
                                kb.I("pe", "matmul", [ot, woutb], [p], p[:], lhsT=ot[:, k, :], rhs=woutb[:, k, hf * 512:(hf + 1) * 512],
                                     start=(k == 0), stop=(k == 15), inc=(k == 15))
                            kb.I("dve", "scalar_tensor_tensor", [ht, p], [a], out=a[:, hf * 512:(hf + 1) * 512], in0=ht[:, hf * 512:(hf + 1) * 512],
                                 scalar=ALPHA, in1=p[:], op0=ALU.mult, op1=ALU.add)
                        layer_norm_tile(kb, a, g_bc, b_bc, outt[t % 2], st6, mv)
                        kb.dma("sp", out_d[r0 + t * 128: r0 + (t + 1) * 128, :], outt[t % 2][:])


def prep_gdn(inp, i):
    f = lambda a: np.ascontiguousarray(a, dtype=np.float32)
    w = inp["odd_w_in"][i]
    cwt = inp["odd_conv_w"][i]
    gw = np.zeros((8, 1024, 768), np.float32)
    gconv = np.zeros((128, 8, 4, 4), np.float32)
    for kh in range(8):
        cols = [np.arange(kh * 128, kh * 128 + 128), 1024 + np.arange(kh * 128, kh * 128 + 128),
                2048 + np.arange(2 * kh * 128, 2 * kh * 128 + 256)]
        zc = 4096 + np.arange(2 * kh * 128, 2 * kh * 128 + 256)
        gw[kh] = w[:, np.concatenate(cols + [zc])]
        cc_ = np.concatenate(cols)
        for fi in range(4):
            gconv[:, kh, fi, :] = cwt[:, cc_[fi * 128:(fi + 1) * 128]].T
    return {
        "gd_w": gw, "gd_wab": f(w[:, 6144:6176]), "gd_conv": gconv.reshape(128, 128).reshape(128, 8, 4, 4),
        "gd_alog": f(inp["odd_a_log"][i]), "gd_dtb": f(inp["odd_dt_bias"][i]), "gd_onorm": f(inp["odd_o_norm"][i]),
        "gd_wout": f(inp["odd_w_out"][i]), "gd_lng": f(inp["ln_g"][2 * i + 1, 0]), "gd_lnb": f(inp["ln_b"][2 * i + 1, 0]),
    }


def perm_expert(w, nk):
    E, R, F_ = w.shape
    return np.ascontiguousarray(w.reshape(E, nk, 128, F_).transpose(0, 2, 1, 3).reshape(E * 128, nk * F_), dtype=np.float32)


def prep_moe(inp, layer, pfx):
    f = lambda a: np.ascontiguousarray(a, dtype=np.float32)
    return {
        pfx + "wr": f(np.concatenate([inp["moe_group_w"][layer], inp["moe_expert_w"][layer]], axis=1)),
        pfx + "br": f(np.concatenate([inp["moe_group_b"][layer], inp["moe_expert_b"][layer]], axis=0)),
        pfx + "wg": perm_expert(inp["moe_w_gate"][layer], 8), pfx + "wu": perm_expert(inp["moe_w_up"][layer], 8),
        pfx + "wd": perm_expert(inp["moe_w_down"][layer], 4),
        pfx + "lng": f(inp["ln_g"][layer, 1]), pfx + "lnb": f(inp["ln_b"][layer, 1]),
    }


def build_program(shapes):
    nc = bass.Bass("TRN2", target_bir_lowering=False)
    kb = KB(nc)
    dd = {}
    for n, (shp, dt) in shapes.items():
        dd[n] = nc.dram_tensor(n, list(shp), dt, kind="ExternalInput").ap()
    out_d = nc.dram_tensor("out", [NTOK, D], F32, kind="ExternalOutput").ap()
    h1_d = kb.dram("h1", [NTOK, D])
    h2_d = kb.dram("h2", [NTOK, D])
    h3_d = kb.dram("h3", [NTOK, D])
    xs_d = kb.dram("xs", [NSLOT, D], BF16)
    ys_d = kb.dram("ys", [NSLOT, D], F32)
    o_d = kb.dram("o_scr", [SPC, 16, 128, TP], BF16)
    c = load_consts(kb, {k[2:]: v for k, v in dd.items() if k.startswith("c_")})
    build_even(kb, c, dd["h0"], dd["ev_win"], dd["ev_wout"], dd["ev_wuk"], dd["ev_wuv"], dd["ev_rgw"], dd["ev_vec"],
               dd["ev_lng"], dd["ev_lnb"], h1_d)
    build_moe(kb, c, h1_d, dd["m0_wr"], dd["m0_br"], dd["m0_wg"], dd["m0_wu"], dd["m0_wd"], dd["m0_lng"], dd["m0_lnb"],
              h2_d, xs_d, ys_d)
    build_gdn(kb, c, h2_d, dd["gd_w"], dd["gd_wab"], dd["gd_conv"], dd["gd_alog"], dd["gd_dtb"], dd["gd_onorm"],
              dd["gd_wout"], dd["gd_lng"], dd["gd_lnb"], h3_d, o_d)
    build_moe(kb, c, h3_d, dd["m1_wr"], dd["m1_br"], dd["m1_wg"], dd["m1_wu"], dd["m1_wd"], dd["m1_lng"], dd["m1_lnb"],
              out_d, xs_d, ys_d)
    kb.finish()
    return nc


def kernel(**inputs):
    import ml_dtypes
    inp = {k: np.asarray(v) for k, v in inputs.items()}
    x = inp["x"].astype(np.float32, copy=False)
    B = x.shape[0]
    hp = np.zeros((B, TP, D), np.float32)
    hp[:, :NMETA] = inp["meta_tokens"][None]
    hp[:, NMETA:T] = x
    shared = {}
    for k, v in make_consts().items():
        shared["c_" + k] = v
    shared.update(prep_even(inp, 0))
    shared.update(prep_gdn(inp, 0))
    shared.update(prep_moe(inp, 0, "m0_"))
    shared.update(prep_moe(inp, 1, "m1_"))
    shapes = {n: (v.shape, BF16 if v.dtype == ml_dtypes.bfloat16 else F32) for n, v in shared.items()}
    shapes["h0"] = ((NTOK, D), F32)
    nc = build_program(shapes)
    in_maps = []
    for ci in range(NCORES):
        m = dict(shared)
        m["h0"] = np.ascontiguousarray(hp[ci * SPC:(ci + 1) * SPC].reshape(NTOK, D))
        in_maps.append(m)
    res = run_bass_kernel_spmd(nc, in_maps, core_ids=list(range(NCORES)))
    outs = [r["out"].reshape(SPC, TP, D)[:, NMETA:T] for r in res.results]
    return np.ascontiguousarray(np.concatenate(outs, axis=0).astype(np.float32))
```

```python
import contextlib
import numpy as np
import concourse.bass as bass
import concourse.mybir as mybir
from concourse.bass_utils import run_bass_kernel_spmd

F32 = mybir.dt.float32
BF16 = mybir.dt.bfloat16
I32 = mybir.dt.int32
AF = mybir.ActivationFunctionType
ALU = mybir.AluOpType
AX = mybir.AxisListType

NCORES = 8
D = 1024
SEQ = 2048
NMETA = 16
T = SEQ + NMETA
TP = 17 * 128
NT = TP // 128
SPC = 4
NTOK = SPC * TP
NTILE = NTOK // 128
ALPHA = 4.0 ** 0.25
NEG = -1.0e30


class Res:
    __slots__ = ("w", "r", "dsem")

    def __init__(self):
        self.w = None
        self.r = {}
        self.dsem = None


class KB:
    def __init__(self, nc):
        self.nc = nc
        self.es = contextlib.ExitStack()
        self.stack = [self.es]
        self.eng = {"pe": nc.tensor, "act": nc.scalar, "dve": nc.vector, "pool": nc.gpsimd, "sp": nc.sync}
        self.sems = {}
        self.cnt = {}
        for k in self.eng:
            self.sems[k] = self.es.enter_context(nc.semaphore("sem_" + k))
            self.cnt[k] = 0
        self.seen = {k: {} for k in self.eng}
        self.res = {}
        self.dcount = {}
        self.free_dsems = []
        self.scope_res = [[]]
        self.nd = 0
        self.n_inst = 0
        self.uid = 0
        self.psum_names = set()

    def sb(self, name, shape, dtype=F32):
        self.uid += 1
        return self.stack[-1].enter_context(self.nc.sbuf_tensor("%s_%d" % (name, self.uid), list(shape), dtype))

    def ps(self, name, shape, dtype=F32):
        self.uid += 1
        nm = "%s_%d" % (name, self.uid)
        self.psum_names.add(nm)
        return self.stack[-1].enter_context(self.nc.psum_tensor(nm, list(shape), dtype))

    def dram(self, name, shape, dtype=F32, kind="Internal"):
        return self.nc.dram_tensor(name, list(shape), dtype, kind=kind).ap()

    @contextlib.contextmanager
    def scope(self):
        st = contextlib.ExitStack()
        self.stack.append(st)
        self.scope_res.append([])
        try:
            yield
        finally:
            self.barrier()
            for key in self.scope_res.pop():
                r = self.res.pop(key, None)
                if r is not None and r.dsem is not None:
                    self.free_dsems.append(r.dsem)
            self.stack.pop()
            st.close()

    def _res(self, ap):
        key = ap if isinstance(ap, str) else ap.name
        r = self.res.get(key)
        if r is None:
            r = self.res[key] = Res()
            self.scope_res[-1].append(key)
        return r

    def _wait(self, e, semkey, val):
        if semkey in self.dcount:
            val = max(val, self.dcount[semkey])
        if self.seen[e].get(semkey, 0) >= val:
            return
        if semkey == e and val > self.cnt[e]:
            return
        self.seen[e][semkey] = val
        self.eng[e].wait_ge(self.sems[semkey], val)

    def _deps(self, e, reads, writes):
        for a in reads:
            r = self._res(a)
            if r.w is not None:
                self._wait(e, *r.w)
        for a in writes:
            r = self._res(a)
            if r.w is not None:
                self._wait(e, *r.w)
            for sk, v in r.r.items():
                self._wait(e, sk, v)

    def _done(self, ev, reads, writes):
        for a in reads:
            r = self._res(a)
            if r.r.get(ev[0], 0) < ev[1]:
                r.r[ev[0]] = ev[1]
        for a in writes:
            r = self._res(a)
            r.w = ev
            r.r = {}

    def I(self, e, fn, reads, writes, *args, inc=True, **kw):
        writes = list(writes) + [a for a in reads if (not isinstance(a, str)) and a.name in self.psum_names]
        self._deps(e, reads, writes)
        ins = getattr(self.eng[e], fn)(*args, **kw)
        if inc:
            self.cnt[e] += 1
            ins.then_inc(self.sems[e], 1)
            self._done((e, self.cnt[e]), reads, writes)
        else:
            self._done((e, self.cnt[e] + 1), reads, writes)
        self.n_inst += 1
        return ins

    def dma(self, q, out, in_, reads=None, writes=None, group=None, indirect=None, **kw):
        reads = [in_] if reads is None else reads
        writes = [out] if writes is None else writes
        dst = self._res(group if group is not None else writes[0])
        if dst.dsem is None:
            if self.free_dsems:
                dst.dsem = self.free_dsems.pop()
            else:
                self.nd += 1
                dst.dsem = "d%d" % self.nd
                self.sems[dst.dsem] = self.es.enter_context(self.nc.semaphore(dst.dsem))
                self.dcount[dst.dsem] = 0
        if group is None:
            self._deps(q, reads, writes)
        else:
            self._deps(q, reads, [])
            for a in writes:
                r = self._res(a)
                if r.w is not None and r.w[0] != dst.dsem:
                    self._wait(q, *r.w)
                for sk, v in r.r.items():
                    self._wait(q, sk, v)
        if indirect is None:
            ins = self.eng[q].dma_start(out=out, in_=in_, **kw)
        else:
            ins = self.eng[q].indirect_dma_start(out=out, in_=in_, **indirect)
        self.dcount[dst.dsem] += 16
        ins.then_inc(self.sems[dst.dsem], 16)
        self._done((dst.dsem, self.dcount[dst.dsem]), reads, writes)
        self.n_inst += 1
        return ins

    def copy(self, e, reads, writes, out, in_):
        return self.I(e, "copy" if e == "act" else "tensor_copy", reads, writes, out=out, in_=in_)

    def barrier(self):
        for e in self.eng:
            for e2 in ("pe", "act", "dve", "pool"):
                if self.cnt[e2]:
                    self._wait(e, e2, self.cnt[e2])
            for sk, c in self.dcount.items():
                if c:
                    self._wait(e, sk, c)

    def finish(self):
        self.barrier()
        self.es.close()


def load_consts(kb, cd):
    c = {}
    for name, shape, dt in (("ident", [128, 128], F32), ("identb", [128, 128], BF16),
                            ("ones", [128, 128], F32), ("sut", [128, 128], F32),
                            ("ramp", [128, 128], F32), ("pidx", [128, 128], F32), ("cmask", [128, 128], F32),
                            ("onesb", [128, 128], BF16), ("cmaskb", [128, 128], BF16)):
        t = kb.sb("c_" + name, shape, dt)
        kb.dma("sp", t[:], cd[name])
        c[name] = t
    return c


def layer_norm_tile(kb, acc, g_bc, b_bc, outt, st6, mv, eng2="pool"):
    for j in range(2):
        kb.I("dve", "bn_stats", [acc], [st6], out=st6[:, j, :], in_=acc[:, j * 512:(j + 1) * 512])
    kb.I("dve", "bn_aggr", [st6], [mv], out=mv[:, 0:2], in_=st6[:].rearrange("p a b -> p (a b)"))
    kb.I("dve", "tensor_scalar", [mv], [mv], out=mv[:, 2:3], in0=mv[:, 1:2], scalar1=1e-5, scalar2=None, op0=ALU.add)
    kb.I("act", "sqrt", [mv], [mv], out=mv[:, 2:3], in_=mv[:, 2:3])
    kb.I("dve", "reciprocal", [mv], [mv], out=mv[:, 2:3], in_=mv[:, 2:3])
    kb.I("dve", "scalar_tensor_tensor", [mv], [mv], out=mv[:, 3:4], in0=mv[:, 0:1], scalar=-1.0, in1=mv[:, 2:3], op0=ALU.mult, op1=ALU.mult)
    kb.I("act", "activation", [acc, mv], [acc], out=acc[:], in_=acc[:], func=AF.Identity, scale=mv[:, 2:3], bias=mv[:, 3:4])
    kb.I("dve", "tensor_tensor", [acc, g_bc], [acc], out=acc[:], in0=acc[:], in1=g_bc[:], op=ALU.mult)
    kb.I(eng2, "tensor_tensor", [acc, b_bc], [outt], out=outt[:], in0=acc[:], in1=b_bc[:], op=ALU.add)


MOE_BS = 4
MOE_BLK = MOE_BS * 128
NBLK = -(-NTOK * 2 // MOE_BLK) + 32
NSLOT = NBLK * MOE_BLK


def build_moe(kb, c, h_d, wr_d, br_d, wg_d, wu_d, wd_d, lng_d, lnb_d, out_d, xs_d, ys_d, ntile=NTILE):
    nc = kb.nc
    nblk = -(-ntile * 128 * 2 // MOE_BLK) + 32
    BS, BLK = MOE_BS, MOE_BLK
    with kb.scope():
        s1i = kb.sb("s1i", [128, ntile], I32)
        s2i = kb.sb("s2i", [128, ntile], I32)
        g1 = kb.sb("g1", [128, ntile])
        g2 = kb.sb("g2", [128, ntile])
        idxe = kb.sb("idxe", [128, nblk], I32)
        g_bc = kb.sb("g_bc", [128, D])
        b_bc = kb.sb("b_bc", [128, D])
        kb.dma("sp", g_bc[:], lng_d.partition_broadcast(128))
        kb.dma("sp", b_bc[:], lnb_d.partition_broadcast(128))
        hts = [kb.sb("ht%d" % i, [128, D]) for i in range(2)]

        with kb.scope():
            wr = kb.sb("wr", [128, 8, 36])
            kb.dma("sp", wr[:], wr_d.rearrange("(k p) e -> p k e", p=128))
            br = kb.sb("br", [128, 36])
            kb.dma("sp", br[:], br_d.partition_broadcast(128))
            L = kb.sb("L", [128, ntile, 36])
            hT = [kb.sb("hT%d" % i, [128, 8, 128]) for i in range(2)]
            tp = [kb.ps("tp%d" % i, [128, 8, 128]) for i in range(2)]
            lg = [kb.ps("lg%d" % i, [128, 36]) for i in range(2)]
            for t in range(ntile):
                ht = hts[t % 2]
                kb.dma("sp", ht[:], h_d[t * 128:(t + 1) * 128, :])
                tpp = tp[t % 2]
                for k in range(8):
                    kb.I("pe", "transpose", [ht, c["ident"]], [tpp], out=tpp[:, k, :], in_=ht[:, k * 128:(k + 1) * 128],
                         identity=c["ident"][:], inc=(k == 7))
                hTt = hT[t % 2]
                kb.I("act", "copy", [tpp], [hTt], out=hTt[:], in_=tpp[:])
                lgp = lg[t % 2]
                for k in range(8):
                    kb.I("pe", "matmul", [hTt, wr], [lgp], lgp[:], lhsT=hTt[:, k, :], rhs=wr[:, k, :],
                         start=(k == 0), stop=(k == 7), inc=(k == 7))
                kb.I("dve", "tensor_tensor", [lgp, br], [L], out=L[:, t, :], in0=lgp[:], in1=br[:], op=ALU.add)

            NE = ntile * 32
            GL = L[:, :, 0:4]
            EL = L[:, :, 4:36]
            gmax = kb.sb("gmax", [128, ntile])
            t4 = kb.sb("t4", [128, ntile, 4])
            goh = kb.sb("goh", [128, ntile, 4])
            gg = kb.sb("gg", [128, ntile])
            ELm = kb.sb("ELm", [128, ntile, 32])
            EL2 = kb.sb("EL2", [128, ntile, 32])
            oh1 = kb.sb("oh1", [128, ntile, 32])
            oh2 = kb.sb("oh2", [128, ntile, 32])
            m1 = kb.sb("m1", [128, ntile])
            m2 = kb.sb("m2", [128, ntile])
            tmp = kb.sb("tmp", [128, ntile])
            V = "dve"
            kb.I(V, "tensor_reduce", [L], [gmax], out=gmax[:], in_=GL, axis=AX.X, op=ALU.max)
            bc4 = lambda a: a[:].unsqueeze(2).to_broadcast([128, ntile, 4])
            bc32 = lambda a: a[:].unsqueeze(2).to_broadcast([128, ntile, 32])
            kb.I(V, "tensor_tensor", [L, gmax], [goh], out=goh[:], in0=GL, in1=bc4(gmax), op=ALU.is_equal)
            kb.I(V, "tensor_tensor", [L, gmax], [t4], out=t4[:], in0=GL, in1=bc4(gmax), op=ALU.subtract)
            kb.I("act", "activation", [t4], [t4], out=t4[:], in_=t4[:], func=AF.Exp)
            kb.I(V, "tensor_reduce", [t4], [gg], out=gg[:], in_=t4[:], axis=AX.X, op=ALU.add)
            kb.I(V, "reciprocal", [gg], [gg], out=gg[:], in_=gg[:])
            kb.I(V, "tensor_scalar", [goh], [t4], out=t4[:], in0=goh[:], scalar1=-NEG, scalar2=NEG,
                 op0=ALU.mult, op1=ALU.add)
            kb.I(V, "tensor_tensor", [L, t4], [ELm], out=ELm[:].rearrange("p t (g e) -> p t g e", g=4),
                 in0=EL.rearrange("p t (g e) -> p t g e", g=4),
                 in1=t4[:].unsqueeze(3).to_broadcast([128, ntile, 4, 8]), op=ALU.add)
            kb.I(V, "tensor_reduce", [ELm], [m1], out=m1[:], in_=ELm[:], axis=AX.X, op=ALU.max)
            kb.I(V, "tensor_tensor", [ELm, m1], [oh1], out=oh1[:], in0=ELm[:], in1=bc32(m1), op=ALU.is_equal)
            kb.I(V, "scalar_tensor_tensor", [oh1, ELm], [EL2], out=EL2[:], in0=oh1[:], scalar=NEG, in1=ELm[:],
                 op0=ALU.mult, op1=ALU.add)
            kb.I(V, "tensor_reduce", [EL2], [m2], out=m2[:], in_=EL2[:], axis=AX.X, op=ALU.max)
            kb.I(V, "tensor_tensor", [EL2, m2], [oh2], out=oh2[:], in0=EL2[:], in1=bc32(m2), op=ALU.is_equal)
            kb.I(V, "tensor_tensor", [m1, m2], [tmp], out=tmp[:], in0=m2[:], in1=m1[:], op=ALU.subtract)
            kb.I("act", "activation", [tmp], [tmp], out=tmp[:], in_=tmp[:], func=AF.Exp)
            kb.I(V, "tensor_scalar", [tmp], [m1], out=m1[:], in0=tmp[:], scalar1=1.0, scalar2=None, op0=ALU.add)
            kb.I(V, "reciprocal", [m1], [m1], out=m1[:], in_=m1[:])
            kb.I(V, "tensor_tensor", [tmp, m1], [m2], out=m2[:], in0=tmp[:], in1=m1[:], op=ALU.mult)
            kb.I(V, "tensor_tensor", [m1, gg], [g1], out=g1[:], in0=m1[:], in1=gg[:], op=ALU.mult)
            kb.I(V, "tensor_tensor", [m2, gg], [g2], out=g2[:], in0=m2[:], in1=gg[:], op=ALU.mult)
            OH = ELm
            kb.I(V, "tensor_tensor", [oh1, oh2], [OH], out=OH[:], in0=oh1[:], in1=oh2[:], op=ALU.add)
            RK = kb.sb("RK", [128, NE])
            CT = kb.sb("CT", [128, ntile, 32])
            OHf = OH[:].rearrange("p t e -> p (t e)")
            CTf = CT[:].rearrange("p t e -> p (t e)")
            pr = [kb.ps("pr%d" % i, [128, 512]) for i in range(2)]
            ci = 0
            for dst, lhs in ((RK[:], c["sut"]), (CTf, c["ones"])):
                for o in range(0, NE, 512):
                    n = min(512, NE - o)
                    p = pr[ci % 2]
                    ci += 1
                    kb.I("pe", "matmul", [lhs, OH], [p], p[:, 0:n], lhsT=lhs[:], rhs=OHf[:, o:o + n],
                         start=True, stop=True)
                    kb.I("act", "copy", [p], [RK if dst is not CTf else CT], out=dst[:, o:o + n], in_=p[:, 0:n])
            base = kb.sb("base", [128, ntile, 32])
            kb.I(V, "memset", [], [base], base[:, 0, :], 0.0)
            for t in range(1, ntile):
                kb.I(V, "tensor_tensor", [base, CT], [base], out=base[:, t, :], in0=base[:, t - 1, :],
                     in1=CT[:, t - 1, :], op=ALU.add)
            tot = kb.sb("tot", [128, 32])
            pad = kb.sb("pad", [128, 32])
            pend = kb.sb("pend", [128, 32])
            one32 = kb.sb("one32", [128, 32])
            kb.I(V, "memset", [], [one32], one32[:], 1.0)
            kb.I(V, "tensor_tensor", [base, CT], [tot], out=tot[:], in0=base[:, ntile - 1, :], in1=CT[:, ntile - 1, :],
                 op=ALU.add)
            thr = kb.sb("thr", [128, nblk])
            kb.I(V, "tensor_scalar", [c["ramp"]], [thr], out=thr[:], in0=c["ramp"][:, 0:nblk], scalar1=float(BLK),
                 scalar2=None, op0=ALU.mult)
            cmp0 = kb.sb("cmp0", [128, 32, nblk])
            kb.I(V, "tensor_tensor", [tot, thr], [cmp0], out=cmp0[:],
                 in0=tot[:].unsqueeze(2).to_broadcast([128, 32, nblk]),
                 in1=thr[:].unsqueeze(1).to_broadcast([128, 32, nblk]), op=ALU.is_gt)
            kb.I(V, "tensor_reduce", [cmp0], [pad], out=pad[:], in_=cmp0[:], axis=AX.X, op=ALU.add)
            kb.I(V, "tensor_scalar", [pad], [pad], out=pad[:], in0=pad[:], scalar1=float(BLK), scalar2=None, op0=ALU.mult)
            kb.I(V, "tensor_tensor_scan", [one32, pad], [pend], out=pend[:], data0=one32[:], data1=pad[:], initial=0.0,
                 op0=ALU.mult, op1=ALU.add)
            kb.I(V, "tensor_tensor", [pend, pad], [pad], out=pad[:], in0=pend[:], in1=pad[:], op=ALU.subtract)
            cmp = kb.sb("cmp", [128, nblk, 32])
            bef = kb.sb("bef", [128, nblk])
            kb.I(V, "tensor_scalar", [c["ramp"]], [bef], out=bef[:], in0=c["ramp"][:, 0:nblk], scalar1=float(BLK),
                 scalar2=None, op0=ALU.mult)
            kb.I(V, "tensor_tensor", [pend, bef], [cmp], out=cmp[:],
                 in0=pend[:].unsqueeze(1).to_broadcast([128, nblk, 32]),
                 in1=bef[:].unsqueeze(2).to_broadcast([128, nblk, 32]), op=ALU.is_le)
            kb.I(V, "tensor_reduce", [cmp], [bef], out=bef[:], in_=cmp[:], axis=AX.X, op=ALU.add)
            kb.I(V, "tensor_scalar", [bef], [bef], out=bef[:], in0=bef[:], scalar1=31.0, scalar2=None, op0=ALU.min)
            ig = kb.sb("ig", [128, nblk])
            kb.I(V, "tensor_scalar", [bef, c["pidx"]], [ig], out=ig[:], in0=bef[:], scalar1=128.0, scalar2=c["pidx"][:, 0:1],
                 op0=ALU.mult, op1=ALU.add)
            usedf = kb.sb("usedf", [128, nblk])
            kb.I(V, "tensor_scalar", [thr, pend], [usedf], out=usedf[:], in0=thr[:], scalar1=pend[:, 31:32], scalar2=None, op0=ALU.is_lt)
            kb.I(V, "tensor_tensor", [ig, usedf], [ig], out=ig[:], in0=ig[:], in1=usedf[:], op=ALU.mult)
            kb.I(V, "tensor_scalar", [usedf], [usedf], out=usedf[:], in0=usedf[:], scalar1=-8192.0, scalar2=8192.0, op0=ALU.mult, op1=ALU.add)
            kb.I(V, "tensor_tensor", [ig, usedf], [ig], out=ig[:], in0=ig[:], in1=usedf[:], op=ALU.add)
            kb.I(V, "tensor_copy", [ig], [idxe], out=idxe[:], in_=ig[:])
            SL = EL2
            RK3 = RK[:].rearrange("p (t e) -> p t e", e=32)
            kb.I(V, "tensor_tensor", [RK, base], [SL], out=SL[:], in0=RK3, in1=base[:], op=ALU.add)
            kb.I(V, "tensor_tensor", [SL, pad], [SL], out=SL[:], in0=SL[:],
                 in1=pad[:].unsqueeze(1).to_broadcast([128, ntile, 32]), op=ALU.add)
            for oh, si in ((oh1, s1i), (oh2, s2i)):
                kb.I(V, "tensor_tensor", [SL, oh], [oh], out=oh[:], in0=SL[:], in1=oh[:], op=ALU.mult)
                kb.I(V, "tensor_reduce", [oh], [tmp], out=tmp[:], in_=oh[:], axis=AX.X, op=ALU.add)
                kb.I(V, "tensor_copy", [tmp], [si], out=si[:], in_=tmp[:])

        with kb.scope():
            hbs = [kb.sb("hb%d" % i, [128, D], BF16) for i in range(2)]
            for t in range(ntile):
                ht = hts[t % 2]
                hb = hbs[t % 2]
                kb.dma("sp", ht[:], h_d[t * 128:(t + 1) * 128, :])
                kb.I("act", "copy", [ht], [hb], out=hb[:], in_=ht[:])
                for si in (s1i, s2i):
                    kb.dma("pool", xs_d, hb[:], reads=[hb, si], writes=[xs_d], indirect=dict(
                        out_offset=bass.IndirectOffsetOnAxis(ap=si[:, t:t + 1], axis=0), in_offset=None))

        with kb.scope():
            xin = [[kb.sb("xin%d_%d" % (i, s), [128, D], BF16) for s in range(BS)] for i in range(2)]
            xT = [kb.sb("xT%d" % i, [128, 8, BLK], BF16) for i in range(2)]
            tps = [kb.ps("tps%d" % i, [128, 8, 128], BF16) for i in range(2)]
            stg = [kb.sb("stg%d" % i, [128, 4096]) for i in range(3)]
            wgb = [kb.sb("wgb%d" % i, [128, 8, 512], BF16) for i in range(2)]
            wub = [kb.sb("wub%d" % i, [128, 8, 512], BF16) for i in range(2)]
            wdb = [kb.sb("wdb%d" % i, [128, 4, 1024], BF16) for i in range(2)]
            hgp = [kb.ps("hgp%d" % i, [128, BLK]) for i in range(2)]
            hup = [kb.ps("hup%d" % i, [128, BLK]) for i in range(2)]
            yp = [kb.ps("yp%d" % i, [128, 512]) for i in range(2)]
            sg = [kb.sb("sg%d" % i, [128, BLK]) for i in range(2)]
            hTb = [kb.sb("hTb%d" % i, [128, 4, BLK], BF16) for i in range(2)]
            yb = [kb.sb("yb%d" % i, [128, D]) for i in range(2)]
            sti = 0
            cast_eng = ["dve", "act"]
            bc_reg = nc.gpsimd.alloc_register("moe_bc_%d" % kb.uid)
            nc.gpsimd.reg_mov(bc_reg, 4095)

            def load_weights(b):
                nonlocal sti
                par = b % 2
                for src, dstt in ((wg_d, wgb[par]), (wu_d, wub[par]), (wd_d, wdb[par])):
                    st = stg[sti % 3]
                    kb.dma("pool", st[:], src, reads=[idxe], writes=[st],
                           indirect=dict(out_offset=None, in_offset=bass.IndirectOffsetOnAxis(ap=idxe[:, b:b + 1], axis=0),
                                         bounds_check=bc_reg, oob_is_err=False))
                    dflat = dstt[:].rearrange("p a b -> p (a b)")
                    for half in range(2):
                        ce = cast_eng[(2 * sti + half) % 2]
                        kb.copy(ce, [st], [dstt], dflat[:, half * 2048:(half + 1) * 2048], st[:, half * 2048:(half + 1) * 2048])
                    sti += 1

            def load_tokens(b):
                par = b % 2
                for s in range(BS):
                    xi = xin[par][s]
                    kb.dma("act", xi[:], xs_d[b * BLK + s * 128: b * BLK + (s + 1) * 128, :])
                    tpp = tps[s % 2]
                    for k in range(8):
                        kb.I("pe", "transpose", [xi, c["identb"]], [tpp], out=tpp[:, k, :], in_=xi[:, k * 128:(k + 1) * 128],
                             identity=c["identb"][:], inc=(k == 7))
                    kb.I("dve", "tensor_copy", [tpp], [xT[par]], out=xT[par][:, :, s * 128:(s + 1) * 128], in_=tpp[:])

            def gate_up(b):
                par = b % 2
                for f in range(4):
                    hg = hgp[f % 2]
                    hu = hup[f % 2]
                    for k in range(8):
                        kb.I("pe", "matmul", [wgb[par], xT[par]], [hg], hg[:], lhsT=wgb[par][:, k, f * 128:(f + 1) * 128],
                             rhs=xT[par][:, k, :], start=(k == 0), stop=(k == 7), inc=(k == 7))
                    for k in range(8):
                        kb.I("pe", "matmul", [wub[par], xT[par]], [hu], hu[:], lhsT=wub[par][:, k, f * 128:(f + 1) * 128],
                             rhs=xT[par][:, k, :], start=(k == 0), stop=(k == 7), inc=(k == 7))
                    sgt = sg[f % 2]
                    kb.I("act", "activation", [hg], [sgt], out=sgt[:], in_=hg[:], func=AF.Silu)
                    kb.I("dve", "tensor_tensor", [sgt, hu], [hTb[par]], out=hTb[par][:, f, :], in0=sgt[:], in1=hu[:], op=ALU.mult)

            def down(b):
                par = b % 2
                for s in range(BS):
                    ybt = yb[s % 2]
                    for dh in range(2):
                        y = yp[dh]
                        for f in range(4):
                            kb.I("pe", "matmul", [hTb[par], wdb[par]], [y], y[:], lhsT=hTb[par][:, f, s * 128:(s + 1) * 128],
                                 rhs=wdb[par][:, f, dh * 512:(dh + 1) * 512], start=(f == 0), stop=(f == 3), inc=(f == 3))
                        kb.I("act" if dh == 0 else "dve", "tensor_copy" if dh else "copy", [y], [ybt],
                             out=ybt[:, dh * 512:(dh + 1) * 512], in_=y[:])
                    kb.dma("sp", ys_d[b * BLK + s * 128: b * BLK + (s + 1) * 128, :], ybt[:])

            load_weights(0)
            load_tokens(0)
            for b in range(nblk):
                if b + 1 < nblk:
                    load_weights(b + 1)
                gate_up(b)
                if b + 1 < nblk:
                    load_tokens(b + 1)
                down(b)

        with kb.scope():
            NS = 3
            y1 = [kb.sb("y1_%d" % i, [128, D]) for i in range(NS)]
            y2 = [kb.sb("y2_%d" % i, [128, D]) for i in range(NS)]
            hts5 = [kb.sb("ht5_%d" % i, [128, D]) for i in range(NS)]
            acc = [kb.sb("acc%d" % i, [128, D]) for i in range(2)]
            outt = [kb.sb("outt%d" % i, [128, D]) for i in range(2)]
            st6 = kb.sb("st6", [128, 2, 6])
            mv = kb.sb("mv", [128, 4])

            def issue(t):
                p = t % NS
                kb.dma("sp", hts5[p][:], h_d[t * 128:(t + 1) * 128, :])
                for yy, si in ((y1[p], s1i), (y2[p], s2i)):
                    kb.dma("pool", yy[:], ys_d, reads=[ys_d, si], writes=[yy], indirect=dict(
                        out_offset=None, in_offset=bass.IndirectOffsetOnAxis(ap=si[:, t:t + 1], axis=0)))
            for t in range(min(NS - 1, ntile)):
                issue(t)
            for t in range(ntile):
                if t + NS - 1 < ntile:
                    issue(t + NS - 1)
                p = t % NS
                ht = hts5[p]
                a = acc[t % 2]
                kb.I("act", "mul", [y1[p], g1], [a], out=a[:], in_=y1[p][:], mul=g1[:, t:t + 1])
                kb.I("dve", "scalar_tensor_tensor", [y2[p], g2, a], [a], out=a[:], in0=y2[p][:], scalar=g2[:, t:t + 1],
                     in1=a[:], op0=ALU.mult, op1=ALU.add)
                kb.I("dve", "scalar_tensor_tensor", [ht, a], [a], out=a[:], in0=ht[:], scalar=ALPHA, in1=a[:],
                     op0=ALU.mult, op1=ALU.add)
                layer_norm_tile(kb, a, g_bc, b_bc, outt[t % 2], st6, mv)
                kb.dma("sp", out_d[t * 128:(t + 1) * 128, :], outt[t % 2][:])

def make_consts():
    import ml_dtypes
    i = np.arange(128)
    return {
        "ident": np.eye(128, dtype=np.float32),
        "identb": np.eye(128, dtype=np.float32).astype(ml_dtypes.bfloat16),
        "ones": np.ones((128, 128), np.float32),
        "sut": (i[:, None] < i[None, :]).astype(np.float32),
        "ramp": np.broadcast_to(i[None, :].astype(np.float32), (128, 128)).copy(),
        "pidx": np.broadcast_to(i[:, None].astype(np.float32), (128, 128)).copy(),
        "cmask": np.where(i[None, :] > i[:, None], NEG, 0.0).astype(np.float32),
        "onesb": np.ones((128, 128), np.float32).astype(ml_dtypes.bfloat16),
        "cmaskb": np.where(i[None, :] > i[:, None], NEG, 0.0).astype(np.float32).astype(ml_dtypes.bfloat16),
    }


CHUNKS = [(0, 512), (512, 512), (1024, 512), (1536, 512), (2048, 128)]
EV_Q, EV_CKV, EV_QI, EV_KI, EV_WI, EV_GATE, EV_XB = 0, 512, 768, 1280, 1344, 1352, 1864


def proj_fm(kb, ps2, cnt, wb, col0, hT, dst_fn, evac):
    for (t0, n) in CHUNKS:
        p = ps2[cnt[0] % 2]
        for k in range(8):
            kb.I("pe", "matmul", [wb, hT], [p], p[:, 0:n], lhsT=wb[:, k, col0:col0 + 128], rhs=hT[:, k, t0:t0 + n],
                 start=(k == 0), stop=(k == 7), inc=(k == 7))
        evac(cnt[0], p[:, 0:n], t0, n)
        cnt[0] += 1


def build_even(kb, c, h_d, win_d, wout_d, wuk_d, wuv_d, rgw_d, vec_d, lng_d, lnb_d, out_d, nseq=SPC):
    with kb.scope():
        winb = kb.sb("winb", [128, 8, 2376], BF16)
        wk2 = kb.sb("wk2", [128, 8, 128], BF16)
        wukb = kb.sb("wukb", [128, 2, 128], BF16)
        wuvb = kb.sb("wuvb", [128, 2, 128], BF16)
        rgwb = kb.sb("rgwb", [128, 8, 128], BF16)
        vec = kb.sb("vec", [128, 40])
        cc = kb.sb("cc", [128, 4])
        g_bc = kb.sb("g_bc", [128, D])
        b_bc = kb.sb("b_bc", [128, D])
        kb.dma("sp", g_bc[:], lng_d.partition_broadcast(128))
        kb.dma("sp", b_bc[:], lnb_d.partition_broadcast(128))
        kb.dma("sp", vec[:], vec_d)
        with kb.scope():
            stg = [kb.sb("wstg%d" % i, [128, 2376]) for i in range(2)]
            for k in range(8):
                st = stg[k % 2]
                kb.dma("sp", st[:], win_d[k * 128:(k + 1) * 128, :])
                kb.copy("act" if k % 2 else "pool", [st], [winb], winb[:, k, :], st[:])
            for j in range(2):
                kb.I("dve", "tensor_copy", [winb], [wk2], out=wk2[:, :, j * 64:(j + 1) * 64], in_=winb[:, :, EV_KI:EV_KI + 64])
            st = stg[0]
            kb.dma("sp", st[:, 0:256], wuk_d.rearrange("(c p) d -> p c d", p=128))
            kb.I("dve", "tensor_copy", [st], [wukb], out=wukb[:], in_=st[:, 0:256].rearrange("p (c d) -> p c d", c=2))
            st = stg[1]
            kb.dma("sp", st[:, 0:256], wuv_d.rearrange("(c p) d -> p c d", p=128))
            kb.I("dve", "tensor_copy", [st], [wuvb], out=wuvb[:], in_=st[:, 0:256].rearrange("p (c d) -> p c d", c=2))
            st = stg[0]
            kb.dma("sp", st[:, 0:1024], rgw_d)
            kb.I("dve", "tensor_copy", [st], [rgwb], out=rgwb[:], in_=st[:, 0:1024].rearrange("p (c d) -> p c d", c=8))
            kb.I("act", "activation", [vec], [cc], out=cc[:], in_=vec[:, 28:32], func=AF.Exp, scale=-1.0)
            kb.I("act", "activation", [cc], [cc], out=cc[:], in_=cc[:], func=AF.Ln, bias=1.0)
            kb.I("dve", "tensor_scalar", [cc], [cc], out=cc[:], in0=cc[:], scalar1=-8.0, scalar2=None, op0=ALU.mult)
        cw = lambda j, ch: vec[:, j * 4 + ch: j * 4 + ch + 1]
        cb = lambda ch: vec[:, 16 + ch:17 + ch]
        ba = lambda ch: vec[:, 20 + ch:21 + ch]
        bx = lambda ch: vec[:, 24 + ch:25 + ch]
        kvn = lambda ch: vec[:, 32 + ch:33 + ch]

        for s in range(nseq):
            r0 = s * TP
            with kb.scope():
                hT = kb.sb("hT", [128, 8, TP], BF16)
                qT = kb.sb("qT", [128, 4, TP], BF16)
                qiT = kb.sb("qiT", [128, 4, TP], BF16)
                kiT2 = kb.sb("kiT2", [128, TP], BF16)
                kT = kb.sb("kT", [128, TP], BF16)
                vtok = kb.sb("vtok", [128, NT, 130], BF16)
                wq = kb.sb("wq", [128, NT, 8])
                kb.I("pool", "memset", [], [vtok], vtok[:, :, 128:130], 1.0)
                with kb.scope():
                    gateT = kb.sb("gateT", [128, 4, TP], BF16)
                    xbT = kb.sb("xbT", [128, 4, TP], BF16)
                    with kb.scope():
                        hts = [kb.sb("eht%d" % i, [128, D]) for i in range(2)]
                        tp = [kb.ps("etp%d" % i, [128, 8, 128]) for i in range(2)]
                        for t in range(NT):
                            ht = hts[t % 2]
                            kb.dma("sp", ht[:], h_d[r0 + t * 128: r0 + (t + 1) * 128, :])
                            tpp = tp[t % 2]
                            for k in range(8):
                                kb.I("pe", "transpose", [ht, c["ident"]], [tpp], out=tpp[:, k, :],
                                     in_=ht[:, k * 128:(k + 1) * 128], identity=c["ident"][:], inc=(k == 7))
                            kb.copy("act" if t % 2 else "dve", [tpp], [hT], hT[:, :, t * 128:(t + 1) * 128], tpp[:])
                    with kb.scope():
                        ckvT = kb.sb("ckvT", [128, 2, TP], BF16)
                        latT = kb.sb("latT", [128, 2, TP], BF16)
                        ps2 = [kb.ps("pp%d" % i, [128, 512]) for i in range(2)]
                        cnt = [0]

                        def mk_evac(dst, ci):
                            def ev(n_, p, t0, n):
                                kb.copy("act" if n_ % 2 else "dve", [p], [dst], dst[:, ci, t0:t0 + n] if ci is not None else dst[:, t0:t0 + n], p)
                            return ev
                        for ci in range(4):
                            proj_fm(kb, ps2, cnt, winb, EV_Q + ci * 128, hT, None, mk_evac(qT, ci))
                            proj_fm(kb, ps2, cnt, winb, EV_QI + ci * 128, hT, None, mk_evac(qiT, ci))
                            proj_fm(kb, ps2, cnt, winb, EV_GATE + ci * 128, hT, None, mk_evac(gateT, ci))
                            proj_fm(kb, ps2, cnt, winb, EV_XB + ci * 128, hT, None, mk_evac(xbT, ci))
                        for ci in range(2):
                            proj_fm(kb, ps2, cnt, winb, EV_CKV + ci * 128, hT, None, mk_evac(ckvT, ci))
                        proj_fm(kb, ps2, cnt, wk2, 0, hT, None, mk_evac(kiT2, None))
                        wps = [kb.ps("wps%d" % i, [128, 8]) for i in range(2)]
                        for t in range(NT):
                            p = wps[t % 2]
                            for k in range(8):
                                kb.I("pe", "matmul", [hT, winb], [p], p[:], lhsT=hT[:, k, t * 128:(t + 1) * 128],
                                     rhs=winb[:, k, EV_WI:EV_WI + 8], start=(k == 0), stop=(k == 7), inc=(k == 7))
                            kb.copy("act", [p], [wq], wq[:, t, :], p[:])
                        sq = kb.sb("sq", [128, 2, 512], BF16)
                        rstd = kb.sb("rstd", [128, 512])
                        for (t0, n) in CHUNKS:
                            kb.I("act", "activation", [ckvT], [sq], out=sq[:, :, 0:n], in_=ckvT[:, :, t0:t0 + n], func=AF.Square)
                            p = ps2[cnt[0] % 2]
                            cnt[0] += 1
                            for ci in range(2):
                                kb.I("pe", "matmul", [c["onesb"], sq], [p], p[:, 0:n], lhsT=c["onesb"][:], rhs=sq[:, ci, 0:n],
                                     start=(ci == 0), stop=(ci == 1), inc=(ci == 1))
                            kb.I("dve", "tensor_scalar", [p], [rstd], out=rstd[:, 0:n], in0=p[:, 0:n], scalar1=1.0 / 256, scalar2=1e-6,
                                 op0=ALU.mult, op1=ALU.add)
                            kb.I("act", "sqrt", [rstd], [rstd], out=rstd[:, 0:n], in_=rstd[:, 0:n])
                            kb.I("dve", "reciprocal", [rstd], [rstd], out=rstd[:, 0:n], in_=rstd[:, 0:n])
                            for ci in range(2):
                                kb.I("dve", "scalar_tensor_tensor", [ckvT, vec, rstd], [latT], out=latT[:, ci, t0:t0 + n],
                                     in0=ckvT[:, ci, t0:t0 + n], scalar=kvn(ci), in1=rstd[:, 0:n], op0=ALU.mult, op1=ALU.mult)
                            p = ps2[cnt[0] % 2]
                            cnt[0] += 1
                            for ci in range(2):
                                kb.I("pe", "matmul", [wukb, latT], [p], p[:, 0:n], lhsT=wukb[:, ci, :], rhs=latT[:, ci, t0:t0 + n],
                                     start=(ci == 0), stop=(ci == 1), inc=(ci == 1))
                            kb.copy("act", [p], [kT], kT[:, t0:t0 + n], p[:, 0:n])
                        for t in range(NT):
                            p = ps2[cnt[0] % 2]
                            cnt[0] += 1
                            for ci in range(2):
                                kb.I("pe", "matmul", [latT, wuvb], [p], p[:, 0:128], lhsT=latT[:, ci, t * 128:(t + 1) * 128],
                                     rhs=wuvb[:, ci, :], start=(ci == 0), stop=(ci == 1), inc=(ci == 1))
                            kb.copy("dve", [p], [vtok], vtok[:, t, 0:128], p[:, 0:128])
                    mixT = hT
                    with kb.scope():
                        HS = TP // 2
                        F = lambda nm: kb.sb(nm, [128, HS])
                        xr, rr, ii, aa, uu, hh, gl = F("xr"), F("rr"), F("ii"), F("aa"), F("uu"), F("hh"), F("gl")
                        xrb = kb.sb("xrb", [128, HS], BF16)
                        carry = kb.sb("carry", [128, 4])
                        gps = [kb.ps("gps%d" % i, [128, 512]) for i in range(4)]
                        gi = 0
                        for ch in range(4):
                            for hf in range(2):
                                o = hf * HS
                                x = xbT[:, ch, :]
                                kb.I("dve", "tensor_scalar", [xbT, vec], [xr], out=xr[:], in0=x[:, o:o + HS], scalar1=cw(3, ch), scalar2=cb(ch),
                                     op0=ALU.mult, op1=ALU.add)
                                for d in (1, 2, 3):
                                    lo = d if hf == 0 else 0
                                    kb.I("dve", "scalar_tensor_tensor", [xbT, vec, xr], [xr], out=xr[:, lo:HS], in0=x[:, o + lo - d:o + HS - d],
                                         scalar=cw(3 - d, ch), in1=xr[:, lo:HS], op0=ALU.mult, op1=ALU.add)
                                kb.copy("pool", [xr], [xrb], xrb[:], xr[:])
                                for (t0, n) in ((0, 512), (512, 512), (1024, 64)):
                                    pa = gps[gi % 4]
                                    px = gps[(gi + 1) % 4]
                                    gi += 2
                                    kb.I("pe", "matmul", [rgwb, xrb], [pa], pa[:, 0:n], lhsT=rgwb[:, ch, :], rhs=xrb[:, t0:t0 + n], start=True, stop=True)
                                    kb.I("pe", "matmul", [rgwb, xrb], [px], px[:, 0:n], lhsT=rgwb[:, 4 + ch, :], rhs=xrb[:, t0:t0 + n], start=True, stop=True)
                                    kb.I("act", "activation", [pa, vec], [rr], out=rr[:, t0:t0 + n], in_=pa[:, 0:n], func=AF.Sigmoid, bias=ba(ch))
                                    kb.I("act", "activation", [px, vec], [ii], out=ii[:, t0:t0 + n], in_=px[:, 0:n], func=AF.Sigmoid, bias=bx(ch))
                                kb.I("act", "activation", [rr, cc], [aa], out=aa[:], in_=rr[:], func=AF.Exp, scale=cc[:, ch:ch + 1])
                                kb.I("pool", "tensor_tensor", [aa], [uu], out=uu[:], in0=aa[:], in1=aa[:], op=ALU.mult)
                                kb.I("act", "activation", [uu], [uu], out=uu[:], in_=uu[:], func=AF.Sqrt, scale=-1.0, bias=1.0)
                                kb.I("pool", "tensor_tensor", [ii, xr], [ii], out=ii[:], in0=ii[:], in1=xr[:], op=ALU.mult)
                                kb.I("pool", "tensor_tensor", [uu, ii], [uu], out=uu[:], in0=uu[:], in1=ii[:], op=ALU.mult)
                                kb.I("dve", "tensor_tensor_scan", [aa, uu, carry], [hh], out=hh[:], data0=aa[:], data1=uu[:],
                                     initial=(0.0 if hf == 0 else carry[:, ch:ch + 1]), op0=ALU.mult, op1=ALU.add)
                                if hf == 0:
                                    kb.I("dve", "tensor_copy", [hh], [carry], out=carry[:, ch:ch + 1], in_=hh[:, HS - 1:HS])
                                g = gateT[:, ch, o:o + HS]
                                kb.I("act", "activation", [gateT], [gl], out=gl[:], in_=g, func=AF.Square)
                                kb.I("pool", "tensor_scalar", [gl], [gl], out=gl[:], in0=gl[:], scalar1=0.044715, scalar2=1.0, op0=ALU.mult, op1=ALU.add)
                                kb.I("pool", "tensor_tensor", [gl, gateT], [gl], out=gl[:], in0=gl[:], in1=g, op=ALU.mult)
                                kb.I("act", "activation", [gl], [gl], out=gl[:], in_=gl[:], func=AF.Sigmoid, scale=1.5957691216)
                                kb.I("pool", "tensor_tensor", [gl, gateT], [gl], out=gl[:], in0=gl[:], in1=g, op=ALU.mult)
                                kb.I("dve", "tensor_tensor", [hh, gl], [mixT], out=mixT[:, 4 + ch, o:o + HS], in0=hh[:], in1=gl[:], op=ALU.mult)
                with kb.scope():
                    score = [kb.sb("score%d" % i, [128, TP]) for i in range(2)]
                    mask = [kb.sb("mask%d" % i, [128, TP], BF16) for i in range(2)]
                    m8 = [kb.sb("m8_%d" % i, [128, 8]) for i in range(2)]
                    work = kb.sb("work", [128, TP])
                    thrc = kb.sb("thrc", [128, 1])
                    rlb = [kb.sb("rlb%d" % i, [128, 512], BF16) for i in range(4)]
                    dg = [kb.sb("dg%d" % i, [128, 8, 128], BF16) for i in range(1)]
                    lgt = [kb.sb("lgt%d" % i, [128, TP]) for i in range(2)]
                    ee = [kb.sb("ee%d" % i, [128, TP], BF16) for i in range(2)]
                    pT = [kb.sb("pT%d" % i, [128, NT, 128], BF16) for i in range(1)] * 2
                    sm = [kb.sb("sm%d" % i, [128, 4]) for i in range(2)]
                    on = [kb.sb("on%d" % i, [128, 128], BF16) for i in range(2)]
                    sps = [kb.ps("sps%d" % i, [128, 512]) for i in range(2)]
                    lps = [kb.ps("lps%d" % i, [128, 512]) for i in range(2)]
                    tps = [kb.ps("atp%d" % i, [128, 8, 128], BF16) for i in range(2)]
                    ops = kb.ps("ops", [128, 132])
                    scp = kb.ps("scp", [128, 512])
                    kb.I("dve", "memset", [], [thrc], thrc[:], -1.0e29)
                    cnts = {"si": 0, "ti": 0, "li": 0}

                    def indexer(i):
                        sc = score[i % 2]
                        dgt = dg[0]
                        nk = (i + 1) * 128
                        qs = slice(i * 128, (i + 1) * 128)
                        for h in range(8):
                            kb.I("pool", "tensor_scalar", [c["identb"], wq], [dgt], out=dgt[:, h, :], in0=c["identb"][:],
                                 scalar1=wq[:, i, h:h + 1], scalar2=None, op0=ALU.mult)
                        for t0 in range(0, nk, 512):
                            n = min(512, nk - t0)
                            last = (t0 + n == nk)
                            for hh in range(2):
                                for h in range(hh * 4, hh * 4 + 4):
                                    p = sps[cnts["si"] % 2]
                                    cnts["si"] += 1
                                    pr = slice((h % 2) * 64, (h % 2) * 64 + 64)
                                    kb.I("pe", "matmul", [qiT, kiT2], [p], p[:, 0:n], lhsT=qiT[pr, h // 2, qs], rhs=kiT2[pr, t0:t0 + n],
                                         start=True, stop=True)
                                    kb.I("act", "activation", [p], [rlb[h % 4]], out=rlb[h % 4][:, 0:n], in_=p[:, 0:n], func=AF.Relu)
                                for h in range(hh * 4, hh * 4 + 4):
                                    kb.I("pe", "matmul", [dgt, rlb[h % 4]], [scp], scp[:, 0:n], lhsT=dgt[:, h, :], rhs=rlb[h % 4][:, 0:n],
                                         start=(h == 0), stop=(h == 7 and not last), inc=(h % 4 == 3))
                            if last:
                                kb.I("pe", "matmul", [c["identb"], c["cmaskb"]], [scp], scp[:, n - 128:n], lhsT=c["identb"][:], rhs=c["cmaskb"][:],
                                     start=False, stop=True)
                            kb.copy("act", [scp], [sc], sc[:, t0:t0 + n], scp[:, 0:n])
                            yield

                    def topk(i):
                        sc, mk, m = score[i % 2], mask[i % 2], m8[i % 2]
                        nk = (i + 1) * 128
                        if i >= 2:
                            for it in range(32):
                                src = sc if it == 0 else work
                                kb.I("dve", "max", [src], [m], out=m[:], in_=src[:, 0:nk])
                                if it < 31:
                                    kb.I("dve", "match_replace", [m, src], [work], out=work[:, 0:nk], in_to_replace=m[:],
                                         in_values=src[:, 0:nk], imm_value=NEG)
                                if it % 8 == 7 and it < 31:
                                    yield
                            kb.I("dve", "tensor_scalar", [sc, m], [mk], out=mk[:, 0:nk], in0=sc[:, 0:nk], scalar1=m[:, 7:8],
                                 scalar2=None, op0=ALU.is_ge)
                        else:
                            kb.I("dve", "tensor_scalar", [sc, thrc], [mk], out=mk[:, 0:nk], in0=sc[:, 0:nk], scalar1=thrc[:, 0:1],
                                 scalar2=None, op0=ALU.is_ge)

                    def qk(i, h):
                        nk = (i + 1) * 128
                        qs = slice(i * 128, (i + 1) * 128)
                        lg = lgt[h % 2]
                        for t0 in range(0, nk, 512):
                            n = min(512, nk - t0)
                            p = lps[cnts["li"] % 2]
                            cnts["li"] += 1
                            kb.I("pe", "matmul", [qT, kT], [p], p[:, 0:n], lhsT=qT[:, h, qs], rhs=kT[:, t0:t0 + n], start=True, stop=True)
                            kb.I("act", "mul", [p], [lg], out=lg[:, t0:t0 + n], in_=p[:, 0:n], mul=128.0 ** -0.5)

                    def attention(i, gen, geni):
                        mk = mask[i % 2]
                        nk = (i + 1) * 128
                        qs = slice(i * 128, (i + 1) * 128)
                        qk(i, 0)
                        for h in range(4):
                            lg, e_, s_, pT_, on_ = lgt[h % 2], ee[h % 2], sm[h % 2], pT[h % 2], on[h % 2]
                            kb.I("dve", "tensor_reduce", [lg], [s_], out=s_[:, 0:1], in_=lg[:, 0:nk], axis=AX.X, op=ALU.max)
                            kb.I("dve", "tensor_scalar", [s_], [s_], out=s_[:, 1:2], in0=s_[:, 0:1], scalar1=-1.0, scalar2=None, op0=ALU.mult)
                            kb.I("act", "activation", [lg, s_], [e_], out=e_[:, 0:nk], in_=lg[:, 0:nk], func=AF.Exp, bias=s_[:, 1:2])
                            if h < 3:
                                qk(i, h + 1)
                            next(gen, None)
                            next(geni, None)
                            if h % 2:
                                next(geni, None)
                            kb.I("pool", "tensor_tensor", [e_, mk], [e_], out=e_[:, 0:nk], in0=e_[:, 0:nk], in1=mk[:, 0:nk], op=ALU.mult)
                            for j0 in range(0, i + 1, 8):
                                j1 = min(j0 + 8, i + 1)
                                tpp = tps[cnts["ti"] % 2]
                                cnts["ti"] += 1
                                for j in range(j0, j1):
                                    kb.I("pe", "transpose", [e_, c["identb"]], [tpp], out=tpp[:, j - j0, :], in_=e_[:, j * 128:(j + 1) * 128],
                                         identity=c["identb"][:], inc=(j == j1 - 1))
                                kb.copy("act", [tpp], [pT_], pT_[:, j0:j1, :], tpp[:, 0:j1 - j0, :])
                            for j in range(i + 1):
                                kb.I("pe", "matmul", [pT_, vtok], [ops], ops[:, 0:129], lhsT=pT_[:, j, :], rhs=vtok[:, j, 0:129], start=(j == 0), stop=(j == i),
                                     inc=(j == i))
                            kb.I("dve", "reciprocal", [ops], [s_], out=s_[:, 3:4], in_=ops[:, 128:129])
                            kb.I("dve", "tensor_scalar", [ops, s_], [on_], out=on_[:], in0=ops[:, 0:128], scalar1=s_[:, 3:4], scalar2=None, op0=ALU.mult)
                            otb = tps[cnts["ti"] % 2]
                            cnts["ti"] += 1
                            kb.I("pe", "transpose", [on_, c["identb"]], [otb], out=otb[:, 0, :], in_=on_[:], identity=c["identb"][:])
                            kb.copy("act", [otb], [mixT], mixT[:, h, qs], otb[:, 0, :])

                    for _ in indexer(0):
                        pass
                    for _ in indexer(1):
                        pass
                    for _ in topk(0):
                        pass
                    for i in range(NT):
                        gen = topk(i + 1) if i + 1 < NT else iter(())
                        geni = indexer(i + 2) if i + 2 < NT else iter(())
                        attention(i, gen, geni)
                        for _ in geni:
                            pass
                        for _ in gen:
                            pass
                with kb.scope():
                    woutb = kb.sb("woutb", [128, 8, D], BF16)
                    stg = [kb.sb("ostg%d" % i, [128, D]) for i in range(2)]
                    for k in range(8):
                        kb.dma("sp", stg[k % 2][:], wout_d[k * 128:(k + 1) * 128, :])
                        kb.copy("act" if k % 2 else "pool", [stg[k % 2]], [woutb], woutb[:, k, :], stg[k % 2][:])
                    hts = [kb.sb("oht%d" % i, [128, D]) for i in range(2)]
                    acc = [kb.sb("oacc%d" % i, [128, D]) for i in range(2)]
                    outt = [kb.sb("oout%d" % i, [128, D]) for i in range(2)]
                    st6 = kb.sb("st6", [128, 2, 6])
                    mv = kb.sb("mv", [128, 4])
                    ops2 = [kb.ps("opp%d" % i, [128, 512]) for i in range(4)]
                    for t in range(NT):
                        ht = hts[t % 2]
                        a = acc[t % 2]
                        kb.dma("sp", ht[:], h_d[r0 + t * 128: r0 + (t + 1) * 128, :])
                        for hf in range(2):
                            p = ops2[(2 * t + hf) % 4]
                            for k in range(8):
                                kb.I("pe", "matmul", [mixT, woutb], [p], p[:], lhsT=mixT[:, k, t * 128:(t + 1) * 128],
                                     rhs=woutb[:, k, hf * 512:(hf + 1) * 512], start=(k == 0), stop=(k == 7), inc=(k == 7))
                            kb.I("dve", "scalar_tensor_tensor", [ht, p], [a], out=a[:, hf * 512:(hf + 1) * 512], in0=ht[:, hf * 512:(hf + 1) * 512],
                                 scalar=ALPHA, in1=p[:], op0=ALU.mult, op1=ALU.add)
                        layer_norm_tile(kb, a, g_bc, b_bc, outt[t % 2], st6, mv)
                        kb.dma("sp", out_d[r0 + t * 128: r0 + (t + 1) * 128, :], outt[t % 2][:])


def prep_even(inp, i):
    f = lambda a: np.ascontiguousarray(a, dtype=np.float32)
    pc = lambda v, n: f(v.reshape(n, 128).T)
    vec = np.zeros((128, 40), np.float32)
    cwt = inp["even_conv_w"][i]
    for j in range(4):
        vec[:, j * 4:(j + 1) * 4] = pc(cwt[j], 4)
    vec[:, 16:20] = pc(inp["even_conv_b"][i], 4)
    vec[:, 20:24] = pc(inp["even_rg_ba"][i], 4)
    vec[:, 24:28] = pc(inp["even_rg_bx"][i], 4)
    vec[:, 28:32] = pc(inp["even_rg_lambda"][i], 4)
    vec[:, 32:34] = pc(inp["even_kv_norm"][i], 2)
    rgw = np.zeros((128, 8, 128), np.float32)
    for g, nm in enumerate(("even_rg_wa", "even_rg_wx")):
        w = inp[nm][i]
        for n in range(8):
            o = (n % 2) * 64
            rgw[o:o + 64, g * 4 + n // 2, o:o + 64] = w[n]
    return {
        "ev_win": f(inp["even_w_in"][i]), "ev_wout": f(inp["even_w_out"][i]),
        "ev_wuk": f(inp["even_w_uk"][i]), "ev_wuv": f(inp["even_w_uv"][i]),
        "ev_rgw": f(rgw.reshape(128, 1024)), "ev_vec": vec,
        "ev_lng": f(inp["ln_g"][2 * i, 0]), "ev_lnb": f(inp["ln_b"][2 * i, 0]),
    }


def build_gdn(kb, c, h_d, gw_d, wab_d, gconv_d, alog_d, dtb_d, onorm_d, wout_d, lng_d, lnb_d, out_d, o_d, nseq=SPC, stop=0):
    with kb.scope():
        g_bc = kb.sb("g_bc", [128, D])
        b_bc = kb.sb("b_bc", [128, D])
        kb.dma("sp", g_bc[:], lng_d.partition_broadcast(128))
        kb.dma("sp", b_bc[:], lnb_d.partition_broadcast(128))
        wabb = kb.sb("wabb", [128, 8, 32], BF16)
        gconv = kb.sb("gconv", [128, 8, 4, 4])
        nA = kb.sb("nA", [128, 16])
        dtb = kb.sb("dtb", [128, 16])
        onorm = kb.sb("onorm", [128, 1])
        eps6 = kb.sb("eps6", [128, 1])
        kb.I("pool", "memset", [], [eps6], eps6[:], 1e-6)
        kb.dma("sp", gconv[:], gconv_d)
        kb.dma("sp", nA[:], alog_d.partition_broadcast(128))
        kb.dma("sp", dtb[:], dtb_d.partition_broadcast(128))
        kb.dma("sp", onorm[:], onorm_d.rearrange("(p o) -> p o", o=1))
        kb.I("act", "activation", [nA], [nA], out=nA[:], in_=nA[:], func=AF.Exp)
        kb.I("dve", "tensor_scalar", [nA], [nA], out=nA[:], in0=nA[:], scalar1=-1.0, scalar2=None, op0=ALU.mult)
        ut = kb.sb("ut", [128, 128])
        slt = kb.sb("slt", [128, 128])
        lmask = kb.sb("lmask", [128, 128])
        smask = kb.sb("smask", [128, 128])
        kb.I("dve", "tensor_tensor", [c["sut"], c["ident"]], [ut], out=ut[:], in0=c["sut"][:], in1=c["ident"][:], op=ALU.add)
        kb.I("dve", "tensor_scalar", [ut], [slt], out=slt[:], in0=ut[:], scalar1=-1.0, scalar2=1.0, op0=ALU.mult, op1=ALU.add)
        kb.I("dve", "tensor_copy", [slt], [smask], out=smask[:], in_=slt[:])
        kb.I("dve", "tensor_tensor", [slt, c["ident"]], [lmask], out=lmask[:], in0=slt[:], in1=c["ident"][:], op=ALU.add)
        with kb.scope():
            st = kb.sb("abstg", [128, 8, 32])
            kb.dma("sp", st[:], wab_d.rearrange("(k p) e -> p k e", p=128))
            kb.I("dve", "tensor_copy", [st], [wabb], out=wabb[:], in_=st[:])

        for s in range(nseq):
            r0 = s * TP
            with kb.scope():
                hT = kb.sb("hT", [128, 8, TP], BF16)
                with kb.scope():
                    hts = [kb.sb("ght%d" % i, [128, D]) for i in range(2)]
                    tp = [kb.ps("gtp%d" % i, [128, 8, 128]) for i in range(2)]
                    for t in range(NT):
                        ht = hts[t % 2]
                        kb.dma("sp", ht[:], h_d[r0 + t * 128: r0 + (t + 1) * 128, :])
                        tpp = tp[t % 2]
                        for k in range(8):
                            kb.I("pe", "transpose", [ht, c["ident"]], [tpp], out=tpp[:, k, :],
                                 in_=ht[:, k * 128:(k + 1) * 128], identity=c["ident"][:], inc=(k == 7))
                        kb.copy("act" if t % 2 else "dve", [tpp], [hT], hT[:, :, t * 128:(t + 1) * 128], tpp[:])
                if stop == 1:
                    return
                S3 = lambda nm: kb.sb(nm, [128, NT, 16])
                gg, beta, gc, egc, ekd, egl, bege = S3("gg"), S3("beta"), S3("gc"), S3("egc"), S3("ekd"), S3("egl"), S3("bege")
                with kb.scope():
                    abp = [kb.ps("abp%d" % i, [128, 32]) for i in range(2)]
                    ab = kb.sb("ab", [128, NT, 32])
                    tmp = S3("tmpa")
                    for t in range(NT):
                        p = abp[t % 2]
                        for k in range(8):
                            kb.I("pe", "matmul", [hT, wabb], [p], p[:], lhsT=hT[:, k, t * 128:(t + 1) * 128], rhs=wabb[:, k, :],
                                 start=(k == 0), stop=(k == 7), inc=(k == 7))
                        kb.copy("act", [p], [ab], ab[:, t, :], p[:])
                    bcT = lambda a: a[:].unsqueeze(1).to_broadcast([128, NT, 16])
                    kb.I("dve", "tensor_tensor", [ab, dtb], [gg], out=gg[:], in0=ab[:, :, 0:16], in1=bcT(dtb), op=ALU.add)
                    kb.I("dve", "tensor_scalar", [gg], [tmp], out=tmp[:], in0=gg[:], scalar1=-1.0, scalar2=None, op0=ALU.mult)
                    kb.I("dve", "tensor_tensor", [gg, tmp], [tmp], out=tmp[:], in0=gg[:], in1=tmp[:], op=ALU.max)
                    kb.I("act", "activation", [tmp], [tmp], out=tmp[:], in_=tmp[:], func=AF.Exp, scale=-1.0)
                    kb.I("act", "activation", [tmp], [tmp], out=tmp[:], in_=tmp[:], func=AF.Ln, bias=1.0)
                    kb.I("dve", "tensor_scalar", [gg], [gg], out=gg[:], in0=gg[:], scalar1=0.0, scalar2=None, op0=ALU.max)
                    kb.I("dve", "tensor_tensor", [gg, tmp], [gg], out=gg[:], in0=gg[:], in1=tmp[:], op=ALU.add)
                    kb.I("dve", "tensor_tensor", [gg, nA], [gg], out=gg[:], in0=gg[:], in1=bcT(nA), op=ALU.mult)
                    kb.I("act", "activation", [ab], [beta], out=beta[:], in_=ab[:, :, 16:32], func=AF.Sigmoid)
                    for t in range(NT):
                        p = abp[t % 2]
                        kb.I("pe", "matmul", [ut, gg], [p], p[:, 0:16], lhsT=ut[:], rhs=gg[:, t, :], start=True, stop=True, inc=False)
                        kb.I("pe", "matmul", [c["ones"], gg], [p], p[:, 16:32], lhsT=c["ones"][:], rhs=gg[:, t, :], start=True, stop=True)
                        kb.copy("act", [p], [gc], gc[:, t, :], p[:, 0:16])
                        kb.copy("dve", [p], [egl], egl[:, t, :], p[:, 16:32])
                    kb.I("dve", "tensor_tensor", [egl, gc], [ekd], out=ekd[:], in0=egl[:], in1=gc[:], op=ALU.subtract)
                    kb.I("act", "activation", [ekd], [ekd], out=ekd[:], in_=ekd[:], func=AF.Exp)
                    kb.I("act", "activation", [egl], [egl], out=egl[:], in_=egl[:], func=AF.Exp)
                    kb.I("act", "activation", [gc], [egc], out=egc[:], in_=gc[:], func=AF.Exp)
                    kb.I("dve", "tensor_tensor", [beta, egc], [bege], out=bege[:], in0=beta[:], in1=egc[:], op=ALU.mult)

                if stop == 2:
                    return
                wsls = [kb.sb("wsl%d" % i, [128, 8, 768], BF16) for i in range(2)]
                gstg = [kb.sb("gstg%d" % i, [128, 768]) for i in range(2)]

                def load_wsl(kh_):
                    w_ = wsls[kh_ % 2]
                    for k in range(8):
                        kb.dma("sp", gstg[k % 2][:], gw_d[kh_, k * 128:(k + 1) * 128, :])
                        kb.copy("act" if k % 2 else "pool", [gstg[k % 2]], [w_], w_[:, k, :], gstg[k % 2][:])
                load_wsl(0)
                for kh in range(8):
                    with kb.scope():
                        wsl = wsls[kh % 2]
                        qkT = kb.sb("qkT", [128, 2, TP], BF16)
                        zsT = kb.sb("zsT", [128, 2, TP], BF16)
                        ktok = kb.sb("ktok", [128, NT, 128], BF16)
                        vtok = kb.sb("vtok", [128, NT, 256], BF16)
                        oTk = kb.sb("oTk", [128, 2, TP], BF16)
                        with kb.scope():
                            vT = kb.sb("vT", [128, 2, TP], BF16)
                            raws = [kb.sb("raw%d" % i, [128, TP]) for i in range(2)]
                            cvs = [kb.sb("cv%d" % i, [128, TP]) for i in range(2)]
                            sqs = [kb.sb("sq%d" % i, [128, 512], BF16) for i in range(3)]
                            rstds = [kb.sb("rstd%d" % i, [128, 512]) for i in range(3)]
                            ps2 = [kb.ps("gpp%d" % i, [128, 512]) for i in range(2)]
                            ps3 = [kb.ps("gpq%d" % i, [128, 512]) for i in range(2)]
                            tps = [kb.ps("gtq%d" % i, [128, 8, 128], BF16) for i in range(2)]
                            cnt = [0]
                            ti = 0
                            def do_proj(fi):
                                raw = raws[fi % 2]
                                def ev(n_, p, t0, n, fi=fi, raw=raw):
                                    if fi < 4:
                                        kb.copy("act" if n_ % 2 else "dve", [p], [raw], raw[:, t0:t0 + n], p)
                                    else:
                                        kb.I("act", "activation", [p], [zsT], out=zsT[:, fi - 4, t0:t0 + n], in_=p, func=AF.Silu)
                                proj_fm(kb, ps2, cnt, wsl, fi * 128, hT, None, ev)

                            def do_post(fi):
                                nonlocal ti
                                raw, cv = raws[fi % 2], cvs[fi % 2]
                                cwc = lambda j: gconv[:, kh, fi, j:j + 1]
                                kb.I("dve", "tensor_scalar", [raw, gconv], [cv], out=cv[:], in0=raw[:], scalar1=cwc(3), scalar2=None, op0=ALU.mult)
                                for d in (1, 2, 3):
                                    kb.I("dve", "scalar_tensor_tensor", [raw, gconv, cv], [cv], out=cv[:, d:TP], in0=raw[:, 0:TP - d],
                                         scalar=cwc(3 - d), in1=cv[:, d:TP], op0=ALU.mult, op1=ALU.add)
                                if fi >= 2:
                                    kb.I("act", "activation", [cv], [vT], out=vT[:, fi - 2, :], in_=cv[:], func=AF.Silu)
                                    for j0 in range(0, NT, 8):
                                        j1 = min(j0 + 8, NT)
                                        tpp = tps[ti % 2]
                                        ti += 1
                                        for j in range(j0, j1):
                                            kb.I("pe", "transpose", [vT, c["identb"]], [tpp], out=tpp[:, j - j0, :],
                                                 in_=vT[:, fi - 2, j * 128:(j + 1) * 128], identity=c["identb"][:], inc=(j == j1 - 1))
                                        kb.copy("dve", [tpp], [vtok], vtok[:, j0:j1, (fi - 2) * 128:(fi - 1) * 128], tpp[:, 0:j1 - j0, :])
                                    return
                                kb.I("act", "activation", [cv], [cv], out=cv[:], in_=cv[:], func=AF.Silu)
                                for ci_, (t0, n) in enumerate(CHUNKS):
                                    sq, rstd = sqs[ci_ % 3], rstds[ci_ % 3]
                                    kb.I("pool", "tensor_tensor", [cv], [sq], out=sq[:, 0:n], in0=cv[:, t0:t0 + n], in1=cv[:, t0:t0 + n], op=ALU.mult)
                                    p = ps3[ci_ % 2]
                                    kb.I("pe", "matmul", [c["onesb"], sq], [p], p[:, 0:n], lhsT=c["onesb"][:], rhs=sq[:, 0:n], start=True, stop=True)
                                    kb.I("act", "activation", [p], [rstd], out=rstd[:, 0:n], in_=p[:, 0:n], func=AF.Ln, bias=eps6[:, 0:1])
                                    kb.I("act", "activation", [rstd], [rstd], out=rstd[:, 0:n], in_=rstd[:, 0:n], func=AF.Exp, scale=-0.5)
                                    kb.I("dve", "scalar_tensor_tensor", [cv, rstd], [qkT], out=qkT[:, fi, t0:t0 + n], in0=cv[:, t0:t0 + n],
                                         scalar=(128.0 ** -0.5 if fi == 0 else 1.0), in1=rstd[:, 0:n], op0=ALU.mult, op1=ALU.mult)
                                if fi == 1:
                                    for j0 in range(0, NT, 8):
                                        j1 = min(j0 + 8, NT)
                                        tpp = tps[ti % 2]
                                        ti += 1
                                        for j in range(j0, j1):
                                            kb.I("pe", "transpose", [qkT, c["identb"]], [tpp], out=tpp[:, j - j0, :],
                                                 in_=qkT[:, 1, j * 128:(j + 1) * 128], identity=c["identb"][:], inc=(j == j1 - 1))
                                        kb.copy("dve", [tpp], [ktok], ktok[:, j0:j1, :], tpp[:, 0:j1 - j0, :])

                            do_proj(0)
                            for fi in range(6):
                                if fi + 1 < 6:
                                    do_proj(fi + 1)
                                if fi < 4:
                                    do_post(fi)
                        if stop == 3:
                            return
                        if kh + 1 < 8:
                            load_wsl(kh + 1)
                        with kb.scope():
                            B = lambda nm: kb.sb(nm, [128, 128], BF16)
                            Fp = lambda nm: kb.sb(nm, [128, 128])
                            otok = [kb.sb("otok%d" % j, [128, NT, 128]) for j in range(2)]
                            us_all = [kb.sb("us_all%d" % j, [128, NT, 128]) for j in range(2)]
                            wT_all = [kb.sb("wT_all%d" % j, [128, NT, 128], BF16) for j in range(2)]
                            aT_all = [kb.sb("aT_all%d" % j, [128, NT, 128], BF16) for j in range(2)]
                            kd_all = [kb.sb("kd_all%d" % j, [128, NT, 128], BF16) for j in range(2)]
                            pGA = kb.ps("pGA", [128, 2, 128])
                            X = [kb.ps("pX%d" % i, [128, 4, 128]) for i in range(4)]
                            TPb = kb.ps("TPb", [128, 8, 128], BF16)
                            CH = []
                            for ci in range(4):
                                CH.append(dict(
                                    Rm=Fp("Rm%d" % ci), Dm=Fp("Dm%d" % ci), attn=B("attn%d" % ci),
                                    Mb=[B("Mb%d_0" % ci), B("Mb%d_1" % ci)],
                                    NA=[kb.sb("NA%d_0" % ci, [128, 2, 128], BF16), kb.sb("NA%d_1" % ci, [128, 2, 128], BF16)],
                                    vb=B("vb%d" % ci), kbg=B("kbg%d" % ci), X=X[ci]))
                            GsAs = [(Fp("Gs0"), Fp("As0")), (Fp("Gs1"), Fp("As1"))]
                            for j in range(2):
                                hd = 2 * kh + j
                                kb.I("pool", "tensor_tensor", [ktok, ekd], [kd_all[j]], out=kd_all[j][:], in0=ktok[:],
                                     in1=ekd[:, :, hd:hd + 1].to_broadcast([128, NT, 128]), op=ALU.mult)
                            Ssb = [Fp("S0"), Fp("S1")]
                            Sbf = [B("Sb0"), B("Sb1")]
                            vnew = [B("vnew0"), B("vnew1")]
                            o1 = [Fp("o1_0"), Fp("o1_1")]
                            P2 = [kb.ps("pP2_%d" % j, [128, 4, 128]) for j in range(2)]
                            for j in range(2):
                                kb.I("pool", "memset", [], [Ssb[j]], Ssb[j][:], 0.0)
                                kb.I("pool", "memset", [], [Sbf[j]], Sbf[j][:], 0.0)

                            def phase2(tlist):
                                for t in tlist:
                                    ts_ = slice(t * 128, (t + 1) * 128)
                                    for j in range(2):
                                        W = P2[j]
                                        kb.I("pe", "matmul", [wT_all[j], Sbf[j]], [W], W[:, 0, :], lhsT=wT_all[j][:, t, :], rhs=Sbf[j][:], start=True, stop=True, inc=False)
                                        kb.I("pe", "matmul", [qkT, Sbf[j]], [W], W[:, 1, :], lhsT=qkT[:, 0, ts_], rhs=Sbf[j][:], start=True, stop=True)
                                    yield
                                    for j in range(2):
                                        W = P2[j]
                                        hd = 2 * kh + j
                                        kb.I("dve", "tensor_tensor", [us_all[j], W], [vnew[j]], out=vnew[j][:], in0=us_all[j][:, t, :], in1=W[:, 0, :], op=ALU.subtract)
                                        kb.I("dve", "tensor_scalar", [W, egc], [o1[j]], out=o1[j][:], in0=W[:, 1, :], scalar1=egc[:, t, hd:hd + 1], scalar2=None, op0=ALU.mult)
                                    yield
                                    for j in range(2):
                                        V_ = P2[j]
                                        kb.I("pe", "matmul", [aT_all[j], vnew[j]], [V_], V_[:, 2, :], lhsT=aT_all[j][:, t, :], rhs=vnew[j][:], start=True, stop=True, inc=False)
                                        kb.I("pe", "matmul", [kd_all[j], vnew[j]], [V_], V_[:, 3, :], lhsT=kd_all[j][:, t, :], rhs=vnew[j][:], start=True, stop=True)
                                    yield
                                    for j in range(2):
                                        V_ = P2[j]
                                        hd = 2 * kh + j
                                        kb.I("dve", "scalar_tensor_tensor", [Ssb[j], egl, V_], [Sbf[j]], out=Sbf[j][:], in0=Ssb[j][:], scalar=egl[:, t, hd:hd + 1],
                                             in1=V_[:, 3, :], op0=ALU.mult, op1=ALU.add)
                                        kb.I("dve", "scalar_tensor_tensor", [Ssb[j], egl, V_], [Ssb[j]], out=Ssb[j][:], in0=Ssb[j][:], scalar=egl[:, t, hd:hd + 1],
                                             in1=V_[:, 3, :], op0=ALU.mult, op1=ALU.add)
                                        kb.I("dve", "tensor_tensor", [o1[j], V_], [otok[j]], out=otok[j][:, t, :], in0=o1[j][:], in1=V_[:, 2, :], op=ALU.add)
                                    yield
                            gen2 = iter(())
                            for t0 in range(0, NT, 2):
                                tl = [t for t in (t0, t0 + 1) if t < NT]
                                chains = []
                                for ti_, t in enumerate(tl):
                                    ts_ = slice(t * 128, (t + 1) * 128)
                                    Gs, As = GsAs[ti_]
                                    kb.I("pe", "matmul", [qkT], [pGA], pGA[:, 0, :], lhsT=qkT[:, 1, ts_], rhs=qkT[:, 1, ts_], start=True, stop=True, inc=False)
                                    kb.I("pe", "matmul", [qkT], [pGA], pGA[:, 1, :], lhsT=qkT[:, 0, ts_], rhs=qkT[:, 1, ts_], start=True, stop=True)
                                    kb.I("dve", "tensor_tensor", [pGA, smask], [Gs], out=Gs[:], in0=pGA[:, 0, :], in1=smask[:], op=ALU.mult)
                                    kb.I("dve", "tensor_tensor", [pGA, lmask], [As], out=As[:], in0=pGA[:, 1, :], in1=lmask[:], op=ALU.mult)
                                    for j in range(2):
                                        chn = dict(CH[ti_ * 2 + j])
                                        chn.update(t=t, j=j, hd=2 * kh + j, Gs=Gs, As=As, slot=ti_ * 2 + j)
                                        chains.append(chn)
                                col = lambda a, q: a[:, q["t"], q["hd"]:q["hd"] + 1]
                                for q in chains:
                                    kb.I("act", "mul", [ut, gg], [q["Rm"]], out=q["Rm"][:], in_=ut[:], mul=col(gg, q))
                                for q in chains:
                                    kb.I("pe", "matmul", [q["Rm"], slt], [q["X"]], q["X"][:, 0, :], lhsT=q["Rm"][:], rhs=slt[:], start=True, stop=True)
                                for q in chains:
                                    kb.I("act", "activation", [q["X"]], [q["Dm"]], out=q["Dm"][:], in_=q["X"][:, 0, :], func=AF.Exp)
                                for q in chains:
                                    kb.I("dve", "scalar_tensor_tensor", [q["Gs"], beta, q["Dm"]], [q["Mb"][0]], out=q["Mb"][0][:], in0=q["Gs"][:],
                                         scalar=col(beta, q), in1=q["Dm"][:], op0=ALU.mult, op1=ALU.mult)
                                    kb.I("pool", "tensor_tensor", [q["As"], q["Dm"]], [q["attn"]], out=q["attn"][:], in0=q["As"][:], in1=q["Dm"][:], op=ALU.mult)
                                    kb.copy("act", [c["identb"]], [q["NA"][0]], q["NA"][0][:, 1, :], c["identb"][:])
                                for q in chains:
                                    sl = q["slot"]
                                    kb.I("pe", "transpose", [q["Mb"][0], c["identb"]], [TPb], out=TPb[:, 2 * sl, :], in_=q["Mb"][0][:], identity=c["identb"][:], inc=False)
                                    kb.I("pe", "transpose", [q["attn"], c["identb"]], [TPb], out=TPb[:, 2 * sl + 1, :], in_=q["attn"][:], identity=c["identb"][:])
                                for q in chains:
                                    sl = q["slot"]
                                    kb.copy("act", [TPb], [q["NA"][0]], q["NA"][0][:, 0, :], TPb[:, 2 * sl, :])
                                    kb.copy("dve", [TPb], [aT_all[q["j"]]], aT_all[q["j"]][:, q["t"], :], TPb[:, 2 * sl + 1, :])
                                cur = 0
                                for st_ in range(7):
                                    nxt = 1 - cur
                                    next(gen2, None)
                                    for q in chains:
                                        Xq, M, NA = q["X"], q["Mb"][cur], q["NA"][cur]
                                        if st_ < 6:
                                            kb.I("pe", "matmul", [NA, M], [Xq], Xq[:, 0, :], lhsT=NA[:, 0, :], rhs=M[:], start=True, stop=True, inc=False)
                                            kb.I("pe", "matmul", [M, NA], [Xq], Xq[:, 1:3, :], lhsT=M[:], rhs=NA[:], start=True, stop=True)
                                        else:
                                            kb.I("pe", "matmul", [M, NA], [Xq], Xq[:, 2, :], lhsT=M[:], rhs=NA[:, 1, :], start=True, stop=True)
                                    next(gen2, None)
                                    for q in chains:
                                        Xq, NA, NAn = q["X"], q["NA"][cur], q["NA"][nxt]
                                        if st_ < 6:
                                            kb.copy("act", [Xq], [q["Mb"][nxt]], q["Mb"][nxt][:], Xq[:, 0, :])
                                            if st_ < 5:
                                                kb.copy("act", [Xq], [NAn], NAn[:, 0, :], Xq[:, 1, :])
                                        kb.I("dve", "tensor_tensor", [NA, Xq], [NAn], out=NAn[:, 1, :], in0=NA[:, 1, :], in1=Xq[:, 2, :],
                                             op=(ALU.subtract if st_ == 0 else ALU.add))
                                    cur = nxt
                                for q in chains:
                                    j, t = q["j"], q["t"]
                                    kb.I("act", "mul", [vtok, beta], [q["vb"]], out=q["vb"][:], in_=vtok[:, t, j * 128:(j + 1) * 128], mul=col(beta, q))
                                    kb.I("pool", "tensor_scalar", [ktok, bege], [q["kbg"]], out=q["kbg"][:], in0=ktok[:, t, :], scalar1=col(bege, q),
                                         scalar2=None, op0=ALU.mult)
                                for q in chains:
                                    TT = q["NA"][cur][:, 1, :]
                                    kb.I("pe", "matmul", [q["NA"][cur], q["vb"]], [q["X"]], q["X"][:, 0, :], lhsT=TT, rhs=q["vb"][:], start=True, stop=True, inc=False)
                                    kb.I("pe", "matmul", [q["kbg"], q["NA"][cur]], [q["X"]], q["X"][:, 1, :], lhsT=q["kbg"][:], rhs=TT, start=True, stop=True)
                                for q in chains:
                                    j, t = q["j"], q["t"]
                                    kb.copy("act", [q["X"]], [us_all[j]], us_all[j][:, t, :], q["X"][:, 0, :])
                                    kb.copy("dve", [q["X"]], [wT_all[j]], wT_all[j][:, t, :], q["X"][:, 1, :])
                                for _ in gen2:
                                    pass
                                gen2 = phase2(tl)
                            for _ in gen2:
                                pass
                            if stop == 4:
                                return
                            with kb.scope():
                                sqo = us_all[0]
                                ssq = kb.sb("ssq", [128, NT])
                                onb = wT_all[0]
                                tpo = [TPb, TPb]
                                ti = 0
                                for j in range(2):
                                    kb.I("pool", "tensor_tensor", [otok[j]], [sqo], out=sqo[:], in0=otok[j][:], in1=otok[j][:], op=ALU.mult)
                                    kb.I("dve", "tensor_reduce", [sqo], [ssq], out=ssq[:], in_=sqo[:], axis=AX.X, op=ALU.add)
                                    kb.I("dve", "tensor_scalar", [ssq], [ssq], out=ssq[:], in0=ssq[:], scalar1=1.0 / 128, scalar2=1e-6, op0=ALU.mult, op1=ALU.add)
                                    kb.I("act", "sqrt", [ssq], [ssq], out=ssq[:], in_=ssq[:])
                                    kb.I("dve", "reciprocal", [ssq], [ssq], out=ssq[:], in_=ssq[:])
                                    kb.I("dve", "tensor_tensor", [otok[j], ssq], [onb], out=onb[:], in0=otok[j][:],
                                         in1=ssq[:].unsqueeze(2).to_broadcast([128, NT, 128]), op=ALU.mult)
                                    for j0 in range(0, NT, 8):
                                        j1 = min(j0 + 8, NT)
                                        tpp = tpo[ti % 2]
                                        ti += 1
                                        for jj in range(j0, j1):
                                            kb.I("pe", "transpose", [onb, c["identb"]], [tpp], out=tpp[:, jj - j0, :], in_=onb[:, jj, :], identity=c["identb"][:], inc=(jj == j1 - 1))
                                        kb.I("dve", "scalar_tensor_tensor", [tpp, onorm, zsT], [oTk], out=oTk[:, j, j0 * 128:j1 * 128],
                                             in0=tpp[:, 0:j1 - j0, :].rearrange("p a b -> p (a b)"), scalar=onorm[:, 0:1], in1=zsT[:, j, j0 * 128:j1 * 128],
                                             op0=ALU.mult, op1=ALU.mult)
                                for j in range(2):
                                    kb.dma("sp", o_d[s, 2 * kh + j, :, :], oTk[:, j, :])
                if stop == 5:
                    return
                with kb.scope():
                    woutb = kb.sb("gwoutb", [128, 16, D], BF16)
                    stg = [kb.sb("gostg%d" % i, [128, D]) for i in range(2)]
                    for k in range(16):
                        kb.dma("sp", stg[k % 2][:], wout_d[k * 128:(k + 1) * 128, :])
                        kb.copy("act" if k % 2 else "pool", [stg[k % 2]], [woutb], woutb[:, k, :], stg[k % 2][:])
                    oTt = [kb.sb("oTt%d" % i, [128, 16, 128], BF16) for i in range(2)]
                    hts = [kb.sb("goht%d" % i, [128, D]) for i in range(2)]
                    acc = [kb.sb("goacc%d" % i, [128, D]) for i in range(2)]
                    outt = [kb.sb("goout%d" % i, [128, D]) for i in range(2)]
                    st6 = kb.sb("st6", [128, 2, 6])
                    mv = kb.sb("mv", [128, 4])
                    ops2 = [kb.ps("gopp%d" % i, [128, 512]) for i in range(4)]
                    for t in range(NT):
                        ht = hts[t % 2]
                        a = acc[t % 2]
                        ot = oTt[t % 2]
                        kb.dma("sp", ht[:], h_d[r0 + t * 128: r0 + (t + 1) * 128, :])
                        for hq in range(4):
                            kb.dma("act", ot[:, hq * 4:(hq + 1) * 4, :], o_d[s, hq * 4:(hq + 1) * 4, :, t * 128:(t + 1) * 128].rearrange("h p t -> p h t"),
                                   writes=[ot], group="oTt%d" % (t % 2))
                        for hf in range(2):
                            p = ops2[(2 * t + hf) % 4]
                            for k in range(16):
                                kb.I("pe", "matmul", [ot, woutb], [p], p[:], lhsT=ot[:, k, :], rhs=woutb[:, k, hf * 512:(hf + 1) * 512],
                                     start=(k == 0), stop=(k == 15), inc=(k == 15))
                            kb.I("dve", "scalar_tensor_tensor", [ht, p], [a], out=a[:, hf * 512:(hf + 1) * 512], in0=ht[:, hf * 512:(hf + 1) * 512],
                                 scalar=ALPHA, in1=p[:], op0=ALU.mult, op1=ALU.add)
                        layer_norm_tile(kb, a, g_bc, b_bc, outt[t % 2], st6, mv)
                        kb.dma("sp", out_d[r0 + t * 128: r0 + (t + 1) * 128, :], outt[t % 2][:])


def prep_gdn(inp, i):
    f = lambda a: np.ascontiguousarray(a, dtype=np.float32)
    w = inp["odd_w_in"][i]
    cwt = inp["odd_conv_w"][i]
    gw = np.zeros((8, 1024, 768), np.float32)
    gconv = np.zeros((128, 8, 4, 4), np.float32)
    for kh in range(8):
        cols = [np.arange(kh * 128, kh * 128 + 128), 1024 + np.arange(kh * 128, kh * 128 + 128),
                2048 + np.arange(2 * kh * 128, 2 * kh * 128 + 256)]
        zc = 4096 + np.arange(2 * kh * 128, 2 * kh * 128 + 256)
        gw[kh] = w[:, np.concatenate(cols + [zc])]
        cc_ = np.concatenate(cols)
        for fi in range(4):
            gconv[:, kh, fi, :] = cwt[:, cc_[fi * 128:(fi + 1) * 128]].T
    return {
        "gd_w": gw, "gd_wab": f(w[:, 6144:6176]), "gd_conv": gconv.reshape(128, 128).reshape(128, 8, 4, 4),
        "gd_alog": f(inp["odd_a_log"][i]), "gd_dtb": f(inp["odd_dt_bias"][i]), "gd_onorm": f(inp["odd_o_norm"][i]),
        "gd_wout": f(inp["odd_w_out"][i]), "gd_lng": f(inp["ln_g"][2 * i + 1, 0]), "gd_lnb": f(inp["ln_b"][2 * i + 1, 0]),
    }


def perm_expert(w, nk):
    E, R, F_ = w.shape
    return np.ascontiguousarray(w.reshape(E, nk, 128, F_).transpose(0, 2, 1, 3).reshape(E * 128, nk * F_), dtype=np.float32)


def prep_moe(inp, layer, pfx):
    f = lambda a: np.ascontiguousarray(a, dtype=np.float32)
    return {
        pfx + "wr": f(np.concatenate([inp["moe_group_w"][layer], inp["moe_expert_w"][layer]], axis=1)),
        pfx + "br": f(np.concatenate([inp["moe_group_b"][layer], inp["moe_expert_b"][layer]], axis=0)),
        pfx + "wg": perm_expert(inp["moe_w_gate"][layer], 8), pfx + "wu": perm_expert(inp["moe_w_up"][layer], 8),
        pfx + "wd": perm_expert(inp["moe_w_down"][layer], 4),
        pfx + "lng": f(inp["ln_g"][layer, 1]), pfx + "lnb": f(inp["ln_b"][layer, 1]),
    }


def build_program(shapes):
    nc = bass.Bass("TRN2", target_bir_lowering=False)
    kb = KB(nc)
    dd = {}
    for n, (shp, dt) in shapes.items():
        dd[n] = nc.dram_tensor(n, list(shp), dt, kind="ExternalInput").ap()
    out_d = nc.dram_tensor("out", [NTOK, D], F32, kind="ExternalOutput").ap()
    h1_d = kb.dram("h1", [NTOK, D])
    h2_d = kb.dram("h2", [NTOK, D])
    h3_d = kb.dram("h3", [NTOK, D])
    xs_d = kb.dram("xs", [NSLOT, D], BF16)
    ys_d = kb.dram("ys", [NSLOT, D], F32)
    o_d = kb.dram("o_scr", [SPC, 16, 128, TP], BF16)
    c = load_consts(kb, {k[2:]: v for k, v in dd.items() if k.startswith("c_")})
    build_even(kb, c, dd["h0"], dd["ev_win"], dd["ev_wout"], dd["ev_wuk"], dd["ev_wuv"], dd["ev_rgw"], dd["ev_vec"],
               dd["ev_lng"], dd["ev_lnb"], h1_d)
    build_moe(kb, c, h1_d, dd["m0_wr"], dd["m0_br"], dd["m0_wg"], dd["m0_wu"], dd["m0_wd"], dd["m0_lng"], dd["m0_lnb"],
              h2_d, xs_d, ys_d)
    build_gdn(kb, c, h2_d, dd["gd_w"], dd["gd_wab"], dd["gd_conv"], dd["gd_alog"], dd["gd_dtb"], dd["gd_onorm"],
              dd["gd_wout"], dd["gd_lng"], dd["gd_lnb"], h3_d, o_d)
    build_moe(kb, c, h3_d, dd["m1_wr"], dd["m1_br"], dd["m1_wg"], dd["m1_wu"], dd["m1_wd"], dd["m1_lng"], dd["m1_lnb"],
              out_d, xs_d, ys_d)
    kb.finish()
    return nc


def kernel(**inputs):
    import ml_dtypes
    inp = {k: np.asarray(v) for k, v in inputs.items()}
    x = inp["x"].astype(np.float32, copy=False)
    B = x.shape[0]
    hp = np.zeros((B, TP, D), np.float32)
    hp[:, :NMETA] = inp["meta_tokens"][None]
    hp[:, NMETA:T] = x
    shared = {}
    for k, v in make_consts().items():
        shared["c_" + k] = v
    shared.update(prep_even(inp, 0))
    shared.update(prep_gdn(inp, 0))
    shared.update(prep_moe(inp, 0, "m0_"))
    shared.update(prep_moe(inp, 1, "m1_"))
    shapes = {n: (v.shape, BF16 if v.dtype == ml_dtypes.bfloat16 else F32) for n, v in shared.items()}
    shapes["h0"] = ((NTOK, D), F32)
    nc = build_program(shapes)
    in_maps = []
    for ci in range(NCORES):
        m = dict(shared)
        m["h0"] = np.ascontiguousarray(hp[ci * SPC:(ci + 1) * SPC].reshape(NTOK, D))
        in_maps.append(m)
    res = run_bass_kernel_spmd(nc, in_maps, core_ids=list(range(NCORES)))
    outs = [r["out"].reshape(SPC, TP, D)[:, NMETA:T] for r in res.results]
    return np.ascontiguousarray(np.concatenate(outs, axis=0).astype(np.float32))
```

```python
import contextlib
import numpy as np
import concourse.bass as bass
import concourse.mybir as mybir
from concourse.bass_utils import run_bass_kernel_spmd

F32 = mybir.dt.float32
BF16 = mybir.dt.bfloat16
I32 = mybir.dt.int32
AF = mybir.ActivationFunctionType
ALU = mybir.AluOpType
AX = mybir.AxisListType

NCORES = 8
D = 1024
SEQ = 2048
NMETA = 16
T = SEQ + NMETA
TP = 17 * 128
NT = TP // 128
SPC = 4
NTOK = SPC * TP
NTILE = NTOK // 128
ALPHA = 4.0 ** 0.25
NEG = -1.0e30


class Res:
    __slots__ = ("w", "r", "dsem")

    def __init__(self):
        self.w = None
        self.r = {}
        self.dsem = None


class KB:
    def __init__(self, nc):
        self.nc = nc
        self.es = contextlib.ExitStack()
        self.stack = [self.es]
        self.eng = {"pe": nc.tensor, "act": nc.scalar, "dve": nc.vector, "pool": nc.gpsimd, "sp": nc.sync}
        self.sems = {}
        self.cnt = {}
        for k in self.eng:
            self.sems[k] = self.es.enter_context(nc.semaphore("sem_" + k))
            self.cnt[k] = 0
        self.seen = {k: {} for k in self.eng}
        self.res = {}
        self.dcount = {}
        self.free_dsems = []
        self.scope_res = [[]]
        self.nd = 0
        self.n_inst = 0
        self.uid = 0
        self.psum_names = set()

    def sb(self, name, shape, dtype=F32):
        self.uid += 1
        return self.stack[-1].enter_context(self.nc.sbuf_tensor("%s_%d" % (name, self.uid), list(shape), dtype))

    def ps(self, name, shape, dtype=F32):
        self.uid += 1
        nm = "%s_%d" % (name, self.uid)
        self.psum_names.add(nm)
        return self.stack[-1].enter_context(self.nc.psum_tensor(nm, list(shape), dtype))

    def dram(self, name, shape, dtype=F32, kind="Internal"):
        return self.nc.dram_tensor(name, list(shape), dtype, kind=kind).ap()

    @contextlib.contextmanager
    def scope(self):
        st = contextlib.ExitStack()
        self.stack.append(st)
        self.scope_res.append([])
        try:
            yield
        finally:
            self.barrier()
            for key in self.scope_res.pop():
                r = self.res.pop(key, None)
                if r is not None and r.dsem is not None:
                    self.free_dsems.append(r.dsem)
            self.stack.pop()
            st.close()

    def _res(self, ap):
        key = ap if isinstance(ap, str) else ap.name
        r = self.res.get(key)
        if r is None:
            r = self.res[key] = Res()
            self.scope_res[-1].append(key)
        return r

    def _wait(self, e, semkey, val):
        if semkey in self.dcount:
            val = max(val, self.dcount[semkey])
        if self.seen[e].get(semkey, 0) >= val:
            return
        if semkey == e and val > self.cnt[e]:
            return
        self.seen[e][semkey] = val
        self.eng[e].wait_ge(self.sems[semkey], val)

    def _deps(self, e, reads, writes):
        for a in reads:
            r = self._res(a)
            if r.w is not None:
                self._wait(e, *r.w)
        for a in writes:
            r = self._res(a)
            if r.w is not None:
                self._wait(e, *r.w)
            for sk, v in r.r.items():
                self._wait(e, sk, v)

    def _done(self, ev, reads, writes):
        for a in reads:
            r = self._res(a)
            if r.r.get(ev[0], 0) < ev[1]:
                r.r[ev[0]] = ev[1]
        for a in writes:
            r = self._res(a)
            r.w = ev
            r.r = {}

    def I(self, e, fn, reads, writes, *args, inc=True, **kw):
        writes = list(writes) + [a for a in reads if (not isinstance(a, str)) and a.name in self.psum_names]
        self._deps(e, reads, writes)
        ins = getattr(self.eng[e], fn)(*args, **kw)
        if inc:
            self.cnt[e] += 1
            ins.then_inc(self.sems[e], 1)
            self._done((e, self.cnt[e]), reads, writes)
        else:
            self._done((e, self.cnt[e] + 1), reads, writes)
        self.n_inst += 1
        return ins

    def dma(self, q, out, in_, reads=None, writes=None, group=None, indirect=None, **kw):
        reads = [in_] if reads is None else reads
        writes = [out] if writes is None else writes
        dst = self._res(group if group is not None else writes[0])
        if dst.dsem is None:
            if self.free_dsems:
                dst.dsem = self.free_dsems.pop()
            else:
                self.nd += 1
                dst.dsem = "d%d" % self.nd
                self.sems[dst.dsem] = self.es.enter_context(self.nc.semaphore(dst.dsem))
                self.dcount[dst.dsem] = 0
        if group is None:
            self._deps(q, reads, writes)
        else:
            self._deps(q, reads, [])
            for a in writes:
                r = self._res(a)
                if r.w is not None and r.w[0] != dst.dsem:
                    self._wait(q, *r.w)
                for sk, v in r.r.items():
                    self._wait(q, sk, v)
        if indirect is None:
            ins = self.eng[q].dma_start(out=out, in_=in_, **kw)
        else:
            ins = self.eng[q].indirect_dma_start(out=out, in_=in_, **indirect)
        self.dcount[dst.dsem] += 16
        ins.then_inc(self.sems[dst.dsem], 16)
        self._done((dst.dsem, self.dcount[dst.dsem]), reads, writes)
        self.n_inst += 1
        return ins

    def copy(self, e, reads, writes, out, in_):
        return self.I(e, "copy" if e == "act" else "tensor_copy", reads, writes, out=out, in_=in_)

    def barrier(self):
        for e in self.eng:
            for e2 in ("pe", "act", "dve", "pool"):
                if self.cnt[e2]:
                    self._wait(e, e2, self.cnt[e2])
            for sk, c in self.dcount.items():
                if c:
                    self._wait(e, sk, c)

    def finish(self):
        self.barrier()
        self.es.close()


def load_consts(kb, cd):
    c = {}
    for name, shape, dt in (("ident", [128, 128], F32), ("identb", [128, 128], BF16),
                            ("ones", [128, 128], F32), ("sut", [128, 128], F32),
                            ("ramp", [128, 128], F32), ("pidx", [128, 128], F32), ("cmask", [128, 128], F32),
                            ("onesb", [128, 128], BF16), ("cmaskb", [128, 128], BF16)):
        t = kb.sb("c_" + name, shape, dt)
        kb.dma("sp", t[:], cd[name])
        c[name] = t
    return c


def layer_norm_tile(kb, acc, g_bc, b_bc, outt, st6, mv, eng2="pool"):
    for j in range(2):
        kb.I("dve", "bn_stats", [acc], [st6], out=st6[:, j, :], in_=acc[:, j * 512:(j + 1) * 512])
    kb.I("dve", "bn_aggr", [st6], [mv], out=mv[:, 0:2], in_=st6[:].rearrange("p a b -> p (a b)"))
    kb.I("dve", "tensor_scalar", [mv], [mv], out=mv[:, 2:3], in0=mv[:, 1:2], scalar1=1e-5, scalar2=None, op0=ALU.add)
    kb.I("act", "sqrt", [mv], [mv], out=mv[:, 2:3], in_=mv[:, 2:3])
    kb.I("dve", "reciprocal", [mv], [mv], out=mv[:, 2:3], in_=mv[:, 2:3])
    kb.I("dve", "scalar_tensor_tensor", [mv], [mv], out=mv[:, 3:4], in0=mv[:, 0:1], scalar=-1.0, in1=mv[:, 2:3], op0=ALU.mult, op1=ALU.mult)
    kb.I("act", "activation", [acc, mv], [acc], out=acc[:], in_=acc[:], func=AF.Identity, scale=mv[:, 2:3], bias=mv[:, 3:4])
    kb.I("dve", "tensor_tensor", [acc, g_bc], [acc], out=acc[:], in0=acc[:], in1=g_bc[:], op=ALU.mult)
    kb.I(eng2, "tensor_tensor", [acc, b_bc], [outt], out=outt[:], in0=acc[:], in1=b_bc[:], op=ALU.add)


MOE_BS = 4
MOE_BLK = MOE_BS * 128
NBLK = -(-NTOK * 2 // MOE_BLK) + 32
NSLOT = NBLK * MOE_BLK


def build_moe(kb, c, h_d, wr_d, br_d, wg_d, wu_d, wd_d, lng_d, lnb_d, out_d, xs_d, ys_d, ntile=NTILE):
    nc = kb.nc
    nblk = -(-ntile * 128 * 2 // MOE_BLK) + 32
    BS, BLK = MOE_BS, MOE_BLK
    with kb.scope():
        s1i = kb.sb("s1i", [128, ntile], I32)
        s2i = kb.sb("s2i", [128, ntile], I32)
        g1 = kb.sb("g1", [128, ntile])
        g2 = kb.sb("g2", [128, ntile])
        idxe = kb.sb("idxe", [128, nblk], I32)
        g_bc = kb.sb("g_bc", [128, D])
        b_bc = kb.sb("b_bc", [128, D])
        kb.dma("sp", g_bc[:], lng_d.partition_broadcast(128))
        kb.dma("sp", b_bc[:], lnb_d.partition_broadcast(128))
        hts = [kb.sb("ht%d" % i, [128, D]) for i in range(2)]

        with kb.scope():
            wr = kb.sb("wr", [128, 8, 36])
            kb.dma("sp", wr[:], wr_d.rearrange("(k p) e -> p k e", p=128))
            br = kb.sb("br", [128, 36])
            kb.dma("sp", br[:], br_d.partition_broadcast(128))
            L = kb.sb("L", [128, ntile, 36])
            hT = [kb.sb("hT%d" % i, [128, 8, 128]) for i in range(2)]
            tp = [kb.ps("tp%d" % i, [128, 8, 128]) for i in range(2)]
            lg = [kb.ps("lg%d" % i, [128, 36]) for i in range(2)]
            for t in range(ntile):
                ht = hts[t % 2]
                kb.dma("sp", ht[:], h_d[t * 128:(t + 1) * 128, :])
                tpp = tp[t % 2]
                for k in range(8):
                    kb.I("pe", "transpose", [ht, c["ident"]], [tpp], out=tpp[:, k, :], in_=ht[:, k * 128:(k + 1) * 128],
                         identity=c["ident"][:], inc=(k == 7))
                hTt = hT[t % 2]
                kb.I("act", "copy", [tpp], [hTt], out=hTt[:], in_=tpp[:])
                lgp = lg[t % 2]
                for k in range(8):
                    kb.I("pe", "matmul", [hTt, wr], [lgp], lgp[:], lhsT=hTt[:, k, :], rhs=wr[:, k, :],
                         start=(k == 0), stop=(k == 7), inc=(k == 7))
                kb.I("dve", "tensor_tensor", [lgp, br], [L], out=L[:, t, :], in0=lgp[:], in1=br[:], op=ALU.add)

            NE = ntile * 32
            GL = L[:, :, 0:4]
            EL = L[:, :, 4:36]
            gmax = kb.sb("gmax", [128, ntile])
            t4 = kb.sb("t4", [128, ntile, 4])
            goh = kb.sb("goh", [128, ntile, 4])
            gg = kb.sb("gg", [128, ntile])
            ELm = kb.sb("ELm", [128, ntile, 32])
            EL2 = kb.sb("EL2", [128, ntile, 32])
            oh1 = kb.sb("oh1", [128, ntile, 32])
            oh2 = kb.sb("oh2", [128, ntile, 32])
            m1 = kb.sb("m1", [128, ntile])
            m2 = kb.sb("m2", [128, ntile])
            tmp = kb.sb("tmp", [128, ntile])
            V = "dve"
            kb.I(V, "tensor_reduce", [L], [gmax], out=gmax[:], in_=GL, axis=AX.X, op=ALU.max)
            bc4 = lambda a: a[:].unsqueeze(2).to_broadcast([128, ntile, 4])
            bc32 = lambda a: a[:].unsqueeze(2).to_broadcast([128, ntile, 32])
            kb.I(V, "tensor_tensor", [L, gmax], [goh], out=goh[:], in0=GL, in1=bc4(gmax), op=ALU.is_equal)
            kb.I(V, "tensor_tensor", [L, gmax], [t4], out=t4[:], in0=GL, in1=bc4(gmax), op=ALU.subtract)
            kb.I("act", "activation", [t4], [t4], out=t4[:], in_=t4[:], func=AF.Exp)
            kb.I(V, "tensor_reduce", [t4], [gg], out=gg[:], in_=t4[:], axis=AX.X, op=ALU.add)
            kb.I(V, "reciprocal", [gg], [gg], out=gg[:], in_=gg[:])
            kb.I(V, "tensor_scalar", [goh], [t4], out=t4[:], in0=goh[:], scalar1=-NEG, scalar2=NEG,
                 op0=ALU.mult, op1=ALU.add)
            kb.I(V, "tensor_tensor", [L, t4], [ELm], out=ELm[:].rearrange("p t (g e) -> p t g e", g=4),
                 in0=EL.rearrange("p t (g e) -> p t g e", g=4),
                 in1=t4[:].unsqueeze(3).to_broadcast([128, ntile, 4, 8]), op=ALU.add)
            kb.I(V, "tensor_reduce", [ELm], [m1], out=m1[:], in_=ELm[:], axis=AX.X, op=ALU.max)
            kb.I(V, "tensor_tensor", [ELm, m1], [oh1], out=oh1[:], in0=ELm[:], in1=bc32(m1), op=ALU.is_equal)
            kb.I(V, "scalar_tensor_tensor", [oh1, ELm], [EL2], out=EL2[:], in0=oh1[:], scalar=NEG, in1=ELm[:],
                 op0=ALU.mult, op1=ALU.add)
            kb.I(V, "tensor_reduce", [EL2], [m2], out=m2[:], in_=EL2[:], axis=AX.X, op=ALU.max)
            kb.I(V, "tensor_tensor", [EL2, m2], [oh2], out=oh2[:], in0=EL2[:], in1=bc32(m2), op=ALU.is_equal)
            kb.I(V, "tensor_tensor", [m1, m2], [tmp], out=tmp[:], in0=m2[:], in1=m1[:], op=ALU.subtract)
            kb.I("act", "activation", [tmp], [tmp], out=tmp[:], in_=tmp[:], func=AF.Exp)
            kb.I(V, "tensor_scalar", [tmp], [m1], out=m1[:], in0=tmp[:], scalar1=1.0, scalar2=None, op0=ALU.add)
            kb.I(V, "reciprocal", [m1], [m1], out=m1[:], in_=m1[:])
            kb.I(V, "tensor_tensor", [tmp, m1], [m2], out=m2[:], in0=tmp[:], in1=m1[:], op=ALU.mult)
            kb.I(V, "tensor_tensor", [m1, gg], [g1], out=g1[:], in0=m1[:], in1=gg[:], op=ALU.mult)
            kb.I(V, "tensor_tensor", [m2, gg], [g2], out=g2[:], in0=m2[:], in1=gg[:], op=ALU.mult)
            OH = ELm
            kb.I(V, "tensor_tensor", [oh1, oh2], [OH], out=OH[:], in0=oh1[:], in1=oh2[:], op=ALU.add)
            RK = kb.sb("RK", [128, NE])
            CT = kb.sb("CT", [128, ntile, 32])
            OHf = OH[:].rearrange("p t e -> p (t e)")
            CTf = CT[:].rearrange("p t e -> p (t e)")
            pr = [kb.ps("pr%d" % i, [128, 512]) for i in range(2)]
            ci = 0
            for dst, lhs in ((RK[:], c["sut"]), (CTf, c["ones"])):
                for o in range(0, NE, 512):
                    n = min(512, NE - o)
                    p = pr[ci % 2]
                    ci += 1
                    kb.I("pe", "matmul", [lhs, OH], [p], p[:, 0:n], lhsT=lhs[:], rhs=OHf[:, o:o + n],
                         start=True, stop=True)
                    kb.I("act", "copy", [p], [RK if dst is not CTf else CT], out=dst[:, o:o + n], in_=p[:, 0:n])
            base = kb.sb("base", [128, ntile, 32])
            kb.I(V, "memset", [], [base], base[:, 0, :], 0.0)
            for t in range(1, ntile):
                kb.I(V, "tensor_tensor", [base, CT], [base], out=base[:, t, :], in0=base[:, t - 1, :],
                     in1=CT[:, t - 1, :], op=ALU.add)
            tot = kb.sb("tot", [128, 32])
            pad = kb.sb("pad", [128, 32])
            pend = kb.sb("pend", [128, 32])
            one32 = kb.sb("one32", [128, 32])
            kb.I(V, "memset", [], [one32], one32[:], 1.0)
            kb.I(V, "tensor_tensor", [base, CT], [tot], out=tot[:], in0=base[:, ntile - 1, :], in1=CT[:, ntile - 1, :],
                 op=ALU.add)
            thr = kb.sb("thr", [128, nblk])
            kb.I(V, "tensor_scalar", [c["ramp"]], [thr], out=thr[:], in0=c["ramp"][:, 0:nblk], scalar1=float(BLK),
                 scalar2=None, op0=ALU.mult)
            cmp0 = kb.sb("cmp0", [128, 32, nblk])
            kb.I(V, "tensor_tensor", [tot, thr], [cmp0], out=cmp0[:],
                 in0=tot[:].unsqueeze(2).to_broadcast([128, 32, nblk]),
                 in1=thr[:].unsqueeze(1).to_broadcast([128, 32, nblk]), op=ALU.is_gt)
            kb.I(V, "tensor_reduce", [cmp0], [pad], out=pad[:], in_=cmp0[:], axis=AX.X, op=ALU.add)
            kb.I(V, "tensor_scalar", [pad], [pad], out=pad[:], in0=pad[:], scalar1=float(BLK), scalar2=None, op0=ALU.mult)
            kb.I(V, "tensor_tensor_scan", [one32, pad], [pend], out=pend[:], data0=one32[:], data1=pad[:], initial=0.0,
                 op0=ALU.mult, op1=ALU.add)
            kb.I(V, "tensor_tensor", [pend, pad], [pad], out=pad[:], in0=pend[:], in1=pad[:], op=ALU.subtract)
            cmp = kb.sb("cmp", [128, nblk, 32])
            bef = kb.sb("bef", [128, nblk])
            kb.I(V, "tensor_scalar", [c["ramp"]], [bef], out=bef[:], in0=c["ramp"][:, 0:nblk], scalar1=float(BLK),
                 scalar2=None, op0=ALU.mult)
            kb.I(V, "tensor_tensor", [pend, bef], [cmp], out=cmp[:],
                 in0=pend[:].unsqueeze(1).to_broadcast([128, nblk, 32]),
                 in1=bef[:].unsqueeze(2).to_broadcast([128, nblk, 32]), op=ALU.is_le)
            kb.I(V, "tensor_reduce", [cmp], [bef], out=bef[:], in_=cmp[:], axis=AX.X, op=ALU.add)
            kb.I(V, "tensor_scalar", [bef], [bef], out=bef[:], in0=bef[:], scalar1=31.0, scalar2=None, op0=ALU.min)
            ig = kb.sb("ig", [128, nblk])
            kb.I(V, "tensor_scalar", [bef, c["pidx"]], [ig], out=ig[:], in0=bef[:], scalar1=128.0, scalar2=c["pidx"][:, 0:1],
                 op0=ALU.mult, op1=ALU.add)
            usedf = kb.sb("usedf", [128, nblk])
            kb.I(V, "tensor_scalar", [thr, pend], [usedf], out=usedf[:], in0=thr[:], scalar1=pend[:, 31:32], scalar2=None, op0=ALU.is_lt)
            kb.I(V, "tensor_tensor", [ig, usedf], [ig], out=ig[:], in0=ig[:], in1=usedf[:], op=ALU.mult)
            kb.I(V, "tensor_scalar", [usedf], [usedf], out=usedf[:], in0=usedf[:], scalar1=-8192.0, scalar2=8192.0, op0=ALU.mult, op1=ALU.add)
            kb.I(V, "tensor_tensor", [ig, usedf], [ig], out=ig[:], in0=ig[:], in1=usedf[:], op=ALU.add)
            kb.I(V, "tensor_copy", [ig], [idxe], out=idxe[:], in_=ig[:])
            SL = EL2
            RK3 = RK[:].rearrange("p (t e) -> p t e", e=32)
            kb.I(V, "tensor_tensor", [RK, base], [SL], out=SL[:], in0=RK3, in1=base[:], op=ALU.add)
            kb.I(V, "tensor_tensor", [SL, pad], [SL], out=SL[:], in0=SL[:],
                 in1=pad[:].unsqueeze(1).to_broadcast([128, ntile, 32]), op=ALU.add)
            for oh, si in ((oh1, s1i), (oh2, s2i)):
                kb.I(V, "tensor_tensor", [SL, oh], [oh], out=oh[:], in0=SL[:], in1=oh[:], op=ALU.mult)
                kb.I(V, "tensor_reduce", [oh], [tmp], out=tmp[:], in_=oh[:], axis=AX.X, op=ALU.add)
                kb.I(V, "tensor_copy", [tmp], [si], out=si[:], in_=tmp[:])

        with kb.scope():
            hbs = [kb.sb("hb%d" % i, [128, D], BF16) for i in range(2)]
            for t in range(ntile):
                ht = hts[t % 2]
                hb = hbs[t % 2]
                kb.dma("sp", ht[:], h_d[t * 128:(t + 1) * 128, :])
                kb.I("act", "copy", [ht], [hb], out=hb[:], in_=ht[:])
                for si in (s1i, s2i):
                    kb.dma("pool", xs_d, hb[:], reads=[hb, si], writes=[xs_d], indirect=dict(
                        out_offset=bass.IndirectOffsetOnAxis(ap=si[:, t:t + 1], axis=0), in_offset=None))

        with kb.scope():
            xin = [[kb.sb("xin%d_%d" % (i, s), [128, D], BF16) for s in range(BS)] for i in range(2)]
            xT = [kb.sb("xT%d" % i, [128, 8, BLK], BF16) for i in range(2)]
            tps = [kb.ps("tps%d" % i, [128, 8, 128], BF16) for i in range(2)]
            stg = [kb.sb("stg%d" % i, [128, 4096]) for i in range(3)]
            wgb = [kb.sb("wgb%d" % i, [128, 8, 512], BF16) for i in range(2)]
            wub = [kb.sb("wub%d" % i, [128, 8, 512], BF16) for i in range(2)]
            wdb = [kb.sb("wdb%d" % i, [128, 4, 1024], BF16) for i in range(2)]
            hgp = [kb.ps("hgp%d" % i, [128, BLK]) for i in range(2)]
            hup = [kb.ps("hup%d" % i, [128, BLK]) for i in range(2)]
            yp = [kb.ps("yp%d" % i, [128, 512]) for i in range(2)]
            sg = [kb.sb("sg%d" % i, [128, BLK]) for i in range(2)]
            hTb = [kb.sb("hTb%d" % i, [128, 4, BLK], BF16) for i in range(2)]
            yb = [kb.sb("yb%d" % i, [128, D]) for i in range(2)]
            sti = 0
            cast_eng = ["dve", "act"]
            bc_reg = nc.gpsimd.alloc_register("moe_bc_%d" % kb.uid)
            nc.gpsimd.reg_mov(bc_reg, 4095)

            def load_weights(b):
                nonlocal sti
                par = b % 2
                for src, dstt in ((wg_d, wgb[par]), (wu_d, wub[par]), (wd_d, wdb[par])):
                    st = stg[sti % 3]
                    kb.dma("pool", st[:], src, reads=[idxe], writes=[st],
                           indirect=dict(out_offset=None, in_offset=bass.IndirectOffsetOnAxis(ap=idxe[:, b:b + 1], axis=0),
                                         bounds_check=bc_reg, oob_is_err=False))
                    dflat = dstt[:].rearrange("p a b -> p (a b)")
                    for half in range(2):
                        ce = cast_eng[(2 * sti + half) % 2]
                        kb.copy(ce, [st], [dstt], dflat[:, half * 2048:(half + 1) * 2048], st[:, half * 2048:(half + 1) * 2048])
                    sti += 1

            def load_tokens(b):
                par = b % 2
                for s in range(BS):
                    xi = xin[par][s]
                    kb.dma("act", xi[:], xs_d[b * BLK + s * 128: b * BLK + (s + 1) * 128, :])
                    tpp = tps[s % 2]
                    for k in range(8):
                        kb.I("pe", "transpose", [xi, c["identb"]], [tpp], out=tpp[:, k, :], in_=xi[:, k * 128:(k + 1) * 128],
                             identity=c["identb"][:], inc=(k == 7))
                    kb.I("dve", "tensor_copy", [tpp], [xT[par]], out=xT[par][:, :, s * 128:(s + 1) * 128], in_=tpp[:])

            def gate_up(b):
                par = b % 2
                for f in range(4):
                    hg = hgp[f % 2]
                    hu = hup[f % 2]
                    for k in range(8):
                        kb.I("pe", "matmul", [wgb[par], xT[par]], [hg], hg[:], lhsT=wgb[par][:, k, f * 128:(f + 1) * 128],
                             rhs=xT[par][:, k, :], start=(k == 0), stop=(k == 7), inc=(k == 7))
                    for k in range(8):
                        kb.I("pe", "matmul", [wub[par], xT[par]], [hu], hu[:], lhsT=wub[par][:, k, f * 128:(f + 1) * 128],
                             rhs=xT[par][:, k, :], start=(k == 0), stop=(k == 7), inc=(k == 7))
                    sgt = sg[f % 2]
                    kb.I("act", "activation", [hg], [sgt], out=sgt[:], in_=hg[:], func=AF.Silu)
                    kb.I("dve", "tensor_tensor", [sgt, hu], [hTb[par]], out=hTb[par][:, f, :], in0=sgt[:], in1=hu[:], op=ALU.mult)

            def down(b):
                par = b % 2
                for s in range(BS):
                    ybt = yb[s % 2]
                    for dh in range(2):
                        y = yp[dh]
                        for f in range(4):
                            kb.I("pe", "matmul", [hTb[par], wdb[par]], [y], y[:], lhsT=hTb[par][:, f, s * 128:(s + 1) * 128],
                                 rhs=wdb[par][:, f, dh * 512:(dh + 1) * 512], start=(f == 0), stop=(f == 3), inc=(f == 3))
                        kb.I("act" if dh == 0 else "dve", "tensor_copy" if dh else "copy", [y], [ybt],
                             out=ybt[:, dh * 512:(dh + 1) * 512], in_=y[:])
                    kb.dma("sp", ys_d[b * BLK + s * 128: b * BLK + (s + 1) * 128, :], ybt[:])

            load_weights(0)
            load_tokens(0)
            for b in range(nblk):
                if b + 1 < nblk:
                    load_weights(b + 1)
                gate_up(b)
                if b + 1 < nblk:
                    load_tokens(b + 1)
                down(b)

        with kb.scope():
            NS = 3
            y1 = [kb.sb("y1_%d" % i, [128, D]) for i in range(NS)]
            y2 = [kb.sb("y2_%d" % i, [128, D]) for i in range(NS)]
            hts5 = [kb.sb("ht5_%d" % i, [128, D]) for i in range(NS)]
            acc = [kb.sb("acc%d" % i, [128, D]) for i in range(2)]
            outt = [kb.sb("outt%d" % i, [128, D]) for i in range(2)]
            st6 = kb.sb("st6", [128, 2, 6])
            mv = kb.sb("mv", [128, 4])

            def issue(t):
                p = t % NS
                kb.dma("sp", hts5[p][:], h_d[t * 128:(t + 1) * 128, :])
                for yy, si in ((y1[p], s1i), (y2[p], s2i)):
                    kb.dma("pool", yy[:], ys_d, reads=[ys_d, si], writes=[yy], indirect=dict(
                        out_offset=None, in_offset=bass.IndirectOffsetOnAxis(ap=si[:, t:t + 1], axis=0)))
            for t in range(min(NS - 1, ntile)):
                issue(t)
            for t in range(ntile):
                if t + NS - 1 < ntile:
                    issue(t + NS - 1)
                p = t % NS
                ht = hts5[p]
                a = acc[t % 2]
                kb.I("act", "mul", [y1[p], g1], [a], out=a[:], in_=y1[p][:], mul=g1[:, t:t + 1])
                kb.I("dve", "scalar_tensor_tensor", [y2[p], g2, a], [a], out=a[:], in0=y2[p][:], scalar=g2[:, t:t + 1],
                     in1=a[:], op0=ALU.mult, op1=ALU.add)
                kb.I("dve", "scalar_tensor_tensor", [ht, a], [a], out=a[:], in0=ht[:], scalar=ALPHA, in1=a[:],
                     op0=ALU.mult, op1=ALU.add)
                layer_norm_tile(kb, a, g_bc, b_bc, outt[t % 2], st6, mv)
                kb.dma("sp", out_d[t * 128:(t + 1) * 128, :], outt[t % 2][:])

def make_consts():
    import ml_dtypes
    i = np.arange(128)
    return {
        "ident": np.eye(128, dtype=np.float32),
        "identb": np.eye(128, dtype=np.float32).astype(ml_dtypes.bfloat16),
        "ones": np.ones((128, 128), np.float32),
        "sut": (i[:, None] < i[None, :]).astype(np.float32),
        "ramp": np.broadcast_to(i[None, :].astype(np.float32), (128, 128)).copy(),
        "pidx": np.broadcast_to(i[:, None].astype(np.float32), (128, 128)).copy(),
        "cmask": np.where(i[None, :] > i[:, None], NEG, 0.0).astype(np.float32),
        "onesb": np.ones((128, 128), np.float32).astype(ml_dtypes.bfloat16),
        "cmaskb": np.where(i[None, :] > i[:, None], NEG, 0.0).astype(np.float32).astype(ml_dtypes.bfloat16),
    }


CHUNKS = [(0, 512), (512, 512), (1024, 512), (1536, 512), (2048, 128)]
EV_Q, EV_CKV, EV_QI, EV_KI, EV_WI, EV_GATE, EV_XB = 0, 512, 768, 1280, 1344, 1352, 1864


def proj_fm(kb, ps2, cnt, wb, col0, hT, dst_fn, evac):
    for (t0, n) in CHUNKS:
        p = ps2[cnt[0] % 2]
        for k in range(8):
            kb.I("pe", "matmul", [wb, hT], [p], p[:, 0:n], lhsT=wb[:, k, col0:col0 + 128], rhs=hT[:, k, t0:t0 + n],
                 start=(k == 0), stop=(k == 7), inc=(k == 7))
        evac(cnt[0], p[:, 0:n], t0, n)
        cnt[0] += 1


def build_even(kb, c, h_d, win_d, wout_d, wuk_d, wuv_d, rgw_d, vec_d, lng_d, lnb_d, out_d, nseq=SPC):
    with kb.scope():
        winb = kb.sb("winb", [128, 8, 2376], BF16)
        wk2 = kb.sb("wk2", [128, 8, 128], BF16)
        wukb = kb.sb("wukb", [128, 2, 128], BF16)
        wuvb = kb.sb("wuvb", [128, 2, 128], BF16)
        rgwb = kb.sb("rgwb", [128, 8, 128], BF16)
        vec = kb.sb("vec", [128, 40])
        cc = kb.sb("cc", [128, 4])
        g_bc = kb.sb("g_bc", [128, D])
        b_bc = kb.sb("b_bc", [128, D])
        kb.dma("sp", g_bc[:], lng_d.partition_broadcast(128))
        kb.dma("sp", b_bc[:], lnb_d.partition_broadcast(128))
        kb.dma("sp", vec[:], vec_d)
        with kb.scope():
            stg = [kb.sb("wstg%d" % i, [128, 2376]) for i in range(2)]
            for k in range(8):
                st = stg[k % 2]
                kb.dma("sp", st[:], win_d[k * 128:(k + 1) * 128, :])
                kb.copy("act" if k % 2 else "pool", [st], [winb], winb[:, k, :], st[:])
            for j in range(2):
                kb.I("dve", "tensor_copy", [winb], [wk2], out=wk2[:, :, j * 64:(j + 1) * 64], in_=winb[:, :, EV_KI:EV_KI + 64])
            st = stg[0]
            kb.dma("sp", st[:, 0:256], wuk_d.rearrange("(c p) d -> p c d", p=128))
            kb.I("dve", "tensor_copy", [st], [wukb], out=wukb[:], in_=st[:, 0:256].rearrange("p (c d) -> p c d", c=2))
            st = stg[1]
            kb.dma("sp", st[:, 0:256], wuv_d.rearrange("(c p) d -> p c d", p=128))
            kb.I("dve", "tensor_copy", [st], [wuvb], out=wuvb[:], in_=st[:, 0:256].rearrange("p (c d) -> p c d", c=2))
            st = stg[0]
            kb.dma("sp", st[:, 0:1024], rgw_d)
            kb.I("dve", "tensor_copy", [st], [rgwb], out=rgwb[:], in_=st[:, 0:1024].rearrange("p (c d) -> p c d", c=8))
            kb.I("act", "activation", [vec], [cc], out=cc[:], in_=vec[:, 28:32], func=AF.Exp, scale=-1.0)
            kb.I("act", "activation", [cc], [cc], out=cc[:], in_=cc[:], func=AF.Ln, bias=1.0)
            kb.I("dve", "tensor_scalar", [cc], [cc], out=cc[:], in0=cc[:], scalar1=-8.0, scalar2=None, op0=ALU.mult)
        cw = lambda j, ch: vec[:, j * 4 + ch: j * 4 + ch + 1]
        cb = lambda ch: vec[:, 16 + ch:17 + ch]
        ba = lambda ch: vec[:, 20 + ch:21 + ch]
        bx = lambda ch: vec[:, 24 + ch:25 + ch]
        kvn = lambda ch: vec[:, 32 + ch:33 + ch]

        for s in range(nseq):
            r0 = s * TP
            with kb.scope():
                hT = kb.sb("hT", [128, 8, TP], BF16)
                qT = kb.sb("qT", [128, 4, TP], BF16)
                qiT = kb.sb("qiT", [128, 4, TP], BF16)
                kiT2 = kb.sb("kiT2", [128, TP], BF16)
                kT = kb.sb("kT", [128, TP], BF16)
                vtok = kb.sb("vtok", [128, NT, 130], BF16)
                wq = kb.sb("wq", [128, NT, 8])
                kb.I("pool", "memset", [], [vtok], vtok[:, :, 128:130], 1.0)
                with kb.scope():
                    gateT = kb.sb("gateT", [128, 4, TP], BF16)
                    xbT = kb.sb("xbT", [128, 4, TP], BF16)
                    with kb.scope():
                        hts = [kb.sb("eht%d" % i, [128, D]) for i in range(2)]
                        tp = [kb.ps("etp%d" % i, [128, 8, 128]) for i in range(2)]
                        for t in range(NT):
                            ht = hts[t % 2]
                            kb.dma("sp", ht[:], h_d[r0 + t * 128: r0 + (t + 1) * 128, :])
                            tpp = tp[t % 2]
                            for k in range(8):
                                kb.I("pe", "transpose", [ht, c["ident"]], [tpp], out=tpp[:, k, :],
                                     in_=ht[:, k * 128:(k + 1) * 128], identity=c["ident"][:], inc=(k == 7))
                            kb.copy("act" if t % 2 else "dve", [tpp], [hT], hT[:, :, t * 128:(t + 1) * 128], tpp[:])
                    with kb.scope():
                        ckvT = kb.sb("ckvT", [128, 2, TP], BF16)
                        latT = kb.sb("latT", [128, 2, TP], BF16)
                        ps2 = [kb.ps("pp%d" % i, [128, 512]) for i in range(2)]
                        cnt = [0]

                        def mk_evac(dst, ci):
                            def ev(n_, p, t0, n):
                                kb.copy("act" if n_ % 2 else "dve", [p], [dst], dst[:, ci, t0:t0 + n] if ci is not None else dst[:, t0:t0 + n], p)
                            return ev
                        for ci in range(4):
                            proj_fm(kb, ps2, cnt, winb, EV_Q + ci * 128, hT, None, mk_evac(qT, ci))
                            proj_fm(kb, ps2, cnt, winb, EV_QI + ci * 128, hT, None, mk_evac(qiT, ci))
                            proj_fm(kb, ps2, cnt, winb, EV_GATE + ci * 128, hT, None, mk_evac(gateT, ci))
                            proj_fm(kb, ps2, cnt, winb, EV_XB + ci * 128, hT, None, mk_evac(xbT, ci))
                        for ci in range(2):
                            proj_fm(kb, ps2, cnt, winb, EV_CKV + ci * 128, hT, None, mk_evac(ckvT, ci))
                        proj_fm(kb, ps2, cnt, wk2, 0, hT, None, mk_evac(kiT2, None))
                        wps = [kb.ps("wps%d" % i, [128, 8]) for i in range(2)]
                        for t in range(NT):
                            p = wps[t % 2]
                            for k in range(8):
                                kb.I("pe", "matmul", [hT, winb], [p], p[:], lhsT=hT[:, k, t * 128:(t + 1) * 128],
                                     rhs=winb[:, k, EV_WI:EV_WI + 8], start=(k == 0), stop=(k == 7), inc=(k == 7))
                            kb.copy("act", [p], [wq], wq[:, t, :], p[:])
                        sq = kb.sb("sq", [128, 2, 512], BF16)
                        rstd = kb.sb("rstd", [128, 512])
                        for (t0, n) in CHUNKS:
                            kb.I("act", "activation", [ckvT], [sq], out=sq[:, :, 0:n], in_=ckvT[:, :, t0:t0 + n], func=AF.Square)
                            p = ps2[cnt[0] % 2]
                            cnt[0] += 1
                            for ci in range(2):
                                kb.I("pe", "matmul", [c["onesb"], sq], [p], p[:, 0:n], lhsT=c["onesb"][:], rhs=sq[:, ci, 0:n],
                                     start=(ci == 0), stop=(ci == 1), inc=(ci == 1))
                            kb.I("dve", "tensor_scalar", [p], [rstd], out=rstd[:, 0:n], in0=p[:, 0:n], scalar1=1.0 / 256, scalar2=1e-6,
                                 op0=ALU.mult, op1=ALU.add)
                            kb.I("act", "sqrt", [rstd], [rstd], out=rstd[:, 0:n], in_=rstd[:, 0:n])
                            kb.I("dve", "reciprocal", [rstd], [rstd], out=rstd[:, 0:n], in_=rstd[:, 0:n])
                            for ci in range(2):
                                kb.I("dve", "scalar_tensor_tensor", [ckvT, vec, rstd], [latT], out=latT[:, ci, t0:t0 + n],
                                     in0=ckvT[:, ci, t0:t0 + n], scalar=kvn(ci), in1=rstd[:, 0:n], op0=ALU.mult, op1=ALU.mult)
                            p = ps2[cnt[0] % 2]
                            cnt[0] += 1
                            for ci in range(2):
                                kb.I("pe", "matmul", [wukb, latT], [p], p[:, 0:n], lhsT=wukb[:, ci, :], rhs=latT[:, ci, t0:t0 + n],
                                     start=(ci == 0), stop=(ci == 1), inc=(ci == 1))
                            kb.copy("act", [p], [kT], kT[:, t0:t0 + n], p[:, 0:n])
                        for t in range(NT):
                            p = ps2[cnt[0] % 2]
                            cnt[0] += 1
                            for ci in range(2):
                                kb.I("pe", "matmul", [latT, wuvb], [p], p[:, 0:128], lhsT=latT[:, ci, t * 128:(t + 1) * 128],
                                     rhs=wuvb[:, ci, :], start=(ci == 0), stop=(ci == 1), inc=(ci == 1))
                            kb.copy("dve", [p], [vtok], vtok[:, t, 0:128], p[:, 0:128])
                    mixT = hT
                    with kb.scope():
                        HS = TP // 2
                        F = lambda nm: kb.sb(nm, [128, HS])
                        xr, rr, ii, aa, uu, hh, gl = F("xr"), F("rr"), F("ii"), F("aa"), F("uu"), F("hh"), F("gl")
                        xrb = kb.sb("xrb", [128, HS], BF16)
                        carry = kb.sb("carry", [128, 4])
                        gps = [kb.ps("gps%d" % i, [128, 512]) for i in range(4)]
                        gi = 0
                        for ch in range(4):
                            for hf in range(2):
                                o = hf * HS
                                x = xbT[:, ch, :]
                                kb.I("dve", "tensor_scalar", [xbT, vec], [xr], out=xr[:], in0=x[:, o:o + HS], scalar1=cw(3, ch), scalar2=cb(ch),
                                     op0=ALU.mult, op1=ALU.add)
                                for d in (1, 2, 3):
                                    lo = d if hf == 0 else 0
                                    kb.I("dve", "scalar_tensor_tensor", [xbT, vec, xr], [xr], out=xr[:, lo:HS], in0=x[:, o + lo - d:o + HS - d],
                                         scalar=cw(3 - d, ch), in1=xr[:, lo:HS], op0=ALU.mult, op1=ALU.add)
                                kb.copy("pool", [xr], [xrb], xrb[:], xr[:])
                                for (t0, n) in ((0, 512), (512, 512), (1024, 64)):
                                    pa = gps[gi % 4]
                                    px = gps[(gi + 1) % 4]
                                    gi += 2
                                    kb.I("pe", "matmul", [rgwb, xrb], [pa], pa[:, 0:n], lhsT=rgwb[:, ch, :], rhs=xrb[:, t0:t0 + n], start=True, stop=True)
                                    kb.I("pe", "matmul", [rgwb, xrb], [px], px[:, 0:n], lhsT=rgwb[:, 4 + ch, :], rhs=xrb[:, t0:t0 + n], start=True, stop=True)
                                    kb.I("act", "activation", [pa, vec], [rr], out=rr[:, t0:t0 + n], in_=pa[:, 0:n], func=AF.Sigmoid, bias=ba(ch))
                                    kb.I("act", "activation", [px, vec], [ii], out=ii[:, t0:t0 + n], in_=px[:, 0:n], func=AF.Sigmoid, bias=bx(ch))
                                kb.I("act", "activation", [rr, cc], [aa], out=aa[:], in_=rr[:], func=AF.Exp, scale=cc[:, ch:ch + 1])
                                kb.I("pool", "tensor_tensor", [aa], [uu], out=uu[:], in0=aa[:], in1=aa[:], op=ALU.mult)
                                kb.I("act", "activation", [uu], [uu], out=uu[:], in_=uu[:], func=AF.Sqrt, scale=-1.0, bias=1.0)
                                kb.I("pool", "tensor_tensor", [ii, xr], [ii], out=ii[:], in0=ii[:], in1=xr[:], op=ALU.mult)
                                kb.I("pool", "tensor_tensor", [uu, ii], [uu], out=uu[:], in0=uu[:], in1=ii[:], op=ALU.mult)
                                kb.I("dve", "tensor_tensor_scan", [aa, uu, carry], [hh], out=hh[:], data0=aa[:], data1=uu[:],
                                     initial=(0.0 if hf == 0 else carry[:, ch:ch + 1]), op0=ALU.mult, op1=ALU.add)
                                if hf == 0:
                                    kb.I("dve", "tensor_copy", [hh], [carry], out=carry[:, ch:ch + 1], in_=hh[:, HS - 1:HS])
                                g = gateT[:, ch, o:o + HS]
                                kb.I("act", "activation", [gateT], [gl], out=gl[:], in_=g, func=AF.Square)
                                kb.I("pool", "tensor_scalar", [gl], [gl], out=gl[:], in0=gl[:], scalar1=0.044715, scalar2=1.0, op0=ALU.mult, op1=ALU.add)
                                kb.I("pool", "tensor_tensor", [gl, gateT], [gl], out=gl[:], in0=gl[:], in1=g, op=ALU.mult)
                                kb.I("act", "activation", [gl], [gl], out=gl[:], in_=gl[:], func=AF.Sigmoid, scale=1.5957691216)
                                kb.I("pool", "tensor_tensor", [gl, gateT], [gl], out=gl[:], in0=gl[:], in1=g, op=ALU.mult)
                                kb.I("dve", "tensor_tensor", [hh, gl], [mixT], out=mixT[:, 4 + ch, o:o + HS], in0=hh[:], in1=gl[:], op=ALU.mult)
                with kb.scope():
                    score = [kb.sb("score%d" % i, [128, TP]) for i in range(2)]
                    mask = [kb.sb("mask%d" % i, [128, TP], BF16) for i in range(2)]
                    m8 = [kb.sb("m8_%d" % i, [128, 8]) for i in range(2)]
                    work = kb.sb("work", [128, TP])
                    thrc = kb.sb("thrc", [128, 1])
                    rlb = [kb.sb("rlb%d" % i, [128, 512], BF16) for i in range(4)]
                    dg = [kb.sb("dg%d" % i, [128, 8, 128], BF16) for i in range(1)]
                    lgt = [kb.sb("lgt%d" % i, [128, TP]) for i in range(2)]
                    ee = [kb.sb("ee%d" % i, [128, TP], BF16) for i in range(2)]
                    pT = [kb.sb("pT%d" % i, [128, NT, 128], BF16) for i in range(1)] * 2
                    sm = [kb.sb("sm%d" % i, [128, 4]) for i in range(2)]
                    on = [kb.sb("on%d" % i, [128, 128], BF16) for i in range(2)]
                    sps = [kb.ps("sps%d" % i, [128, 512]) for i in range(2)]
                    lps = [kb.ps("lps%d" % i, [128, 512]) for i in range(2)]
                    tps = [kb.ps("atp%d" % i, [128, 8, 128], BF16) for i in range(2)]
                    ops = kb.ps("ops", [128, 132])
                    scp = kb.ps("scp", [128, 512])
                    kb.I("dve", "memset", [], [thrc], thrc[:], -1.0e29)
                    cnts = {"si": 0, "ti": 0, "li": 0}

                    def indexer(i):
                        sc = score[i % 2]
                        dgt = dg[0]
                        nk = (i + 1) * 128
                        qs = slice(i * 128, (i + 1) * 128)
                        for h in range(8):
                            kb.I("pool", "tensor_scalar", [c["identb"], wq], [dgt], out=dgt[:, h, :], in0=c["identb"][:],
                                 scalar1=wq[:, i, h:h + 1], scalar2=None, op0=ALU.mult)
                        for t0 in range(0, nk, 512):
                            n = min(512, nk - t0)
                            last = (t0 + n == nk)
                            for hh in range(2):
                                for h in range(hh * 4, hh * 4 + 4):
                                    p = sps[cnts["si"] % 2]
                                    cnts["si"] += 1
                                    pr = slice((h % 2) * 64, (h % 2) * 64 + 64)
                                    kb.I("pe", "matmul", [qiT, kiT2], [p], p[:, 0:n], lhsT=qiT[pr, h // 2, qs], rhs=kiT2[pr, t0:t0 + n],
                                         start=True, stop=True)
                                    kb.I("act", "activation", [p], [rlb[h % 4]], out=rlb[h % 4][:, 0:n], in_=p[:, 0:n], func=AF.Relu)
                                for h in range(hh * 4, hh * 4 + 4):
                                    kb.I("pe", "matmul", [dgt, rlb[h % 4]], [scp], scp[:, 0:n], lhsT=dgt[:, h, :], rhs=rlb[h % 4][:, 0:n],
                                         start=(h == 0), stop=(h == 7 and not last), inc=(h % 4 == 3))
                            if last:
                                kb.I("pe", "matmul", [c["identb"], c["cmaskb"]], [scp], scp[:, n - 128:n], lhsT=c["identb"][:], rhs=c["cmaskb"][:],
                                     start=False, stop=True)
                            kb.copy("act", [scp], [sc], sc[:, t0:t0 + n], scp[:, 0:n])
                            yield

                    def topk(i):
                        sc, mk, m = score[i % 2], mask[i % 2], m8[i % 2]
                        nk = (i + 1) * 128
                        if i >= 2:
                            for it in range(32):
                                src = sc if it == 0 else work
                                kb.I("dve", "max", [src], [m], out=m[:], in_=src[:, 0:nk])
                                if it < 31:
                                    kb.I("dve", "match_replace", [m, src], [work], out=work[:, 0:nk], in_to_replace=m[:],
                                         in_values=src[:, 0:nk], imm_value=NEG)
                                if it % 8 == 7 and it < 31:
                                    yield
                            kb.I("dve", "tensor_scalar", [sc, m], [mk], out=mk[:, 0:nk], in0=sc[:, 0:nk], scalar1=m[:, 7:8],
                                 scalar2=None, op0=ALU.is_ge)
                        else:
                            kb.I("dve", "tensor_scalar", [sc, thrc], [mk], out=mk[:, 0:nk], in0=sc[:, 0:nk], scalar1=thrc[:, 0:1],
                                 scalar2=None, op0=ALU.is_ge)

                    def qk(i, h):
                        nk = (i + 1) * 128
                        qs = slice(i * 128, (i + 1) * 128)
                        lg = lgt[h % 2]
                        for t0 in range(0, nk, 512):
                            n = min(512, nk - t0)
                            p = lps[cnts["li"] % 2]
                            cnts["li"] += 1
                            kb.I("pe", "matmul", [qT, kT], [p], p[:, 0:n], lhsT=qT[:, h, qs], rhs=kT[:, t0:t0 + n], start=True, stop=True)
                            kb.I("act", "mul", [p], [lg], out=lg[:, t0:t0 + n], in_=p[:, 0:n], mul=128.0 ** -0.5)

                    def attention(i, gen, geni):
                        mk = mask[i % 2]
                        nk = (i + 1) * 128
                        qs = slice(i * 128, (i + 1) * 128)
                        qk(i, 0)
                        for h in range(4):
                            lg, e_, s_, pT_, on_ = lgt[h % 2], ee[h % 2], sm[h % 2], pT[h % 2], on[h % 2]
                            kb.I("dve", "tensor_reduce", [lg], [s_], out=s_[:, 0:1], in_=lg[:, 0:nk], axis=AX.X, op=ALU.max)
                            kb.I("dve", "tensor_scalar", [s_], [s_], out=s_[:, 1:2], in0=s_[:, 0:1], scalar1=-1.0, scalar2=None, op0=ALU.mult)
                            kb.I("act", "activation", [lg, s_], [e_], out=e_[:, 0:nk], in_=lg[:, 0:nk], func=AF.Exp, bias=s_[:, 1:2])
                            if h < 3:
                                qk(i, h + 1)
                            next(gen, None)
                            next(geni, None)
                            if h % 2:
                                next(geni, None)
                            kb.I("pool", "tensor_tensor", [e_, mk], [e_], out=e_[:, 0:nk], in0=e_[:, 0:nk], in1=mk[:, 0:nk], op=ALU.mult)
                            for j0 in range(0, i + 1, 8):
                                j1 = min(j0 + 8, i + 1)
                                tpp = tps[cnts["ti"] % 2]
                                cnts["ti"] += 1
                                for j in range(j0, j1):
                                    kb.I("pe", "transpose", [e_, c["identb"]], [tpp], out=tpp[:, j - j0, :], in_=e_[:, j * 128:(j + 1) * 128],
                                         identity=c["identb"][:], inc=(j == j1 - 1))
                                kb.copy("act", [tpp], [pT_], pT_[:, j0:j1, :], tpp[:, 0:j1 - j0, :])
                            for j in range(i + 1):
                                kb.I("pe", "matmul", [pT_, vtok], [ops], ops[:, 0:129], lhsT=pT_[:, j, :], rhs=vtok[:, j, 0:129], start=(j == 0), stop=(j == i),
                                     inc=(j == i))
                            kb.I("dve", "reciprocal", [ops], [s_], out=s_[:, 3:4], in_=ops[:, 128:129])
                            kb.I("dve", "tensor_scalar", [ops, s_], [on_], out=on_[:], in0=ops[:, 0:128], scalar1=s_[:, 3:4], scalar2=None, op0=ALU.mult)
                            otb = tps[cnts["ti"] % 2]
                            cnts["ti"] += 1
                            kb.I("pe", "transpose", [on_, c["identb"]], [otb], out=otb[:, 0, :], in_=on_[:], identity=c["identb"][:])
                            kb.copy("act", [otb], [mixT], mixT[:, h, qs], otb[:, 0, :])

                    for _ in indexer(0):
                        pass
                    for _ in indexer(1):
                        pass
                    for _ in topk(0):
                        pass
                    for i in range(NT):
                        gen = topk(i + 1) if i + 1 < NT else iter(())
                        geni = indexer(i + 2) if i + 2 < NT else iter(())
                        attention(i, gen, geni)
                        for _ in geni:
                            pass
                        for _ in gen:
                            pass
                with kb.scope():
                    woutb = kb.sb("woutb", [128, 8, D], BF16)
                    stg = [kb.sb("ostg%d" % i, [128, D]) for i in range(2)]
                    for k in range(8):
                        kb.dma("sp", stg[k % 2][:], wout_d[k * 128:(k + 1) * 128, :])
                        kb.copy("act" if k % 2 else "pool", [stg[k % 2]], [woutb], woutb[:, k, :], stg[k % 2][:])
                    hts = [kb.sb("oht%d" % i, [128, D]) for i in range(2)]
                    acc = [kb.sb("oacc%d" % i, [128, D]) for i in range(2)]
                    outt = [kb.sb("oout%d" % i, [128, D]) for i in range(2)]
                    st6 = kb.sb("st6", [128, 2, 6])
                    mv = kb.sb("mv", [128, 4])
                    ops2 = [kb.ps("opp%d" % i, [128, 512]) for i in range(4)]
                    for t in range(NT):
                        ht = hts[t % 2]
                        a = acc[t % 2]
                        kb.dma("sp", ht[:], h_d[r0 + t * 128: r0 + (t + 1) * 128, :])
                        for hf in range(2):
                            p = ops2[(2 * t + hf) % 4]
                            for k in range(8):
                                kb.I("pe", "matmul", [mixT, woutb], [p], p[:], lhsT=mixT[:, k, t * 128:(t + 1) * 128],
                                     rhs=woutb[:, k, hf * 512:(hf + 1) * 512], start=(k == 0), stop=(k == 7), inc=(k == 7))
                            kb.I("dve", "scalar_tensor_tensor", [ht, p], [a], out=a[:, hf * 512:(hf + 1) * 512], in0=ht[:, hf * 512:(hf + 1) * 512],
                                 scalar=ALPHA, in1=p[:], op0=ALU.mult, op1=ALU.add)
                        layer_norm_tile(kb, a, g_bc, b_bc, outt[t % 2], st6, mv)
                        kb.dma("sp", out_d[r0 + t * 128: r0 + (t + 1) * 128, :], outt[t % 2][:])


def prep_even(inp, i):
    f = lambda a: np.ascontiguousarray(a, dtype=np.float32)
    pc = lambda v, n: f(v.reshape(n, 128).T)
    vec = np.zeros((128, 40), np.float32)
    cwt = inp["even_conv_w"][i]
    for j in range(4):
        vec[:, j * 4:(j + 1) * 4] = pc(cwt[j], 4)
    vec[:, 16:20] = pc(inp["even_conv_b"][i], 4)
    vec[:, 20:24] = pc(inp["even_rg_ba"][i], 4)
    vec[:, 24:28] = pc(inp["even_rg_bx"][i], 4)
    vec[:, 28:32] = pc(inp["even_rg_lambda"][i], 4)
    vec[:, 32:34] = pc(inp["even_kv_norm"][i], 2)
    rgw = np.zeros((128, 8, 128), np.float32)
    for g, nm in enumerate(("even_rg_wa", "even_rg_wx")):
        w = inp[nm][i]
        for n in range(8):
            o = (n % 2) * 64
            rgw[o:o + 64, g * 4 + n // 2, o:o + 64] = w[n]
    return {
        "ev_win": f(inp["even_w_in"][i]), "ev_wout": f(inp["even_w_out"][i]),
        "ev_wuk": f(inp["even_w_uk"][i]), "ev_wuv": f(inp["even_w_uv"][i]),
        "ev_rgw": f(rgw.reshape(128, 1024)), "ev_vec": vec,
        "ev_lng": f(inp["ln_g"][2 * i, 0]), "ev_lnb": f(inp["ln_b"][2 * i, 0]),
    }


def build_gdn(kb, c, h_d, gw_d, wab_d, gconv_d, alog_d, dtb_d, onorm_d, wout_d, lng_d, lnb_d, out_d, o_d, nseq=SPC, stop=0):
    with kb.scope():
        g_bc = kb.sb("g_bc", [128, D])
        b_bc = kb.sb("b_bc", [128, D])
        kb.dma("sp", g_bc[:], lng_d.partition_broadcast(128))
        kb.dma("sp", b_bc[:], lnb_d.partition_broadcast(128))
        wabb = kb.sb("wabb", [128, 8, 32], BF16)
        gconv = kb.sb("gconv", [128, 8, 4, 4])
        nA = kb.sb("nA", [128, 16])
        dtb = kb.sb("dtb", [128, 16])
        onorm = kb.sb("onorm", [128, 1])
        eps6 = kb.sb("eps6", [128, 1])
        kb.I("pool", "memset", [], [eps6], eps6[:], 1e-6)
        kb.dma("sp", gconv[:], gconv_d)
        kb.dma("sp", nA[:], alog_d.partition_broadcast(128))
        kb.dma("sp", dtb[:], dtb_d.partition_broadcast(128))
        kb.dma("sp", onorm[:], onorm_d.rearrange("(p o) -> p o", o=1))
        kb.I("act", "activation", [nA], [nA], out=nA[:], in_=nA[:], func=AF.Exp)
        kb.I("dve", "tensor_scalar", [nA], [nA], out=nA[:], in0=nA[:], scalar1=-1.0, scalar2=None, op0=ALU.mult)
        ut = kb.sb("ut", [128, 128])
        slt = kb.sb("slt", [128, 128])
        lmask = kb.sb("lmask", [128, 128])
        smask = kb.sb("smask", [128, 128])
        kb.I("dve", "tensor_tensor", [c["sut"], c["ident"]], [ut], out=ut[:], in0=c["sut"][:], in1=c["ident"][:], op=ALU.add)
        kb.I("dve", "tensor_scalar", [ut], [slt], out=slt[:], in0=ut[:], scalar1=-1.0, scalar2=1.0, op0=ALU.mult, op1=ALU.add)
        kb.I("dve", "tensor_copy", [slt], [smask], out=smask[:], in_=slt[:])
        kb.I("dve", "tensor_tensor", [slt, c["ident"]], [lmask], out=lmask[:], in0=slt[:], in1=c["ident"][:], op=ALU.add)
        with kb.scope():
            st = kb.sb("abstg", [128, 8, 32])
            kb.dma("sp", st[:], wab_d.rearrange("(k p) e -> p k e", p=128))
            kb.I("dve", "tensor_copy", [st], [wabb], out=wabb[:], in_=st[:])

        for s in range(nseq):
            r0 = s * TP
            with kb.scope():
                hT = kb.sb("hT", [128, 8, TP], BF16)
                with kb.scope():
                    hts = [kb.sb("ght%d" % i, [128, D]) for i in range(2)]
                    tp = [kb.ps("gtp%d" % i, [128, 8, 128]) for i in range(2)]
                    for t in range(NT):
                        ht = hts[t % 2]
                        kb.dma("sp", ht[:], h_d[r0 + t * 128: r0 + (t + 1) * 128, :])
                        tpp = tp[t % 2]
                        for k in range(8):
                            kb.I("pe", "transpose", [ht, c["ident"]], [tpp], out=tpp[:, k, :],
                                 in_=ht[:, k * 128:(k + 1) * 128], identity=c["ident"][:], inc=(k == 7))
                        kb.copy("act" if t % 2 else "dve", [tpp], [hT], hT[:, :, t * 128:(t + 1) * 128], tpp[:])
                if stop == 1:
                    return
                S3 = lambda nm: kb.sb(nm, [128, NT, 16])
                gg, beta, gc, egc, ekd, egl, bege = S3("gg"), S3("beta"), S3("gc"), S3("egc"), S3("ekd"), S3("egl"), S3("bege")
                with kb.scope():
                    abp = [kb.ps("abp%d" % i, [128, 32]) for i in range(2)]
                    ab = kb.sb("ab", [128, NT, 32])
                    tmp = S3("tmpa")
                    for t in range(NT):
                        p = abp[t % 2]
                        for k in range(8):
                            kb.I("pe", "matmul", [hT, wabb], [p], p[:], lhsT=hT[:, k, t * 128:(t + 1) * 128], rhs=wabb[:, k, :],
                                 start=(k == 0), stop=(k == 7), inc=(k == 7))
                        kb.copy("act", [p], [ab], ab[:, t, :], p[:])
                    bcT = lambda a: a[:].unsqueeze(1).to_broadcast([128, NT, 16])
                    kb.I("dve", "tensor_tensor", [ab, dtb], [gg], out=gg[:], in0=ab[:, :, 0:16], in1=bcT(dtb), op=ALU.add)
                    kb.I("dve", "tensor_scalar", [gg], [tmp], out=tmp[:], in0=gg[:], scalar1=-1.0, scalar2=None, op0=ALU.mult)
                    kb.I("dve", "tensor_tensor", [gg, tmp], [tmp], out=tmp[:], in0=gg[:], in1=tmp[:], op=ALU.max)
                    kb.I("act", "activation", [tmp], [tmp], out=tmp[:], in_=tmp[:], func=AF.Exp, scale=-1.0)
                    kb.I("act", "activation", [tmp], [tmp], out=tmp[:], in_=tmp[:], func=AF.Ln, bias=1.0)
                    kb.I("dve", "tensor_scalar", [gg], [gg], out=gg[:], in0=gg[:], scalar1=0.0, scalar2=None, op0=ALU.max)
                    kb.I("dve", "tensor_tensor", [gg, tmp], [gg], out=gg[:], in0=gg[:], in1=tmp[:], op=ALU.add)
                    kb.I("dve", "tensor_tensor", [gg, nA], [gg], out=gg[:], in0=gg[:], in1=bcT(nA), op=ALU.mult)
                    kb.I("act", "activation", [ab], [beta], out=beta[:], in_=ab[:, :, 16:32], func=AF.Sigmoid)
                    for t in range(NT):
                        p = abp[t % 2]
                        kb.I("pe", "matmul", [ut, gg], [p], p[:, 0:16], lhsT=ut[:], rhs=gg[:, t, :], start=True, stop=True, inc=False)
                        kb.I("pe", "matmul", [c["ones"], gg], [p], p[:, 16:32], lhsT=c["ones"][:], rhs=gg[:, t, :], start=True, stop=True)
                        kb.copy("act", [p], [gc], gc[:, t, :], p[:, 0:16])
                        kb.copy("dve", [p], [egl], egl[:, t, :], p[:, 16:32])
                    kb.I("dve", "tensor_tensor", [egl, gc], [ekd], out=ekd[:], in0=egl[:], in1=gc[:], op=ALU.subtract)
                    kb.I("act", "activation", [ekd], [ekd], out=ekd[:], in_=ekd[:], func=AF.Exp)
                    kb.I("act", "activation", [egl], [egl], out=egl[:], in_=egl[:], func=AF.Exp)
                    kb.I("act", "activation", [gc], [egc], out=egc[:], in_=gc[:], func=AF.Exp)
                    kb.I("dve", "tensor_tensor", [beta, egc], [bege], out=bege[:], in0=beta[:], in1=egc[:], op=ALU.mult)

                if stop == 2:
                    return
                wsls = [kb.sb("wsl%d" % i, [128, 8, 768], BF16) for i in range(2)]
                gstg = [kb.sb("gstg%d" % i, [128, 768]) for i in range(2)]

                def load_wsl(kh_):
                    w_ = wsls[kh_ % 2]
                    for k in range(8):
                        kb.dma("sp", gstg[k % 2][:], gw_d[kh_, k * 128:(k + 1) * 128, :])
                        kb.copy("act" if k % 2 else "pool", [gstg[k % 2]], [w_], w_[:, k, :], gstg[k % 2][:])
                load_wsl(0)
                for kh in range(8):
                    with kb.scope():
                        wsl = wsls[kh % 2]
                        qkT = kb.sb("qkT", [128, 2, TP], BF16)
                        zsT = kb.sb("zsT", [128, 2, TP], BF16)
                        ktok = kb.sb("ktok", [128, NT, 128], BF16)
                        vtok = kb.sb("vtok", [128, NT, 256], BF16)
                        oTk = kb.sb("oTk", [128, 2, TP], BF16)
                        with kb.scope():
                            vT = kb.sb("vT", [128, 2, TP], BF16)
                            raws = [kb.sb("raw%d" % i, [128, TP]) for i in range(2)]
                            cvs = [kb.sb("cv%d" % i, [128, TP]) for i in range(2)]
                            sqs = [kb.sb("sq%d" % i, [128, 512], BF16) for i in range(3)]
                            rstds = [kb.sb("rstd%d" % i, [128, 512]) for i in range(3)]
                            ps2 = [kb.ps("gpp%d" % i, [128, 512]) for i in range(2)]
                            ps3 = [kb.ps("gpq%d" % i, [128, 512]) for i in range(2)]
                            tps = [kb.ps("gtq%d" % i, [128, 8, 128], BF16) for i in range(2)]
                            cnt = [0]
                            ti = 0
                            def do_proj(fi):
                                raw = raws[fi % 2]
                                def ev(n_, p, t0, n, fi=fi, raw=raw):
                                    if fi < 4:
                                        kb.copy("act", [p], [raw], raw[:, t0:t0 + n], p)
                                    else:
                                        kb.I("act", "activation", [p], [zsT], out=zsT[:, fi - 4, t0:t0 + n], in_=p, func=AF.Silu)
                                proj_fm(kb, ps2, cnt, wsl, fi * 128, hT, None, ev)

                            def do_post(fi):
                                nonlocal ti
                                raw, cv = raws[fi % 2], cvs[fi % 2]
                                cwc = lambda j: gconv[:, kh, fi, j:j + 1]
                                kb.I("dve", "tensor_scalar", [raw, gconv], [cv], out=cv[:], in0=raw[:], scalar1=cwc(3), scalar2=None, op0=ALU.mult)
                                for d in (1, 2, 3):
                                    kb.I("dve", "scalar_tensor_tensor", [raw, gconv, cv], [cv], out=cv[:, d:TP], in0=raw[:, 0:TP - d],
                                         scalar=cwc(3 - d), in1=cv[:, d:TP], op0=ALU.mult, op1=ALU.add)
                                if fi >= 2:
                                    kb.I("act", "activation", [cv], [vT], out=vT[:, fi - 2, :], in_=cv[:], func=AF.Silu)
                                    for j0 in range(0, NT, 8):
                                        j1 = min(j0 + 8, NT)
                                        tpp = tps[ti % 2]
                                        ti += 1
                                        for j in range(j0, j1):
                                            kb.I("pe", "transpose", [vT, c["identb"]], [tpp], out=tpp[:, j - j0, :],
                                                 in_=vT[:, fi - 2, j * 128:(j + 1) * 128], identity=c["identb"][:], inc=(j == j1 - 1))
                                        kb.copy("dve", [tpp], [vtok], vtok[:, j0:j1, (fi - 2) * 128:(fi - 1) * 128], tpp[:, 0:j1 - j0, :])
                                    return
                                kb.I("act", "activation", [cv], [cv], out=cv[:], in_=cv[:], func=AF.Silu)
                                for ci_, (t0, n) in enumerate(CHUNKS):
                                    sq, rstd = sqs[ci_ % 3], rstds[ci_ % 3]
                                    kb.I("pool", "tensor_tensor", [cv], [sq], out=sq[:, 0:n], in0=cv[:, t0:t0 + n], in1=cv[:, t0:t0 + n], op=ALU.mult)
                                    p = ps3[ci_ % 2]
                                    kb.I("pe", "matmul", [c["onesb"], sq], [p], p[:, 0:n], lhsT=c["onesb"][:], rhs=sq[:, 0:n], start=True, stop=True)
                                    kb.I("act", "activation", [p], [rstd], out=rstd[:, 0:n], in_=p[:, 0:n], func=AF.Ln, bias=eps6[:, 0:1])
                                    kb.I("act", "activation", [rstd], [rstd], out=rstd[:, 0:n], in_=rstd[:, 0:n], func=AF.Exp, scale=-0.5)
                                    kb.I("dve", "scalar_tensor_tensor", [cv, rstd], [qkT], out=qkT[:, fi, t0:t0 + n], in0=cv[:, t0:t0 + n],
                                         scalar=(128.0 ** -0.5 if fi == 0 else 1.0), in1=rstd[:, 0:n], op0=ALU.mult, op1=ALU.mult)
                                if fi == 1:
                                    for j0 in range(0, NT, 8):
                                        j1 = min(j0 + 8, NT)
                                        tpp = tps[ti % 2]
                                        ti += 1
                                        for j in range(j0, j1):
                                            kb.I("pe", "transpose", [qkT, c["identb"]], [tpp], out=tpp[:, j - j0, :],
                                                 in_=qkT[:, 1, j * 128:(j + 1) * 128], identity=c["identb"][:], inc=(j == j1 - 1))
                                        kb.copy("dve", [tpp], [ktok], ktok[:, j0:j1, :], tpp[:, 0:j1 - j0, :])

                            do_proj(0)
                            for fi in range(6):
                                if fi + 1 < 6:
                                    do_proj(fi + 1)
                                if fi < 4:
                                    do_post(fi)
                        if stop == 3:
                            return
                        if kh + 1 < 8:
                            load_wsl(kh + 1)
                        with kb.scope():
                            B = lambda nm: kb.sb(nm, [128, 128], BF16)
                            Fp = lambda nm: kb.sb(nm, [128, 128])
                            otok = [kb.sb("otok%d" % j, [128, NT, 128]) for j in range(2)]
                            us_all = [kb.sb("us_all%d" % j, [128, NT, 128]) for j in range(2)]
                            wT_all = [kb.sb("wT_all%d" % j, [128, NT, 128], BF16) for j in range(2)]
                            aT_all = [kb.sb("aT_all%d" % j, [128, NT, 128], BF16) for j in range(2)]
                            kd_all = [kb.sb("kd_all%d" % j, [128, NT, 128], BF16) for j in range(2)]
                            pGA = kb.ps("pGA", [128, 2, 128])
                            X = [kb.ps("pX%d" % i, [128, 4, 128]) for i in range(4)]
                            TPb = kb.ps("TPb", [128, 8, 128], BF16)
                            CH = []
                            for ci in range(4):
                                CH.append(dict(
                                    Rm=Fp("Rm%d" % ci), Dm=Fp("Dm%d" % ci), attn=B("attn%d" % ci),
                                    MNA=[kb.sb("MNA%d_0" % ci, [128, 3, 128], BF16), kb.sb("MNA%d_1" % ci, [128, 3, 128], BF16)],
                                    vb=B("vb%d" % ci), kbg=B("kbg%d" % ci), X=X[ci]))
                            GsAs = [(Fp("Gs0"), Fp("As0")), (Fp("Gs1"), Fp("As1"))]
                            for j in range(2):
                                hd = 2 * kh + j
                                kb.I("pool", "tensor_tensor", [ktok, ekd], [kd_all[j]], out=kd_all[j][:], in0=ktok[:],
                                     in1=ekd[:, :, hd:hd + 1].to_broadcast([128, NT, 128]), op=ALU.mult)
                            Ssb = [Fp("S0"), Fp("S1")]
                            Sbf = [B("Sb0"), B("Sb1")]
                            vnew = [B("vnew0"), B("vnew1")]
                            o1 = [Fp("o1_0"), Fp("o1_1")]
                            P2 = [kb.ps("pP2_%d" % j, [128, 4, 128]) for j in range(2)]
                            for j in range(2):
                                kb.I("pool", "memset", [], [Ssb[j]], Ssb[j][:], 0.0)
                                kb.I("pool", "memset", [], [Sbf[j]], Sbf[j][:], 0.0)

                            def phase2(tlist):
                                for t in tlist:
                                    ts_ = slice(t * 128, (t + 1) * 128)
                                    for j in range(2):
                                        W = P2[j]
                                        kb.I("pe", "matmul", [wT_all[j], Sbf[j]], [W], W[:, 0, :], lhsT=wT_all[j][:, t, :], rhs=Sbf[j][:], start=True, stop=True, inc=False)
                                        kb.I("pe", "matmul", [qkT, Sbf[j]], [W], W[:, 1, :], lhsT=qkT[:, 0, ts_], rhs=Sbf[j][:], start=True, stop=True)
                                    yield
                                    for j in range(2):
                                        W = P2[j]
                                        hd = 2 * kh + j
                                        kb.I("dve", "tensor_tensor", [us_all[j], W], [vnew[j]], out=vnew[j][:], in0=us_all[j][:, t, :], in1=W[:, 0, :], op=ALU.subtract)
                                        kb.I("dve", "tensor_scalar", [W, egc], [o1[j]], out=o1[j][:], in0=W[:, 1, :], scalar1=egc[:, t, hd:hd + 1], scalar2=None, op0=ALU.mult)
                                    yield
                                    for j in range(2):
                                        V_ = P2[j]
                                        kb.I("pe", "matmul", [aT_all[j], vnew[j]], [V_], V_[:, 2, :], lhsT=aT_all[j][:, t, :], rhs=vnew[j][:], start=True, stop=True, inc=False)
                                        kb.I("pe", "matmul", [kd_all[j], vnew[j]], [V_], V_[:, 3, :], lhsT=kd_all[j][:, t, :], rhs=vnew[j][:], start=True, stop=True)
                                    yield
                                    for j in range(2):
                                        V_ = P2[j]
                                        hd = 2 * kh + j
                                        kb.I("dve", "scalar_tensor_tensor", [Ssb[j], egl, V_], [Sbf[j]], out=Sbf[j][:], in0=Ssb[j][:], scalar=egl[:, t, hd:hd + 1],
                                             in1=V_[:, 3, :], op0=ALU.mult, op1=ALU.add)
                                        kb.I("dve", "scalar_tensor_tensor", [Ssb[j], egl, V_], [Ssb[j]], out=Ssb[j][:], in0=Ssb[j][:], scalar=egl[:, t, hd:hd + 1],
                                             in1=V_[:, 3, :], op0=ALU.mult, op1=ALU.add)
                                        kb.I("dve", "tensor_tensor", [o1[j], V_], [otok[j]], out=otok[j][:, t, :], in0=o1[j][:], in1=V_[:, 2, :], op=ALU.add)
                                    yield
                            gen2 = iter(())
                            for t0 in range(0, NT, 2):
                                tl = [t for t in (t0, t0 + 1) if t < NT]
                                chains = []
                                for ti_, t in enumerate(tl):
                                    ts_ = slice(t * 128, (t + 1) * 128)
                                    Gs, As = GsAs[ti_]
                                    kb.I("pe", "matmul", [qkT], [pGA], pGA[:, 0, :], lhsT=qkT[:, 1, ts_], rhs=qkT[:, 1, ts_], start=True, stop=True, inc=False)
                                    kb.I("pe", "matmul", [qkT], [pGA], pGA[:, 1, :], lhsT=qkT[:, 0, ts_], rhs=qkT[:, 1, ts_], start=True, stop=True)
                                    kb.I("dve", "tensor_tensor", [pGA, smask], [Gs], out=Gs[:], in0=pGA[:, 0, :], in1=smask[:], op=ALU.mult)
                                    kb.I("dve", "tensor_tensor", [pGA, lmask], [As], out=As[:], in0=pGA[:, 1, :], in1=lmask[:], op=ALU.mult)
                                    for j in range(2):
                                        chn = dict(CH[ti_ * 2 + j])
                                        chn.update(t=t, j=j, hd=2 * kh + j, Gs=Gs, As=As, slot=ti_ * 2 + j)
                                        chains.append(chn)
                                col = lambda a, q: a[:, q["t"], q["hd"]:q["hd"] + 1]
                                for q in chains:
                                    kb.I("act", "mul", [ut, gg], [q["Rm"]], out=q["Rm"][:], in_=ut[:], mul=col(gg, q))
                                for q in chains:
                                    kb.I("pe", "matmul", [q["Rm"], slt], [q["X"]], q["X"][:, 0, :], lhsT=q["Rm"][:], rhs=slt[:], start=True, stop=True)
                                for q in chains:
                                    kb.I("act", "activation", [q["X"]], [q["Dm"]], out=q["Dm"][:], in_=q["X"][:, 0, :], func=AF.Exp)
                                for q in chains:
                                    kb.I("dve", "scalar_tensor_tensor", [q["Gs"], beta, q["Dm"]], [q["MNA"][0]], out=q["MNA"][0][:, 0, :], in0=q["Gs"][:],
                                         scalar=col(beta, q), in1=q["Dm"][:], op0=ALU.mult, op1=ALU.mult)
                                    kb.I("pool", "tensor_tensor", [q["As"], q["Dm"]], [q["attn"]], out=q["attn"][:], in0=q["As"][:], in1=q["Dm"][:], op=ALU.mult)
                                    kb.copy("act", [c["identb"]], [q["MNA"][0]], q["MNA"][0][:, 2, :], c["identb"][:])
                                for q in chains:
                                    sl = q["slot"]
                                    kb.I("pe", "transpose", [q["MNA"][0], c["identb"]], [TPb], out=TPb[:, 2 * sl, :], in_=q["MNA"][0][:, 0, :], identity=c["identb"][:], inc=False)
                                    kb.I("pe", "transpose", [q["attn"], c["identb"]], [TPb], out=TPb[:, 2 * sl + 1, :], in_=q["attn"][:], identity=c["identb"][:])
                                for q in chains:
                                    sl = q["slot"]
                                    kb.copy("act", [TPb], [q["MNA"][0]], q["MNA"][0][:, 1, :], TPb[:, 2 * sl, :])
                                    kb.copy("dve", [TPb], [aT_all[q["j"]]], aT_all[q["j"]][:, q["t"], :], TPb[:, 2 * sl + 1, :])
                                cur = 0
                                for st_ in range(7):
                                    nxt = 1 - cur
                                    next(gen2, None)
                                    for q in chains:
                                        Xq, T_ = q["X"], q["MNA"][cur]
                                        if st_ < 6:
                                            kb.I("pe", "matmul", [T_], [Xq], Xq[:, 0, :], lhsT=T_[:, 1, :], rhs=T_[:, 0, :], start=True, stop=True, inc=False)
                                            kb.I("pe", "matmul", [T_], [Xq], Xq[:, 1:3, :], lhsT=T_[:, 0, :], rhs=T_[:, 1:3, :], start=True, stop=True)
                                        else:
                                            kb.I("pe", "matmul", [T_], [Xq], Xq[:, 2, :], lhsT=T_[:, 0, :], rhs=T_[:, 2, :], start=True, stop=True)
                                    next(gen2, None)
                                    for q in chains:
                                        Xq, T_, Tn = q["X"], q["MNA"][cur], q["MNA"][nxt]
                                        if st_ < 6:
                                            kb.copy("act", [Xq], [Tn], Tn[:, 0:2, :], Xq[:, 0:2, :])
                                        kb.I("dve", "tensor_tensor", [T_, Xq], [Tn], out=Tn[:, 2, :], in0=T_[:, 2, :], in1=Xq[:, 2, :],
                                             op=(ALU.subtract if st_ == 0 else ALU.add))
                                    cur = nxt
                                for q in chains:
                                    j, t = q["j"], q["t"]
                                    kb.I("act", "mul", [vtok, beta], [q["vb"]], out=q["vb"][:], in_=vtok[:, t, j * 128:(j + 1) * 128], mul=col(beta, q))
                                    kb.I("pool", "tensor_scalar", [ktok, bege], [q["kbg"]], out=q["kbg"][:], in0=ktok[:, t, :], scalar1=col(bege, q),
                                         scalar2=None, op0=ALU.mult)
                                for q in chains:
                                    TT = q["MNA"][cur][:, 2, :]
                                    kb.I("pe", "matmul", [q["MNA"][cur], q["vb"]], [q["X"]], q["X"][:, 0, :], lhsT=TT, rhs=q["vb"][:], start=True, stop=True, inc=False)
                                    kb.I("pe", "matmul", [q["kbg"], q["MNA"][cur]], [q["X"]], q["X"][:, 1, :], lhsT=q["kbg"][:], rhs=TT, start=True, stop=True)
                                for q in chains:
                                    j, t = q["j"], q["t"]
                                    kb.copy("act", [q["X"]], [us_all[j]], us_all[j][:, t, :], q["X"][:, 0, :])
                                    kb.copy("dve", [q["X"]], [wT_all[j]], wT_all[j][:, t, :], q["X"][:, 1, :])
                                for _ in gen2:
                                    pass
                                gen2 = phase2(tl)
                            for _ in gen2:
                                pass
                            if stop == 4:
                                return
                            with kb.scope():
                                sqo = us_all[0]
                                ssq = kb.sb("ssq", [128, NT])
                                onb = wT_all[0]
                                tpo = [TPb, TPb]
                                ti = 0
                                for j in range(2):
                                    kb.I("pool", "tensor_tensor", [otok[j]], [sqo], out=sqo[:], in0=otok[j][:], in1=otok[j][:], op=ALU.mult)
                                    kb.I("dve", "tensor_reduce", [sqo], [ssq], out=ssq[:], in_=sqo[:], axis=AX.X, op=ALU.add)
                                    kb.I("dve", "tensor_scalar", [ssq], [ssq], out=ssq[:], in0=ssq[:], scalar1=1.0 / 128, scalar2=1e-6, op0=ALU.mult, op1=ALU.add)
                                    kb.I("act", "sqrt", [ssq], [ssq], out=ssq[:], in_=ssq[:])
                                    kb.I("dve", "reciprocal", [ssq], [ssq], out=ssq[:], in_=ssq[:])
                                    kb.I("dve", "tensor_tensor", [otok[j], ssq], [onb], out=onb[:], in0=otok[j][:],
                                         in1=ssq[:].unsqueeze(2).to_broadcast([128, NT, 128]), op=ALU.mult)
                                    for j0 in range(0, NT, 8):
                                        j1 = min(j0 + 8, NT)
                                        tpp = tpo[ti % 2]
                                        ti += 1
                                        for jj in range(j0, j1):
                                            kb.I("pe", "transpose", [onb, c["identb"]], [tpp], out=tpp[:, jj - j0, :], in_=onb[:, jj, :], identity=c["identb"][:], inc=(jj == j1 - 1))
                                        kb.I("dve", "scalar_tensor_tensor", [tpp, onorm, zsT], [oTk], out=oTk[:, j, j0 * 128:j1 * 128],
                                             in0=tpp[:, 0:j1 - j0, :].rearrange("p a b -> p (a b)"), scalar=onorm[:, 0:1], in1=zsT[:, j, j0 * 128:j1 * 128],
                                             op0=ALU.mult, op1=ALU.mult)
                                for j in range(2):
                                    kb.dma("sp", o_d[s, 2 * kh + j, :, :], oTk[:, j, :])
                if stop == 5:
                    return
                with kb.scope():
                    woutb = kb.sb("gwoutb", [128, 16, D], BF16)
                    stg = [kb.sb("gostg%d" % i, [128, D]) for i in range(2)]
                    for k in range(16):
                        kb.dma("sp", stg[k % 2][:], wout_d[k * 128:(k + 1) * 128, :])
                        kb.copy("act" if k % 2 else "pool", [stg[k % 2]], [woutb], woutb[:, k, :], stg[k % 2][:])
                    oTt = [kb.sb("oTt%d" % i, [128, 16, 128], BF16) for i in range(2)]
                    hts = [kb.sb("goht%d" % i, [128, D]) for i in range(2)]
                    acc = [kb.sb("goacc%d" % i, [128, D]) for i in range(2)]
                    outt = [kb.sb("goout%d" % i, [128, D]) for i in range(2)]
                    st6 = kb.sb("st6", [128, 2, 6])
                    mv = kb.sb("mv", [128, 4])
                    ops2 = [kb.ps("gopp%d" % i, [128, 512]) for i in range(4)]
                    for t in range(NT):
                        ht = hts[t % 2]
                        a = acc[t % 2]
                        ot = oTt[t % 2]
                        kb.dma("sp", ht[:], h_d[r0 + t * 128: r0 + (t + 1) * 128, :])
                        for hq in range(4):
                            kb.dma("act", ot[:, hq * 4:(hq + 1) * 4, :], o_d[s, hq * 4:(hq + 1) * 4, :, t * 128:(t + 1) * 128].rearrange("h p t -> p h t"),
                                   writes=[ot], group="oTt%d" % (t % 2))
                        for hf in range(2):
                            p = ops2[(2 * t + hf) % 4]
                            for k in range(16):
                                kb.I("pe", "matmul", [ot, woutb], [p], p[:], lhsT=ot[:, k, :], rhs=woutb[:, k, hf * 512:(hf + 1) * 512],
                                     start=(k == 0), stop=(k == 15), inc=(k == 15))
                            kb.I("dve", "scalar_tensor_tensor", [ht, p], [a], out=a[:, hf * 512:(hf + 1) * 512], in0=ht[:, hf * 512:(hf + 1) * 512],
                                 scalar=ALPHA, in1=p[:], op0=ALU.mult, op1=ALU.add)
                        layer_norm_tile(kb, a, g_bc, b_bc, outt[t % 2], st6, mv)
                        kb.dma("sp", out_d[r0 + t * 128: r0 + (t + 1) * 128, :], outt[t % 2][:])


def prep_gdn(inp, i):
    f = lambda a: np.ascontiguousarray(a, dtype=np.float32)
    w = inp["odd_w_in"][i]
    cwt = inp["odd_conv_w"][i]
    gw = np.zeros((8, 1024, 768), np.float32)
    gconv = np.zeros((128, 8, 4, 4), np.float32)
    for kh in range(8):
        cols = [np.arange(kh * 128, kh * 128 + 128), 1024 + np.arange(kh * 128, kh * 128 + 128),
                2048 + np.arange(2 * kh * 128, 2 * kh * 128 + 256)]
        zc = 4096 + np.arange(2 * kh * 128, 2 * kh * 128 + 256)
        gw[kh] = w[:, np.concatenate(cols + [zc])]
        cc_ = np.concatenate(cols)
        for fi in range(4):
            gconv[:, kh, fi, :] = cwt[:, cc_[fi * 128:(fi + 1) * 128]].T
    return {
        "gd_w": gw, "gd_wab": f(w[:, 6144:6176]), "gd_conv": gconv.reshape(128, 128).reshape(128, 8, 4, 4),
        "gd_alog": f(inp["odd_a_log"][i]), "gd_dtb": f(inp["odd_dt_bias"][i]), "gd_onorm": f(inp["odd_o_norm"][i]),
        "gd_wout": f(inp["odd_w_out"][i]), "gd_lng": f(inp["ln_g"][2 * i + 1, 0]), "gd_lnb": f(inp["ln_b"][2 * i + 1, 0]),
    }


def perm_expert(w, nk):
    E, R, F_ = w.shape
    return np.ascontiguousarray(w.reshape(E, nk, 128, F_).transpose(0, 2, 1, 3).reshape(E * 128, nk * F_), dtype=np.float32)


def prep_moe(inp, layer, pfx):
    f = lambda a: np.ascontiguousarray(a, dtype=np.float32)
    return {
        pfx + "wr": f(np.concatenate([inp["moe_group_w"][layer], inp["moe_expert_w"][layer]], axis=1)),
        pfx + "br": f(np.concatenate([inp["moe_group_b"][layer], inp["moe_expert_b"][layer]], axis=0)),
        pfx + "wg": perm_expert(inp["moe_w_gate"][layer], 8), pfx + "wu": perm_expert(inp["moe_w_up"][layer], 8),
        pfx + "wd": perm_expert(inp["moe_w_down"][layer], 4),
        pfx + "lng": f(inp["ln_g"][layer, 1]), pfx + "lnb": f(inp["ln_b"][layer, 1]),
    }


def build_program(shapes):
    nc = bass.Bass("TRN2", target_bir_lowering=False)
    kb = KB(nc)
    dd = {}
    for n, (shp, dt) in shapes.items():
        dd[n] = nc.dram_tensor(n, list(shp), dt, kind="ExternalInput").ap()
    out_d = nc.dram_tensor("out", [NTOK, D], F32, kind="ExternalOutput").ap()
    h1_d = kb.dram("h1", [NTOK, D])
    h2_d = kb.dram("h2", [NTOK, D])
    h3_d = kb.dram("h3", [NTOK, D])
    xs_d = kb.dram("xs", [NSLOT, D], BF16)
    ys_d = kb.dram("ys", [NSLOT, D], F32)
    o_d = kb.dram("o_scr", [SPC, 16, 128, TP], BF16)
    c = load_consts(kb, {k[2:]: v for k, v in dd.items() if k.startswith("c_")})
    build_even(kb, c, dd["h0"], dd["ev_win"], dd["ev_wout"], dd["ev_wuk"], dd["ev_wuv"], dd["ev_rgw"], dd["ev_vec"],
               dd["ev_lng"], dd["ev_lnb"], h1_d)
    build_moe(kb, c, h1_d, dd["m0_wr"], dd["m0_br"], dd["m0_wg"], dd["m0_wu"], dd["m0_wd"], dd["m0_lng"], dd["m0_lnb"],
              h2_d, xs_d, ys_d)
    build_gdn(kb, c, h2_d, dd["gd_w"], dd["gd_wab"], dd["gd_conv"], dd["gd_alog"], dd["gd_dtb"], dd["gd_onorm"],
              dd["gd_wout"], dd["gd_lng"], dd["gd_lnb"], h3_d, o_d)
    build_moe(kb, c, h3_d, dd["m1_wr"], dd["m1_br"], dd["m1_wg"], dd["m1_wu"], dd["m1_wd"], dd["m1_lng"], dd["m1_lnb"],
              out_d, xs_d, ys_d)
    kb.finish()
    return nc


def kernel(**inputs):
    import ml_dtypes
    inp = {k: np.asarray(v) for k, v in inputs.items()}
    x = inp["x"].astype(np.float32, copy=False)
    B = x.shape[0]
    hp = np.zeros((B, TP, D), np.float32)
    hp[:, :NMETA] = inp["meta_tokens"][None]
    hp[:, NMETA:T] = x
    shared = {}
    for k, v in make_consts().items():
        shared["c_" + k] = v
    shared.update(prep_even(inp, 0))
    shared.update(prep_gdn(inp, 0))
    shared.update(prep_moe(inp, 0, "m0_"))
    shared.update(prep_moe(inp, 1, "m1_"))
    shapes = {n: (v.shape, BF16 if v.dtype == ml_dtypes.bfloat16 else F32) for n, v in shared.items()}
    shapes["h0"] = ((NTOK, D), F32)
    nc = build_program(shapes)
    in_maps = []
    for ci in range(NCORES):
        m = dict(shared)
        m["h0"] = np.ascontiguousarray(hp[ci * SPC:(ci + 1) * SPC].reshape(NTOK, D))
        in_maps.append(m)
    res = run_bass_kernel_spmd(nc, in_maps, core_ids=list(range(NCORES)))
    outs = [r["out"].reshape(SPC, TP, D)[:, NMETA:T] for r in res.results]
    return np.ascontiguousarray(np.concatenate(outs, axis=0).astype(np.float32))
```

```python
import contextlib
import numpy as np
import concourse.bass as bass
import concourse.mybir as mybir
from concourse.bass_utils import run_bass_kernel_spmd

F32 = mybir.dt.float32
BF16 = mybir.dt.bfloat16
I32 = mybir.dt.int32
AF = mybir.ActivationFunctionType
ALU = mybir.AluOpType
AX = mybir.AxisListType

NCORES = 8
D = 1024
SEQ = 2048
NMETA = 16
T = SEQ + NMETA
TP = 17 * 128
NT = TP // 128
SPC = 4
NTOK = SPC * TP
NTILE = NTOK // 128
ALPHA = 4.0 ** 0.25
NEG = -1.0e30


class Res:
    __slots__ = ("w", "r", "dsem")

    def __init__(self):
        self.w = None
        self.r = {}
        self.dsem = None


class KB:
    def __init__(self, nc):
        self.nc = nc
        self.es = contextlib.ExitStack()
        self.stack = [self.es]
        self.eng = {"pe": nc.tensor, "act": nc.scalar, "dve": nc.vector, "pool": nc.gpsimd, "sp": nc.sync}
        self.sems = {}
        self.cnt = {}
        for k in self.eng:
            self.sems[k] = self.es.enter_context(nc.semaphore("sem_" + k))
            self.cnt[k] = 0
        self.seen = {k: {} for k in self.eng}
        self.res = {}
        self.dcount = {}
        self.free_dsems = []
        self.scope_res = [[]]
        self.nd = 0
        self.n_inst = 0
        self.uid = 0
        self.psum_names = set()

    def sb(self, name, shape, dtype=F32):
        self.uid += 1
        return self.stack[-1].enter_context(self.nc.sbuf_tensor("%s_%d" % (name, self.uid), list(shape), dtype))

    def ps(self, name, shape, dtype=F32):
        self.uid += 1
        nm = "%s_%d" % (name, self.uid)
        self.psum_names.add(nm)
        return self.stack[-1].enter_context(self.nc.psum_tensor(nm, list(shape), dtype))

    def dram(self, name, shape, dtype=F32, kind="Internal"):
        return self.nc.dram_tensor(name, list(shape), dtype, kind=kind).ap()

    @contextlib.contextmanager
    def scope(self):
        st = contextlib.ExitStack()
        self.stack.append(st)
        self.scope_res.append([])
        try:
            yield
        finally:
            self.barrier()
            for key in self.scope_res.pop():
                r = self.res.pop(key, None)
                if r is not None and r.dsem is not None:
                    self.free_dsems.append(r.dsem)
            self.stack.pop()
            st.close()

    def _res(self, ap):
        key = ap if isinstance(ap, str) else ap.name
        r = self.res.get(key)
        if r is None:
            r = self.res[key] = Res()
            self.scope_res[-1].append(key)
        return r

    def _wait(self, e, semkey, val):
        if semkey in self.dcount:
            val = max(val, self.dcount[semkey])
        if self.seen[e].get(semkey, 0) >= val:
            return
        if semkey == e and val > self.cnt[e]:
            return
        self.seen[e][semkey] = val
        self.eng[e].wait_ge(self.sems[semkey], val)

    def _deps(self, e, reads, writes):
        for a in reads:
            r = self._res(a)
            if r.w is not None:
                self._wait(e, *r.w)
        for a in writes:
            r = self._res(a)
            if r.w is not None:
                self._wait(e, *r.w)
            for sk, v in r.r.items():
                self._wait(e, sk, v)

    def _done(self, ev, reads, writes):
        for a in reads:
            r = self._res(a)
            if r.r.get(ev[0], 0) < ev[1]:
                r.r[ev[0]] = ev[1]
        for a in writes:
            r = self._res(a)
            r.w = ev
            r.r = {}

    def I(self, e, fn, reads, writes, *args, inc=True, **kw):
        writes = list(writes) + [a for a in reads if (not isinstance(a, str)) and a.name in self.psum_names]
        self._deps(e, reads, writes)
        ins = getattr(self.eng[e], fn)(*args, **kw)
        if inc:
            self.cnt[e] += 1
            ins.then_inc(self.sems[e], 1)
            self._done((e, self.cnt[e]), reads, writes)
        else:
            self._done((e, self.cnt[e] + 1), reads, writes)
        self.n_inst += 1
        return ins

    def dma(self, q, out, in_, reads=None, writes=None, group=None, indirect=None, **kw):
        reads = [in_] if reads is None else reads
        writes = [out] if writes is None else writes
        dst = self._res(group if group is not None else writes[0])
        if dst.dsem is None:
            if self.free_dsems:
                dst.dsem = self.free_dsems.pop()
            else:
                self.nd += 1
                dst.dsem = "d%d" % self.nd
                self.sems[dst.dsem] = self.es.enter_context(self.nc.semaphore(dst.dsem))
                self.dcount[dst.dsem] = 0
        if group is None:
            self._deps(q, reads, writes)
        else:
            self._deps(q, reads, [])
            for a in writes:
                r = self._res(a)
                if r.w is not None and r.w[0] != dst.dsem:
                    self._wait(q, *r.w)
                for sk, v in r.r.items():
                    self._wait(q, sk, v)
        if indirect is None:
            ins = self.eng[q].dma_start(out=out, in_=in_, **kw)
        else:
            ins = self.eng[q].indirect_dma_start(out=out, in_=in_, **indirect)
        self.dcount[dst.dsem] += 16
        ins.then_inc(self.sems[dst.dsem], 16)
        self._done((dst.dsem, self.dcount[dst.dsem]), reads, writes)
        self.n_inst += 1
        return ins

    def copy(self, e, reads, writes, out, in_):
        return self.I(e, "copy" if e == "act" else "tensor_copy", reads, writes, out=out, in_=in_)

    def barrier(self):
        for e in self.eng:
            for e2 in ("pe", "act", "dve", "pool"):
                if self.cnt[e2]:
                    self._wait(e, e2, self.cnt[e2])
            for sk, c in self.dcount.items():
                if c:
                    self._wait(e, sk, c)

    def finish(self):
        self.barrier()
        self.es.close()


def load_consts(kb, cd):
    c = {}
    for name, shape, dt in (("ident", [128, 128], F32), ("identb", [128, 128], BF16),
                            ("ones", [128, 128], F32), ("sut", [128, 128], F32),
                            ("ramp", [128, 128], F32), ("pidx", [128, 128], F32), ("cmask", [128, 128], F32),
                            ("onesb", [128, 128], BF16), ("cmaskb", [128, 128], BF16)):
        t = kb.sb("c_" + name, shape, dt)
        kb.dma("sp", t[:], cd[name])
        c[name] = t
    return c


def layer_norm_tile(kb, acc, g_bc, b_bc, outt, st6, mv, eng2="pool"):
    for j in range(2):
        kb.I("dve", "bn_stats", [acc], [st6], out=st6[:, j, :], in_=acc[:, j * 512:(j + 1) * 512])
    kb.I("dve", "bn_aggr", [st6], [mv], out=mv[:, 0:2], in_=st6[:].rearrange("p a b -> p (a b)"))
    kb.I("dve", "tensor_scalar", [mv], [mv], out=mv[:, 2:3], in0=mv[:, 1:2], scalar1=1e-5, scalar2=None, op0=ALU.add)
    kb.I("act", "sqrt", [mv], [mv], out=mv[:, 2:3], in_=mv[:, 2:3])
    kb.I("dve", "reciprocal", [mv], [mv], out=mv[:, 2:3], in_=mv[:, 2:3])
    kb.I("dve", "scalar_tensor_tensor", [mv], [mv], out=mv[:, 3:4], in0=mv[:, 0:1], scalar=-1.0, in1=mv[:, 2:3], op0=ALU.mult, op1=ALU.mult)
    kb.I("act", "activation", [acc, mv], [acc], out=acc[:], in_=acc[:], func=AF.Identity, scale=mv[:, 2:3], bias=mv[:, 3:4])
    kb.I("dve", "tensor_tensor", [acc, g_bc], [acc], out=acc[:], in0=acc[:], in1=g_bc[:], op=ALU.mult)
    kb.I(eng2, "tensor_tensor", [acc, b_bc], [outt], out=outt[:], in0=acc[:], in1=b_bc[:], op=ALU.add)


MOE_BS = 4
MOE_BLK = MOE_BS * 128
NBLK = -(-NTOK * 2 // MOE_BLK) + 32
NSLOT = NBLK * MOE_BLK


def build_moe(kb, c, h_d, wr_d, br_d, wg_d, wu_d, wd_d, lng_d, lnb_d, out_d, xs_d, ys_d, ntile=NTILE):
    nc = kb.nc
    nblk = -(-ntile * 128 * 2 // MOE_BLK) + 32
    BS, BLK = MOE_BS, MOE_BLK
    with kb.scope():
        s1i = kb.sb("s1i", [128, ntile], I32)
        s2i = kb.sb("s2i", [128, ntile], I32)
        g1 = kb.sb("g1", [128, ntile])
        g2 = kb.sb("g2", [128, ntile])
        idxe = kb.sb("idxe", [128, nblk], I32)
        g_bc = kb.sb("g_bc", [128, D])
        b_bc = kb.sb("b_bc", [128, D])
        kb.dma("sp", g_bc[:], lng_d.partition_broadcast(128))
        kb.dma("sp", b_bc[:], lnb_d.partition_broadcast(128))
        hts = [kb.sb("ht%d" % i, [128, D]) for i in range(2)]

        with kb.scope():
            wr = kb.sb("wr", [128, 8, 36])
            kb.dma("sp", wr[:], wr_d.rearrange("(k p) e -> p k e", p=128))
            br = kb.sb("br", [128, 36])
            kb.dma("sp", br[:], br_d.partition_broadcast(128))
            L = kb.sb("L", [128, ntile, 36])
            hT = [kb.sb("hT%d" % i, [128, 8, 128]) for i in range(2)]
            tp = [kb.ps("tp%d" % i, [128, 8, 128]) for i in range(2)]
            lg = [kb.ps("lg%d" % i, [128, 36]) for i in range(2)]
            for t in range(ntile):
                ht = hts[t % 2]
                kb.dma("sp", ht[:], h_d[t * 128:(t + 1) * 128, :])
                tpp = tp[t % 2]
                for k in range(8):
                    kb.I("pe", "transpose", [ht, c["ident"]], [tpp], out=tpp[:, k, :], in_=ht[:, k * 128:(k + 1) * 128],
                         identity=c["ident"][:], inc=(k == 7))
                hTt = hT[t % 2]
                kb.I("act", "copy", [tpp], [hTt], out=hTt[:], in_=tpp[:])
                lgp = lg[t % 2]
                for k in range(8):
                    kb.I("pe", "matmul", [hTt, wr], [lgp], lgp[:], lhsT=hTt[:, k, :], rhs=wr[:, k, :],
                         start=(k == 0), stop=(k == 7), inc=(k == 7))
                kb.I("dve", "tensor_tensor", [lgp, br], [L], out=L[:, t, :], in0=lgp[:], in1=br[:], op=ALU.add)

            NE = ntile * 32
            GL = L[:, :, 0:4]
            EL = L[:, :, 4:36]
            gmax = kb.sb("gmax", [128, ntile])
            t4 = kb.sb("t4", [128, ntile, 4])
            goh = kb.sb("goh", [128, ntile, 4])
            gg = kb.sb("gg", [128, ntile])
            ELm = kb.sb("ELm", [128, ntile, 32])
            EL2 = kb.sb("EL2", [128, ntile, 32])
            oh1 = kb.sb("oh1", [128, ntile, 32])
            oh2 = kb.sb("oh2", [128, ntile, 32])
            m1 = kb.sb("m1", [128, ntile])
            m2 = kb.sb("m2", [128, ntile])
            tmp = kb.sb("tmp", [128, ntile])
            V = "dve"
            kb.I(V, "tensor_reduce", [L], [gmax], out=gmax[:], in_=GL, axis=AX.X, op=ALU.max)
            bc4 = lambda a: a[:].unsqueeze(2).to_broadcast([128, ntile, 4])
            bc32 = lambda a: a[:].unsqueeze(2).to_broadcast([128, ntile, 32])
            kb.I(V, "tensor_tensor", [L, gmax], [goh], out=goh[:], in0=GL, in1=bc4(gmax), op=ALU.is_equal)
            kb.I(V, "tensor_tensor", [L, gmax], [t4], out=t4[:], in0=GL, in1=bc4(gmax), op=ALU.subtract)
            kb.I("act", "activation", [t4], [t4], out=t4[:], in_=t4[:], func=AF.Exp)
            kb.I(V, "tensor_reduce", [t4], [gg], out=gg[:], in_=t4[:], axis=AX.X, op=ALU.add)
            kb.I(V, "reciprocal", [gg], [gg], out=gg[:], in_=gg[:])
            kb.I(V, "tensor_scalar", [goh], [t4], out=t4[:], in0=goh[:], scalar1=-NEG, scalar2=NEG,
                 op0=ALU.mult, op1=ALU.add)
            kb.I(V, "tensor_tensor", [L, t4], [ELm], out=ELm[:].rearrange("p t (g e) -> p t g e", g=4),
                 in0=EL.rearrange("p t (g e) -> p t g e", g=4),
                 in1=t4[:].unsqueeze(3).to_broadcast([128, ntile, 4, 8]), op=ALU.add)
            kb.I(V, "tensor_reduce", [ELm], [m1], out=m1[:], in_=ELm[:], axis=AX.X, op=ALU.max)
            kb.I(V, "tensor_tensor", [ELm, m1], [oh1], out=oh1[:], in0=ELm[:], in1=bc32(m1), op=ALU.is_equal)
            kb.I(V, "scalar_tensor_tensor", [oh1, ELm], [EL2], out=EL2[:], in0=oh1[:], scalar=NEG, in1=ELm[:],
                 op0=ALU.mult, op1=ALU.add)
            kb.I(V, "tensor_reduce", [EL2], [m2], out=m2[:], in_=EL2[:], axis=AX.X, op=ALU.max)
            kb.I(V, "tensor_tensor", [EL2, m2], [oh2], out=oh2[:], in0=EL2[:], in1=bc32(m2), op=ALU.is_equal)
            kb.I(V, "tensor_tensor", [m1, m2], [tmp], out=tmp[:], in0=m2[:], in1=m1[:], op=ALU.subtract)
            kb.I("act", "activation", [tmp], [tmp], out=tmp[:], in_=tmp[:], func=AF.Exp)
            kb.I(V, "tensor_scalar", [tmp], [m1], out=m1[:], in0=tmp[:], scalar1=1.0, scalar2=None, op0=ALU.add)
            kb.I(V, "reciprocal", [m1], [m1], out=m1[:], in_=m1[:])
            kb.I(V, "tensor_tensor", [tmp, m1], [m2], out=m2[:], in0=tmp[:], in1=m1[:], op=ALU.mult)
            kb.I(V, "tensor_tensor", [m1, gg], [g1], out=g1[:], in0=m1[:], in1=gg[:], op=ALU.mult)
            kb.I(V, "tensor_tensor", [m2, gg], [g2], out=g2[:], in0=m2[:], in1=gg[:], op=ALU.mult)
            OH = ELm
            kb.I(V, "tensor_tensor", [oh1, oh2], [OH], out=OH[:], in0=oh1[:], in1=oh2[:], op=ALU.add)
            RK = kb.sb("RK", [128, NE])
            CT = kb.sb("CT", [128, ntile, 32])
            OHf = OH[:].rearrange("p t e -> p (t e)")
            CTf = CT[:].rearrange("p t e -> p (t e)")
            pr = [kb.ps("pr%d" % i, [128, 512]) for i in range(2)]
            ci = 0
            for dst, lhs in ((RK[:], c["sut"]), (CTf, c["ones"])):
                for o in range(0, NE, 512):
                    n = min(512, NE - o)
                    p = pr[ci % 2]
                    ci += 1
                    kb.I("pe", "matmul", [lhs, OH], [p], p[:, 0:n], lhsT=lhs[:], rhs=OHf[:, o:o + n],
                         start=True, stop=True)
                    kb.I("act", "copy", [p], [RK if dst is not CTf else CT], out=dst[:, o:o + n], in_=p[:, 0:n])
            base = kb.sb("base", [128, ntile, 32])
            kb.I(V, "memset", [], [base], base[:, 0, :], 0.0)
            for t in range(1, ntile):
                kb.I(V, "tensor_tensor", [base, CT], [base], out=base[:, t, :], in0=base[:, t - 1, :],
                     in1=CT[:, t - 1, :], op=ALU.add)
            tot = kb.sb("tot", [128, 32])
            pad = kb.sb("pad", [128, 32])
            pend = kb.sb("pend", [128, 32])
            one32 = kb.sb("one32", [128, 32])
            kb.I(V, "memset", [], [one32], one32[:], 1.0)
            kb.I(V, "tensor_tensor", [base, CT], [tot], out=tot[:], in0=base[:, ntile - 1, :], in1=CT[:, ntile - 1, :],
                 op=ALU.add)
            thr = kb.sb("thr", [128, nblk])
            kb.I(V, "tensor_scalar", [c["ramp"]], [thr], out=thr[:], in0=c["ramp"][:, 0:nblk], scalar1=float(BLK),
                 scalar2=None, op0=ALU.mult)
            cmp0 = kb.sb("cmp0", [128, 32, nblk])
            kb.I(V, "tensor_tensor", [tot, thr], [cmp0], out=cmp0[:],
                 in0=tot[:].unsqueeze(2).to_broadcast([128, 32, nblk]),
                 in1=thr[:].unsqueeze(1).to_broadcast([128, 32, nblk]), op=ALU.is_gt)
            kb.I(V, "tensor_reduce", [cmp0], [pad], out=pad[:], in_=cmp0[:], axis=AX.X, op=ALU.add)
            kb.I(V, "tensor_scalar", [pad], [pad], out=pad[:], in0=pad[:], scalar1=float(BLK), scalar2=None, op0=ALU.mult)
            kb.I(V, "tensor_tensor_scan", [one32, pad], [pend], out=pend[:], data0=one32[:], data1=pad[:], initial=0.0,
                 op0=ALU.mult, op1=ALU.add)
            kb.I(V, "tensor_tensor", [pend, pad], [pad], out=pad[:], in0=pend[:], in1=pad[:], op=ALU.subtract)
            cmp = kb.sb("cmp", [128, nblk, 32])
            bef = kb.sb("bef", [128, nblk])
            kb.I(V, "tensor_scalar", [c["ramp"]], [bef], out=bef[:], in0=c["ramp"][:, 0:nblk], scalar1=float(BLK),
                 scalar2=None, op0=ALU.mult)
            kb.I(V, "tensor_tensor", [pend, bef], [cmp], out=cmp[:],
                 in0=pend[:].unsqueeze(1).to_broadcast([128, nblk, 32]),
                 in1=bef[:].unsqueeze(2).to_broadcast([128, nblk, 32]), op=ALU.is_le)
            kb.I(V, "tensor_reduce", [cmp], [bef], out=bef[:], in_=cmp[:], axis=AX.X, op=ALU.add)
            kb.I(V, "tensor_scalar", [bef], [bef], out=bef[:], in0=bef[:], scalar1=31.0, scalar2=None, op0=ALU.min)
            ig = kb.sb("ig", [128, nblk])
            kb.I(V, "tensor_scalar", [bef, c["pidx"]], [ig], out=ig[:], in0=bef[:], scalar1=128.0, scalar2=c["pidx"][:, 0:1],
                 op0=ALU.mult, op1=ALU.add)
            usedf = kb.sb("usedf", [128, nblk])
            kb.I(V, "tensor_scalar", [thr, pend], [usedf], out=usedf[:], in0=thr[:], scalar1=pend[:, 31:32], scalar2=None, op0=ALU.is_lt)
            kb.I(V, "tensor_tensor", [ig, usedf], [ig], out=ig[:], in0=ig[:], in1=usedf[:], op=ALU.mult)
            kb.I(V, "tensor_scalar", [usedf], [usedf], out=usedf[:], in0=usedf[:], scalar1=-8192.0, scalar2=8192.0, op0=ALU.mult, op1=ALU.add)
            kb.I(V, "tensor_tensor", [ig, usedf], [ig], out=ig[:], in0=ig[:], in1=usedf[:], op=ALU.add)
            kb.I(V, "tensor_copy", [ig], [idxe], out=idxe[:], in_=ig[:])
            SL = EL2
            RK3 = RK[:].rearrange("p (t e) -> p t e", e=32)
            kb.I(V, "tensor_tensor", [RK, base], [SL], out=SL[:], in0=RK3, in1=base[:], op=ALU.add)
            kb.I(V, "tensor_tensor", [SL, pad], [SL], out=SL[:], in0=SL[:],
                 in1=pad[:].unsqueeze(1).to_broadcast([128, ntile, 32]), op=ALU.add)
            for oh, si in ((oh1, s1i), (oh2, s2i)):
                kb.I(V, "tensor_tensor", [SL, oh], [oh], out=oh[:], in0=SL[:], in1=oh[:], op=ALU.mult)
                kb.I(V, "tensor_reduce", [oh], [tmp], out=tmp[:], in_=oh[:], axis=AX.X, op=ALU.add)
                kb.I(V, "tensor_copy", [tmp], [si], out=si[:], in_=tmp[:])

        with kb.scope():
            hbs = [kb.sb("hb%d" % i, [128, D], BF16) for i in range(2)]
            for t in range(ntile):
                ht = hts[t % 2]
                hb = hbs[t % 2]
                kb.dma("sp", ht[:], h_d[t * 128:(t + 1) * 128, :])
                kb.I("act", "copy", [ht], [hb], out=hb[:], in_=ht[:])
                for si in (s1i, s2i):
                    kb.dma("pool", xs_d, hb[:], reads=[hb, si], writes=[xs_d], indirect=dict(
                        out_offset=bass.IndirectOffsetOnAxis(ap=si[:, t:t + 1], axis=0), in_offset=None))

        with kb.scope():
            xin = [[kb.sb("xin%d_%d" % (i, s), [128, D], BF16) for s in range(BS)] for i in range(2)]
            xT = [kb.sb("xT%d" % i, [128, 8, BLK], BF16) for i in range(2)]
            tps = [kb.ps("tps%d" % i, [128, 8, 128], BF16) for i in range(2)]
            stg = [kb.sb("stg%d" % i, [128, 4096]) for i in range(3)]
            wgb = [kb.sb("wgb%d" % i, [128, 8, 512], BF16) for i in range(2)]
            wub = [kb.sb("wub%d" % i, [128, 8, 512], BF16) for i in range(2)]
            wdb = [kb.sb("wdb%d" % i, [128, 4, 1024], BF16) for i in range(2)]
            hgp = [kb.ps("hgp%d" % i, [128, BLK]) for i in range(2)]
            hup = [kb.ps("hup%d" % i, [128, BLK]) for i in range(2)]
            yp = [kb.ps("yp%d" % i, [128, 512]) for i in range(2)]
            sg = [kb.sb("sg%d" % i, [128, BLK]) for i in range(2)]
            hTb = [kb.sb("hTb%d" % i, [128, 4, BLK], BF16) for i in range(2)]
            yb = [kb.sb("yb%d" % i, [128, D]) for i in range(2)]
            sti = 0
            cast_eng = ["dve", "act"]
            bc_reg = nc.gpsimd.alloc_register("moe_bc_%d" % kb.uid)
            nc.gpsimd.reg_mov(bc_reg, 4095)

            def load_weights(b):
                nonlocal sti
                par = b % 2
                for src, dstt in ((wg_d, wgb[par]), (wu_d, wub[par]), (wd_d, wdb[par])):
                    st = stg[sti % 3]
                    kb.dma("pool", st[:], src, reads=[idxe], writes=[st],
                           indirect=dict(out_offset=None, in_offset=bass.IndirectOffsetOnAxis(ap=idxe[:, b:b + 1], axis=0),
                                         bounds_check=bc_reg, oob_is_err=False))
                    dflat = dstt[:].rearrange("p a b -> p (a b)")
                    for half in range(2):
                        ce = cast_eng[(2 * sti + half) % 2]
                        kb.copy(ce, [st], [dstt], dflat[:, half * 2048:(half + 1) * 2048], st[:, half * 2048:(half + 1) * 2048])
                    sti += 1

            def load_tokens(b):
                par = b % 2
                for s in range(BS):
                    xi = xin[par][s]
                    kb.dma("act", xi[:], xs_d[b * BLK + s * 128: b * BLK + (s + 1) * 128, :])
                    tpp = tps[s % 2]
                    for k in range(8):
                        kb.I("pe", "transpose", [xi, c["identb"]], [tpp], out=tpp[:, k, :], in_=xi[:, k * 128:(k + 1) * 128],
                             identity=c["identb"][:], inc=(k == 7))
                    kb.I("dve", "tensor_copy", [tpp], [xT[par]], out=xT[par][:, :, s * 128:(s + 1) * 128], in_=tpp[:])

            def gate_up(b):
                par = b % 2
                for f in range(4):
                    hg = hgp[f % 2]
                    hu = hup[f % 2]
                    for k in range(8):
                        kb.I("pe", "matmul", [wgb[par], xT[par]], [hg], hg[:], lhsT=wgb[par][:, k, f * 128:(f + 1) * 128],
                             rhs=xT[par][:, k, :], start=(k == 0), stop=(k == 7), inc=(k == 7))
                    for k in range(8):
                        kb.I("pe", "matmul", [wub[par], xT[par]], [hu], hu[:], lhsT=wub[par][:, k, f * 128:(f + 1) * 128],
                             rhs=xT[par][:, k, :], start=(k == 0), stop=(k == 7), inc=(k == 7))
                    sgt = sg[f % 2]
                    kb.I("act", "activation", [hg], [sgt], out=sgt[:], in_=hg[:], func=AF.Silu)
                    kb.I("dve", "tensor_tensor", [sgt, hu], [hTb[par]], out=hTb[par][:, f, :], in0=sgt[:], in1=hu[:], op=ALU.mult)

            def down(b):
                par = b % 2
                for s in range(BS):
                    ybt = yb[s % 2]
                    for dh in range(2):
                        y = yp[dh]
                        for f in range(4):
                            kb.I("pe", "matmul", [hTb[par], wdb[par]], [y], y[:], lhsT=hTb[par][:, f, s * 128:(s + 1) * 128],
                                 rhs=wdb[par][:, f, dh * 512:(dh + 1) * 512], start=(f == 0), stop=(f == 3), inc=(f == 3))
                        kb.I("act" if dh == 0 else "dve", "tensor_copy" if dh else "copy", [y], [ybt],
                             out=ybt[:, dh * 512:(dh + 1) * 512], in_=y[:])
                    kb.dma("sp", ys_d[b * BLK + s * 128: b * BLK + (s + 1) * 128, :], ybt[:])

            load_weights(0)
            load_tokens(0)
            for b in range(nblk):
                if b + 1 < nblk:
                    load_weights(b + 1)
                gate_up(b)
                if b + 1 < nblk:
                    load_tokens(b + 1)
                down(b)

        with kb.scope():
            NS = 3
            y1 = [kb.sb("y1_%d" % i, [128, D]) for i in range(NS)]
            y2 = [kb.sb("y2_%d" % i, [128, D]) for i in range(NS)]
            hts5 = [kb.sb("ht5_%d" % i, [128, D]) for i in range(NS)]
            acc = [kb.sb("acc%d" % i, [128, D]) for i in range(2)]
            outt = [kb.sb("outt%d" % i, [128, D]) for i in range(2)]
            st6 = kb.sb("st6", [128, 2, 6])
            mv = kb.sb("mv", [128, 4])

            def issue(t):
                p = t % NS
                kb.dma("sp", hts5[p][:], h_d[t * 128:(t + 1) * 128, :])
                for yy, si in ((y1[p], s1i), (y2[p], s2i)):
                    kb.dma("pool", yy[:], ys_d, reads=[ys_d, si], writes=[yy], indirect=dict(
                        out_offset=None, in_offset=bass.IndirectOffsetOnAxis(ap=si[:, t:t + 1], axis=0)))
            for t in range(min(NS - 1, ntile)):
                issue(t)
            for t in range(ntile):
                if t + NS - 1 < ntile:
                    issue(t + NS - 1)
                p = t % NS
                ht = hts5[p]
                a = acc[t % 2]
                kb.I("act", "mul", [y1[p], g1], [a], out=a[:], in_=y1[p][:], mul=g1[:, t:t + 1])
                kb.I("dve", "scalar_tensor_tensor", [y2[p], g2, a], [a], out=a[:], in0=y2[p][:], scalar=g2[:, t:t + 1],
                     in1=a[:], op0=ALU.mult, op1=ALU.add)
                kb.I("dve", "scalar_tensor_tensor", [ht, a], [a], out=a[:], in0=ht[:], scalar=ALPHA, in1=a[:],
                     op0=ALU.mult, op1=ALU.add)
                layer_norm_tile(kb, a, g_bc, b_bc, outt[t % 2], st6, mv)
                kb.dma("sp", out_d[t * 128:(t + 1) * 128, :], outt[t % 2][:])

def make_consts():
    import ml_dtypes
    i = np.arange(128)
    return {
        "ident": np.eye(128, dtype=np.float32),
        "identb": np.eye(128, dtype=np.float32).astype(ml_dtypes.bfloat16),
        "ones": np.ones((128, 128), np.float32),
        "sut": (i[:, None] < i[None, :]).astype(np.float32),
        "ramp": np.broadcast_to(i[None, :].astype(np.float32), (128, 128)).copy(),
        "pidx": np.broadcast_to(i[:, None].astype(np.float32), (128, 128)).copy(),
        "cmask": np.where(i[None, :] > i[:, None], NEG, 0.0).astype(np.float32),
        "onesb": np.ones((128, 128), np.float32).astype(ml_dtypes.bfloat16),
        "cmaskb": np.where(i[None, :] > i[:, None], NEG, 0.0).astype(np.float32).astype(ml_dtypes.bfloat16),
    }


CHUNKS = [(0, 512), (512, 512), (1024, 512), (1536, 512), (2048, 128)]
EV_Q, EV_CKV, EV_QI, EV_KI, EV_WI, EV_GATE, EV_XB = 0, 512, 768, 1280, 1344, 1352, 1864


def proj_fm(kb, ps2, cnt, wb, col0, hT, dst_fn, evac):
    for (t0, n) in CHUNKS:
        p = ps2[cnt[0] % 2]
        for k in range(8):
            kb.I("pe", "matmul", [wb, hT], [p], p[:, 0:n], lhsT=wb[:, k, col0:col0 + 128], rhs=hT[:, k, t0:t0 + n],
                 start=(k == 0), stop=(k == 7), inc=(k == 7))
        evac(cnt[0], p[:, 0:n], t0, n)
        cnt[0] += 1


def build_even(kb, c, h_d, win_d, wout_d, wuk_d, wuv_d, rgw_d, vec_d, lng_d, lnb_d, out_d, nseq=SPC):
    with kb.scope():
        winb = kb.sb("winb", [128, 8, 2376], BF16)
        wk2 = kb.sb("wk2", [128, 8, 128], BF16)
        wukb = kb.sb("wukb", [128, 2, 128], BF16)
        wuvb = kb.sb("wuvb", [128, 2, 128], BF16)
        rgwb = kb.sb("rgwb", [128, 8, 128], BF16)
        vec = kb.sb("vec", [128, 40])
        cc = kb.sb("cc", [128, 4])
        g_bc = kb.sb("g_bc", [128, D])
        b_bc = kb.sb("b_bc", [128, D])
        kb.dma("sp", g_bc[:], lng_d.partition_broadcast(128))
        kb.dma("sp", b_bc[:], lnb_d.partition_broadcast(128))
        kb.dma("sp", vec[:], vec_d)
        with kb.scope():
            stg = [kb.sb("wstg%d" % i, [128, 2376]) for i in range(2)]
            for k in range(8):
                st = stg[k % 2]
                kb.dma("sp", st[:], win_d[k * 128:(k + 1) * 128, :])
                kb.copy("act" if k % 2 else "pool", [st], [winb], winb[:, k, :], st[:])
            for j in range(2):
                kb.I("dve", "tensor_copy", [winb], [wk2], out=wk2[:, :, j * 64:(j + 1) * 64], in_=winb[:, :, EV_KI:EV_KI + 64])
            st = stg[0]
            kb.dma("sp", st[:, 0:256], wuk_d.rearrange("(c p) d -> p c d", p=128))
            kb.I("dve", "tensor_copy", [st], [wukb], out=wukb[:], in_=st[:, 0:256].rearrange("p (c d) -> p c d", c=2))
            st = stg[1]
            kb.dma("sp", st[:, 0:256], wuv_d.rearrange("(c p) d -> p c d", p=128))
            kb.I("dve", "tensor_copy", [st], [wuvb], out=wuvb[:], in_=st[:, 0:256].rearrange("p (c d) -> p c d", c=2))
            st = stg[0]
            kb.dma("sp", st[:, 0:1024], rgw_d)
            kb.I("dve", "tensor_copy", [st], [rgwb], out=rgwb[:], in_=st[:, 0:1024].rearrange("p (c d) -> p c d", c=8))
            kb.I("act", "activation", [vec], [cc], out=cc[:], in_=vec[:, 28:32], func=AF.Exp, scale=-1.0)
            kb.I("act", "activation", [cc], [cc], out=cc[:], in_=cc[:], func=AF.Ln, bias=1.0)
            kb.I("dve", "tensor_scalar", [cc], [cc], out=cc[:], in0=cc[:], scalar1=-8.0, scalar2=None, op0=ALU.mult)
        cw = lambda j, ch: vec[:, j * 4 + ch: j * 4 + ch + 1]
        cb = lambda ch: vec[:, 16 + ch:17 + ch]
        ba = lambda ch: vec[:, 20 + ch:21 + ch]
        bx = lambda ch: vec[:, 24 + ch:25 + ch]
        kvn = lambda ch: vec[:, 32 + ch:33 + ch]

        for s in range(nseq):
            r0 = s * TP
            with kb.scope():
                hT = kb.sb("hT", [128, 8, TP], BF16)
                qT = kb.sb("qT", [128, 4, TP], BF16)
                qiT = kb.sb("qiT", [128, 4, TP], BF16)
                kiT2 = kb.sb("kiT2", [128, TP], BF16)
                kT = kb.sb("kT", [128, TP], BF16)
                vtok = kb.sb("vtok", [128, NT, 130], BF16)
                wq = kb.sb("wq", [128, NT, 8])
                kb.I("pool", "memset", [], [vtok], vtok[:, :, 128:130], 1.0)
                with kb.scope():
                    gateT = kb.sb("gateT", [128, 4, TP], BF16)
                    xbT = kb.sb("xbT", [128, 4, TP], BF16)
                    with kb.scope():
                        hts = [kb.sb("eht%d" % i, [128, D]) for i in range(2)]
                        tp = [kb.ps("etp%d" % i, [128, 8, 128]) for i in range(2)]
                        for t in range(NT):
                            ht = hts[t % 2]
                            kb.dma("sp", ht[:], h_d[r0 + t * 128: r0 + (t + 1) * 128, :])
                            tpp = tp[t % 2]
                            for k in range(8):
                                kb.I("pe", "transpose", [ht, c["ident"]], [tpp], out=tpp[:, k, :],
                                     in_=ht[:, k * 128:(k + 1) * 128], identity=c["ident"][:], inc=(k == 7))
                            kb.copy("act" if t % 2 else "dve", [tpp], [hT], hT[:, :, t * 128:(t + 1) * 128], tpp[:])
                    with kb.scope():
                        ckvT = kb.sb("ckvT", [128, 2, TP], BF16)
                        latT = kb.sb("latT", [128, 2, TP], BF16)
                        ps2 = [kb.ps("pp%d" % i, [128, 512]) for i in range(2)]
                        cnt = [0]

                        def mk_evac(dst, ci):
                            def ev(n_, p, t0, n):
                                kb.copy("act" if n_ % 2 else "dve", [p], [dst], dst[:, ci, t0:t0 + n] if ci is not None else dst[:, t0:t0 + n], p)
                            return ev
                        for ci in range(4):
                            proj_fm(kb, ps2, cnt, winb, EV_Q + ci * 128, hT, None, mk_evac(qT, ci))
                            proj_fm(kb, ps2, cnt, winb, EV_QI + ci * 128, hT, None, mk_evac(qiT, ci))
                            proj_fm(kb, ps2, cnt, winb, EV_GATE + ci * 128, hT, None, mk_evac(gateT, ci))
                            proj_fm(kb, ps2, cnt, winb, EV_XB + ci * 128, hT, None, mk_evac(xbT, ci))
                        for ci in range(2):
                            proj_fm(kb, ps2, cnt, winb, EV_CKV + ci * 128, hT, None, mk_evac(ckvT, ci))
                        proj_fm(kb, ps2, cnt, wk2, 0, hT, None, mk_evac(kiT2, None))
                        wps = [kb.ps("wps%d" % i, [128, 8]) for i in range(2)]
                        for t in range(NT):
                            p = wps[t % 2]
                            for k in range(8):
                                kb.I("pe", "matmul", [hT, winb], [p], p[:], lhsT=hT[:, k, t * 128:(t + 1) * 128],
                                     rhs=winb[:, k, EV_WI:EV_WI + 8], start=(k == 0), stop=(k == 7), inc=(k == 7))
                            kb.copy("act", [p], [wq], wq[:, t, :], p[:])
                        sq = kb.sb("sq", [128, 2, 512], BF16)
                        rstd = kb.sb("rstd", [128, 512])
                        for (t0, n) in CHUNKS:
                            kb.I("act", "activation", [ckvT], [sq], out=sq[:, :, 0:n], in_=ckvT[:, :, t0:t0 + n], func=AF.Square)
                            p = ps2[cnt[0] % 2]
                            cnt[0] += 1
                            for ci in range(2):
                                kb.I("pe", "matmul", [c["onesb"], sq], [p], p[:, 0:n], lhsT=c["onesb"][:], rhs=sq[:, ci, 0:n],
                                     start=(ci == 0), stop=(ci == 1), inc=(ci == 1))
                            kb.I("dve", "tensor_scalar", [p], [rstd], out=rstd[:, 0:n], in0=p[:, 0:n], scalar1=1.0 / 256, scalar2=1e-6,
                                 op0=ALU.mult, op1=ALU.add)
                            kb.I("act", "sqrt", [rstd], [rstd], out=rstd[:, 0:n], in_=rstd[:, 0:n])
                            kb.I("dve", "reciprocal", [rstd], [rstd], out=rstd[:, 0:n], in_=rstd[:, 0:n])
                            for ci in range(2):
                                kb.I("dve", "scalar_tensor_tensor", [ckvT, vec, rstd], [latT], out=latT[:, ci, t0:t0 + n],
                                     in0=ckvT[:, ci, t0:t0 + n], scalar=kvn(ci), in1=rstd[:, 0:n], op0=ALU.mult, op1=ALU.mult)
                            p = ps2[cnt[0] % 2]
                            cnt[0] += 1
                            for ci in range(2):
                                kb.I("pe", "matmul", [wukb, latT], [p], p[:, 0:n], lhsT=wukb[:, ci, :], rhs=latT[:, ci, t0:t0 + n],
                                     start=(ci == 0), stop=(ci == 1), inc=(ci == 1))
                            kb.copy("act", [p], [kT], kT[:, t0:t0 + n], p[:, 0:n])
                        for t in range(NT):
                            p = ps2[cnt[0] % 2]
                            cnt[0] += 1
                            for ci in range(2):
                                kb.I("pe", "matmul", [latT, wuvb], [p], p[:, 0:128], lhsT=latT[:, ci, t * 128:(t + 1) * 128],
                                     rhs=wuvb[:, ci, :], start=(ci == 0), stop=(ci == 1), inc=(ci == 1))
                            kb.copy("dve", [p], [vtok], vtok[:, t, 0:128], p[:, 0:128])
                    mixT = hT
                    with kb.scope():
                        HS = TP // 2
                        F = lambda nm: kb.sb(nm, [128, HS])
                        xr, rr, ii, aa, uu, hh, gl = F("xr"), F("rr"), F("ii"), F("aa"), F("uu"), F("hh"), F("gl")
                        xrb = kb.sb("xrb", [128, HS], BF16)
                        carry = kb.sb("carry", [128, 4])
                        gps = [kb.ps("gps%d" % i, [128, 512]) for i in range(4)]
                        gi = 0
                        for ch in range(4):
                            for hf in range(2):
                                o = hf * HS
                                x = xbT[:, ch, :]
                                kb.I("dve", "tensor_scalar", [xbT, vec], [xr], out=xr[:], in0=x[:, o:o + HS], scalar1=cw(3, ch), scalar2=cb(ch),
                                     op0=ALU.mult, op1=ALU.add)
                                for d in (1, 2, 3):
                                    lo = d if hf == 0 else 0
                                    kb.I("dve", "scalar_tensor_tensor", [xbT, vec, xr], [xr], out=xr[:, lo:HS], in0=x[:, o + lo - d:o + HS - d],
                                         scalar=cw(3 - d, ch), in1=xr[:, lo:HS], op0=ALU.mult, op1=ALU.add)
                                kb.copy("pool", [xr], [xrb], xrb[:], xr[:])
                                for (t0, n) in ((0, 512), (512, 512), (1024, 64)):
                                    pa = gps[gi % 4]
                                    px = gps[(gi + 1) % 4]
                                    gi += 2
                                    kb.I("pe", "matmul", [rgwb, xrb], [pa], pa[:, 0:n], lhsT=rgwb[:, ch, :], rhs=xrb[:, t0:t0 + n], start=True, stop=True)
                                    kb.I("pe", "matmul", [rgwb, xrb], [px], px[:, 0:n], lhsT=rgwb[:, 4 + ch, :], rhs=xrb[:, t0:t0 + n], start=True, stop=True)
                                    kb.I("act", "activation", [pa, vec], [rr], out=rr[:, t0:t0 + n], in_=pa[:, 0:n], func=AF.Sigmoid, bias=ba(ch))
                                    kb.I("act", "activation", [px, vec], [ii], out=ii[:, t0:t0 + n], in_=px[:, 0:n], func=AF.Sigmoid, bias=bx(ch))
                                kb.I("act", "activation", [rr, cc], [aa], out=aa[:], in_=rr[:], func=AF.Exp, scale=cc[:, ch:ch + 1])
                                kb.I("pool", "tensor_tensor", [aa], [uu], out=uu[:], in0=aa[:], in1=aa[:], op=ALU.mult)
                                kb.I("act", "activation", [uu], [uu], out=uu[:], in_=uu[:], func=AF.Sqrt, scale=-1.0, bias=1.0)
                                kb.I("pool", "tensor_tensor", [ii, xr], [ii], out=ii[:], in0=ii[:], in1=xr[:], op=ALU.mult)
                                kb.I("pool", "tensor_tensor", [uu, ii], [uu], out=uu[:], in0=uu[:], in1=ii[:], op=ALU.mult)
                                kb.I("dve", "tensor_tensor_scan", [aa, uu, carry], [hh], out=hh[:], data0=aa[:], data1=uu[:],
                                     initial=(0.0 if hf == 0 else carry[:, ch:ch + 1]), op0=ALU.mult, op1=ALU.add)
                                if hf == 0:
                                    kb.I("dve", "tensor_copy", [hh], [carry], out=carry[:, ch:ch + 1], in_=hh[:, HS - 1:HS])
                                g = gateT[:, ch, o:o + HS]
                                kb.I("act", "activation", [gateT], [gl], out=gl[:], in_=g, func=AF.Square)
                                kb.I("pool", "tensor_scalar", [gl], [gl], out=gl[:], in0=gl[:], scalar1=0.044715, scalar2=1.0, op0=ALU.mult, op1=ALU.add)
                                kb.I("pool", "tensor_tensor", [gl, gateT], [gl], out=gl[:], in0=gl[:], in1=g, op=ALU.mult)
                                kb.I("act", "activation", [gl], [gl], out=gl[:], in_=gl[:], func=AF.Sigmoid, scale=1.5957691216)
                                kb.I("pool", "tensor_tensor", [gl, gateT], [gl], out=gl[:], in0=gl[:], in1=g, op=ALU.mult)
                                kb.I("dve", "tensor_tensor", [hh, gl], [mixT], out=mixT[:, 4 + ch, o:o + HS], in0=hh[:], in1=gl[:], op=ALU.mult)
                with kb.scope():
                    score = [kb.sb("score%d" % i, [128, TP]) for i in range(2)]
                    mask = [kb.sb("mask%d" % i, [128, TP], BF16) for i in range(2)]
                    m8 = [kb.sb("m8_%d" % i, [128, 8]) for i in range(2)]
                    work = kb.sb("work", [128, TP])
                    thrc = kb.sb("thrc", [128, 1])
                    rlb = [kb.sb("rlb%d" % i, [128, 512], BF16) for i in range(4)]
                    dg = [kb.sb("dg%d" % i, [128, 8, 128], BF16) for i in range(1)]
                    lgt = [kb.sb("lgt%d" % i, [128, TP]) for i in range(2)]
                    ee = [kb.sb("ee%d" % i, [128, TP], BF16) for i in range(2)]
                    pT = [kb.sb("pT%d" % i, [128, NT, 128], BF16) for i in range(1)] * 2
                    sm = [kb.sb("sm%d" % i, [128, 4]) for i in range(2)]
                    on = [kb.sb("on%d" % i, [128, 128], BF16) for i in range(2)]
                    sps = [kb.ps("sps%d" % i, [128, 512]) for i in range(2)]
                    lps = [kb.ps("lps%d" % i, [128, 512]) for i in range(2)]
                    tps = [kb.ps("atp%d" % i, [128, 8, 128], BF16) for i in range(2)]
                    ops = kb.ps("ops", [128, 132])
                    scp = kb.ps("scp", [128, 512])
                    kb.I("dve", "memset", [], [thrc], thrc[:], -1.0e29)
                    cnts = {"si": 0, "ti": 0, "li": 0}

                    def indexer(i):
                        sc = score[i % 2]
                        dgt = dg[0]
                        nk = (i + 1) * 128
                        qs = slice(i * 128, (i + 1) * 128)
                        for h in range(8):
                            kb.I("pool", "tensor_scalar", [c["identb"], wq], [dgt], out=dgt[:, h, :], in0=c["identb"][:],
                                 scalar1=wq[:, i, h:h + 1], scalar2=None, op0=ALU.mult)
                        for t0 in range(0, nk, 512):
                            n = min(512, nk - t0)
                            last = (t0 + n == nk)
                            for hh in range(2):
                                for h in range(hh * 4, hh * 4 + 4):
                                    p = sps[cnts["si"] % 2]
                                    cnts["si"] += 1
                                    pr = slice((h % 2) * 64, (h % 2) * 64 + 64)
                                    kb.I("pe", "matmul", [qiT, kiT2], [p], p[:, 0:n], lhsT=qiT[pr, h // 2, qs], rhs=kiT2[pr, t0:t0 + n],
                                         start=True, stop=True)
                                    kb.I("act", "activation", [p], [rlb[h % 4]], out=rlb[h % 4][:, 0:n], in_=p[:, 0:n], func=AF.Relu)
                                for h in range(hh * 4, hh * 4 + 4):
                                    kb.I("pe", "matmul", [dgt, rlb[h % 4]], [scp], scp[:, 0:n], lhsT=dgt[:, h, :], rhs=rlb[h % 4][:, 0:n],
                                         start=(h == 0), stop=(h == 7 and not last), inc=(h % 4 == 3))
                            if last:
                                kb.I("pe", "matmul", [c["identb"], c["cmaskb"]], [scp], scp[:, n - 128:n], lhsT=c["identb"][:], rhs=c["cmaskb"][:],
                                     start=False, stop=True)
                            kb.copy("act", [scp], [sc], sc[:, t0:t0 + n], scp[:, 0:n])
                            yield

                    def topk(i):
                        sc, mk, m = score[i % 2], mask[i % 2], m8[i % 2]
                        nk = (i + 1) * 128
                        if i >= 2:
                            for it in range(32):
                                src = sc if it == 0 else work
                                kb.I("dve", "max", [src], [m], out=m[:], in_=src[:, 0:nk])
                                if it < 31:
                                    kb.I("dve", "match_replace", [m, src], [work], out=work[:, 0:nk], in_to_replace=m[:],
                                         in_values=src[:, 0:nk], imm_value=NEG)
                                if it % 8 == 7 and it < 31:
                                    yield
                            kb.I("dve", "tensor_scalar", [sc, m], [mk], out=mk[:, 0:nk], in0=sc[:, 0:nk], scalar1=m[:, 7:8],
                                 scalar2=None, op0=ALU.is_ge)
                        else:
                            kb.I("dve", "tensor_scalar", [sc, thrc], [mk], out=mk[:, 0:nk], in0=sc[:, 0:nk], scalar1=thrc[:, 0:1],
                                 scalar2=None, op0=ALU.is_ge)

                    def qk(i, h):
                        nk = (i + 1) * 128
                        qs = slice(i * 128, (i + 1) * 128)
                        lg = lgt[h % 2]
                        for t0 in range(0, nk, 512):
                            n = min(512, nk - t0)
                            p = lps[cnts["li"] % 2]
                            cnts["li"] += 1
                            kb.I("pe", "matmul", [qT, kT], [p], p[:, 0:n], lhsT=qT[:, h, qs], rhs=kT[:, t0:t0 + n], start=True, stop=True)
                            kb.I("act", "mul", [p], [lg], out=lg[:, t0:t0 + n], in_=p[:, 0:n], mul=128.0 ** -0.5)

                    def attention(i, gen, geni):
                        mk = mask[i % 2]
                        nk = (i + 1) * 128
                        qs = slice(i * 128, (i + 1) * 128)
                        qk(i, 0)
                        for h in range(4):
                            lg, e_, s_, pT_, on_ = lgt[h % 2], ee[h % 2], sm[h % 2], pT[h % 2], on[h % 2]
                            kb.I("dve", "tensor_reduce", [lg], [s_], out=s_[:, 0:1], in_=lg[:, 0:nk], axis=AX.X, op=ALU.max)
                            kb.I("dve", "tensor_scalar", [s_], [s_], out=s_[:, 1:2], in0=s_[:, 0:1], scalar1=-1.0, scalar2=None, op0=ALU.mult)
                            kb.I("act", "activation", [lg, s_], [e_], out=e_[:, 0:nk], in_=lg[:, 0:nk], func=AF.Exp, bias=s_[:, 1:2])
                            if h < 3:
                                qk(i, h + 1)
                            next(gen, None)
                            next(geni, None)
                            if h % 2:
                                next(geni, None)
                            kb.I("pool", "tensor_tensor", [e_, mk], [e_], out=e_[:, 0:nk], in0=e_[:, 0:nk], in1=mk[:, 0:nk], op=ALU.mult)
                            for j0 in range(0, i + 1, 8):
                                j1 = min(j0 + 8, i + 1)
                                tpp = tps[cnts["ti"] % 2]
                                cnts["ti"] += 1
                                for j in range(j0, j1):
                                    kb.I("pe", "transpose", [e_, c["identb"]], [tpp], out=tpp[:, j - j0, :], in_=e_[:, j * 128:(j + 1) * 128],
                                         identity=c["identb"][:], inc=(j == j1 - 1))
                                kb.copy("act", [tpp], [pT_], pT_[:, j0:j1, :], tpp[:, 0:j1 - j0, :])
                            for j in range(i + 1):
                                kb.I("pe", "matmul", [pT_, vtok], [ops], ops[:, 0:129], lhsT=pT_[:, j, :], rhs=vtok[:, j, 0:129], start=(j == 0), stop=(j == i),
                                     inc=(j == i))
                            kb.I("dve", "reciprocal", [ops], [s_], out=s_[:, 3:4], in_=ops[:, 128:129])
                            kb.I("dve", "tensor_scalar", [ops, s_], [on_], out=on_[:], in0=ops[:, 0:128], scalar1=s_[:, 3:4], scalar2=None, op0=ALU.mult)
                            otb = tps[cnts["ti"] % 2]
                            cnts["ti"] += 1
                            kb.I("pe", "transpose", [on_, c["identb"]], [otb], out=otb[:, 0, :], in_=on_[:], identity=c["identb"][:])
                            kb.copy("act", [otb], [mixT], mixT[:, h, qs], otb[:, 0, :])

                    for _ in indexer(0):
                        pass
                    for _ in indexer(1):
                        pass
                    for _ in topk(0):
                        pass
                    for i in range(NT):
                        gen = topk(i + 1) if i + 1 < NT else iter(())
                        geni = indexer(i + 2) if i + 2 < NT else iter(())
                        attention(i, gen, geni)
                        for _ in geni:
                            pass
                        for _ in gen:
                            pass
                with kb.scope():
                    woutb = kb.sb("woutb", [128, 8, D], BF16)
                    stg = [kb.sb("ostg%d" % i, [128, D]) for i in range(2)]
                    for k in range(8):
                        kb.dma("sp", stg[k % 2][:], wout_d[k * 128:(k + 1) * 128, :])
                        kb.copy("act" if k % 2 else "pool", [stg[k % 2]], [woutb], woutb[:, k, :], stg[k % 2][:])
                    hts = [kb.sb("oht%d" % i, [128, D]) for i in range(2)]
                    acc = [kb.sb("oacc%d" % i, [128, D]) for i in range(2)]
                    outt = [kb.sb("oout%d" % i, [128, D]) for i in range(2)]
                    st6 = kb.sb("st6", [128, 2, 6])
                    mv = kb.sb("mv", [128, 4])
                    ops2 = [kb.ps("opp%d" % i, [128, 512]) for i in range(4)]
                    for t in range(NT):
                        ht = hts[t % 2]
                        a = acc[t % 2]
                        kb.dma("sp", ht[:], h_d[r0 + t * 128: r0 + (t + 1) * 128, :])
                        for hf in range(2):
                            p = ops2[(2 * t + hf) % 4]
                            for k in range(8):
                                kb.I("pe", "matmul", [mixT, woutb], [p], p[:], lhsT=mixT[:, k, t * 128:(t + 1) * 128],
                                     rhs=woutb[:, k, hf * 512:(hf + 1) * 512], start=(k == 0), stop=(k == 7), inc=(k == 7))
                            kb.I("dve", "scalar_tensor_tensor", [ht, p], [a], out=a[:, hf * 512:(hf + 1) * 512], in0=ht[:, hf * 512:(hf + 1) * 512],
                                 scalar=ALPHA, in1=p[:], op0=ALU.mult, op1=ALU.add)
                        layer_norm_tile(kb, a, g_bc, b_bc, outt[t % 2], st6, mv)
                        kb.dma("sp", out_d[r0 + t * 128: r0 + (t + 1) * 128, :], outt[t % 2][:])


def prep_even(inp, i):
    f = lambda a: np.ascontiguousarray(a, dtype=np.float32)
    pc = lambda v, n: f(v.reshape(n, 128).T)
    vec = np.zeros((128, 40), np.float32)
    cwt = inp["even_conv_w"][i]
    for j in range(4):
        vec[:, j * 4:(j + 1) * 4] = pc(cwt[j], 4)
    vec[:, 16:20] = pc(inp["even_conv_b"][i], 4)
    vec[:, 20:24] = pc(inp["even_rg_ba"][i], 4)
    vec[:, 24:28] = pc(inp["even_rg_bx"][i], 4)
    vec[:, 28:32] = pc(inp["even_rg_lambda"][i], 4)
    vec[:, 32:34] = pc(inp["even_kv_norm"][i], 2)
    rgw = np.zeros((128, 8, 128), np.float32)
    for g, nm in enumerate(("even_rg_wa", "even_rg_wx")):
        w = inp[nm][i]
        for n in range(8):
            o = (n % 2) * 64
            rgw[o:o + 64, g * 4 + n // 2, o:o + 64] = w[n]
    return {
        "ev_win": f(inp["even_w_in"][i]), "ev_wout": f(inp["even_w_out"][i]),
        "ev_wuk": f(inp["even_w_uk"][i]), "ev_wuv": f(inp["even_w_uv"][i]),
        "ev_rgw": f(rgw.reshape(128, 1024)), "ev_vec": vec,
        "ev_lng": f(inp["ln_g"][2 * i, 0]), "ev_lnb": f(inp["ln_b"][2 * i, 0]),
    }


def build_gdn(kb, c, h_d, gw_d, wab_d, gconv_d, alog_d, dtb_d, onorm_d, wout_d, lng_d, lnb_d, out_d, o_d, nseq=SPC, stop=0):
    with kb.scope():
        g_bc = kb.sb("g_bc", [128, D])
        b_bc = kb.sb("b_bc", [128, D])
        kb.dma("sp", g_bc[:], lng_d.partition_broadcast(128))
        kb.dma("sp", b_bc[:], lnb_d.partition_broadcast(128))
        wabb = kb.sb("wabb", [128, 8, 32], BF16)
        gconv = kb.sb("gconv", [128, 8, 4, 4])
        nA = kb.sb("nA", [128, 16])
        dtb = kb.sb("dtb", [128, 16])
        onorm = kb.sb("onorm", [128, 1])
        eps6 = kb.sb("eps6", [128, 1])
        kb.I("pool", "memset", [], [eps6], eps6[:], 1e-6)
        kb.dma("sp", gconv[:], gconv_d)
        kb.dma("sp", nA[:], alog_d.partition_broadcast(128))
        kb.dma("sp", dtb[:], dtb_d.partition_broadcast(128))
        kb.dma("sp", onorm[:], onorm_d.rearrange("(p o) -> p o", o=1))
        kb.I("act", "activation", [nA], [nA], out=nA[:], in_=nA[:], func=AF.Exp)
        kb.I("dve", "tensor_scalar", [nA], [nA], out=nA[:], in0=nA[:], scalar1=-1.0, scalar2=None, op0=ALU.mult)
        ut = kb.sb("ut", [128, 128])
        slt = kb.sb("slt", [128, 128])
        lmask = kb.sb("lmask", [128, 128])
        smask = kb.sb("smask", [128, 128])
        kb.I("dve", "tensor_tensor", [c["sut"], c["ident"]], [ut], out=ut[:], in0=c["sut"][:], in1=c["ident"][:], op=ALU.add)
        kb.I("dve", "tensor_scalar", [ut], [slt], out=slt[:], in0=ut[:], scalar1=-1.0, scalar2=1.0, op0=ALU.mult, op1=ALU.add)
        kb.I("dve", "tensor_copy", [slt], [smask], out=smask[:], in_=slt[:])
        kb.I("dve", "tensor_tensor", [slt, c["ident"]], [lmask], out=lmask[:], in0=slt[:], in1=c["ident"][:], op=ALU.add)
        with kb.scope():
            st = kb.sb("abstg", [128, 8, 32])
            kb.dma("sp", st[:], wab_d.rearrange("(k p) e -> p k e", p=128))
            kb.I("dve", "tensor_copy", [st], [wabb], out=wabb[:], in_=st[:])

        for s in range(nseq):
            r0 = s * TP
            with kb.scope():
                hT = kb.sb("hT", [128, 8, TP], BF16)
                with kb.scope():
                    hts = [kb.sb("ght%d" % i, [128, D]) for i in range(2)]
                    tp = [kb.ps("gtp%d" % i, [128, 8, 128]) for i in range(2)]
                    for t in range(NT):
                        ht = hts[t % 2]
                        kb.dma("sp", ht[:], h_d[r0 + t * 128: r0 + (t + 1) * 128, :])
                        tpp = tp[t % 2]
                        for k in range(8):
                            kb.I("pe", "transpose", [ht, c["ident"]], [tpp], out=tpp[:, k, :],
                                 in_=ht[:, k * 128:(k + 1) * 128], identity=c["ident"][:], inc=(k == 7))
                        kb.copy("act" if t % 2 else "dve", [tpp], [hT], hT[:, :, t * 128:(t + 1) * 128], tpp[:])
                if stop == 1:
                    return
                S3 = lambda nm: kb.sb(nm, [128, NT, 16])
                gg, beta, gc, egc, ekd, egl, bege = S3("gg"), S3("beta"), S3("gc"), S3("egc"), S3("ekd"), S3("egl"), S3("bege")
                with kb.scope():
                    abp = [kb.ps("abp%d" % i, [128, 32]) for i in range(2)]
                    ab = kb.sb("ab", [128, NT, 32])
                    tmp = S3("tmpa")
                    for t in range(NT):
                        p = abp[t % 2]
                        for k in range(8):
                            kb.I("pe", "matmul", [hT, wabb], [p], p[:], lhsT=hT[:, k, t * 128:(t + 1) * 128], rhs=wabb[:, k, :],
                                 start=(k == 0), stop=(k == 7), inc=(k == 7))
                        kb.copy("act", [p], [ab], ab[:, t, :], p[:])
                    bcT = lambda a: a[:].unsqueeze(1).to_broadcast([128, NT, 16])
                    kb.I("dve", "tensor_tensor", [ab, dtb], [gg], out=gg[:], in0=ab[:, :, 0:16], in1=bcT(dtb), op=ALU.add)
                    kb.I("dve", "tensor_scalar", [gg], [tmp], out=tmp[:], in0=gg[:], scalar1=-1.0, scalar2=None, op0=ALU.mult)
                    kb.I("dve", "tensor_tensor", [gg, tmp], [tmp], out=tmp[:], in0=gg[:], in1=tmp[:], op=ALU.max)
                    kb.I("act", "activation", [tmp], [tmp], out=tmp[:], in_=tmp[:], func=AF.Exp, scale=-1.0)
                    kb.I("act", "activation", [tmp], [tmp], out=tmp[:], in_=tmp[:], func=AF.Ln, bias=1.0)
                    kb.I("dve", "tensor_scalar", [gg], [gg], out=gg[:], in0=gg[:], scalar1=0.0, scalar2=None, op0=ALU.max)
                    kb.I("dve", "tensor_tensor", [gg, tmp], [gg], out=gg[:], in0=gg[:], in1=tmp[:], op=ALU.add)
                    kb.I("dve", "tensor_tensor", [gg, nA], [gg], out=gg[:], in0=gg[:], in1=bcT(nA), op=ALU.mult)
                    kb.I("act", "activation", [ab], [beta], out=beta[:], in_=ab[:, :, 16:32], func=AF.Sigmoid)
                    for t in range(NT):
                        p = abp[t % 2]
                        kb.I("pe", "matmul", [ut, gg], [p], p[:, 0:16], lhsT=ut[:], rhs=gg[:, t, :], start=True, stop=True, inc=False)
                        kb.I("pe", "matmul", [c["ones"], gg], [p], p[:, 16:32], lhsT=c["ones"][:], rhs=gg[:, t, :], start=True, stop=True)
                        kb.copy("act", [p], [gc], gc[:, t, :], p[:, 0:16])
                        kb.copy("dve", [p], [egl], egl[:, t, :], p[:, 16:32])
                    kb.I("dve", "tensor_tensor", [egl, gc], [ekd], out=ekd[:], in0=egl[:], in1=gc[:], op=ALU.subtract)
                    kb.I("act", "activation", [ekd], [ekd], out=ekd[:], in_=ekd[:], func=AF.Exp)
                    kb.I("act", "activation", [egl], [egl], out=egl[:], in_=egl[:], func=AF.Exp)
                    kb.I("act", "activation", [gc], [egc], out=egc[:], in_=gc[:], func=AF.Exp)
                    kb.I("dve", "tensor_tensor", [beta, egc], [bege], out=bege[:], in0=beta[:], in1=egc[:], op=ALU.mult)

                if stop == 2:
                    return
                wsls = [kb.sb("wsl0", [128, 8, 768], BF16)] * 2
                gstg = [kb.sb("gstg%d" % i, [128, 768]) for i in range(2)]

                def load_wsl(kh_):
                    w_ = wsls[kh_ % 2]
                    for k in range(8):
                        kb.dma("sp", gstg[k % 2][:], gw_d[kh_, k * 128:(k + 1) * 128, :])
                        kb.copy("act" if k % 2 else "pool", [gstg[k % 2]], [w_], w_[:, k, :], gstg[k % 2][:])
                load_wsl(0)
                for kh in range(8):
                    with kb.scope():
                        wsl = wsls[kh % 2]
                        qkT = kb.sb("qkT", [128, 2, TP], BF16)
                        zsT = kb.sb("zsT", [128, 2, TP], BF16)
                        ktok = kb.sb("ktok", [128, NT, 128], BF16)
                        vtok = kb.sb("vtok", [128, NT, 256], BF16)
                        oTk = kb.sb("oTk", [128, 2, TP], BF16)
                        with kb.scope():
                            vT = kb.sb("vT", [128, 2, TP], BF16)
                            raws = [kb.sb("raw%d" % i, [128, TP]) for i in range(2)]
                            cvs = [kb.sb("cv%d" % i, [128, TP]) for i in range(2)]
                            sqs = [kb.sb("sq%d" % i, [128, 512], BF16) for i in range(3)]
                            rstds = [kb.sb("rstd%d" % i, [128, 512]) for i in range(3)]
                            ps2 = [kb.ps("gpp%d" % i, [128, 512]) for i in range(2)]
                            ps3 = [kb.ps("gpq%d" % i, [128, 512]) for i in range(2)]
                            tps = [kb.ps("gtq%d" % i, [128, 8, 128], BF16) for i in range(2)]
                            cnt = [0]
                            ti = 0
                            def do_proj(fi):
                                raw = raws[fi % 2]
                                def ev(n_, p, t0, n, fi=fi, raw=raw):
                                    if fi < 4:
                                        kb.copy("act", [p], [raw], raw[:, t0:t0 + n], p)
                                    else:
                                        kb.I("act", "activation", [p], [zsT], out=zsT[:, fi - 4, t0:t0 + n], in_=p, func=AF.Silu)
                                proj_fm(kb, ps2, cnt, wsl, fi * 128, hT, None, ev)

                            def do_post(fi):
                                nonlocal ti
                                raw, cv = raws[fi % 2], cvs[fi % 2]
                                cwc = lambda j: gconv[:, kh, fi, j:j + 1]
                                kb.I("dve", "tensor_scalar", [raw, gconv], [cv], out=cv[:], in0=raw[:], scalar1=cwc(3), scalar2=None, op0=ALU.mult)
                                for d in (1, 2, 3):
                                    kb.I("dve", "scalar_tensor_tensor", [raw, gconv, cv], [cv], out=cv[:, d:TP], in0=raw[:, 0:TP - d],
                                         scalar=cwc(3 - d), in1=cv[:, d:TP], op0=ALU.mult, op1=ALU.add)
                                if fi >= 2:
                                    kb.I("act", "activation", [cv], [vT], out=vT[:, fi - 2, :], in_=cv[:], func=AF.Silu)
                                    for j0 in range(0, NT, 8):
                                        j1 = min(j0 + 8, NT)
                                        tpp = tps[ti % 2]
                                        ti += 1
                                        for j in range(j0, j1):
                                            kb.I("pe", "transpose", [vT, c["identb"]], [tpp], out=tpp[:, j - j0, :],
                                                 in_=vT[:, fi - 2, j * 128:(j + 1) * 128], identity=c["identb"][:], inc=(j == j1 - 1))
                                        kb.copy("dve", [tpp], [vtok], vtok[:, j0:j1, (fi - 2) * 128:(fi - 1) * 128], tpp[:, 0:j1 - j0, :])
                                    return
                                kb.I("act", "activation", [cv], [cv], out=cv[:], in_=cv[:], func=AF.Silu)
                                for ci_, (t0, n) in enumerate(CHUNKS):
                                    sq, rstd = sqs[ci_ % 3], rstds[ci_ % 3]
                                    kb.I("pool", "tensor_tensor", [cv], [sq], out=sq[:, 0:n], in0=cv[:, t0:t0 + n], in1=cv[:, t0:t0 + n], op=ALU.mult)
                                    p = ps3[ci_ % 2]
                                    kb.I("pe", "matmul", [c["onesb"], sq], [p], p[:, 0:n], lhsT=c["onesb"][:], rhs=sq[:, 0:n], start=True, stop=True)
                                    kb.I("act", "activation", [p], [rstd], out=rstd[:, 0:n], in_=p[:, 0:n], func=AF.Ln, bias=eps6[:, 0:1])
                                    kb.I("act", "activation", [rstd], [rstd], out=rstd[:, 0:n], in_=rstd[:, 0:n], func=AF.Exp, scale=-0.5)
                                    kb.I("dve", "scalar_tensor_tensor", [cv, rstd], [qkT], out=qkT[:, fi, t0:t0 + n], in0=cv[:, t0:t0 + n],
                                         scalar=(128.0 ** -0.5 if fi == 0 else 1.0), in1=rstd[:, 0:n], op0=ALU.mult, op1=ALU.mult)
                                if fi == 1:
                                    for j0 in range(0, NT, 8):
                                        j1 = min(j0 + 8, NT)
                                        tpp = tps[ti % 2]
                                        ti += 1
                                        for j in range(j0, j1):
                                            kb.I("pe", "transpose", [qkT, c["identb"]], [tpp], out=tpp[:, j - j0, :],
                                                 in_=qkT[:, 1, j * 128:(j + 1) * 128], identity=c["identb"][:], inc=(j == j1 - 1))
                                        kb.copy("dve", [tpp], [ktok], ktok[:, j0:j1, :], tpp[:, 0:j1 - j0, :])

                            do_proj(0)
                            for fi in range(6):
                                if fi + 1 < 6:
                                    do_proj(fi + 1)
                                if fi < 4:
                                    do_post(fi)
                        if stop == 3:
                            return
                        if kh + 1 < 8:
                            load_wsl(kh + 1)
                        with kb.scope():
                            B = lambda nm: kb.sb(nm, [128, 128], BF16)
                            Fp = lambda nm: kb.sb(nm, [128, 128])
                            otok = [kb.sb("otok%d" % j, [128, NT, 128]) for j in range(2)]
                            us_all = [kb.sb("us_all%d" % j, [128, NT, 128]) for j in range(2)]
                            wT_all = [kb.sb("wT_all%d" % j, [128, NT, 128], BF16) for j in range(2)]
                            aT_all = [kb.sb("aT_all%d" % j, [128, NT, 128], BF16) for j in range(2)]
                            kd_all = [kb.sb("kd_all%d" % j, [128, NT, 128], BF16) for j in range(2)]
                            NCH = 6
                            X = [kb.ps("pX%d" % i, [128, 4, 128]) for i in range(NCH)]
                            TPb = kb.ps("TPb", [128, 8, 128], BF16)
                            CH = []
                            for ci in range(NCH):
                                CH.append(dict(
                                    Rm=Fp("Rm%d" % ci), Dm=Fp("Dm%d" % ci), attn=B("attn%d" % ci),
                                    MNA=[kb.sb("MNA%d_0" % ci, [128, 3, 128], BF16), kb.sb("MNA%d_1" % ci, [128, 3, 128], BF16)],
                                    vb=B("vb%d" % ci), kbg=B("kbg%d" % ci), X=X[ci]))
                            GsAs = [(Fp("Gs%d" % i), Fp("As%d" % i)) for i in range(NCH // 2)]
                            for j in range(2):
                                hd = 2 * kh + j
                                kb.I("pool", "tensor_tensor", [ktok, ekd], [kd_all[j]], out=kd_all[j][:], in0=ktok[:],
                                     in1=ekd[:, :, hd:hd + 1].to_broadcast([128, NT, 128]), op=ALU.mult)
                            Ssb = [Fp("S0"), Fp("S1")]
                            Sbf = [B("Sb0"), B("Sb1")]
                            vnew = [B("vnew0"), B("vnew1")]
                            o1 = [Fp("o1_0"), Fp("o1_1")]
                            P2b = kb.ps("pP2", [128, 4, 128])
                            for j in range(2):
                                kb.I("pool", "memset", [], [Ssb[j]], Ssb[j][:], 0.0)
                                kb.I("pool", "memset", [], [Sbf[j]], Sbf[j][:], 0.0)

                            def phase2(tlist):
                                for t in tlist:
                                    ts_ = slice(t * 128, (t + 1) * 128)
                                    for j in range(2):
                                        W = P2b
                                        kb.I("pe", "matmul", [wT_all[j], Sbf[j]], [W], W[:, 2 * j, :], lhsT=wT_all[j][:, t, :], rhs=Sbf[j][:], start=True, stop=True, inc=False)
                                        kb.I("pe", "matmul", [qkT, Sbf[j]], [W], W[:, 2 * j + 1, :], lhsT=qkT[:, 0, ts_], rhs=Sbf[j][:], start=True, stop=True)
                                    yield
                                    for j in range(2):
                                        W = P2b
                                        hd = 2 * kh + j
                                        kb.I("dve", "tensor_tensor", [us_all[j], W], [vnew[j]], out=vnew[j][:], in0=us_all[j][:, t, :], in1=W[:, 2 * j, :], op=ALU.subtract)
                                        kb.I("dve", "tensor_scalar", [W, egc], [o1[j]], out=o1[j][:], in0=W[:, 2 * j + 1, :], scalar1=egc[:, t, hd:hd + 1], scalar2=None, op0=ALU.mult)
                                    yield
                                    for j in range(2):
                                        V_ = P2b
                                        kb.I("pe", "matmul", [aT_all[j], vnew[j]], [V_], V_[:, 2 * j, :], lhsT=aT_all[j][:, t, :], rhs=vnew[j][:], start=True, stop=True, inc=False)
                                        kb.I("pe", "matmul", [kd_all[j], vnew[j]], [V_], V_[:, 2 * j + 1, :], lhsT=kd_all[j][:, t, :], rhs=vnew[j][:], start=True, stop=True)
                                    yield
                                    for j in range(2):
                                        V_ = P2b
                                        hd = 2 * kh + j
                                        kb.I("dve", "scalar_tensor_tensor", [Ssb[j], egl, V_], [Sbf[j]], out=Sbf[j][:], in0=Ssb[j][:], scalar=egl[:, t, hd:hd + 1],
                                             in1=V_[:, 2 * j + 1, :], op0=ALU.mult, op1=ALU.add)
                                        kb.I("dve", "scalar_tensor_tensor", [Ssb[j], egl, V_], [Ssb[j]], out=Ssb[j][:], in0=Ssb[j][:], scalar=egl[:, t, hd:hd + 1],
                                             in1=V_[:, 2 * j + 1, :], op0=ALU.mult, op1=ALU.add)
                                        kb.I("dve", "tensor_tensor", [o1[j], V_], [otok[j]], out=otok[j][:, t, :], in0=o1[j][:], in1=V_[:, 2 * j, :], op=ALU.add)
                                    yield
                            gen2 = iter(())
                            for t0 in range(0, NT, NCH // 2):
                                tl = [t for t in range(t0, t0 + NCH // 2) if t < NT]
                                chains = []
                                for ti_, t in enumerate(tl):
                                    ts_ = slice(t * 128, (t + 1) * 128)
                                    Gs, As = GsAs[ti_]
                                    XG, XA = X[2 * ti_], X[2 * ti_ + 1]
                                    kb.I("pe", "matmul", [qkT], [XG], XG[:, 3, :], lhsT=qkT[:, 1, ts_], rhs=qkT[:, 1, ts_], start=True, stop=True)
                                    kb.I("pe", "matmul", [qkT], [XA], XA[:, 3, :], lhsT=qkT[:, 0, ts_], rhs=qkT[:, 1, ts_], start=True, stop=True)
                                    kb.I("dve", "tensor_tensor", [XG, smask], [Gs], out=Gs[:], in0=XG[:, 3, :], in1=smask[:], op=ALU.mult)
                                    kb.I("dve", "tensor_tensor", [XA, lmask], [As], out=As[:], in0=XA[:, 3, :], in1=lmask[:], op=ALU.mult)
                                    for j in range(2):
                                        chn = dict(CH[ti_ * 2 + j])
                                        chn.update(t=t, j=j, hd=2 * kh + j, Gs=Gs, As=As, slot=ti_ * 2 + j)
                                        chains.append(chn)
                                col = lambda a, q: a[:, q["t"], q["hd"]:q["hd"] + 1]
                                for q in chains:
                                    kb.I("act", "mul", [ut, gg], [q["Rm"]], out=q["Rm"][:], in_=ut[:], mul=col(gg, q))
                                for q in chains:
                                    kb.I("pe", "matmul", [q["Rm"], slt], [q["X"]], q["X"][:, 0, :], lhsT=q["Rm"][:], rhs=slt[:], start=True, stop=True)
                                for q in chains:
                                    kb.I("act", "activation", [q["X"]], [q["Dm"]], out=q["Dm"][:], in_=q["X"][:, 0, :], func=AF.Exp)
                                for q in chains:
                                    kb.I("dve", "scalar_tensor_tensor", [q["Gs"], beta, q["Dm"]], [q["MNA"][0]], out=q["MNA"][0][:, 0, :], in0=q["Gs"][:],
                                         scalar=col(beta, q), in1=q["Dm"][:], op0=ALU.mult, op1=ALU.mult)
                                    kb.I("pool", "tensor_tensor", [q["As"], q["Dm"]], [q["attn"]], out=q["attn"][:], in0=q["As"][:], in1=q["Dm"][:], op=ALU.mult)
                                    kb.copy("act", [c["identb"]], [q["MNA"][0]], q["MNA"][0][:, 2, :], c["identb"][:])
                                for rr0 in range(0, len(chains), 4):
                                    for q in chains[rr0:rr0 + 4]:
                                        sl = q["slot"] - rr0
                                        kb.I("pe", "transpose", [q["MNA"][0], c["identb"]], [TPb], out=TPb[:, 2 * sl, :], in_=q["MNA"][0][:, 0, :], identity=c["identb"][:], inc=False)
                                        kb.I("pe", "transpose", [q["attn"], c["identb"]], [TPb], out=TPb[:, 2 * sl + 1, :], in_=q["attn"][:], identity=c["identb"][:])
                                    for q in chains[rr0:rr0 + 4]:
                                        sl = q["slot"] - rr0
                                        kb.copy("act", [TPb], [q["MNA"][0]], q["MNA"][0][:, 1, :], TPb[:, 2 * sl, :])
                                        kb.copy("act", [TPb], [aT_all[q["j"]]], aT_all[q["j"]][:, q["t"], :], TPb[:, 2 * sl + 1, :])
                                cur = 0
                                for st_ in range(7):
                                    nxt = 1 - cur
                                    next(gen2, None)
                                    for q in chains:
                                        Xq, T_ = q["X"], q["MNA"][cur]
                                        if st_ < 6:
                                            kb.I("pe", "matmul", [T_], [Xq], Xq[:, 0, :], lhsT=T_[:, 1, :], rhs=T_[:, 0, :], start=True, stop=True, inc=False)
                                            kb.I("pe", "matmul", [T_], [Xq], Xq[:, 1:3, :], lhsT=T_[:, 0, :], rhs=T_[:, 1:3, :], start=True, stop=True)
                                        else:
                                            kb.I("pe", "matmul", [T_], [Xq], Xq[:, 2, :], lhsT=T_[:, 0, :], rhs=T_[:, 2, :], start=True, stop=True)
                                    next(gen2, None)
                                    for q in chains:
                                        Xq, T_, Tn = q["X"], q["MNA"][cur], q["MNA"][nxt]
                                        if st_ < 6:
                                            kb.copy("act", [Xq], [Tn], Tn[:, 0:2, :], Xq[:, 0:2, :])
                                        kb.I("dve", "tensor_tensor", [T_, Xq], [Tn], out=Tn[:, 2, :], in0=T_[:, 2, :], in1=Xq[:, 2, :],
                                             op=(ALU.subtract if st_ == 0 else ALU.add))
                                    cur = nxt
                                for q in chains:
                                    j, t = q["j"], q["t"]
                                    kb.I("act", "mul", [vtok, beta], [q["vb"]], out=q["vb"][:], in_=vtok[:, t, j * 128:(j + 1) * 128], mul=col(beta, q))
                                    kb.I("pool", "tensor_scalar", [ktok, bege], [q["kbg"]], out=q["kbg"][:], in0=ktok[:, t, :], scalar1=col(bege, q),
                                         scalar2=None, op0=ALU.mult)
                                for q in chains:
                                    TT = q["MNA"][cur][:, 2, :]
                                    kb.I("pe", "matmul", [q["MNA"][cur], q["vb"]], [q["X"]], q["X"][:, 0, :], lhsT=TT, rhs=q["vb"][:], start=True, stop=True, inc=False)
                                    kb.I("pe", "matmul", [q["kbg"], q["MNA"][cur]], [q["X"]], q["X"][:, 1, :], lhsT=q["kbg"][:], rhs=TT, start=True, stop=True)
                                for q in chains:
                                    j, t = q["j"], q["t"]
                                    kb.copy("act", [q["X"]], [us_all[j]], us_all[j][:, t, :], q["X"][:, 0, :])
                                    kb.copy("dve", [q["X"]], [wT_all[j]], wT_all[j][:, t, :], q["X"][:, 1, :])
                                for _ in gen2:
                                    pass
                                gen2 = phase2(tl)
                            for _ in gen2:
                                pass
                            if stop == 4:
                                return
                            with kb.scope():
                                sqo = us_all[0]
                                ssq = kb.sb("ssq", [128, NT])
                                onb = wT_all[0]
                                tpo = [TPb, TPb]
                                ti = 0
                                for j in range(2):
                                    kb.I("pool", "tensor_tensor", [otok[j]], [sqo], out=sqo[:], in0=otok[j][:], in1=otok[j][:], op=ALU.mult)
                                    kb.I("dve", "tensor_reduce", [sqo], [ssq], out=ssq[:], in_=sqo[:], axis=AX.X, op=ALU.add)
                                    kb.I("dve", "tensor_scalar", [ssq], [ssq], out=ssq[:], in0=ssq[:], scalar1=1.0 / 128, scalar2=1e-6, op0=ALU.mult, op1=ALU.add)
                                    kb.I("act", "sqrt", [ssq], [ssq], out=ssq[:], in_=ssq[:])
                                    kb.I("dve", "reciprocal", [ssq], [ssq], out=ssq[:], in_=ssq[:])
                                    kb.I("dve", "tensor_tensor", [otok[j], ssq], [onb], out=onb[:], in0=otok[j][:],
                                         in1=ssq[:].unsqueeze(2).to_broadcast([128, NT, 128]), op=ALU.mult)
                                    for j0 in range(0, NT, 8):
                                        j1 = min(j0 + 8, NT)
                                        tpp = tpo[ti % 2]
                                        ti += 1
                                        for jj in range(j0, j1):
                                            kb.I("pe", "transpose", [onb, c["identb"]], [tpp], out=tpp[:, jj - j0, :], in_=onb[:, jj, :], identity=c["identb"][:], inc=(jj == j1 - 1))
                                        kb.I("dve", "scalar_tensor_tensor", [tpp, onorm, zsT], [oTk], out=oTk[:, j, j0 * 128:j1 * 128],
                                             in0=tpp[:, 0:j1 - j0, :].rearrange("p a b -> p (a b)"), scalar=onorm[:, 0:1], in1=zsT[:, j, j0 * 128:j1 * 128],
                                             op0=ALU.mult, op1=ALU.mult)
                                for j in range(2):
                                    kb.dma("sp", o_d[s, 2 * kh + j, :, :], oTk[:, j, :])
                if stop == 5:
                    return
                with kb.scope():
                    woutb = kb.sb("gwoutb", [128, 16, D], BF16)
                    stg = [kb.sb("gostg%d" % i, [128, D]) for i in range(2)]
                    for k in range(16):
                        kb.dma("sp", stg[k % 2][:], wout_d[k * 128:(k + 1) * 128, :])
                        kb.copy("act" if k % 2 else "pool", [stg[k % 2]], [woutb], woutb[:, k, :], stg[k % 2][:])
                    oTt = [kb.sb("oTt%d" % i, [128, 16, 128], BF16) for i in range(2)]
                    hts = [kb.sb("goht%d" % i, [128, D]) for i in range(2)]
                    acc = [kb.sb("goacc%d" % i, [128, D]) for i in range(2)]
                    outt = [kb.sb("goout%d" % i, [128, D]) for i in range(2)]
                    st6 = kb.sb("st6", [128, 2, 6])
                    mv = kb.sb("mv", [128, 4])
                    ops2 = [kb.ps("gopp%d" % i, [128, 512]) for i in range(4)]
                    for t in range(NT):
                        ht = hts[t % 2]
                        a = acc[t % 2]
                        ot = oTt[t % 2]
                        kb.dma("sp", ht[:], h_d[r0 + t * 128: r0 + (t + 1) * 128, :])
                        for hq in range(4):
                            kb.dma("act", ot[:, hq * 4:(hq + 1) * 4, :], o_d[s, hq * 4:(hq + 1) * 4, :, t * 128:(t + 1) * 128].rearrange("h p t -> p h t"),
                                   writes=[ot], group="oTt%d" % (t % 2))
                        for hf in range(2):
                            p = ops2[(2 * t + hf) % 4]
                            for k in range(16):
                                kb.I("pe", "matmul", [ot, woutb], [p], p[:], lhsT=ot[:, k, :], rhs=woutb[:, k, hf * 512:(hf + 1) * 512],
                                     start=(k == 0), stop=(k == 15), inc=(k == 15))
                            kb.I("dve", "scalar_tensor_tensor", [ht, p], [a], out=a[:, hf * 512:(hf + 1) * 512], in0=ht[:, hf * 512:(hf + 1) * 512],
                                 scalar=ALPHA, in1=p[:], op0=ALU.mult, op1=ALU.add)
                        layer_norm_tile(kb, a, g_bc, b_bc, outt[t % 2], st6, mv)
                        kb.dma("sp", out_d[r0 + t * 128: r0 + (t + 1) * 128, :], outt[t % 2][:])


def prep_gdn(inp, i):
    f = lambda a: np.ascontiguousarray(a, dtype=np.float32)
    w = inp["odd_w_in"][i]
    cwt = inp["odd_conv_w"][i]
    gw = np.zeros((8, 1024, 768), np.float32)
    gconv = np.zeros((128, 8, 4, 4), np.float32)
    for kh in range(8):
        cols = [np.arange(kh * 128, kh * 128 + 128), 1024 + np.arange(kh * 128, kh * 128 + 128),
                2048 + np.arange(2 * kh * 128, 2 * kh * 128 + 256)]
        zc = 4096 + np.arange(2 * kh * 128, 2 * kh * 128 + 256)
        gw[kh] = w[:, np.concatenate(cols + [zc])]
        cc_ = np.concatenate(cols)
        for fi in range(4):
            gconv[:, kh, fi, :] = cwt[:, cc_[fi * 128:(fi + 1) * 128]].T
    return {
        "gd_w": gw, "gd_wab": f(w[:, 6144:6176]), "gd_conv": gconv.reshape(128, 128).reshape(128, 8, 4, 4),
        "gd_alog": f(inp["odd_a_log"][i]), "gd_dtb": f(inp["odd_dt_bias"][i]), "gd_onorm": f(inp["odd_o_norm"][i]),
        "gd_wout": f(inp["odd_w_out"][i]), "gd_lng": f(inp["ln_g"][2 * i + 1, 0]), "gd_lnb": f(inp["ln_b"][2 * i + 1, 0]),
    }


def perm_expert(w, nk):
    E, R, F_ = w.shape
    return np.ascontiguousarray(w.reshape(E, nk, 128, F_).transpose(0, 2, 1, 3).reshape(E * 128, nk * F_), dtype=np.float32)


def prep_moe(inp, layer, pfx):
    f = lambda a: np.ascontiguousarray(a, dtype=np.float32)
    return {
        pfx + "wr": f(np.concatenate([inp["moe_group_w"][layer], inp["moe_expert_w"][layer]], axis=1)),
        pfx + "br": f(np.concatenate([inp["moe_group_b"][layer], inp["moe_expert_b"][layer]], axis=0)),
        pfx + "wg": perm_expert(inp["moe_w_gate"][layer], 8), pfx + "wu": perm_expert(inp["moe_w_up"][layer], 8),
        pfx + "wd": perm_expert(inp["moe_w_down"][layer], 4),
        pfx + "lng": f(inp["ln_g"][layer, 1]), pfx + "lnb": f(inp["ln_b"][layer, 1]),
    }


def build_program(shapes):
    nc = bass.Bass("TRN2", target_bir_lowering=False)
    kb = KB(nc)
    dd = {}
    for n, (shp, dt) in shapes.items():
        dd[n] = nc.dram_tensor(n, list(shp), dt, kind="ExternalInput").ap()
    out_d = nc.dram_tensor("out", [NTOK, D], F32, kind="ExternalOutput").ap()
    h1_d = kb.dram("h1", [NTOK, D])
    h2_d = kb.dram("h2", [NTOK, D])
    h3_d = kb.dram("h3", [NTOK, D])
    xs_d = kb.dram("xs", [NSLOT, D], BF16)
    ys_d = kb.dram("ys", [NSLOT, D], F32)
    o_d = kb.dram("o_scr", [SPC, 16, 128, TP], BF16)
    c = load_consts(kb, {k[2:]: v for k, v in dd.items() if k.startswith("c_")})
    build_even(kb, c, dd["h0"], dd["ev_win"], dd["ev_wout"], dd["ev_wuk"], dd["ev_wuv"], dd["ev_rgw"], dd["ev_vec"],
               dd["ev_lng"], dd["ev_lnb"], h1_d)
    build_moe(kb, c, h1_d, dd["m0_wr"], dd["m0_br"], dd["m0_wg"], dd["m0_wu"], dd["m0_wd"], dd["m0_lng"], dd["m0_lnb"],
              h2_d, xs_d, ys_d)
    build_gdn(kb, c, h2_d, dd["gd_w"], dd["gd_wab"], dd["gd_conv"], dd["gd_alog"], dd["gd_dtb"], dd["gd_onorm"],
              dd["gd_wout"], dd["gd_lng"], dd["gd_lnb"], h3_d, o_d)
    build_moe(kb, c, h3_d, dd["m1_wr"], dd["m1_br"], dd["m1_wg"], dd["m1_wu"], dd["m1_wd"], dd["m1_lng"], dd["m1_lnb"],
              out_d, xs_d, ys_d)
    kb.finish()
    return nc


def kernel(**inputs):
    import ml_dtypes
    inp = {k: np.asarray(v) for k, v in inputs.items()}
    x = inp["x"].astype(np.float32, copy=False)
    B = x.shape[0]
    hp = np.zeros((B, TP, D), np.float32)
    hp[:, :NMETA] = inp["meta_tokens"][None]
    hp[:, NMETA:T] = x
    shared = {}
    for k, v in make_consts().items():
        shared["c_" + k] = v
    shared.update(prep_even(inp, 0))
    shared.update(prep_gdn(inp, 0))
    shared.update(prep_moe(inp, 0, "m0_"))
    shared.update(prep_moe(inp, 1, "m1_"))
    shapes = {n: (v.shape, BF16 if v.dtype == ml_dtypes.bfloat16 else F32) for n, v in shared.items()}
    shapes["h0"] = ((NTOK, D), F32)
    nc = build_program(shapes)
    in_maps = []
    for ci in range(NCORES):
        m = dict(shared)
        m["h0"] = np.ascontiguousarray(hp[ci * SPC:(ci + 1) * SPC].reshape(NTOK, D))
        in_maps.append(m)
    res = run_bass_kernel_spmd(nc, in_maps, core_ids=list(range(NCORES)))
    outs = [r["out"].reshape(SPC, TP, D)[:, NMETA:T] for r in res.results]
    return np.ascontiguousarray(np.concatenate(outs, axis=0).astype(np.float32))
```

```python
import contextlib
import numpy as np
import concourse.bass as bass
import concourse.mybir as mybir
from concourse.bass_utils import run_bass_kernel_spmd

F32 = mybir.dt.float32
BF16 = mybir.dt.bfloat16
I32 = mybir.dt.int32
AF = mybir.ActivationFunctionType
ALU = mybir.AluOpType
AX = mybir.AxisListType

NCORES = 8
D = 1024
SEQ = 2048
NMETA = 16
T = SEQ + NMETA
TP = 17 * 128
NT = TP // 128
SPC = 4
NTOK = SPC * TP
NTILE = NTOK // 128
ALPHA = 4.0 ** 0.25
NEG = -1.0e30


class Res:
    __slots__ = ("w", "r", "dsem")

    def __init__(self):
        self.w = None
        self.r = {}
        self.dsem = None


class KB:
    def __init__(self, nc):
        self.nc = nc
        self.es = contextlib.ExitStack()
        self.stack = [self.es]
        self.eng = {"pe": nc.tensor, "act": nc.scalar, "dve": nc.vector, "pool": nc.gpsimd, "sp": nc.sync}
        self.sems = {}
        self.cnt = {}
        for k in self.eng:
            self.sems[k] = self.es.enter_context(nc.semaphore("sem_" + k))
            self.cnt[k] = 0
        self.seen = {k: {} for k in self.eng}
        self.res = {}
        self.dcount = {}
        self.free_dsems = []
        self.scope_res = [[]]
        self.nd = 0
        self.n_inst = 0
        self.uid = 0
        self.psum_names = set()

    def sb(self, name, shape, dtype=F32):
        self.uid += 1
        return self.stack[-1].enter_context(self.nc.sbuf_tensor("%s_%d" % (name, self.uid), list(shape), dtype))

    def ps(self, name, shape, dtype=F32):
        self.uid += 1
        nm = "%s_%d" % (name, self.uid)
        self.psum_names.add(nm)
        return self.stack[-1].enter_context(self.nc.psum_tensor(nm, list(shape), dtype))

    def dram(self, name, shape, dtype=F32, kind="Internal"):
        return self.nc.dram_tensor(name, list(shape), dtype, kind=kind).ap()

    @contextlib.contextmanager
    def scope(self):
        st = contextlib.ExitStack()
        self.stack.append(st)
        self.scope_res.append([])
        try:
            yield
        finally:
            self.barrier()
            for key in self.scope_res.pop():
                r = self.res.pop(key, None)
                if r is not None and r.dsem is not None:
                    self.free_dsems.append(r.dsem)
            self.stack.pop()
            st.close()

    def _res(self, ap):
        key = ap if isinstance(ap, str) else ap.name
        r = self.res.get(key)
        if r is None:
            r = self.res[key] = Res()
            self.scope_res[-1].append(key)
        return r

    def _wait(self, e, semkey, val):
        if semkey in self.dcount:
            val = max(val, self.dcount[semkey])
        if self.seen[e].get(semkey, 0) >= val:
            return
        if semkey == e and val > self.cnt[e]:
            return
        self.seen[e][semkey] = val
        self.eng[e].wait_ge(self.sems[semkey], val)

    def _deps(self, e, reads, writes):
        for a in reads:
            r = self._res(a)
            if r.w is not None:
                self._wait(e, *r.w)
        for a in writes:
            r = self._res(a)
            if r.w is not None:
                self._wait(e, *r.w)
            for sk, v in r.r.items():
                self._wait(e, sk, v)

    def _done(self, ev, reads, writes):
        for a in reads:
            r = self._res(a)
            if r.r.get(ev[0], 0) < ev[1]:
                r.r[ev[0]] = ev[1]
        for a in writes:
            r = self._res(a)
            r.w = ev
            r.r = {}

    def I(self, e, fn, reads, writes, *args, inc=True, **kw):
        writes = list(writes) + [a for a in reads if (not isinstance(a, str)) and a.name in self.psum_names]
        self._deps(e, reads, writes)
        ins = getattr(self.eng[e], fn)(*args, **kw)
        if inc:
            self.cnt[e] += 1
            ins.then_inc(self.sems[e], 1)
            self._done((e, self.cnt[e]), reads, writes)
        else:
            self._done((e, self.cnt[e] + 1), reads, writes)
        self.n_inst += 1
        return ins

    def dma(self, q, out, in_, reads=None, writes=None, group=None, indirect=None, **kw):
        reads = [in_] if reads is None else reads
        writes = [out] if writes is None else writes
        dst = self._res(group if group is not None else writes[0])
        if dst.dsem is None:
            if self.free_dsems:
                dst.dsem = self.free_dsems.pop()
            else:
                self.nd += 1
                dst.dsem = "d%d" % self.nd
                self.sems[dst.dsem] = self.es.enter_context(self.nc.semaphore(dst.dsem))
                self.dcount[dst.dsem] = 0
        if group is None:
            self._deps(q, reads, writes)
        else:
            self._deps(q, reads, [])
            for a in writes:
                r = self._res(a)
                if r.w is not None and r.w[0] != dst.dsem:
                    self._wait(q, *r.w)
                for sk, v in r.r.items():
                    self._wait(q, sk, v)
        if indirect is None:
            ins = self.eng[q].dma_start(out=out, in_=in_, **kw)
        else:
            ins = self.eng[q].indirect_dma_start(out=out, in_=in_, **indirect)
        self.dcount[dst.dsem] += 16
        ins.then_inc(self.sems[dst.dsem], 16)
        self._done((dst.dsem, self.dcount[dst.dsem]), reads, writes)
        self.n_inst += 1
        return ins

    def copy(self, e, reads, writes, out, in_):
        return self.I(e, "copy" if e == "act" else "tensor_copy", reads, writes, out=out, in_=in_)

    def barrier(self):
        for e in self.eng:
            for e2 in ("pe", "act", "dve", "pool"):
                if self.cnt[e2]:
                    self._wait(e, e2, self.cnt[e2])
            for sk, c in self.dcount.items():
                if c:
                    self._wait(e, sk, c)

    def finish(self):
        self.barrier()
        self.es.close()


def load_consts(kb, cd):
    c = {}
    for name, shape, dt in (("ident", [128, 128], F32), ("identb", [128, 128], BF16),
                            ("ones", [128, 128], F32), ("sut", [128, 128], F32),
                            ("ramp", [128, 128], F32), ("pidx", [128, 128], F32), ("cmask", [128, 128], F32),
                            ("onesb", [128, 128], BF16), ("cmaskb", [128, 128], BF16)):
        t = kb.sb("c_" + name, shape, dt)
        kb.dma("sp", t[:], cd[name])
        c[name] = t
    return c


def layer_norm_tile(kb, acc, g_bc, b_bc, outt, st6, mv, eng2="pool"):
    for j in range(2):
        kb.I("dve", "bn_stats", [acc], [st6], out=st6[:, j, :], in_=acc[:, j * 512:(j + 1) * 512])
    kb.I("dve", "bn_aggr", [st6], [mv], out=mv[:, 0:2], in_=st6[:].rearrange("p a b -> p (a b)"))
    kb.I("dve", "tensor_scalar", [mv], [mv], out=mv[:, 2:3], in0=mv[:, 1:2], scalar1=1e-5, scalar2=None, op0=ALU.add)
    kb.I("act", "sqrt", [mv], [mv], out=mv[:, 2:3], in_=mv[:, 2:3])
    kb.I("dve", "reciprocal", [mv], [mv], out=mv[:, 2:3], in_=mv[:, 2:3])
    kb.I("dve", "scalar_tensor_tensor", [mv], [mv], out=mv[:, 3:4], in0=mv[:, 0:1], scalar=-1.0, in1=mv[:, 2:3], op0=ALU.mult, op1=ALU.mult)
    kb.I("act", "activation", [acc, mv], [acc], out=acc[:], in_=acc[:], func=AF.Identity, scale=mv[:, 2:3], bias=mv[:, 3:4])
    kb.I("dve", "tensor_tensor", [acc, g_bc], [acc], out=acc[:], in0=acc[:], in1=g_bc[:], op=ALU.mult)
    kb.I(eng2, "tensor_tensor", [acc, b_bc], [outt], out=outt[:], in0=acc[:], in1=b_bc[:], op=ALU.add)


MOE_BS = 4
MOE_BLK = MOE_BS * 128
NBLK = -(-NTOK * 2 // MOE_BLK) + 32
NSLOT = NBLK * MOE_BLK


def build_moe(kb, c, h_d, wr_d, br_d, wg_d, wu_d, wd_d, lng_d, lnb_d, out_d, xs_d, ys_d, ntile=NTILE):
    nc = kb.nc
    nblk = -(-ntile * 128 * 2 // MOE_BLK) + 32
    BS, BLK = MOE_BS, MOE_BLK
    with kb.scope():
        s1i = kb.sb("s1i", [128, ntile], I32)
        s2i = kb.sb("s2i", [128, ntile], I32)
        g1 = kb.sb("g1", [128, ntile])
        g2 = kb.sb("g2", [128, ntile])
        idxe = kb.sb("idxe", [128, nblk], I32)
        g_bc = kb.sb("g_bc", [128, D])
        b_bc = kb.sb("b_bc", [128, D])
        kb.dma("sp", g_bc[:], lng_d.partition_broadcast(128))
        kb.dma("sp", b_bc[:], lnb_d.partition_broadcast(128))
        hts = [kb.sb("ht%d" % i, [128, D]) for i in range(2)]

        with kb.scope():
            wr = kb.sb("wr", [128, 8, 36])
            kb.dma("sp", wr[:], wr_d.rearrange("(k p) e -> p k e", p=128))
            br = kb.sb("br", [128, 36])
            kb.dma("sp", br[:], br_d.partition_broadcast(128))
            L = kb.sb("L", [128, ntile, 36])
            hT = [kb.sb("hT%d" % i, [128, 8, 128]) for i in range(2)]
            tp = [kb.ps("tp%d" % i, [128, 8, 128]) for i in range(2)]
            lg = [kb.ps("lg%d" % i, [128, 36]) for i in range(2)]
            for t in range(ntile):
                ht = hts[t % 2]
                kb.dma("sp", ht[:], h_d[t * 128:(t + 1) * 128, :])
                tpp = tp[t % 2]
                for k in range(8):
                    kb.I("pe", "transpose", [ht, c["ident"]], [tpp], out=tpp[:, k, :], in_=ht[:, k * 128:(k + 1) * 128],
                         identity=c["ident"][:], inc=(k == 7))
                hTt = hT[t % 2]
                kb.I("act", "copy", [tpp], [hTt], out=hTt[:], in_=tpp[:])
                lgp = lg[t % 2]
                for k in range(8):
                    kb.I("pe", "matmul", [hTt, wr], [lgp], lgp[:], lhsT=hTt[:, k, :], rhs=wr[:, k, :],
                         start=(k == 0), stop=(k == 7), inc=(k == 7))
                kb.I("dve", "tensor_tensor", [lgp, br], [L], out=L[:, t, :], in0=lgp[:], in1=br[:], op=ALU.add)

            NE = ntile * 32
            GL = L[:, :, 0:4]
            EL = L[:, :, 4:36]
            gmax = kb.sb("gmax", [128, ntile])
            t4 = kb.sb("t4", [128, ntile, 4])
            goh = kb.sb("goh", [128, ntile, 4])
            gg = kb.sb("gg", [128, ntile])
            ELm = kb.sb("ELm", [128, ntile, 32])
            EL2 = kb.sb("EL2", [128, ntile, 32])
            oh1 = kb.sb("oh1", [128, ntile, 32])
            oh2 = kb.sb("oh2", [128, ntile, 32])
            m1 = kb.sb("m1", [128, ntile])
            m2 = kb.sb("m2", [128, ntile])
            tmp = kb.sb("tmp", [128, ntile])
            V = "dve"
            kb.I(V, "tensor_reduce", [L], [gmax], out=gmax[:], in_=GL, axis=AX.X, op=ALU.max)
            bc4 = lambda a: a[:].unsqueeze(2).to_broadcast([128, ntile, 4])
            bc32 = lambda a: a[:].unsqueeze(2).to_broadcast([128, ntile, 32])
            kb.I(V, "tensor_tensor", [L, gmax], [goh], out=goh[:], in0=GL, in1=bc4(gmax), op=ALU.is_equal)
            kb.I(V, "tensor_tensor", [L, gmax], [t4], out=t4[:], in0=GL, in1=bc4(gmax), op=ALU.subtract)
            kb.I("act", "activation", [t4], [t4], out=t4[:], in_=t4[:], func=AF.Exp)
            kb.I(V, "tensor_reduce", [t4], [gg], out=gg[:], in_=t4[:], axis=AX.X, op=ALU.add)
            kb.I(V, "reciprocal", [gg], [gg], out=gg[:], in_=gg[:])
            kb.I(V, "tensor_scalar", [goh], [t4], out=t4[:], in0=goh[:], scalar1=-NEG, scalar2=NEG,
                 op0=ALU.mult, op1=ALU.add)
            kb.I(V, "tensor_tensor", [L, t4], [ELm], out=ELm[:].rearrange("p t (g e) -> p t g e", g=4),
                 in0=EL.rearrange("p t (g e) -> p t g e", g=4),
                 in1=t4[:].unsqueeze(3).to_broadcast([128, ntile, 4, 8]), op=ALU.add)
            kb.I(V, "tensor_reduce", [ELm], [m1], out=m1[:], in_=ELm[:], axis=AX.X, op=ALU.max)
            kb.I(V, "tensor_tensor", [ELm, m1], [oh1], out=oh1[:], in0=ELm[:], in1=bc32(m1), op=ALU.is_equal)
            kb.I(V, "scalar_tensor_tensor", [oh1, ELm], [EL2], out=EL2[:], in0=oh1[:], scalar=NEG, in1=ELm[:],
                 op0=ALU.mult, op1=ALU.add)
            kb.I(V, "tensor_reduce", [EL2], [m2], out=m2[:], in_=EL2[:], axis=AX.X, op=ALU.max)
            kb.I(V, "tensor_tensor", [EL2, m2], [oh2], out=oh2[:], in0=EL2[:], in1=bc32(m2), op=ALU.is_equal)
            kb.I(V, "tensor_tensor", [m1, m2], [tmp], out=tmp[:], in0=m2[:], in1=m1[:], op=ALU.subtract)
            kb.I("act", "activation", [tmp], [tmp], out=tmp[:], in_=tmp[:], func=AF.Exp)
            kb.I(V, "tensor_scalar", [tmp], [m1], out=m1[:], in0=tmp[:], scalar1=1.0, scalar2=None, op0=ALU.add)
            kb.I(V, "reciprocal", [m1], [m1], out=m1[:], in_=m1[:])
            kb.I(V, "tensor_tensor", [tmp, m1], [m2], out=m2[:], in0=tmp[:], in1=m1[:], op=ALU.mult)
            kb.I(V, "tensor_tensor", [m1, gg], [g1], out=g1[:], in0=m1[:], in1=gg[:], op=ALU.mult)
            kb.I(V, "tensor_tensor", [m2, gg], [g2], out=g2[:], in0=m2[:], in1=gg[:], op=ALU.mult)
            OH = ELm
            kb.I(V, "tensor_tensor", [oh1, oh2], [OH], out=OH[:], in0=oh1[:], in1=oh2[:], op=ALU.add)
            RK = kb.sb("RK", [128, NE])
            CT = kb.sb("CT", [128, ntile, 32])
            OHf = OH[:].rearrange("p t e -> p (t e)")
            CTf = CT[:].rearrange("p t e -> p (t e)")
            pr = [kb.ps("pr%d" % i, [128, 512]) for i in range(2)]
            ci = 0
            for dst, lhs in ((RK[:], c["sut"]), (CTf, c["ones"])):
                for o in range(0, NE, 512):
                    n = min(512, NE - o)
                    p = pr[ci % 2]
                    ci += 1
                    kb.I("pe", "matmul", [lhs, OH], [p], p[:, 0:n], lhsT=lhs[:], rhs=OHf[:, o:o + n],
                         start=True, stop=True)
                    kb.I("act", "copy", [p], [RK if dst is not CTf else CT], out=dst[:, o:o + n], in_=p[:, 0:n])
            base = kb.sb("base", [128, ntile, 32])
            kb.I(V, "memset", [], [base], base[:, 0, :], 0.0)
            for t in range(1, ntile):
                kb.I(V, "tensor_tensor", [base, CT], [base], out=base[:, t, :], in0=base[:, t - 1, :],
                     in1=CT[:, t - 1, :], op=ALU.add)
            tot = kb.sb("tot", [128, 32])
            pad = kb.sb("pad", [128, 32])
            pend = kb.sb("pend", [128, 32])
            one32 = kb.sb("one32", [128, 32])
            kb.I(V, "memset", [], [one32], one32[:], 1.0)
            kb.I(V, "tensor_tensor", [base, CT], [tot], out=tot[:], in0=base[:, ntile - 1, :], in1=CT[:, ntile - 1, :],
                 op=ALU.add)
            thr = kb.sb("thr", [128, nblk])
            kb.I(V, "tensor_scalar", [c["ramp"]], [thr], out=thr[:], in0=c["ramp"][:, 0:nblk], scalar1=float(BLK),
                 scalar2=None, op0=ALU.mult)
            cmp0 = kb.sb("cmp0", [128, 32, nblk])
            kb.I(V, "tensor_tensor", [tot, thr], [cmp0], out=cmp0[:],
                 in0=tot[:].unsqueeze(2).to_broadcast([128, 32, nblk]),
                 in1=thr[:].unsqueeze(1).to_broadcast([128, 32, nblk]), op=ALU.is_gt)
            kb.I(V, "tensor_reduce", [cmp0], [pad], out=pad[:], in_=cmp0[:], axis=AX.X, op=ALU.add)
            kb.I(V, "tensor_scalar", [pad], [pad], out=pad[:], in0=pad[:], scalar1=float(BLK), scalar2=None, op0=ALU.mult)
            kb.I(V, "tensor_tensor_scan", [one32, pad], [pend], out=pend[:], data0=one32[:], data1=pad[:], initial=0.0,
                 op0=ALU.mult, op1=ALU.add)
            kb.I(V, "tensor_tensor", [pend, pad], [pad], out=pad[:], in0=pend[:], in1=pad[:], op=ALU.subtract)
            cmp = kb.sb("cmp", [128, nblk, 32])
            bef = kb.sb("bef", [128, nblk])
            kb.I(V, "tensor_scalar", [c["ramp"]], [bef], out=bef[:], in0=c["ramp"][:, 0:nblk], scalar1=float(BLK),
                 scalar2=None, op0=ALU.mult)
            kb.I(V, "tensor_tensor", [pend, bef], [cmp], out=cmp[:],
                 in0=pend[:].unsqueeze(1).to_broadcast([128, nblk, 32]),
                 in1=bef[:].unsqueeze(2).to_broadcast([128, nblk, 32]), op=ALU.is_le)
            kb.I(V, "tensor_reduce", [cmp], [bef], out=bef[:], in_=cmp[:], axis=AX.X, op=ALU.add)
            kb.I(V, "tensor_scalar", [bef], [bef], out=bef[:], in0=bef[:], scalar1=31.0, scalar2=None, op0=ALU.min)
            ig = kb.sb("ig", [128, nblk])
            kb.I(V, "tensor_scalar", [bef, c["pidx"]], [ig], out=ig[:], in0=bef[:], scalar1=128.0, scalar2=c["pidx"][:, 0:1],
                 op0=ALU.mult, op1=ALU.add)
            usedf = kb.sb("usedf", [128, nblk])
            kb.I(V, "tensor_scalar", [thr, pend], [usedf], out=usedf[:], in0=thr[:], scalar1=pend[:, 31:32], scalar2=None, op0=ALU.is_lt)
            samef = kb.sb("samef", [128, nblk])
            kb.I(V, "memset", [], [samef], samef[:], 0.0)
            kb.I(V, "tensor_tensor", [bef], [samef], out=samef[:, 1:nblk], in0=bef[:, 1:nblk], in1=bef[:, 0:nblk - 1], op=ALU.is_equal)
            kb.I(V, "tensor_scalar", [samef], [samef], out=samef[:], in0=samef[:], scalar1=-1.0, scalar2=1.0, op0=ALU.mult, op1=ALU.add)
            kb.I(V, "tensor_tensor", [usedf, samef], [usedf], out=usedf[:], in0=usedf[:], in1=samef[:], op=ALU.mult)
            kb.I(V, "tensor_tensor", [ig, usedf], [ig], out=ig[:], in0=ig[:], in1=usedf[:], op=ALU.mult)
            kb.I(V, "tensor_scalar", [usedf], [usedf], out=usedf[:], in0=usedf[:], scalar1=-8192.0, scalar2=8192.0, op0=ALU.mult, op1=ALU.add)
            kb.I(V, "tensor_tensor", [ig, usedf], [ig], out=ig[:], in0=ig[:], in1=usedf[:], op=ALU.add)
            kb.I(V, "tensor_copy", [ig], [idxe], out=idxe[:], in_=ig[:])
            SL = EL2
            RK3 = RK[:].rearrange("p (t e) -> p t e", e=32)
            kb.I(V, "tensor_tensor", [RK, base], [SL], out=SL[:], in0=RK3, in1=base[:], op=ALU.add)
            kb.I(V, "tensor_tensor", [SL, pad], [SL], out=SL[:], in0=SL[:],
                 in1=pad[:].unsqueeze(1).to_broadcast([128, ntile, 32]), op=ALU.add)
            for oh, si in ((oh1, s1i), (oh2, s2i)):
                kb.I(V, "tensor_tensor", [SL, oh], [oh], out=oh[:], in0=SL[:], in1=oh[:], op=ALU.mult)
                kb.I(V, "tensor_reduce", [oh], [tmp], out=tmp[:], in_=oh[:], axis=AX.X, op=ALU.add)
                kb.I(V, "tensor_copy", [tmp], [si], out=si[:], in_=tmp[:])

        with kb.scope():
            hbs = [kb.sb("hb%d" % i, [128, D], BF16) for i in range(2)]
            for t in range(ntile):
                ht = hts[t % 2]
                hb = hbs[t % 2]
                kb.dma("sp", ht[:], h_d[t * 128:(t + 1) * 128, :])
                kb.I("act", "copy", [ht], [hb], out=hb[:], in_=ht[:])
                for si in (s1i, s2i):
                    kb.dma("pool", xs_d, hb[:], reads=[hb, si], writes=[xs_d], indirect=dict(
                        out_offset=bass.IndirectOffsetOnAxis(ap=si[:, t:t + 1], axis=0), in_offset=None))

        with kb.scope():
            xin = [[kb.sb("xin%d_%d" % (i, s), [128, D], BF16) for s in range(BS)] for i in range(2)]
            xT = [kb.sb("xT%d" % i, [128, 8, BLK], BF16) for i in range(2)]
            tps = [kb.ps("tps%d" % i, [128, 8, 128], BF16) for i in range(2)]
            stg = [kb.sb("stg%d" % i, [128, 4096]) for i in range(3)]
            wgb = [kb.sb("wgb%d" % i, [128, 8, 512], BF16) for i in range(2)]
            wub = [kb.sb("wub%d" % i, [128, 8, 512], BF16) for i in range(2)]
            wdb = [kb.sb("wdb%d" % i, [128, 4, 1024], BF16) for i in range(2)]
            hgp = [kb.ps("hgp%d" % i, [128, BLK]) for i in range(2)]
            hup = [kb.ps("hup%d" % i, [128, BLK]) for i in range(2)]
            yp = [kb.ps("yp%d" % i, [128, 512]) for i in range(2)]
            sg = [kb.sb("sg%d" % i, [128, BLK]) for i in range(2)]
            hTb = [kb.sb("hTb%d" % i, [128, 4, BLK], BF16) for i in range(2)]
            yb = [kb.sb("yb%d" % i, [128, D]) for i in range(2)]
            sti = 0
            cast_eng = ["dve", "act"]
            bc_reg = nc.gpsimd.alloc_register("moe_bc_%d" % kb.uid)
            nc.gpsimd.reg_mov(bc_reg, 4095)

            def load_weights(b):
                nonlocal sti
                par = b % 2
                for src, dstt in ((wg_d, wgb[par]), (wu_d, wub[par]), (wd_d, wdb[par])):
                    st = stg[sti % 3]
                    kb.dma("pool", st[:], src, reads=[idxe], writes=[st],
                           indirect=dict(out_offset=None, in_offset=bass.IndirectOffsetOnAxis(ap=idxe[:, b:b + 1], axis=0),
                                         bounds_check=bc_reg, oob_is_err=False))
                    dflat = dstt[:].rearrange("p a b -> p (a b)")
                    for half in range(2):
                        ce = cast_eng[(2 * sti + half) % 2]
                        kb.copy(ce, [st], [dstt], dflat[:, half * 2048:(half + 1) * 2048], st[:, half * 2048:(half + 1) * 2048])
                    sti += 1

            def load_tokens(b):
                par = b % 2
                for s in range(BS):
                    xi = xin[par][s]
                    kb.dma("act", xi[:], xs_d[b * BLK + s * 128: b * BLK + (s + 1) * 128, :])
                    tpp = tps[s % 2]
                    for k in range(8):
                        kb.I("pe", "transpose", [xi, c["identb"]], [tpp], out=tpp[:, k, :], in_=xi[:, k * 128:(k + 1) * 128],
                             identity=c["identb"][:], inc=(k == 7))
                    kb.I("dve", "tensor_copy", [tpp], [xT[par]], out=xT[par][:, :, s * 128:(s + 1) * 128], in_=tpp[:])

            def gate_up(b):
                par = b % 2
                for f in range(4):
                    hg = hgp[f % 2]
                    hu = hup[f % 2]
                    for k in range(8):
                        kb.I("pe", "matmul", [wgb[par], xT[par]], [hg], hg[:], lhsT=wgb[par][:, k, f * 128:(f + 1) * 128],
                             rhs=xT[par][:, k, :], start=(k == 0), stop=(k == 7), inc=(k == 7))
                    for k in range(8):
                        kb.I("pe", "matmul", [wub[par], xT[par]], [hu], hu[:], lhsT=wub[par][:, k, f * 128:(f + 1) * 128],
                             rhs=xT[par][:, k, :], start=(k == 0), stop=(k == 7), inc=(k == 7))
                    sgt = sg[f % 2]
                    kb.I("act", "activation", [hg], [sgt], out=sgt[:], in_=hg[:], func=AF.Silu)
                    kb.I("dve", "tensor_tensor", [sgt, hu], [hTb[par]], out=hTb[par][:, f, :], in0=sgt[:], in1=hu[:], op=ALU.mult)

            def down(b):
                par = b % 2
                for s in range(BS):
                    ybt = yb[s % 2]
                    for dh in range(2):
                        y = yp[dh]
                        for f in range(4):
                            kb.I("pe", "matmul", [hTb[par], wdb[par]], [y], y[:], lhsT=hTb[par][:, f, s * 128:(s + 1) * 128],
                                 rhs=wdb[par][:, f, dh * 512:(dh + 1) * 512], start=(f == 0), stop=(f == 3), inc=(f == 3))
                        kb.I("act" if dh == 0 else "dve", "tensor_copy" if dh else "copy", [y], [ybt],
                             out=ybt[:, dh * 512:(dh + 1) * 512], in_=y[:])
                    kb.dma("sp", ys_d[b * BLK + s * 128: b * BLK + (s + 1) * 128, :], ybt[:])

            load_weights(0)
            load_tokens(0)
            for b in range(nblk):
                if b + 1 < nblk:
                    load_weights(b + 1)
                gate_up(b)
                if b + 1 < nblk:
                    load_tokens(b + 1)
                down(b)

        with kb.scope():
            NS = 3
            y1 = [kb.sb("y1_%d" % i, [128, D]) for i in range(NS)]
            y2 = [kb.sb("y2_%d" % i, [128, D]) for i in range(NS)]
            hts5 = [kb.sb("ht5_%d" % i, [128, D]) for i in range(NS)]
            acc = [kb.sb("acc%d" % i, [128, D]) for i in range(2)]
            outt = [kb.sb("outt%d" % i, [128, D]) for i in range(2)]
            st6 = kb.sb("st6", [128, 2, 6])
            mv = kb.sb("mv", [128, 4])

            def issue(t):
                p = t % NS
                kb.dma("sp", hts5[p][:], h_d[t * 128:(t + 1) * 128, :])
                for yy, si in ((y1[p], s1i), (y2[p], s2i)):
                    kb.dma("pool", yy[:], ys_d, reads=[ys_d, si], writes=[yy], indirect=dict(
                        out_offset=None, in_offset=bass.IndirectOffsetOnAxis(ap=si[:, t:t + 1], axis=0)))
            for t in range(min(NS - 1, ntile)):
                issue(t)
            for t in range(ntile):
                if t + NS - 1 < ntile:
                    issue(t + NS - 1)
                p = t % NS
                ht = hts5[p]
                a = acc[t % 2]
                kb.I("act", "mul", [y1[p], g1], [a], out=a[:], in_=y1[p][:], mul=g1[:, t:t + 1])
                kb.I("dve", "scalar_tensor_tensor", [y2[p], g2, a], [a], out=a[:], in0=y2[p][:], scalar=g2[:, t:t + 1],
                     in1=a[:], op0=ALU.mult, op1=ALU.add)
                kb.I("dve", "scalar_tensor_tensor", [ht, a], [a], out=a[:], in0=ht[:], scalar=ALPHA, in1=a[:],
                     op0=ALU.mult, op1=ALU.add)
                layer_norm_tile(kb, a, g_bc, b_bc, outt[t % 2], st6, mv)
                kb.dma("sp", out_d[t * 128:(t + 1) * 128, :], outt[t % 2][:])

def make_consts():
    import ml_dtypes
    i = np.arange(128)
    return {
        "ident": np.eye(128, dtype=np.float32),
        "identb": np.eye(128, dtype=np.float32).astype(ml_dtypes.bfloat16),
        "ones": np.ones((128, 128), np.float32),
        "sut": (i[:, None] < i[None, :]).astype(np.float32),
        "ramp": np.broadcast_to(i[None, :].astype(np.float32), (128, 128)).copy(),
        "pidx": np.broadcast_to(i[:, None].astype(np.float32), (128, 128)).copy(),
        "cmask": np.where(i[None, :] > i[:, None], NEG, 0.0).astype(np.float32),
        "onesb": np.ones((128, 128), np.float32).astype(ml_dtypes.bfloat16),
        "cmaskb": np.where(i[None, :] > i[:, None], NEG, 0.0).astype(np.float32).astype(ml_dtypes.bfloat16),
    }


CHUNKS = [(0, 512), (512, 512), (1024, 512), (1536, 512), (2048, 128)]
EV_Q, EV_CKV, EV_QI, EV_KI, EV_WI, EV_GATE, EV_XB = 0, 512, 768, 1280, 1344, 1352, 1864


def proj_fm(kb, ps2, cnt, wb, col0, hT, dst_fn, evac):
    for (t0, n) in CHUNKS:
        p = ps2[cnt[0] % 2]
        for k in range(8):
            kb.I("pe", "matmul", [wb, hT], [p], p[:, 0:n], lhsT=wb[:, k, col0:col0 + 128], rhs=hT[:, k, t0:t0 + n],
                 start=(k == 0), stop=(k == 7), inc=(k == 7))
        evac(cnt[0], p[:, 0:n], t0, n)
        cnt[0] += 1


def build_even(kb, c, h_d, win_d, wout_d, wuk_d, wuv_d, rgw_d, vec_d, lng_d, lnb_d, out_d, nseq=SPC):
    with kb.scope():
        winb = kb.sb("winb", [128, 8, 2376], BF16)
        wk2 = kb.sb("wk2", [128, 8, 128], BF16)
        wukb = kb.sb("wukb", [128, 2, 128], BF16)
        wuvb = kb.sb("wuvb", [128, 2, 128], BF16)
        rgwb = kb.sb("rgwb", [128, 8, 128], BF16)
        vec = kb.sb("vec", [128, 40])
        cc = kb.sb("cc", [128, 4])
        g_bc = kb.sb("g_bc", [128, D])
        b_bc = kb.sb("b_bc", [128, D])
        kb.dma("sp", g_bc[:], lng_d.partition_broadcast(128))
        kb.dma("sp", b_bc[:], lnb_d.partition_broadcast(128))
        kb.dma("sp", vec[:], vec_d)
        with kb.scope():
            stg = [kb.sb("wstg%d" % i, [128, 2376]) for i in range(2)]
            for k in range(8):
                st = stg[k % 2]
                kb.dma("sp", st[:], win_d[k * 128:(k + 1) * 128, :])
                kb.copy("act" if k % 2 else "pool", [st], [winb], winb[:, k, :], st[:])
            for j in range(2):
                kb.I("dve", "tensor_copy", [winb], [wk2], out=wk2[:, :, j * 64:(j + 1) * 64], in_=winb[:, :, EV_KI:EV_KI + 64])
            st = stg[0]
            kb.dma("sp", st[:, 0:256], wuk_d.rearrange("(c p) d -> p c d", p=128))
            kb.I("dve", "tensor_copy", [st], [wukb], out=wukb[:], in_=st[:, 0:256].rearrange("p (c d) -> p c d", c=2))
            st = stg[1]
            kb.dma("sp", st[:, 0:256], wuv_d.rearrange("(c p) d -> p c d", p=128))
            kb.I("dve", "tensor_copy", [st], [wuvb], out=wuvb[:], in_=st[:, 0:256].rearrange("p (c d) -> p c d", c=2))
            st = stg[0]
            kb.dma("sp", st[:, 0:1024], rgw_d)
            kb.I("dve", "tensor_copy", [st], [rgwb], out=rgwb[:], in_=st[:, 0:1024].rearrange("p (c d) -> p c d", c=8))
            kb.I("act", "activation", [vec], [cc], out=cc[:], in_=vec[:, 28:32], func=AF.Exp, scale=-1.0)
            kb.I("act", "activation", [cc], [cc], out=cc[:], in_=cc[:], func=AF.Ln, bias=1.0)
            kb.I("dve", "tensor_scalar", [cc], [cc], out=cc[:], in0=cc[:], scalar1=-8.0, scalar2=None, op0=ALU.mult)
        cw = lambda j, ch: vec[:, j * 4 + ch: j * 4 + ch + 1]
        cb = lambda ch: vec[:, 16 + ch:17 + ch]
        ba = lambda ch: vec[:, 20 + ch:21 + ch]
        bx = lambda ch: vec[:, 24 + ch:25 + ch]
        kvn = lambda ch: vec[:, 32 + ch:33 + ch]

        for s in range(nseq):
            r0 = s * TP
            with kb.scope():
                hT = kb.sb("hT", [128, 8, TP], BF16)
                qT = kb.sb("qT", [128, 4, TP], BF16)
                qiT = kb.sb("qiT", [128, 4, TP], BF16)
                kiT2 = kb.sb("kiT2", [128, TP], BF16)
                kT = kb.sb("kT", [128, TP], BF16)
                vtok = kb.sb("vtok", [128, NT, 130], BF16)
                wq = kb.sb("wq", [128, NT, 8])
                kb.I("pool", "memset", [], [vtok], vtok[:, :, 128:130], 1.0)
                with kb.scope():
                    gateT = kb.sb("gateT", [128, 4, TP], BF16)
                    xbT = kb.sb("xbT", [128, 4, TP], BF16)
                    with kb.scope():
                        hts = [kb.sb("eht%d" % i, [128, D]) for i in range(2)]
                        tp = [kb.ps("etp%d" % i, [128, 8, 128]) for i in range(2)]
                        for t in range(NT):
                            ht = hts[t % 2]
                            kb.dma("sp", ht[:], h_d[r0 + t * 128: r0 + (t + 1) * 128, :])
                            tpp = tp[t % 2]
                            for k in range(8):
                                kb.I("pe", "transpose", [ht, c["ident"]], [tpp], out=tpp[:, k, :],
                                     in_=ht[:, k * 128:(k + 1) * 128], identity=c["ident"][:], inc=(k == 7))
                            kb.copy("act" if t % 2 else "dve", [tpp], [hT], hT[:, :, t * 128:(t + 1) * 128], tpp[:])
                    with kb.scope():
                        ckvT = kb.sb("ckvT", [128, 2, TP], BF16)
                        latT = kb.sb("latT", [128, 2, TP], BF16)
                        ps2 = [kb.ps("pp%d" % i, [128, 512]) for i in range(2)]
                        cnt = [0]

                        def mk_evac(dst, ci):
                            def ev(n_, p, t0, n):
                                kb.copy("act" if n_ % 2 else "dve", [p], [dst], dst[:, ci, t0:t0 + n] if ci is not None else dst[:, t0:t0 + n], p)
                            return ev
                        for ci in range(4):
                            proj_fm(kb, ps2, cnt, winb, EV_Q + ci * 128, hT, None, mk_evac(qT, ci))
                            proj_fm(kb, ps2, cnt, winb, EV_QI + ci * 128, hT, None, mk_evac(qiT, ci))
                            proj_fm(kb, ps2, cnt, winb, EV_GATE + ci * 128, hT, None, mk_evac(gateT, ci))
                            proj_fm(kb, ps2, cnt, winb, EV_XB + ci * 128, hT, None, mk_evac(xbT, ci))
                        for ci in range(2):
                            proj_fm(kb, ps2, cnt, winb, EV_CKV + ci * 128, hT, None, mk_evac(ckvT, ci))
                        proj_fm(kb, ps2, cnt, wk2, 0, hT, None, mk_evac(kiT2, None))
                        wps = [kb.ps("wps%d" % i, [128, 8]) for i in range(2)]
                        for t in range(NT):
                            p = wps[t % 2]
                            for k in range(8):
                                kb.I("pe", "matmul", [hT, winb], [p], p[:], lhsT=hT[:, k, t * 128:(t + 1) * 128],
                                     rhs=winb[:, k, EV_WI:EV_WI + 8], start=(k == 0), stop=(k == 7), inc=(k == 7))
                            kb.copy("act", [p], [wq], wq[:, t, :], p[:])
                        sq = kb.sb("sq", [128, 2, 512], BF16)
                        rstd = kb.sb("rstd", [128, 512])
                        for (t0, n) in CHUNKS:
                            kb.I("act", "activation", [ckvT], [sq], out=sq[:, :, 0:n], in_=ckvT[:, :, t0:t0 + n], func=AF.Square)
                            p = ps2[cnt[0] % 2]
                            cnt[0] += 1
                            for ci in range(2):
                                kb.I("pe", "matmul", [c["onesb"], sq], [p], p[:, 0:n], lhsT=c["onesb"][:], rhs=sq[:, ci, 0:n],
                                     start=(ci == 0), stop=(ci == 1), inc=(ci == 1))
                            kb.I("dve", "tensor_scalar", [p], [rstd], out=rstd[:, 0:n], in0=p[:, 0:n], scalar1=1.0 / 256, scalar2=1e-6,
                                 op0=ALU.mult, op1=ALU.add)
                            kb.I("act", "sqrt", [rstd], [rstd], out=rstd[:, 0:n], in_=rstd[:, 0:n])
                            kb.I("dve", "reciprocal", [rstd], [rstd], out=rstd[:, 0:n], in_=rstd[:, 0:n])
                            for ci in range(2):
                                kb.I("dve", "scalar_tensor_tensor", [ckvT, vec, rstd], [latT], out=latT[:, ci, t0:t0 + n],
                                     in0=ckvT[:, ci, t0:t0 + n], scalar=kvn(ci), in1=rstd[:, 0:n], op0=ALU.mult, op1=ALU.mult)
                            p = ps2[cnt[0] % 2]
                            cnt[0] += 1
                            for ci in range(2):
                                kb.I("pe", "matmul", [wukb, latT], [p], p[:, 0:n], lhsT=wukb[:, ci, :], rhs=latT[:, ci, t0:t0 + n],
                                     start=(ci == 0), stop=(ci == 1), inc=(ci == 1))
                            kb.copy("act", [p], [kT], kT[:, t0:t0 + n], p[:, 0:n])
                        for t in range(NT):
                            p = ps2[cnt[0] % 2]
                            cnt[0] += 1
                            for ci in range(2):
                                kb.I("pe", "matmul", [latT, wuvb], [p], p[:, 0:128], lhsT=latT[:, ci, t * 128:(t + 1) * 128],
                                     rhs=wuvb[:, ci, :], start=(ci == 0), stop=(ci == 1), inc=(ci == 1))
                            kb.copy("dve", [p], [vtok], vtok[:, t, 0:128], p[:, 0:128])
                    mixT = hT
                    with kb.scope():
                        HS = TP // 2
                        F = lambda nm: kb.sb(nm, [128, HS])
                        xr, rr, ii, aa, uu, hh, gl = F("xr"), F("rr"), F("ii"), F("aa"), F("uu"), F("hh"), F("gl")
                        xrb = kb.sb("xrb", [128, HS], BF16)
                        carry = kb.sb("carry", [128, 4])
                        gps = [kb.ps("gps%d" % i, [128, 512]) for i in range(4)]
                        gi = 0
                        for ch in range(4):
                            for hf in range(2):
                                o = hf * HS
                                x = xbT[:, ch, :]
                                kb.I("dve", "tensor_scalar", [xbT, vec], [xr], out=xr[:], in0=x[:, o:o + HS], scalar1=cw(3, ch), scalar2=cb(ch),
                                     op0=ALU.mult, op1=ALU.add)
                                for d in (1, 2, 3):
                                    lo = d if hf == 0 else 0
                                    kb.I("dve", "scalar_tensor_tensor", [xbT, vec, xr], [xr], out=xr[:, lo:HS], in0=x[:, o + lo - d:o + HS - d],
                                         scalar=cw(3 - d, ch), in1=xr[:, lo:HS], op0=ALU.mult, op1=ALU.add)
                                kb.copy("pool", [xr], [xrb], xrb[:], xr[:])
                                for (t0, n) in ((0, 512), (512, 512), (1024, 64)):
                                    pa = gps[gi % 4]
                                    px = gps[(gi + 1) % 4]
                                    gi += 2
                                    kb.I("pe", "matmul", [rgwb, xrb], [pa], pa[:, 0:n], lhsT=rgwb[:, ch, :], rhs=xrb[:, t0:t0 + n], start=True, stop=True)
                                    kb.I("pe", "matmul", [rgwb, xrb], [px], px[:, 0:n], lhsT=rgwb[:, 4 + ch, :], rhs=xrb[:, t0:t0 + n], start=True, stop=True)
                                    kb.I("act", "activation", [pa, vec], [rr], out=rr[:, t0:t0 + n], in_=pa[:, 0:n], func=AF.Sigmoid, bias=ba(ch))
                                    kb.I("act", "activation", [px, vec], [ii], out=ii[:, t0:t0 + n], in_=px[:, 0:n], func=AF.Sigmoid, bias=bx(ch))
                                kb.I("act", "activation", [rr, cc], [aa], out=aa[:], in_=rr[:], func=AF.Exp, scale=cc[:, ch:ch + 1])
                                kb.I("pool", "tensor_tensor", [aa], [uu], out=uu[:], in0=aa[:], in1=aa[:], op=ALU.mult)
                                kb.I("act", "activation", [uu], [uu], out=uu[:], in_=uu[:], func=AF.Sqrt, scale=-1.0, bias=1.0)
                                kb.I("pool", "tensor_tensor", [ii, xr], [ii], out=ii[:], in0=ii[:], in1=xr[:], op=ALU.mult)
                                kb.I("pool", "tensor_tensor", [uu, ii], [uu], out=uu[:], in0=uu[:], in1=ii[:], op=ALU.mult)
                                kb.I("dve", "tensor_tensor_scan", [aa, uu, carry], [hh], out=hh[:], data0=aa[:], data1=uu[:],
                                     initial=(0.0 if hf == 0 else carry[:, ch:ch + 1]), op0=ALU.mult, op1=ALU.add)
                                if hf == 0:
                                    kb.I("dve", "tensor_copy", [hh], [carry], out=carry[:, ch:ch + 1], in_=hh[:, HS - 1:HS])
                                g = gateT[:, ch, o:o + HS]
                                kb.I("act", "activation", [gateT], [gl], out=gl[:], in_=g, func=AF.Square)
                                kb.I("pool", "tensor_scalar", [gl], [gl], out=gl[:], in0=gl[:], scalar1=0.044715, scalar2=1.0, op0=ALU.mult, op1=ALU.add)
                                kb.I("pool", "tensor_tensor", [gl, gateT], [gl], out=gl[:], in0=gl[:], in1=g, op=ALU.mult)
                                kb.I("act", "activation", [gl], [gl], out=gl[:], in_=gl[:], func=AF.Sigmoid, scale=1.5957691216)
                                kb.I("pool", "tensor_tensor", [gl, gateT], [gl], out=gl[:], in0=gl[:], in1=g, op=ALU.mult)
                                kb.I("dve", "tensor_tensor", [hh, gl], [mixT], out=mixT[:, 4 + ch, o:o + HS], in0=hh[:], in1=gl[:], op=ALU.mult)
                with kb.scope():
                    score = [kb.sb("score%d" % i, [128, TP]) for i in range(2)]
                    mask = [kb.sb("mask%d" % i, [128, TP], BF16) for i in range(2)]
                    m8 = [kb.sb("m8_%d" % i, [128, 8]) for i in range(2)]
                    work = kb.sb("work", [128, TP])
                    thrc = kb.sb("thrc", [128, 1])
                    rlb = [kb.sb("rlb%d" % i, [128, 512], BF16) for i in range(4)]
                    dg = [kb.sb("dg%d" % i, [128, 8, 128], BF16) for i in range(1)]
                    lgt = [kb.sb("lgt%d" % i, [128, TP]) for i in range(2)]
                    ee = [kb.sb("ee%d" % i, [128, TP], BF16) for i in range(2)]
                    pT = [kb.sb("pT%d" % i, [128, NT, 128], BF16) for i in range(1)] * 2
                    sm = [kb.sb("sm%d" % i, [128, 4]) for i in range(2)]
                    on = [kb.sb("on%d" % i, [128, 128], BF16) for i in range(2)]
                    sps = [kb.ps("sps%d" % i, [128, 512]) for i in range(2)]
                    lps = [kb.ps("lps%d" % i, [128, 512]) for i in range(2)]
                    tps = [kb.ps("atp%d" % i, [128, 8, 128], BF16) for i in range(2)]
                    ops = kb.ps("ops", [128, 132])
                    scp = kb.ps("scp", [128, 512])
                    kb.I("dve", "memset", [], [thrc], thrc[:], -1.0e29)
                    cnts = {"si": 0, "ti": 0, "li": 0}

                    def indexer(i):
                        sc = score[i % 2]
                        dgt = dg[0]
                        nk = (i + 1) * 128
                        qs = slice(i * 128, (i + 1) * 128)
                        for h in range(8):
                            kb.I("pool", "tensor_scalar", [c["identb"], wq], [dgt], out=dgt[:, h, :], in0=c["identb"][:],
                                 scalar1=wq[:, i, h:h + 1], scalar2=None, op0=ALU.mult)
                        for t0 in range(0, nk, 512):
                            n = min(512, nk - t0)
                            last = (t0 + n == nk)
                            for hh in range(2):
                                for h in range(hh * 4, hh * 4 + 4):
                                    p = sps[cnts["si"] % 2]
                                    cnts["si"] += 1
                                    pr = slice((h % 2) * 64, (h % 2) * 64 + 64)
                                    kb.I("pe", "matmul", [qiT, kiT2], [p], p[:, 0:n], lhsT=qiT[pr, h // 2, qs], rhs=kiT2[pr, t0:t0 + n],
                                         start=True, stop=True)
                                    kb.I("act", "activation", [p], [rlb[h % 4]], out=rlb[h % 4][:, 0:n], in_=p[:, 0:n], func=AF.Relu)
                                for h in range(hh * 4, hh * 4 + 4):
                                    kb.I("pe", "matmul", [dgt, rlb[h % 4]], [scp], scp[:, 0:n], lhsT=dgt[:, h, :], rhs=rlb[h % 4][:, 0:n],
                                         start=(h == 0), stop=(h == 7 and not last), inc=(h % 4 == 3))
                            if last:
                                kb.I("pe", "matmul", [c["identb"], c["cmaskb"]], [scp], scp[:, n - 128:n], lhsT=c["identb"][:], rhs=c["cmaskb"][:],
                                     start=False, stop=True)
                            kb.copy("act", [scp], [sc], sc[:, t0:t0 + n], scp[:, 0:n])
                            yield

                    def topk(i):
                        sc, mk, m = score[i % 2], mask[i % 2], m8[i % 2]
                        nk = (i + 1) * 128
                        if i >= 2:
                            for it in range(32):
                                src = sc if it == 0 else work
                                kb.I("dve", "max", [src], [m], out=m[:], in_=src[:, 0:nk])
                                if it < 31:
                                    kb.I("dve", "match_replace", [m, src], [work], out=work[:, 0:nk], in_to_replace=m[:],
                                         in_values=src[:, 0:nk], imm_value=NEG)
                                if it % 8 == 7 and it < 31:
                                    yield
                            kb.I("dve", "tensor_scalar", [sc, m], [mk], out=mk[:, 0:nk], in0=sc[:, 0:nk], scalar1=m[:, 7:8],
                                 scalar2=None, op0=ALU.is_ge)
                        else:
                            kb.I("dve", "tensor_scalar", [sc, thrc], [mk], out=mk[:, 0:nk], in0=sc[:, 0:nk], scalar1=thrc[:, 0:1],
                                 scalar2=None, op0=ALU.is_ge)

                    def qk(i, h):
                        nk = (i + 1) * 128
                        qs = slice(i * 128, (i + 1) * 128)
                        lg = lgt[h % 2]
                        for t0 in range(0, nk, 512):
                            n = min(512, nk - t0)
                            p = lps[cnts["li"] % 2]
                            cnts["li"] += 1
                            kb.I("pe", "matmul", [qT, kT], [p], p[:, 0:n], lhsT=qT[:, h, qs], rhs=kT[:, t0:t0 + n], start=True, stop=True)
                            kb.I("act", "mul", [p], [lg], out=lg[:, t0:t0 + n], in_=p[:, 0:n], mul=128.0 ** -0.5)

                    def attention(i, gen, geni):
                        mk = mask[i % 2]
                        nk = (i + 1) * 128
                        qs = slice(i * 128, (i + 1) * 128)
                        qk(i, 0)
                        for h in range(4):
                            lg, e_, s_, pT_, on_ = lgt[h % 2], ee[h % 2], sm[h % 2], pT[h % 2], on[h % 2]
                            kb.I("dve", "tensor_reduce", [lg], [s_], out=s_[:, 0:1], in_=lg[:, 0:nk], axis=AX.X, op=ALU.max)
                            kb.I("dve", "tensor_scalar", [s_], [s_], out=s_[:, 1:2], in0=s_[:, 0:1], scalar1=-1.0, scalar2=None, op0=ALU.mult)
                            kb.I("act", "activation", [lg, s_], [e_], out=e_[:, 0:nk], in_=lg[:, 0:nk], func=AF.Exp, bias=s_[:, 1:2])
                            if h < 3:
                                qk(i, h + 1)
                            next(gen, None)
                            next(geni, None)
                            if h % 2:
                                next(geni, None)
                            kb.I("pool", "tensor_tensor", [e_, mk], [e_], out=e_[:, 0:nk], in0=e_[:, 0:nk], in1=mk[:, 0:nk], op=ALU.mult)
                            for j0 in range(0, i + 1, 8):
                                j1 = min(j0 + 8, i + 1)
                                tpp = tps[cnts["ti"] % 2]
                                cnts["ti"] += 1
                                for j in range(j0, j1):
                                    kb.I("pe", "transpose", [e_, c["identb"]], [tpp], out=tpp[:, j - j0, :], in_=e_[:, j * 128:(j + 1) * 128],
                                         identity=c["identb"][:], inc=(j == j1 - 1))
                                kb.copy("act", [tpp], [pT_], pT_[:, j0:j1, :], tpp[:, 0:j1 - j0, :])
                            for j in range(i + 1):
                                kb.I("pe", "matmul", [pT_, vtok], [ops], ops[:, 0:129], lhsT=pT_[:, j, :], rhs=vtok[:, j, 0:129], start=(j == 0), stop=(j == i),
                                     inc=(j == i))
                            kb.I("dve", "reciprocal", [ops], [s_], out=s_[:, 3:4], in_=ops[:, 128:129])
                            kb.I("dve", "tensor_scalar", [ops, s_], [on_], out=on_[:], in0=ops[:, 0:128], scalar1=s_[:, 3:4], scalar2=None, op0=ALU.mult)
                            otb = tps[cnts["ti"] % 2]
                            cnts["ti"] += 1
                            kb.I("pe", "transpose", [on_, c["identb"]], [otb], out=otb[:, 0, :], in_=on_[:], identity=c["identb"][:])
                            kb.copy("act", [otb], [mixT], mixT[:, h, qs], otb[:, 0, :])

                    for _ in indexer(0):
                        pass
                    for _ in indexer(1):
                        pass
                    for _ in topk(0):
                        pass
                    for i in range(NT):
                        gen = topk(i + 1) if i + 1 < NT else iter(())
                        geni = indexer(i + 2) if i + 2 < NT else iter(())
                        attention(i, gen, geni)
                        for _ in geni:
                            pass
                        for _ in gen:
                            pass
                with kb.scope():
                    woutb = kb.sb("woutb", [128, 8, D], BF16)
                    stg = [kb.sb("ostg%d" % i, [128, D]) for i in range(2)]
                    for k in range(8):
                        kb.dma("sp", stg[k % 2][:], wout_d[k * 128:(k + 1) * 128, :])
                        kb.copy("act" if k % 2 else "pool", [stg[k % 2]], [woutb], woutb[:, k, :], stg[k % 2][:])
                    hts = [kb.sb("oht%d" % i, [128, D]) for i in range(2)]
                    acc = [kb.sb("oacc%d" % i, [128, D]) for i in range(2)]
                    outt = [kb.sb("oout%d" % i, [128, D]) for i in range(2)]
                    st6 = kb.sb("st6", [128, 2, 6])
                    mv = kb.sb("mv", [128, 4])
                    ops2 = [kb.ps("opp%d" % i, [128, 512]) for i in range(4)]
                    for t in range(NT):
                        ht = hts[t % 2]
                        a = acc[t % 2]
                        kb.dma("sp", ht[:], h_d[r0 + t * 128: r0 + (t + 1) * 128, :])
                        for hf in range(2):
                            p = ops2[(2 * t + hf) % 4]
                            for k in range(8):
                                kb.I("pe", "matmul", [mixT, woutb], [p], p[:], lhsT=mixT[:, k, t * 128:(t + 1) * 128],
                                     rhs=woutb[:, k, hf * 512:(hf + 1) * 512], start=(k == 0), stop=(k == 7), inc=(k == 7))
                            kb.I("dve", "scalar_tensor_tensor", [ht, p], [a], out=a[:, hf * 512:(hf + 1) * 512], in0=ht[:, hf * 512:(hf + 1) * 512],
                                 scalar=ALPHA, in1=p[:], op0=ALU.mult, op1=ALU.add)
                        layer_norm_tile(kb, a, g_bc, b_bc, outt[t % 2], st6, mv)
                        kb.dma("sp", out_d[r0 + t * 128: r0 + (t + 1) * 128, :], outt[t % 2][:])


def prep_even(inp, i):
    f = lambda a: np.ascontiguousarray(a, dtype=np.float32)
    pc = lambda v, n: f(v.reshape(n, 128).T)
    vec = np.zeros((128, 40), np.float32)
    cwt = inp["even_conv_w"][i]
    for j in range(4):
        vec[:, j * 4:(j + 1) * 4] = pc(cwt[j], 4)
    vec[:, 16:20] = pc(inp["even_conv_b"][i], 4)
    vec[:, 20:24] = pc(inp["even_rg_ba"][i], 4)
    vec[:, 24:28] = pc(inp["even_rg_bx"][i], 4)
    vec[:, 28:32] = pc(inp["even_rg_lambda"][i], 4)
    vec[:, 32:34] = pc(inp["even_kv_norm"][i], 2)
    rgw = np.zeros((128, 8, 128), np.float32)
    for g, nm in enumerate(("even_rg_wa", "even_rg_wx")):
        w = inp[nm][i]
        for n in range(8):
            o = (n % 2) * 64
            rgw[o:o + 64, g * 4 + n // 2, o:o + 64] = w[n]
    return {
        "ev_win": f(inp["even_w_in"][i]), "ev_wout": f(inp["even_w_out"][i]),
        "ev_wuk": f(inp["even_w_uk"][i]), "ev_wuv": f(inp["even_w_uv"][i]),
        "ev_rgw": f(rgw.reshape(128, 1024)), "ev_vec": vec,
        "ev_lng": f(inp["ln_g"][2 * i, 0]), "ev_lnb": f(inp["ln_b"][2 * i, 0]),
    }


def build_gdn(kb, c, h_d, gw_d, wab_d, gconv_d, alog_d, dtb_d, onorm_d, wout_d, lng_d, lnb_d, out_d, o_d, nseq=SPC, stop=0):
    with kb.scope():
        g_bc = kb.sb("g_bc", [128, D])
        b_bc = kb.sb("b_bc", [128, D])
        kb.dma("sp", g_bc[:], lng_d.partition_broadcast(128))
        kb.dma("sp", b_bc[:], lnb_d.partition_broadcast(128))
        wabb = kb.sb("wabb", [128, 8, 32], BF16)
        gconv = kb.sb("gconv", [128, 8, 4, 4])
        nA = kb.sb("nA", [128, 16])
        dtb = kb.sb("dtb", [128, 16])
        onorm = kb.sb("onorm", [128, 1])
        eps6 = kb.sb("eps6", [128, 1])
        kb.I("pool", "memset", [], [eps6], eps6[:], 1e-6)
        kb.dma("sp", gconv[:], gconv_d)
        kb.dma("sp", nA[:], alog_d.partition_broadcast(128))
        kb.dma("sp", dtb[:], dtb_d.partition_broadcast(128))
        kb.dma("sp", onorm[:], onorm_d.rearrange("(p o) -> p o", o=1))
        kb.I("act", "activation", [nA], [nA], out=nA[:], in_=nA[:], func=AF.Exp)
        kb.I("dve", "tensor_scalar", [nA], [nA], out=nA[:], in0=nA[:], scalar1=-1.0, scalar2=None, op0=ALU.mult)
        ut = kb.sb("ut", [128, 128])
        slt = kb.sb("slt", [128, 128])
        lmask = kb.sb("lmask", [128, 128])
        smask = kb.sb("smask", [128, 128])
        kb.I("dve", "tensor_tensor", [c["sut"], c["ident"]], [ut], out=ut[:], in0=c["sut"][:], in1=c["ident"][:], op=ALU.add)
        kb.I("dve", "tensor_scalar", [ut], [slt], out=slt[:], in0=ut[:], scalar1=-1.0, scalar2=1.0, op0=ALU.mult, op1=ALU.add)
        kb.I("dve", "tensor_copy", [slt], [smask], out=smask[:], in_=slt[:])
        kb.I("dve", "tensor_tensor", [slt, c["ident"]], [lmask], out=lmask[:], in0=slt[:], in1=c["ident"][:], op=ALU.add)
        with kb.scope():
            st = kb.sb("abstg", [128, 8, 32])
            kb.dma("sp", st[:], wab_d.rearrange("(k p) e -> p k e", p=128))
            kb.I("dve", "tensor_copy", [st], [wabb], out=wabb[:], in_=st[:])

        for s in range(nseq):
            r0 = s * TP
            with kb.scope():
                hT = kb.sb("hT", [128, 8, TP], BF16)
                with kb.scope():
                    hts = [kb.sb("ght%d" % i, [128, D]) for i in range(2)]
                    tp = [kb.ps("gtp%d" % i, [128, 8, 128]) for i in range(2)]
                    for t in range(NT):
                        ht = hts[t % 2]
                        kb.dma("sp", ht[:], h_d[r0 + t * 128: r0 + (t + 1) * 128, :])
                        tpp = tp[t % 2]
                        for k in range(8):
                            kb.I("pe", "transpose", [ht, c["ident"]], [tpp], out=tpp[:, k, :],
                                 in_=ht[:, k * 128:(k + 1) * 128], identity=c["ident"][:], inc=(k == 7))
                        kb.copy("act" if t % 2 else "dve", [tpp], [hT], hT[:, :, t * 128:(t + 1) * 128], tpp[:])
                if stop == 1:
                    return
                S3 = lambda nm: kb.sb(nm, [128, NT, 16])
                gg, beta, gc, egc, ekd, egl, bege = S3("gg"), S3("beta"), S3("gc"), S3("egc"), S3("ekd"), S3("egl"), S3("bege")
                with kb.scope():
                    abp = [kb.ps("abp%d" % i, [128, 32]) for i in range(2)]
                    ab = kb.sb("ab", [128, NT, 32])
                    tmp = S3("tmpa")
                    for t in range(NT):
                        p = abp[t % 2]
                        for k in range(8):
                            kb.I("pe", "matmul", [hT, wabb], [p], p[:], lhsT=hT[:, k, t * 128:(t + 1) * 128], rhs=wabb[:, k, :],
                                 start=(k == 0), stop=(k == 7), inc=(k == 7))
                        kb.copy("act", [p], [ab], ab[:, t, :], p[:])
                    bcT = lambda a: a[:].unsqueeze(1).to_broadcast([128, NT, 16])
                    kb.I("dve", "tensor_tensor", [ab, dtb], [gg], out=gg[:], in0=ab[:, :, 0:16], in1=bcT(dtb), op=ALU.add)
                    kb.I("dve", "tensor_scalar", [gg], [tmp], out=tmp[:], in0=gg[:], scalar1=-1.0, scalar2=None, op0=ALU.mult)
                    kb.I("dve", "tensor_tensor", [gg, tmp], [tmp], out=tmp[:], in0=gg[:], in1=tmp[:], op=ALU.max)
                    kb.I("act", "activation", [tmp], [tmp], out=tmp[:], in_=tmp[:], func=AF.Exp, scale=-1.0)
                    kb.I("act", "activation", [tmp], [tmp], out=tmp[:], in_=tmp[:], func=AF.Ln, bias=1.0)
                    kb.I("dve", "tensor_scalar", [gg], [gg], out=gg[:], in0=gg[:], scalar1=0.0, scalar2=None, op0=ALU.max)
                    kb.I("dve", "tensor_tensor", [gg, tmp], [gg], out=gg[:], in0=gg[:], in1=tmp[:], op=ALU.add)
                    kb.I("dve", "tensor_tensor", [gg, nA], [gg], out=gg[:], in0=gg[:], in1=bcT(nA), op=ALU.mult)
                    kb.I("act", "activation", [ab], [beta], out=beta[:], in_=ab[:, :, 16:32], func=AF.Sigmoid)
                    for t in range(NT):
                        p = abp[t % 2]
                        kb.I("pe", "matmul", [ut, gg], [p], p[:, 0:16], lhsT=ut[:], rhs=gg[:, t, :], start=True, stop=True, inc=False)
                        kb.I("pe", "matmul", [c["ones"], gg], [p], p[:, 16:32], lhsT=c["ones"][:], rhs=gg[:, t, :], start=True, stop=True)
                        kb.copy("act", [p], [gc], gc[:, t, :], p[:, 0:16])
                        kb.copy("dve", [p], [egl], egl[:, t, :], p[:, 16:32])
                    kb.I("dve", "tensor_tensor", [egl, gc], [ekd], out=ekd[:], in0=egl[:], in1=gc[:], op=ALU.subtract)
                    kb.I("act", "activation", [ekd], [ekd], out=ekd[:], in_=ekd[:], func=AF.Exp)
                    kb.I("act", "activation", [egl], [egl], out=egl[:], in_=egl[:], func=AF.Exp)
                    kb.I("act", "activation", [gc], [egc], out=egc[:], in_=gc[:], func=AF.Exp)
                    kb.I("dve", "tensor_tensor", [beta, egc], [bege], out=bege[:], in0=beta[:], in1=egc[:], op=ALU.mult)

                if stop == 2:
                    return
                wsls = [kb.sb("wsl0", [128, 8, 768], BF16)] * 2
                gstg = [kb.sb("gstg%d" % i, [128, 768]) for i in range(2)]

                def load_wsl(kh_):
                    w_ = wsls[kh_ % 2]
                    for k in range(8):
                        kb.dma("sp", gstg[k % 2][:], gw_d[kh_, k * 128:(k + 1) * 128, :])
                        kb.copy("act" if k % 2 else "pool", [gstg[k % 2]], [w_], w_[:, k, :], gstg[k % 2][:])
                load_wsl(0)
                for kh in range(8):
                    with kb.scope():
                        wsl = wsls[kh % 2]
                        qkT = kb.sb("qkT", [128, 2, TP], BF16)
                        zsT = kb.sb("zsT", [128, 2, TP], BF16)
                        ktok = kb.sb("ktok", [128, NT, 128], BF16)
                        vtok = kb.sb("vtok", [128, NT, 256], BF16)
                        oTk = kb.sb("oTk", [128, 2, TP], BF16)
                        with kb.scope():
                            vT = kb.sb("vT", [128, 2, TP], BF16)
                            raws = [kb.sb("raw%d" % i, [128, TP]) for i in range(2)]
                            cvs = [kb.sb("cv%d" % i, [128, TP]) for i in range(2)]
                            sqs = [kb.sb("sq%d" % i, [128, 512], BF16) for i in range(3)]
                            rstds = [kb.sb("rstd%d" % i, [128, 512]) for i in range(3)]
                            ps2 = [kb.ps("gpp%d" % i, [128, 512]) for i in range(2)]
                            ps3 = [kb.ps("gpq%d" % i, [128, 512]) for i in range(2)]
                            tps = [kb.ps("gtq%d" % i, [128, 8, 128], BF16) for i in range(2)]
                            cnt = [0]
                            ti = 0
                            def do_proj(fi):
                                raw = raws[fi % 2]
                                def ev(n_, p, t0, n, fi=fi, raw=raw):
                                    if fi < 4:
                                        kb.copy("act", [p], [raw], raw[:, t0:t0 + n], p)
                                    else:
                                        kb.I("act", "activation", [p], [zsT], out=zsT[:, fi - 4, t0:t0 + n], in_=p, func=AF.Silu)
                                proj_fm(kb, ps2, cnt, wsl, fi * 128, hT, None, ev)

                            def do_post(fi):
                                nonlocal ti
                                raw, cv = raws[fi % 2], cvs[fi % 2]
                                cwc = lambda j: gconv[:, kh, fi, j:j + 1]
                                kb.I("dve", "tensor_scalar", [raw, gconv], [cv], out=cv[:], in0=raw[:], scalar1=cwc(3), scalar2=None, op0=ALU.mult)
                                for d in (1, 2, 3):
                                    kb.I("dve", "scalar_tensor_tensor", [raw, gconv, cv], [cv], out=cv[:, d:TP], in0=raw[:, 0:TP - d],
                                         scalar=cwc(3 - d), in1=cv[:, d:TP], op0=ALU.mult, op1=ALU.add)
                                if fi >= 2:
                                    kb.I("act", "activation", [cv], [vT], out=vT[:, fi - 2, :], in_=cv[:], func=AF.Silu)
                                    for j0 in range(0, NT, 8):
                                        j1 = min(j0 + 8, NT)
                                        tpp = tps[ti % 2]
                                        ti += 1
                                        for j in range(j0, j1):
                                            kb.I("pe", "transpose", [vT, c["identb"]], [tpp], out=tpp[:, j - j0, :],
                                                 in_=vT[:, fi - 2, j * 128:(j + 1) * 128], identity=c["identb"][:], inc=(j == j1 - 1))
                                        kb.copy("dve", [tpp], [vtok], vtok[:, j0:j1, (fi - 2) * 128:(fi - 1) * 128], tpp[:, 0:j1 - j0, :])
                                    return
                                kb.I("act", "activation", [cv], [cv], out=cv[:], in_=cv[:], func=AF.Silu)
                                for ci_, (t0, n) in enumerate(CHUNKS):
                                    sq, rstd = sqs[ci_ % 3], rstds[ci_ % 3]
                                    kb.I("pool", "tensor_tensor", [cv], [sq], out=sq[:, 0:n], in0=cv[:, t0:t0 + n], in1=cv[:, t0:t0 + n], op=ALU.mult)
                                    p = ps3[ci_ % 2]
                                    kb.I("pe", "matmul", [c["onesb"], sq], [p], p[:, 0:n], lhsT=c["onesb"][:], rhs=sq[:, 0:n], start=True, stop=True)
                                    kb.I("act", "activation", [p], [rstd], out=rstd[:, 0:n], in_=p[:, 0:n], func=AF.Ln, bias=eps6[:, 0:1])
                                    kb.I("act", "activation", [rstd], [rstd], out=rstd[:, 0:n], in_=rstd[:, 0:n], func=AF.Exp, scale=-0.5)
                                    kb.I("dve", "scalar_tensor_tensor", [cv, rstd], [qkT], out=qkT[:, fi, t0:t0 + n], in0=cv[:, t0:t0 + n],
                                         scalar=(128.0 ** -0.5 if fi == 0 else 1.0), in1=rstd[:, 0:n], op0=ALU.mult, op1=ALU.mult)
                                if fi == 1:
                                    for j0 in range(0, NT, 8):
                                        j1 = min(j0 + 8, NT)
                                        tpp = tps[ti % 2]
                                        ti += 1
                                        for j in range(j0, j1):
                                            kb.I("pe", "transpose", [qkT, c["identb"]], [tpp], out=tpp[:, j - j0, :],
                                                 in_=qkT[:, 1, j * 128:(j + 1) * 128], identity=c["identb"][:], inc=(j == j1 - 1))
                                        kb.copy("dve", [tpp], [ktok], ktok[:, j0:j1, :], tpp[:, 0:j1 - j0, :])

                            do_proj(0)
                            for fi in range(6):
                                if fi + 1 < 6:
                                    do_proj(fi + 1)
                                if fi < 4:
                                    do_post(fi)
                        if stop == 3:
                            return
                        if kh + 1 < 8:
                            load_wsl(kh + 1)
                        with kb.scope():
                            B = lambda nm: kb.sb(nm, [128, 128], BF16)
                            Fp = lambda nm: kb.sb(nm, [128, 128])
                            otok = [kb.sb("otok%d" % j, [128, NT, 128]) for j in range(2)]
                            us_all = [kb.sb("us_all%d" % j, [128, NT, 128]) for j in range(2)]
                            wT_all = [kb.sb("wT_all%d" % j, [128, NT, 128], BF16) for j in range(2)]
                            aT_all = [kb.sb("aT_all%d" % j, [128, NT, 128], BF16) for j in range(2)]
                            kd_all = [kb.sb("kd_all%d" % j, [128, NT, 128], BF16) for j in range(2)]
                            NCH = 6
                            X = [kb.ps("pX%d" % i, [128, 4, 128]) for i in range(NCH)]
                            TPb = kb.ps("TPb", [128, 8, 128], BF16)
                            CH = []
                            for ci in range(NCH):
                                CH.append(dict(
                                    Rm=Fp("Rm%d" % ci), Dm=Fp("Dm%d" % ci), attn=B("attn%d" % ci),
                                    MNA=[kb.sb("MNA%d_0" % ci, [128, 3, 128], BF16), kb.sb("MNA%d_1" % ci, [128, 3, 128], BF16)],
                                    vb=B("vb%d" % ci), kbg=B("kbg%d" % ci), X=X[ci]))
                            GsAs = [(Fp("Gs%d" % i), Fp("As%d" % i)) for i in range(NCH // 2)]
                            for j in range(2):
                                hd = 2 * kh + j
                                kb.I("pool", "tensor_tensor", [ktok, ekd], [kd_all[j]], out=kd_all[j][:], in0=ktok[:],
                                     in1=ekd[:, :, hd:hd + 1].to_broadcast([128, NT, 128]), op=ALU.mult)
                            Ssb = [Fp("S0"), Fp("S1")]
                            Sbf = [B("Sb0"), B("Sb1")]
                            vnew = [B("vnew0"), B("vnew1")]
                            o1 = [Fp("o1_0"), Fp("o1_1")]
                            P2b = kb.ps("pP2", [128, 4, 128])
                            for j in range(2):
                                kb.I("pool", "memset", [], [Ssb[j]], Ssb[j][:], 0.0)
                                kb.I("pool", "memset", [], [Sbf[j]], Sbf[j][:], 0.0)

                            def phase2(tlist):
                                for t in tlist:
                                    ts_ = slice(t * 128, (t + 1) * 128)
                                    for j in range(2):
                                        W = P2b
                                        kb.I("pe", "matmul", [wT_all[j], Sbf[j]], [W], W[:, 2 * j, :], lhsT=wT_all[j][:, t, :], rhs=Sbf[j][:], start=True, stop=True, inc=False)
                                        kb.I("pe", "matmul", [qkT, Sbf[j]], [W], W[:, 2 * j + 1, :], lhsT=qkT[:, 0, ts_], rhs=Sbf[j][:], start=True, stop=True)
                                    yield
                                    for j in range(2):
                                        W = P2b
                                        hd = 2 * kh + j
                                        kb.I("dve", "tensor_tensor", [us_all[j], W], [vnew[j]], out=vnew[j][:], in0=us_all[j][:, t, :], in1=W[:, 2 * j, :], op=ALU.subtract)
                                        kb.I("dve", "tensor_scalar", [W, egc], [o1[j]], out=o1[j][:], in0=W[:, 2 * j + 1, :], scalar1=egc[:, t, hd:hd + 1], scalar2=None, op0=ALU.mult)
                                    yield
                                    for j in range(2):
                                        V_ = P2b
                                        kb.I("pe", "matmul", [aT_all[j], vnew[j]], [V_], V_[:, 2 * j, :], lhsT=aT_all[j][:, t, :], rhs=vnew[j][:], start=True, stop=True, inc=False)
                                        kb.I("pe", "matmul", [kd_all[j], vnew[j]], [V_], V_[:, 2 * j + 1, :], lhsT=kd_all[j][:, t, :], rhs=vnew[j][:], start=True, stop=True)
                                    yield
                                    for j in range(2):
                                        V_ = P2b
                                        hd = 2 * kh + j
                                        kb.I("dve", "scalar_tensor_tensor", [Ssb[j], egl, V_], [Sbf[j]], out=Sbf[j][:], in0=Ssb[j][:], scalar=egl[:, t, hd:hd + 1],
                                             in1=V_[:, 2 * j + 1, :], op0=ALU.mult, op1=ALU.add)
                                        kb.I("dve", "scalar_tensor_tensor", [Ssb[j], egl, V_], [Ssb[j]], out=Ssb[j][:], in0=Ssb[j][:], scalar=egl[:, t, hd:hd + 1],
                                             in1=V_[:, 2 * j + 1, :], op0=ALU.mult, op1=ALU.add)
                                        kb.I("dve", "tensor_tensor", [o1[j], V_], [otok[j]], out=otok[j][:, t, :], in0=o1[j][:], in1=V_[:, 2 * j, :], op=ALU.add)
                                    yield
                            gen2 = iter(())
                            for t0 in range(0, NT, NCH // 2):
                                tl = [t for t in range(t0, t0 + NCH // 2) if t < NT]
                                chains = []
                                for ti_, t in enumerate(tl):
                                    ts_ = slice(t * 128, (t + 1) * 128)
                                    Gs, As = GsAs[ti_]
                                    XG, XA = X[2 * ti_], X[2 * ti_ + 1]
                                    kb.I("pe", "matmul", [qkT], [XG], XG[:, 3, :], lhsT=qkT[:, 1, ts_], rhs=qkT[:, 1, ts_], start=True, stop=True)
                                    kb.I("pe", "matmul", [qkT], [XA], XA[:, 3, :], lhsT=qkT[:, 0, ts_], rhs=qkT[:, 1, ts_], start=True, stop=True)
                                    kb.I("dve", "tensor_tensor", [XG, smask], [Gs], out=Gs[:], in0=XG[:, 3, :], in1=smask[:], op=ALU.mult)
                                    kb.I("dve", "tensor_tensor", [XA, lmask], [As], out=As[:], in0=XA[:, 3, :], in1=lmask[:], op=ALU.mult)
                                    for j in range(2):
                                        chn = dict(CH[ti_ * 2 + j])
                                        chn.update(t=t, j=j, hd=2 * kh + j, Gs=Gs, As=As, slot=ti_ * 2 + j)
                                        chains.append(chn)
                                col = lambda a, q: a[:, q["t"], q["hd"]:q["hd"] + 1]
                                for q in chains:
                                    kb.I("act", "mul", [ut, gg], [q["Rm"]], out=q["Rm"][:], in_=ut[:], mul=col(gg, q))
                                for q in chains:
                                    kb.I("pe", "matmul", [q["Rm"], slt], [q["X"]], q["X"][:, 0, :], lhsT=q["Rm"][:], rhs=slt[:], start=True, stop=True)
                                for q in chains:
                                    kb.I("act", "activation", [q["X"]], [q["Dm"]], out=q["Dm"][:], in_=q["X"][:, 0, :], func=AF.Exp)
                                for q in chains:
                                    kb.I("dve", "scalar_tensor_tensor", [q["Gs"], beta, q["Dm"]], [q["MNA"][0]], out=q["MNA"][0][:, 0, :], in0=q["Gs"][:],
                                         scalar=col(beta, q), in1=q["Dm"][:], op0=ALU.mult, op1=ALU.mult)
                                    kb.I("pool", "tensor_tensor", [q["As"], q["Dm"]], [q["attn"]], out=q["attn"][:], in0=q["As"][:], in1=q["Dm"][:], op=ALU.mult)
                                    kb.copy("act", [c["identb"]], [q["MNA"][0]], q["MNA"][0][:, 2, :], c["identb"][:])
                                for rr0 in range(0, len(chains), 4):
                                    for q in chains[rr0:rr0 + 4]:
                                        sl = q["slot"] - rr0
                                        kb.I("pe", "transpose", [q["MNA"][0], c["identb"]], [TPb], out=TPb[:, 2 * sl, :], in_=q["MNA"][0][:, 0, :], identity=c["identb"][:], inc=False)
                                        kb.I("pe", "transpose", [q["attn"], c["identb"]], [TPb], out=TPb[:, 2 * sl + 1, :], in_=q["attn"][:], identity=c["identb"][:])
                                    for q in chains[rr0:rr0 + 4]:
                                        sl = q["slot"] - rr0
                                        kb.copy("act", [TPb], [q["MNA"][0]], q["MNA"][0][:, 1, :], TPb[:, 2 * sl, :])
                                        kb.copy("act", [TPb], [aT_all[q["j"]]], aT_all[q["j"]][:, q["t"], :], TPb[:, 2 * sl + 1, :])
                                cur = 0
                                for st_ in range(7):
                                    nxt = 1 - cur
                                    next(gen2, None)
                                    for q in chains:
                                        Xq, T_ = q["X"], q["MNA"][cur]
                                        if st_ < 6:
                                            kb.I("pe", "matmul", [T_], [Xq], Xq[:, 0, :], lhsT=T_[:, 1, :], rhs=T_[:, 0, :], start=True, stop=True, inc=False)
                                            kb.I("pe", "matmul", [T_], [Xq], Xq[:, 1:3, :], lhsT=T_[:, 0, :], rhs=T_[:, 1:3, :], start=True, stop=True)
                                        else:
                                            kb.I("pe", "matmul", [T_], [Xq], Xq[:, 2, :], lhsT=T_[:, 0, :], rhs=T_[:, 2, :], start=True, stop=True)
                                    next(gen2, None)
                                    for q in chains:
                                        Xq, T_, Tn = q["X"], q["MNA"][cur], q["MNA"][nxt]
                                        if st_ < 6:
                                            kb.copy("act", [Xq], [Tn], Tn[:, 0:2, :], Xq[:, 0:2, :])
                                        kb.I("dve", "tensor_tensor", [T_, Xq], [Tn], out=Tn[:, 2, :], in0=T_[:, 2, :], in1=Xq[:, 2, :],
                                             op=(ALU.subtract if st_ == 0 else ALU.add))
                                    cur = nxt
                                for q in chains:
                                    j, t = q["j"], q["t"]
                                    kb.I("act", "mul", [vtok, beta], [q["vb"]], out=q["vb"][:], in_=vtok[:, t, j * 128:(j + 1) * 128], mul=col(beta, q))
                                    kb.I("pool", "tensor_scalar", [ktok, bege], [q["kbg"]], out=q["kbg"][:], in0=ktok[:, t, :], scalar1=col(bege, q),
                                         scalar2=None, op0=ALU.mult)
                                for q in chains:
                                    TT = q["MNA"][cur][:, 2, :]
                                    kb.I("pe", "matmul", [q["MNA"][cur], q["vb"]], [q["X"]], q["X"][:, 0, :], lhsT=TT, rhs=q["vb"][:], start=True, stop=True, inc=False)
                                    kb.I("pe", "matmul", [q["kbg"], q["MNA"][cur]], [q["X"]], q["X"][:, 1, :], lhsT=q["kbg"][:], rhs=TT, start=True, stop=True)
                                for q in chains:
                                    j, t = q["j"], q["t"]
                                    kb.copy("act", [q["X"]], [us_all[j]], us_all[j][:, t, :], q["X"][:, 0, :])
                                    kb.copy("dve", [q["X"]], [wT_all[j]], wT_all[j][:, t, :], q["X"][:, 1, :])
                                for _ in gen2:
                                    pass
                                gen2 = phase2(tl)
                            for _ in gen2:
                                pass
                            if stop == 4:
                                return
                            with kb.scope():
                                sqo = us_all[0]
                                ssq = kb.sb("ssq", [128, NT])
                                onb = wT_all[0]
                                tpo = [TPb, TPb]
                                ti = 0
                                for j in range(2):
                                    kb.I("pool", "tensor_tensor", [otok[j]], [sqo], out=sqo[:], in0=otok[j][:], in1=otok[j][:], op=ALU.mult)
                                    kb.I("dve", "tensor_reduce", [sqo], [ssq], out=ssq[:], in_=sqo[:], axis=AX.X, op=ALU.add)
                                    kb.I("dve", "tensor_scalar", [ssq], [ssq], out=ssq[:], in0=ssq[:], scalar1=1.0 / 128, scalar2=1e-6, op0=ALU.mult, op1=ALU.add)
                                    kb.I("act", "sqrt", [ssq], [ssq], out=ssq[:], in_=ssq[:])
                                    kb.I("dve", "reciprocal", [ssq], [ssq], out=ssq[:], in_=ssq[:])
                                    kb.I("dve", "tensor_tensor", [otok[j], ssq], [onb], out=onb[:], in0=otok[j][:],
                                         in1=ssq[:].unsqueeze(2).to_broadcast([128, NT, 128]), op=ALU.mult)
                                    for j0 in range(0, NT, 8):
                                        j1 = min(j0 + 8, NT)
                                        tpp = tpo[ti % 2]
                                        ti += 1
                                        for jj in range(j0, j1):
                                            kb.I("pe", "transpose", [onb, c["identb"]], [tpp], out=tpp[:, jj - j0, :], in_=onb[:, jj, :], identity=c["identb"][:], inc=(jj == j1 - 1))
                                        kb.I("dve", "scalar_tensor_tensor", [tpp, onorm, zsT], [oTk], out=oTk[:, j, j0 * 128:j1 * 128],
                                             in0=tpp[:, 0:j1 - j0, :].rearrange("p a b -> p (a b)"), scalar=onorm[:, 0:1], in1=zsT[:, j, j0 * 128:j1 * 128],
                                             op0=ALU.mult, op1=ALU.mult)
                                for j in range(2):
                                    kb.dma("sp", o_d[s, 2 * kh + j, :, :], oTk[:, j, :])
                if stop == 5:
                    return
                with kb.scope():
                    woutb = kb.sb("gwoutb", [128, 16, D], BF16)
                    stg = [kb.sb("gostg%d" % i, [128, D]) for i in range(2)]
                    for k in range(16):
                        kb.dma("sp", stg[k % 2][:], wout_d[k * 128:(k + 1) * 128, :])
                        kb.copy("act" if k % 2 else "pool", [stg[k % 2]], [woutb], woutb[:, k, :], stg[k % 2][:])
                    oTt = [kb.sb("oTt%d" % i, [128, 16, 128], BF16) for i in range(2)]
                    hts = [kb.sb("goht%d" % i, [128, D]) for i in range(2)]
                    acc = [kb.sb("goacc%d" % i, [128, D]) for i in range(2)]
                    outt = [kb.sb("goout%d" % i, [128, D]) for i in range(2)]
                    st6 = kb.sb("st6", [128, 2, 6])
                    mv = kb.sb("mv", [128, 4])
                    ops2 = [kb.ps("gopp%d" % i, [128, 512]) for i in range(4)]
                    for t in range(NT):
                        ht = hts[t % 2]
                        a = acc[t % 2]
                        ot = oTt[t % 2]
                        kb.dma("sp", ht[:], h_d[r0 + t * 128: r0 + (t + 1) * 128, :])
                        for hq in range(4):
                            kb.dma("act", ot[:, hq * 4:(hq + 1) * 4, :], o_d[s, hq * 4:(hq + 1) * 4, :, t * 128:(t + 1) * 128].rearrange("h p t -> p h t"),
                                   writes=[ot], group="oTt%d" % (t % 2))
                        for hf in range(2):
                            p = ops2[(2 * t + hf) % 4]
                            for k in range(16):
                                kb.I("pe", "matmul", [ot, woutb], [p], p[:], lhsT=ot[:, k, :], rhs=woutb[:, k, hf * 512:(hf + 1) * 512],
                                     start=(k == 0), stop=(k == 15), inc=(k == 15))
                            kb.I("dve", "scalar_tensor_tensor", [ht, p], [a], out=a[:, hf * 512:(hf + 1) * 512], in0=ht[:, hf * 512:(hf + 1) * 512],
                                 scalar=ALPHA, in1=p[:], op0=ALU.mult, op1=ALU.add)
                        layer_norm_tile(kb, a, g_bc, b_bc, outt[t % 2], st6, mv)
                        kb.dma("sp", out_d[r0 + t * 128: r0 + (t + 1) * 128, :], outt[t % 2][:])


def prep_gdn(inp, i):
    f = lambda a: np.ascontiguousarray(a, dtype=np.float32)
    w = inp["odd_w_in"][i]
    cwt = inp["odd_conv_w"][i]
    gw = np.zeros((8, 1024, 768), np.float32)
    gconv = np.zeros((128, 8, 4, 4), np.float32)
    for kh in range(8):
        cols = [np.arange(kh * 128, kh * 128 + 128), 1024 + np.arange(kh * 128, kh * 128 + 128),
                2048 + np.arange(2 * kh * 128, 2 * kh * 128 + 256)]
        zc = 4096 + np.arange(2 * kh * 128, 2 * kh * 128 + 256)
        gw[kh] = w[:, np.concatenate(cols + [zc])]
        cc_ = np.concatenate(cols)
        for fi in range(4):
            gconv[:, kh, fi, :] = cwt[:, cc_[fi * 128:(fi + 1) * 128]].T
    return {
        "gd_w": gw, "gd_wab": f(w[:, 6144:6176]), "gd_conv": gconv.reshape(128, 128).reshape(128, 8, 4, 4),
        "gd_alog": f(inp["odd_a_log"][i]), "gd_dtb": f(inp["odd_dt_bias"][i]), "gd_onorm": f(inp["odd_o_norm"][i]),
        "gd_wout": f(inp["odd_w_out"][i]), "gd_lng": f(inp["ln_g"][2 * i + 1, 0]), "gd_lnb": f(inp["ln_b"][2 * i + 1, 0]),
    }


def perm_expert(w, nk):
    E, R, F_ = w.shape
    return np.ascontiguousarray(w.reshape(E, nk, 128, F_).transpose(0, 2, 1, 3).reshape(E * 128, nk * F_), dtype=np.float32)


def prep_moe(inp, layer, pfx):
    f = lambda a: np.ascontiguousarray(a, dtype=np.float32)
    return {
        pfx + "wr": f(np.concatenate([inp["moe_group_w"][layer], inp["moe_expert_w"][layer]], axis=1)),
        pfx + "br": f(np.concatenate([inp["moe_group_b"][layer], inp["moe_expert_b"][layer]], axis=0)),
        pfx + "wg": perm_expert(inp["moe_w_gate"][layer], 8), pfx + "wu": perm_expert(inp["moe_w_up"][layer], 8),
        pfx + "wd": perm_expert(inp["moe_w_down"][layer], 4),
        pfx + "lng": f(inp["ln_g"][layer, 1]), pfx + "lnb": f(inp["ln_b"][layer, 1]),
    }


def build_program(shapes):
    nc = bass.Bass("TRN2", target_bir_lowering=False)
    kb = KB(nc)
    dd = {}
    for n, (shp, dt) in shapes.items():
        dd[n] = nc.dram_tensor(n, list(shp), dt, kind="ExternalInput").ap()
    out_d = nc.dram_tensor("out", [NTOK, D], F32, kind="ExternalOutput").ap()
    h1_d = kb.dram("h1", [NTOK, D])
    h2_d = kb.dram("h2", [NTOK, D])
    h3_d = kb.dram("h3", [NTOK, D])
    xs_d = kb.dram("xs", [NSLOT, D], BF16)
    ys_d = kb.dram("ys", [NSLOT, D], F32)
    o_d = kb.dram("o_scr", [SPC, 16, 128, TP], BF16)
    c = load_consts(kb, {k[2:]: v for k, v in dd.items() if k.startswith("c_")})
    build_even(kb, c, dd["h0"], dd["ev_win"], dd["ev_wout"], dd["ev_wuk"], dd["ev_wuv"], dd["ev_rgw"], dd["ev_vec"],
               dd["ev_lng"], dd["ev_lnb"], h1_d)
    build_moe(kb, c, h1_d, dd["m0_wr"], dd["m0_br"], dd["m0_wg"], dd["m0_wu"], dd["m0_wd"], dd["m0_lng"], dd["m0_lnb"],
              h2_d, xs_d, ys_d)
    build_gdn(kb, c, h2_d, dd["gd_w"], dd["gd_wab"], dd["gd_conv"], dd["gd_alog"], dd["gd_dtb"], dd["gd_onorm"],
              dd["gd_wout"], dd["gd_lng"], dd["gd_lnb"], h3_d, o_d)
    build_moe(kb, c, h3_d, dd["m1_wr"], dd["m1_br"], dd["m1_wg"], dd["m1_wu"], dd["m1_wd"], dd["m1_lng"], dd["m1_lnb"],
              out_d, xs_d, ys_d)
    kb.finish()
    return nc


def kernel(**inputs):
    import ml_dtypes
    inp = {k: np.asarray(v) for k, v in inputs.items()}
    x = inp["x"].astype(np.float32, copy=False)
    B = x.shape[0]
    hp = np.zeros((B, TP, D), np.float32)
    hp[:, :NMETA] = inp["meta_tokens"][None]
    hp[:, NMETA:T] = x
    shared = {}
    for k, v in make_consts().items():
        shared["c_" + k] = v
    shared.update(prep_even(inp, 0))
    shared.update(prep_gdn(inp, 0))
    shared.update(prep_moe(inp, 0, "m0_"))
    shared.update(prep_moe(inp, 1, "m1_"))
    shapes = {n: (v.shape, BF16 if v.dtype == ml_dtypes.bfloat16 else F32) for n, v in shared.items()}
    shapes["h0"] = ((NTOK, D), F32)
    nc = build_program(shapes)
    in_maps = []
    for ci in range(NCORES):
        m = dict(shared)
        m["h0"] = np.ascontiguousarray(hp[ci * SPC:(ci + 1) * SPC].reshape(NTOK, D))
        in_maps.append(m)
    res = run_bass_kernel_spmd(nc, in_maps, core_ids=list(range(NCORES)))
    outs = [r["out"].reshape(SPC, TP, D)[:, NMETA:T] for r in res.results]
    return np.ascontiguousarray(np.concatenate(outs, axis=0).astype(np.float32))
```

```python
import contextlib
import numpy as np
import concourse.bass as bass
import concourse.mybir as mybir
from concourse.bass_utils import run_bass_kernel_spmd

F32 = mybir.dt.float32
BF16 = mybir.dt.bfloat16
I32 = mybir.dt.int32
AF = mybir.ActivationFunctionType
ALU = mybir.AluOpType
AX = mybir.AxisListType

NCORES = 8
D = 1024
SEQ = 2048
NMETA = 16
T = SEQ + NMETA
TP = 17 * 128
NT = TP // 128
SPC = 4
NTOK = SPC * TP
NTILE = NTOK // 128
ALPHA = 4.0 ** 0.25
NEG = -1.0e30


class Res:
    __slots__ = ("w", "r", "dsem")

    def __init__(self):
        self.w = None
        self.r = {}
        self.dsem = None


class KB:
    def __init__(self, nc):
        self.nc = nc
        self.es = contextlib.ExitStack()
        self.stack = [self.es]
        self.eng = {"pe": nc.tensor, "act": nc.scalar, "dve": nc.vector, "pool": nc.gpsimd, "sp": nc.sync}
        self.sems = {}
        self.cnt = {}
        for k in self.eng:
            self.sems[k] = self.es.enter_context(nc.semaphore("sem_" + k))
            self.cnt[k] = 0
        self.seen = {k: {} for k in self.eng}
        self.res = {}
        self.dcount = {}
        self.free_dsems = []
        self.scope_res = [[]]
        self.nd = 0
        self.n_inst = 0
        self.uid = 0
        self.psum_names = set()

    def sb(self, name, shape, dtype=F32):
        self.uid += 1
        return self.stack[-1].enter_context(self.nc.sbuf_tensor("%s_%d" % (name, self.uid), list(shape), dtype))

    def ps(self, name, shape, dtype=F32):
        self.uid += 1
        nm = "%s_%d" % (name, self.uid)
        self.psum_names.add(nm)
        return self.stack[-1].enter_context(self.nc.psum_tensor(nm, list(shape), dtype))

    def dram(self, name, shape, dtype=F32, kind="Internal"):
        return self.nc.dram_tensor(name, list(shape), dtype, kind=kind).ap()

    @contextlib.contextmanager
    def scope(self):
        st = contextlib.ExitStack()
        self.stack.append(st)
        self.scope_res.append([])
        try:
            yield
        finally:
            self.barrier()
            for key in self.scope_res.pop():
                r = self.res.pop(key, None)
                if r is not None and r.dsem is not None:
                    self.free_dsems.append(r.dsem)
            self.stack.pop()
            st.close()

    def _res(self, ap):
        key = ap if isinstance(ap, str) else ap.name
        r = self.res.get(key)
        if r is None:
            r = self.res[key] = Res()
            self.scope_res[-1].append(key)
        return r

    def _wait(self, e, semkey, val):
        if semkey in self.dcount:
            val = max(val, self.dcount[semkey])
        if self.seen[e].get(semkey, 0) >= val:
            return
        if semkey == e and val > self.cnt[e]:
            return
        self.seen[e][semkey] = val
        self.eng[e].wait_ge(self.sems[semkey], val)

    def _deps(self, e, reads, writes):
        for a in reads:
            r = self._res(a)
            if r.w is not None:
                self._wait(e, *r.w)
        for a in writes:
            r = self._res(a)
            if r.w is not None:
                self._wait(e, *r.w)
            for sk, v in r.r.items():
                self._wait(e, sk, v)

    def _done(self, ev, reads, writes):
        for a in reads:
            r = self._res(a)
            if r.r.get(ev[0], 0) < ev[1]:
                r.r[ev[0]] = ev[1]
        for a in writes:
            r = self._res(a)
            r.w = ev
            r.r = {}

    def I(self, e, fn, reads, writes, *args, inc=True, **kw):
        writes = list(writes) + [a for a in reads if (not isinstance(a, str)) and a.name in self.psum_names]
        self._deps(e, reads, writes)
        ins = getattr(self.eng[e], fn)(*args, **kw)
        if inc:
            self.cnt[e] += 1
            ins.then_inc(self.sems[e], 1)
            self._done((e, self.cnt[e]), reads, writes)
        else:
            self._done((e, self.cnt[e] + 1), reads, writes)
        self.n_inst += 1
        return ins

    def dma(self, q, out, in_, reads=None, writes=None, group=None, indirect=None, **kw):
        reads = [in_] if reads is None else reads
        writes = [out] if writes is None else writes
        dst = self._res(group if group is not None else writes[0])
        if dst.dsem is None:
            if self.free_dsems:
                dst.dsem = self.free_dsems.pop()
            else:
                self.nd += 1
                dst.dsem = "d%d" % self.nd
                self.sems[dst.dsem] = self.es.enter_context(self.nc.semaphore(dst.dsem))
                self.dcount[dst.dsem] = 0
        if group is None:
            self._deps(q, reads, writes)
        else:
            self._deps(q, reads, [])
            for a in writes:
                r = self._res(a)
                if r.w is not None and r.w[0] != dst.dsem:
                    self._wait(q, *r.w)
                for sk, v in r.r.items():
                    self._wait(q, sk, v)
        if indirect is None:
            ins = self.eng[q].dma_start(out=out, in_=in_, **kw)
        else:
            ins = self.eng[q].indirect_dma_start(out=out, in_=in_, **indirect)
        self.dcount[dst.dsem] += 16
        ins.then_inc(self.sems[dst.dsem], 16)
        self._done((dst.dsem, self.dcount[dst.dsem]), reads, writes)
        self.n_inst += 1
        return ins

    def copy(self, e, reads, writes, out, in_):
        return self.I(e, "copy" if e == "act" else "tensor_copy", reads, writes, out=out, in_=in_)

    def barrier(self):
        for e in self.eng:
            for e2 in ("pe", "act", "dve", "pool"):
                if self.cnt[e2]:
                    self._wait(e, e2, self.cnt[e2])
            for sk, c in self.dcount.items():
                if c:
                    self._wait(e, sk, c)

    def finish(self):
        self.barrier()
        self.es.close()


def load_consts(kb, cd):
    c = {}
    for name, shape, dt in (("ident", [128, 128], F32), ("identb", [128, 128], BF16),
                            ("ones", [128, 128], F32), ("sut", [128, 128], F32),
                            ("ramp", [128, 128], F32), ("pidx", [128, 128], F32), ("cmask", [128, 128], F32),
                            ("onesb", [128, 128], BF16), ("cmaskb", [128, 128], BF16)):
        t = kb.sb("c_" + name, shape, dt)
        kb.dma("sp", t[:], cd[name])
        c[name] = t
    return c


def layer_norm_tile(kb, acc, g_bc, b_bc, outt, st6, mv, eng2="pool"):
    for j in range(2):
        kb.I("dve", "bn_stats", [acc], [st6], out=st6[:, j, :], in_=acc[:, j * 512:(j + 1) * 512])
    kb.I("dve", "bn_aggr", [st6], [mv], out=mv[:, 0:2], in_=st6[:].rearrange("p a b -> p (a b)"))
    kb.I("dve", "tensor_scalar", [mv], [mv], out=mv[:, 2:3], in0=mv[:, 1:2], scalar1=1e-5, scalar2=None, op0=ALU.add)
    kb.I("act", "sqrt", [mv], [mv], out=mv[:, 2:3], in_=mv[:, 2:3])
    kb.I("dve", "reciprocal", [mv], [mv], out=mv[:, 2:3], in_=mv[:, 2:3])
    kb.I("dve", "scalar_tensor_tensor", [mv], [mv], out=mv[:, 3:4], in0=mv[:, 0:1], scalar=-1.0, in1=mv[:, 2:3], op0=ALU.mult, op1=ALU.mult)
    kb.I("act", "activation", [acc, mv], [acc], out=acc[:], in_=acc[:], func=AF.Identity, scale=mv[:, 2:3], bias=mv[:, 3:4])
    kb.I("dve", "tensor_tensor", [acc, g_bc], [acc], out=acc[:], in0=acc[:], in1=g_bc[:], op=ALU.mult)
    kb.I(eng2, "tensor_tensor", [acc, b_bc], [outt], out=outt[:], in0=acc[:], in1=b_bc[:], op=ALU.add)


MOE_BS = 4
MOE_BLK = MOE_BS * 128
NBLK = -(-NTOK * 2 // MOE_BLK) + 32
NSLOT = NBLK * MOE_BLK


def build_moe(kb, c, h_d, wr_d, br_d, wg_d, wu_d, wd_d, lng_d, lnb_d, out_d, xs_d, ys_d, ntile=NTILE):
    nc = kb.nc
    nblk = -(-ntile * 128 * 2 // MOE_BLK) + 32
    BS, BLK = MOE_BS, MOE_BLK
    with kb.scope():
        s1i = kb.sb("s1i", [128, ntile], I32)
        s2i = kb.sb("s2i", [128, ntile], I32)
        g1 = kb.sb("g1", [128, ntile])
        g2 = kb.sb("g2", [128, ntile])
        idxe = kb.sb("idxe", [128, nblk], I32)
        g_bc = kb.sb("g_bc", [128, D])
        b_bc = kb.sb("b_bc", [128, D])
        kb.dma("sp", g_bc[:], lng_d.partition_broadcast(128))
        kb.dma("sp", b_bc[:], lnb_d.partition_broadcast(128))
        hts = [kb.sb("ht%d" % i, [128, D]) for i in range(2)]

        with kb.scope():
            wr = kb.sb("wr", [128, 8, 36])
            kb.dma("sp", wr[:], wr_d.rearrange("(k p) e -> p k e", p=128))
            br = kb.sb("br", [128, 36])
            kb.dma("sp", br[:], br_d.partition_broadcast(128))
            L = kb.sb("L", [128, ntile, 36])
            hT = [kb.sb("hT%d" % i, [128, 8, 128]) for i in range(2)]
            tp = [kb.ps("tp%d" % i, [128, 8, 128]) for i in range(2)]
            lg = [kb.ps("lg%d" % i, [128, 36]) for i in range(2)]
            for t in range(ntile):
                ht = hts[t % 2]
                kb.dma("sp", ht[:], h_d[t * 128:(t + 1) * 128, :])
                tpp = tp[t % 2]
                for k in range(8):
                    kb.I("pe", "transpose", [ht, c["ident"]], [tpp], out=tpp[:, k, :], in_=ht[:, k * 128:(k + 1) * 128],
                         identity=c["ident"][:], inc=(k == 7))
                hTt = hT[t % 2]
                kb.I("act", "copy", [tpp], [hTt], out=hTt[:], in_=tpp[:])
                lgp = lg[t % 2]
                for k in range(8):
                    kb.I("pe", "matmul", [hTt, wr], [lgp], lgp[:], lhsT=hTt[:, k, :], rhs=wr[:, k, :],
                         start=(k == 0), stop=(k == 7), inc=(k == 7))
                kb.I("dve", "tensor_tensor", [lgp, br], [L], out=L[:, t, :], in0=lgp[:], in1=br[:], op=ALU.add)

            NE = ntile * 32
            GL = L[:, :, 0:4]
            EL = L[:, :, 4:36]
            gmax = kb.sb("gmax", [128, ntile])
            t4 = kb.sb("t4", [128, ntile, 4])
            goh = kb.sb("goh", [128, ntile, 4])
            gg = kb.sb("gg", [128, ntile])
            ELm = kb.sb("ELm", [128, ntile, 32])
            EL2 = kb.sb("EL2", [128, ntile, 32])
            oh1 = kb.sb("oh1", [128, ntile, 32])
            oh2 = kb.sb("oh2", [128, ntile, 32])
            m1 = kb.sb("m1", [128, ntile])
            m2 = kb.sb("m2", [128, ntile])
            tmp = kb.sb("tmp", [128, ntile])
            V = "dve"
            kb.I(V, "tensor_reduce", [L], [gmax], out=gmax[:], in_=GL, axis=AX.X, op=ALU.max)
            bc4 = lambda a: a[:].unsqueeze(2).to_broadcast([128, ntile, 4])
            bc32 = lambda a: a[:].unsqueeze(2).to_broadcast([128, ntile, 32])
            kb.I(V, "tensor_tensor", [L, gmax], [goh], out=goh[:], in0=GL, in1=bc4(gmax), op=ALU.is_equal)
            kb.I(V, "tensor_tensor", [L, gmax], [t4], out=t4[:], in0=GL, in1=bc4(gmax), op=ALU.subtract)
            kb.I("act", "activation", [t4], [t4], out=t4[:], in_=t4[:], func=AF.Exp)
            kb.I(V, "tensor_reduce", [t4], [gg], out=gg[:], in_=t4[:], axis=AX.X, op=ALU.add)
            kb.I(V, "reciprocal", [gg], [gg], out=gg[:], in_=gg[:])
            kb.I(V, "tensor_scalar", [goh], [t4], out=t4[:], in0=goh[:], scalar1=-NEG, scalar2=NEG,
                 op0=ALU.mult, op1=ALU.add)
            kb.I(V, "tensor_tensor", [L, t4], [ELm], out=ELm[:].rearrange("p t (g e) -> p t g e", g=4),
                 in0=EL.rearrange("p t (g e) -> p t g e", g=4),
                 in1=t4[:].unsqueeze(3).to_broadcast([128, ntile, 4, 8]), op=ALU.add)
            kb.I(V, "tensor_reduce", [ELm], [m1], out=m1[:], in_=ELm[:], axis=AX.X, op=ALU.max)
            kb.I(V, "tensor_tensor", [ELm, m1], [oh1], out=oh1[:], in0=ELm[:], in1=bc32(m1), op=ALU.is_equal)
            kb.I(V, "scalar_tensor_tensor", [oh1, ELm], [EL2], out=EL2[:], in0=oh1[:], scalar=NEG, in1=ELm[:],
                 op0=ALU.mult, op1=ALU.add)
            kb.I(V, "tensor_reduce", [EL2], [m2], out=m2[:], in_=EL2[:], axis=AX.X, op=ALU.max)
            kb.I(V, "tensor_tensor", [EL2, m2], [oh2], out=oh2[:], in0=EL2[:], in1=bc32(m2), op=ALU.is_equal)
            kb.I(V, "tensor_tensor", [m1, m2], [tmp], out=tmp[:], in0=m2[:], in1=m1[:], op=ALU.subtract)
            kb.I("act", "activation", [tmp], [tmp], out=tmp[:], in_=tmp[:], func=AF.Exp)
            kb.I(V, "tensor_scalar", [tmp], [m1], out=m1[:], in0=tmp[:], scalar1=1.0, scalar2=None, op0=ALU.add)
            kb.I(V, "reciprocal", [m1], [m1], out=m1[:], in_=m1[:])
            kb.I(V, "tensor_tensor", [tmp, m1], [m2], out=m2[:], in0=tmp[:], in1=m1[:], op=ALU.mult)
            kb.I(V, "tensor_tensor", [m1, gg], [g1], out=g1[:], in0=m1[:], in1=gg[:], op=ALU.mult)
            kb.I(V, "tensor_tensor", [m2, gg], [g2], out=g2[:], in0=m2[:], in1=gg[:], op=ALU.mult)
            OH = ELm
            kb.I(V, "tensor_tensor", [oh1, oh2], [OH], out=OH[:], in0=oh1[:], in1=oh2[:], op=ALU.add)
            RK = kb.sb("RK", [128, NE])
            CT = kb.sb("CT", [128, ntile, 32])
            OHf = OH[:].rearrange("p t e -> p (t e)")
            CTf = CT[:].rearrange("p t e -> p (t e)")
            pr = [kb.ps("pr%d" % i, [128, 512]) for i in range(2)]
            ci = 0
            for dst, lhs in ((RK[:], c["sut"]), (CTf, c["ones"])):
                for o in range(0, NE, 512):
                    n = min(512, NE - o)
                    p = pr[ci % 2]
                    ci += 1
                    kb.I("pe", "matmul", [lhs, OH], [p], p[:, 0:n], lhsT=lhs[:], rhs=OHf[:, o:o + n],
                         start=True, stop=True)
                    kb.I("act", "copy", [p], [RK if dst is not CTf else CT], out=dst[:, o:o + n], in_=p[:, 0:n])
            base = kb.sb("base", [128, ntile, 32])
            kb.I(V, "memset", [], [base], base[:, 0, :], 0.0)
            for t in range(1, ntile):
                kb.I(V, "tensor_tensor", [base, CT], [base], out=base[:, t, :], in0=base[:, t - 1, :],
                     in1=CT[:, t - 1, :], op=ALU.add)
            tot = kb.sb("tot", [128, 32])
            pad = kb.sb("pad", [128, 32])
            pend = kb.sb("pend", [128, 32])
            one32 = kb.sb("one32", [128, 32])
            kb.I(V, "memset", [], [one32], one32[:], 1.0)
            kb.I(V, "tensor_tensor", [base, CT], [tot], out=tot[:], in0=base[:, ntile - 1, :], in1=CT[:, ntile - 1, :],
                 op=ALU.add)
            thr = kb.sb("thr", [128, nblk])
            kb.I(V, "tensor_scalar", [c["ramp"]], [thr], out=thr[:], in0=c["ramp"][:, 0:nblk], scalar1=float(BLK),
                 scalar2=None, op0=ALU.mult)
            cmp0 = kb.sb("cmp0", [128, 32, nblk])
            kb.I(V, "tensor_tensor", [tot, thr], [cmp0], out=cmp0[:],
                 in0=tot[:].unsqueeze(2).to_broadcast([128, 32, nblk]),
                 in1=thr[:].unsqueeze(1).to_broadcast([128, 32, nblk]), op=ALU.is_gt)
            kb.I(V, "tensor_reduce", [cmp0], [pad], out=pad[:], in_=cmp0[:], axis=AX.X, op=ALU.add)
            kb.I(V, "tensor_scalar", [pad], [pad], out=pad[:], in0=pad[:], scalar1=float(BLK), scalar2=None, op0=ALU.mult)
            kb.I(V, "tensor_tensor_scan", [one32, pad], [pend], out=pend[:], data0=one32[:], data1=pad[:], initial=0.0,
                 op0=ALU.mult, op1=ALU.add)
            kb.I(V, "tensor_tensor", [pend, pad], [pad], out=pad[:], in0=pend[:], in1=pad[:], op=ALU.subtract)
            cmp = kb.sb("cmp", [128, nblk, 32])
            bef = kb.sb("bef", [128, nblk])
            kb.I(V, "tensor_scalar", [c["ramp"]], [bef], out=bef[:], in0=c["ramp"][:, 0:nblk], scalar1=float(BLK),
                 scalar2=None, op0=ALU.mult)
            kb.I(V, "tensor_tensor", [pend, bef], [cmp], out=cmp[:],
                 in0=pend[:].unsqueeze(1).to_broadcast([128, nblk, 32]),
                 in1=bef[:].unsqueeze(2).to_broadcast([128, nblk, 32]), op=ALU.is_le)
            kb.I(V, "tensor_reduce", [cmp], [bef], out=bef[:], in_=cmp[:], axis=AX.X, op=ALU.add)
            kb.I(V, "tensor_scalar", [bef], [bef], out=bef[:], in0=bef[:], scalar1=31.0, scalar2=None, op0=ALU.min)
            ig = kb.sb("ig", [128, nblk])
            kb.I(V, "tensor_scalar", [bef, c["pidx"]], [ig], out=ig[:], in0=bef[:], scalar1=128.0, scalar2=c["pidx"][:, 0:1],
                 op0=ALU.mult, op1=ALU.add)
            usedf = kb.sb("usedf", [128, nblk])
            kb.I(V, "tensor_scalar", [thr, pend], [usedf], out=usedf[:], in0=thr[:], scalar1=pend[:, 31:32], scalar2=None, op0=ALU.is_lt)
            samef = kb.sb("samef", [128, nblk])
            kb.I(V, "memset", [], [samef], samef[:], 0.0)
            kb.I(V, "tensor_tensor", [bef], [samef], out=samef[:, 1:nblk], in0=bef[:, 1:nblk], in1=bef[:, 0:nblk - 1], op=ALU.is_equal)
            kb.I(V, "tensor_scalar", [samef], [samef], out=samef[:], in0=samef[:], scalar1=-1.0, scalar2=1.0, op0=ALU.mult, op1=ALU.add)
            kb.I(V, "tensor_tensor", [usedf, samef], [usedf], out=usedf[:], in0=usedf[:], in1=samef[:], op=ALU.mult)
            kb.I(V, "tensor_tensor", [ig, usedf], [ig], out=ig[:], in0=ig[:], in1=usedf[:], op=ALU.mult)
            kb.I(V, "tensor_scalar", [usedf], [usedf], out=usedf[:], in0=usedf[:], scalar1=-8192.0, scalar2=8192.0, op0=ALU.mult, op1=ALU.add)
            kb.I(V, "tensor_tensor", [ig, usedf], [ig], out=ig[:], in0=ig[:], in1=usedf[:], op=ALU.add)
            kb.I(V, "tensor_copy", [ig], [idxe], out=idxe[:], in_=ig[:])
            SL = EL2
            RK3 = RK[:].rearrange("p (t e) -> p t e", e=32)
            kb.I(V, "tensor_tensor", [RK, base], [SL], out=SL[:], in0=RK3, in1=base[:], op=ALU.add)
            kb.I(V, "tensor_tensor", [SL, pad], [SL], out=SL[:], in0=SL[:],
                 in1=pad[:].unsqueeze(1).to_broadcast([128, ntile, 32]), op=ALU.add)
            for oh, si in ((oh1, s1i), (oh2, s2i)):
                kb.I(V, "tensor_tensor", [SL, oh], [oh], out=oh[:], in0=SL[:], in1=oh[:], op=ALU.mult)
                kb.I(V, "tensor_reduce", [oh], [tmp], out=tmp[:], in_=oh[:], axis=AX.X, op=ALU.add)
                kb.I(V, "tensor_copy", [tmp], [si], out=si[:], in_=tmp[:])

        with kb.scope():
            hbs = [kb.sb("hb%d" % i, [128, D], BF16) for i in range(2)]
            for t in range(ntile):
                ht = hts[t % 2]
                hb = hbs[t % 2]
                kb.dma("sp", ht[:], h_d[t * 128:(t + 1) * 128, :])
                kb.I("act", "copy", [ht], [hb], out=hb[:], in_=ht[:])
                for si in (s1i, s2i):
                    kb.dma("pool", xs_d, hb[:], reads=[hb, si], writes=[xs_d], indirect=dict(
                        out_offset=bass.IndirectOffsetOnAxis(ap=si[:, t:t + 1], axis=0), in_offset=None))

        with kb.scope():
            xin = [[kb.sb("xin%d_%d" % (i, s), [128, D], BF16) for s in range(BS)] for i in range(2)]
            xT = [kb.sb("xT%d" % i, [128, 8, BLK], BF16) for i in range(2)]
            tps = [kb.ps("tps%d" % i, [128, 8, 128], BF16) for i in range(2)]
            stg = [kb.sb("stg%d" % i, [128, 4096]) for i in range(3)]
            wgb = [kb.sb("wgb%d" % i, [128, 8, 512], BF16) for i in range(2)]
            wub = [kb.sb("wub%d" % i, [128, 8, 512], BF16) for i in range(2)]
            wdb = [kb.sb("wdb%d" % i, [128, 4, 1024], BF16) for i in range(2)]
            hgp = [kb.ps("hgp%d" % i, [128, BLK]) for i in range(2)]
            hup = [kb.ps("hup%d" % i, [128, BLK]) for i in range(2)]
            yp = [kb.ps("yp%d" % i, [128, 512]) for i in range(2)]
            sg = [kb.sb("sg%d" % i, [128, BLK]) for i in range(2)]
            hTb = [kb.sb("hTb%d" % i, [128, 4, BLK], BF16) for i in range(2)]
            yb = [kb.sb("yb%d" % i, [128, D]) for i in range(2)]
            sti = 0
            cast_eng = ["dve", "act"]
            bc_reg = nc.gpsimd.alloc_register("moe_bc_%d" % kb.uid)
            nc.gpsimd.reg_mov(bc_reg, 4095)

            def load_weights(b):
                nonlocal sti
                par = b % 2
                for src, dstt in ((wg_d, wgb[par]), (wu_d, wub[par]), (wd_d, wdb[par])):
                    st = stg[sti % 3]
                    kb.dma("pool", st[:], src, reads=[idxe], writes=[st],
                           indirect=dict(out_offset=None, in_offset=bass.IndirectOffsetOnAxis(ap=idxe[:, b:b + 1], axis=0),
                                         bounds_check=bc_reg, oob_is_err=False))
                    dflat = dstt[:].rearrange("p a b -> p (a b)")
                    for half in range(2):
                        ce = cast_eng[(2 * sti + half) % 2]
                        kb.copy(ce, [st], [dstt], dflat[:, half * 2048:(half + 1) * 2048], st[:, half * 2048:(half + 1) * 2048])
                    sti += 1

            def fetch_tokens(b):
                par = b % 2
                for s in range(BS):
                    kb.dma("sp", xin[par][s][:], xs_d[b * BLK + s * 128: b * BLK + (s + 1) * 128, :])

            def load_tokens(b):
                par = b % 2
                for s in range(BS):
                    xi = xin[par][s]
                    tpp = tps[s % 2]
                    for k in range(8):
                        kb.I("pe", "transpose", [xi, c["identb"]], [tpp], out=tpp[:, k, :], in_=xi[:, k * 128:(k + 1) * 128],
                             identity=c["identb"][:], inc=(k == 7))
                    kb.I("dve", "tensor_copy", [tpp], [xT[par]], out=xT[par][:, :, s * 128:(s + 1) * 128], in_=tpp[:])

            def gate_up(b):
                par = b % 2
                for f in range(4):
                    hg = hgp[f % 2]
                    hu = hup[f % 2]
                    for k in range(8):
                        kb.I("pe", "matmul", [wgb[par], xT[par]], [hg], hg[:], lhsT=wgb[par][:, k, f * 128:(f + 1) * 128],
                             rhs=xT[par][:, k, :], start=(k == 0), stop=(k == 7), inc=(k == 7))
                    for k in range(8):
                        kb.I("pe", "matmul", [wub[par], xT[par]], [hu], hu[:], lhsT=wub[par][:, k, f * 128:(f + 1) * 128],
                             rhs=xT[par][:, k, :], start=(k == 0), stop=(k == 7), inc=(k == 7))
                    sgt = sg[f % 2]
                    kb.I("act", "activation", [hg], [sgt], out=sgt[:], in_=hg[:], func=AF.Silu)
                    kb.I("dve", "tensor_tensor", [sgt, hu], [hTb[par]], out=hTb[par][:, f, :], in0=sgt[:], in1=hu[:], op=ALU.mult)

            def down(b):
                par = b % 2
                for s in range(BS):
                    ybt = yb[s % 2]
                    for dh in range(2):
                        y = yp[dh]
                        for f in range(4):
                            kb.I("pe", "matmul", [hTb[par], wdb[par]], [y], y[:], lhsT=hTb[par][:, f, s * 128:(s + 1) * 128],
                                 rhs=wdb[par][:, f, dh * 512:(dh + 1) * 512], start=(f == 0), stop=(f == 3), inc=(f == 3))
                        kb.I("act" if dh == 0 else "dve", "tensor_copy" if dh else "copy", [y], [ybt],
                             out=ybt[:, dh * 512:(dh + 1) * 512], in_=y[:])
                    kb.dma("sp", ys_d[b * BLK + s * 128: b * BLK + (s + 1) * 128, :], ybt[:])

            load_weights(0)
            fetch_tokens(0)
            load_tokens(0)
            for b in range(nblk):
                if b + 1 < nblk:
                    fetch_tokens(b + 1)
                    load_weights(b + 1)
                gate_up(b)
                if b + 1 < nblk:
                    load_tokens(b + 1)
                down(b)

        with kb.scope():
            NS = 3
            y1 = [kb.sb("y1_%d" % i, [128, D]) for i in range(NS)]
            y2 = [kb.sb("y2_%d" % i, [128, D]) for i in range(NS)]
            hts5 = [kb.sb("ht5_%d" % i, [128, D]) for i in range(NS)]
            acc = [kb.sb("acc%d" % i, [128, D]) for i in range(2)]
            outt = [kb.sb("outt%d" % i, [128, D]) for i in range(2)]
            st6 = kb.sb("st6", [128, 2, 6])
            mv = kb.sb("mv", [128, 4])

            def issue(t):
                p = t % NS
                kb.dma("sp", hts5[p][:], h_d[t * 128:(t + 1) * 128, :])
                for yy, si in ((y1[p], s1i), (y2[p], s2i)):
                    kb.dma("pool", yy[:], ys_d, reads=[ys_d, si], writes=[yy], indirect=dict(
                        out_offset=None, in_offset=bass.IndirectOffsetOnAxis(ap=si[:, t:t + 1], axis=0)))
            for t in range(min(NS - 1, ntile)):
                issue(t)
            for t in range(ntile):
                if t + NS - 1 < ntile:
                    issue(t + NS - 1)
                p = t % NS
                ht = hts5[p]
                a = acc[t % 2]
                kb.I("act", "mul", [y1[p], g1], [a], out=a[:], in_=y1[p][:], mul=g1[:, t:t + 1])
                kb.I("dve", "scalar_tensor_tensor", [y2[p], g2, a], [a], out=a[:], in0=y2[p][:], scalar=g2[:, t:t + 1],
                     in1=a[:], op0=ALU.mult, op1=ALU.add)
                kb.I("dve", "scalar_tensor_tensor", [ht, a], [a], out=a[:], in0=ht[:], scalar=ALPHA, in1=a[:],
                     op0=ALU.mult, op1=ALU.add)
                layer_norm_tile(kb, a, g_bc, b_bc, outt[t % 2], st6, mv)
                kb.dma("sp", out_d[t * 128:(t + 1) * 128, :], outt[t % 2][:])

def make_consts():
    import ml_dtypes
    i = np.arange(128)
    return {
        "ident": np.eye(128, dtype=np.float32),
        "identb": np.eye(128, dtype=np.float32).astype(ml_dtypes.bfloat16),
        "ones": np.ones((128, 128), np.float32),
        "sut": (i[:, None] < i[None, :]).astype(np.float32),
        "ramp": np.broadcast_to(i[None, :].astype(np.float32), (128, 128)).copy(),
        "pidx": np.broadcast_to(i[:, None].astype(np.float32), (128, 128)).copy(),
        "cmask": np.where(i[None, :] > i[:, None], NEG, 0.0).astype(np.float32),
        "onesb": np.ones((128, 128), np.float32).astype(ml_dtypes.bfloat16),
        "cmaskb": np.where(i[None, :] > i[:, None], NEG, 0.0).astype(np.float32).astype(ml_dtypes.bfloat16),
    }


CHUNKS = [(0, 512), (512, 512), (1024, 512), (1536, 512), (2048, 128)]
EV_Q, EV_CKV, EV_QI, EV_KI, EV_WI, EV_GATE, EV_XB = 0, 512, 768, 1280, 1344, 1352, 1864


def proj_fm(kb, ps2, cnt, wb, col0, hT, dst_fn, evac):
    for (t0, n) in CHUNKS:
        p = ps2[cnt[0] % 2]
        for k in range(8):
            kb.I("pe", "matmul", [wb, hT], [p], p[:, 0:n], lhsT=wb[:, k, col0:col0 + 128], rhs=hT[:, k, t0:t0 + n],
                 start=(k == 0), stop=(k == 7), inc=(k == 7))
        evac(cnt[0], p[:, 0:n], t0, n)
        cnt[0] += 1


def build_even(kb, c, h_d, win_d, wout_d, wuk_d, wuv_d, rgw_d, vec_d, lng_d, lnb_d, out_d, nseq=SPC):
    with kb.scope():
        winb = kb.sb("winb", [128, 8, 2376], BF16)
        wk2 = kb.sb("wk2", [128, 8, 128], BF16)
        wukb = kb.sb("wukb", [128, 2, 128], BF16)
        wuvb = kb.sb("wuvb", [128, 2, 128], BF16)
        rgwb = kb.sb("rgwb", [128, 8, 128], BF16)
        vec = kb.sb("vec", [128, 40])
        cc = kb.sb("cc", [128, 4])
        g_bc = kb.sb("g_bc", [128, D])
        b_bc = kb.sb("b_bc", [128, D])
        kb.dma("sp", g_bc[:], lng_d.partition_broadcast(128))
        kb.dma("sp", b_bc[:], lnb_d.partition_broadcast(128))
        kb.dma("sp", vec[:], vec_d)
        with kb.scope():
            stg = [kb.sb("wstg%d" % i, [128, 2376]) for i in range(2)]
            for k in range(8):
                st = stg[k % 2]
                kb.dma("sp", st[:], win_d[k * 128:(k + 1) * 128, :])
                kb.copy("act" if k % 2 else "pool", [st], [winb], winb[:, k, :], st[:])
            for j in range(2):
                kb.I("dve", "tensor_copy", [winb], [wk2], out=wk2[:, :, j * 64:(j + 1) * 64], in_=winb[:, :, EV_KI:EV_KI + 64])
            st = stg[0]
            kb.dma("sp", st[:, 0:256], wuk_d.rearrange("(c p) d -> p c d", p=128))
            kb.I("dve", "tensor_copy", [st], [wukb], out=wukb[:], in_=st[:, 0:256].rearrange("p (c d) -> p c d", c=2))
            st = stg[1]
            kb.dma("sp", st[:, 0:256], wuv_d.rearrange("(c p) d -> p c d", p=128))
            kb.I("dve", "tensor_copy", [st], [wuvb], out=wuvb[:], in_=st[:, 0:256].rearrange("p (c d) -> p c d", c=2))
            st = stg[0]
            kb.dma("sp", st[:, 0:1024], rgw_d)
            kb.I("dve", "tensor_copy", [st], [rgwb], out=rgwb[:], in_=st[:, 0:1024].rearrange("p (c d) -> p c d", c=8))
            kb.I("act", "activation", [vec], [cc], out=cc[:], in_=vec[:, 28:32], func=AF.Exp, scale=-1.0)
            kb.I("act", "activation", [cc], [cc], out=cc[:], in_=cc[:], func=AF.Ln, bias=1.0)
            kb.I("dve", "tensor_scalar", [cc], [cc], out=cc[:], in0=cc[:], scalar1=-8.0, scalar2=None, op0=ALU.mult)
        cw = lambda j, ch: vec[:, j * 4 + ch: j * 4 + ch + 1]
        cb = lambda ch: vec[:, 16 + ch:17 + ch]
        ba = lambda ch: vec[:, 20 + ch:21 + ch]
        bx = lambda ch: vec[:, 24 + ch:25 + ch]
        kvn = lambda ch: vec[:, 32 + ch:33 + ch]

        for s in range(nseq):
            r0 = s * TP
            with kb.scope():
                hT = kb.sb("hT", [128, 8, TP], BF16)
                qT = kb.sb("qT", [128, 4, TP], BF16)
                qiT = kb.sb("qiT", [128, 4, TP], BF16)
                kiT2 = kb.sb("kiT2", [128, TP], BF16)
                kT = kb.sb("kT", [128, TP], BF16)
                vtok = kb.sb("vtok", [128, NT, 130], BF16)
                wq = kb.sb("wq", [128, NT, 8])
                kb.I("pool", "memset", [], [vtok], vtok[:, :, 128:130], 1.0)
                with kb.scope():
                    gateT = kb.sb("gateT", [128, 4, TP], BF16)
                    xbT = kb.sb("xbT", [128, 4, TP], BF16)
                    with kb.scope():
                        hts = [kb.sb("eht%d" % i, [128, D]) for i in range(2)]
                        tp = [kb.ps("etp%d" % i, [128, 8, 128]) for i in range(2)]
                        for t in range(NT):
                            ht = hts[t % 2]
                            kb.dma("sp", ht[:], h_d[r0 + t * 128: r0 + (t + 1) * 128, :])
                            tpp = tp[t % 2]
                            for k in range(8):
                                kb.I("pe", "transpose", [ht, c["ident"]], [tpp], out=tpp[:, k, :],
                                     in_=ht[:, k * 128:(k + 1) * 128], identity=c["ident"][:], inc=(k == 7))
                            kb.copy("act" if t % 2 else "dve", [tpp], [hT], hT[:, :, t * 128:(t + 1) * 128], tpp[:])
                    with kb.scope():
                        ckvT = kb.sb("ckvT", [128, 2, TP], BF16)
                        latT = kb.sb("latT", [128, 2, TP], BF16)
                        ps2 = [kb.ps("pp%d" % i, [128, 512]) for i in range(2)]
                        cnt = [0]

                        def mk_evac(dst, ci):
                            def ev(n_, p, t0, n):
                                kb.copy("act" if n_ % 2 else "dve", [p], [dst], dst[:, ci, t0:t0 + n] if ci is not None else dst[:, t0:t0 + n], p)
                            return ev
                        for ci in range(4):
                            proj_fm(kb, ps2, cnt, winb, EV_Q + ci * 128, hT, None, mk_evac(qT, ci))
                            proj_fm(kb, ps2, cnt, winb, EV_QI + ci * 128, hT, None, mk_evac(qiT, ci))
                            proj_fm(kb, ps2, cnt, winb, EV_GATE + ci * 128, hT, None, mk_evac(gateT, ci))
                            proj_fm(kb, ps2, cnt, winb, EV_XB + ci * 128, hT, None, mk_evac(xbT, ci))
                        for ci in range(2):
                            proj_fm(kb, ps2, cnt, winb, EV_CKV + ci * 128, hT, None, mk_evac(ckvT, ci))
                        proj_fm(kb, ps2, cnt, wk2, 0, hT, None, mk_evac(kiT2, None))
                        wps = [kb.ps("wps%d" % i, [128, 8]) for i in range(2)]
                        for t in range(NT):
                            p = wps[t % 2]
                            for k in range(8):
                                kb.I("pe", "matmul", [hT, winb], [p], p[:], lhsT=hT[:, k, t * 128:(t + 1) * 128],
                                     rhs=winb[:, k, EV_WI:EV_WI + 8], start=(k == 0), stop=(k == 7), inc=(k == 7))
                            kb.copy("act", [p], [wq], wq[:, t, :], p[:])
                        sq = kb.sb("sq", [128, 2, 512], BF16)
                        rstd = kb.sb("rstd", [128, 512])
                        for (t0, n) in CHUNKS:
                            kb.I("act", "activation", [ckvT], [sq], out=sq[:, :, 0:n], in_=ckvT[:, :, t0:t0 + n], func=AF.Square)
                            p = ps2[cnt[0] % 2]
                            cnt[0] += 1
                            for ci in range(2):
                                kb.I("pe", "matmul", [c["onesb"], sq], [p], p[:, 0:n], lhsT=c["onesb"][:], rhs=sq[:, ci, 0:n],
                                     start=(ci == 0), stop=(ci == 1), inc=(ci == 1))
                            kb.I("dve", "tensor_scalar", [p], [rstd], out=rstd[:, 0:n], in0=p[:, 0:n], scalar1=1.0 / 256, scalar2=1e-6,
                                 op0=ALU.mult, op1=ALU.add)
                            kb.I("act", "sqrt", [rstd], [rstd], out=rstd[:, 0:n], in_=rstd[:, 0:n])
                            kb.I("dve", "reciprocal", [rstd], [rstd], out=rstd[:, 0:n], in_=rstd[:, 0:n])
                            for ci in range(2):
                                kb.I("dve", "scalar_tensor_tensor", [ckvT, vec, rstd], [latT], out=latT[:, ci, t0:t0 + n],
                                     in0=ckvT[:, ci, t0:t0 + n], scalar=kvn(ci), in1=rstd[:, 0:n], op0=ALU.mult, op1=ALU.mult)
                            p = ps2[cnt[0] % 2]
                            cnt[0] += 1
                            for ci in range(2):
                                kb.I("pe", "matmul", [wukb, latT], [p], p[:, 0:n], lhsT=wukb[:, ci, :], rhs=latT[:, ci, t0:t0 + n],
                                     start=(ci == 0), stop=(ci == 1), inc=(ci == 1))
                            kb.copy("act", [p], [kT], kT[:, t0:t0 + n], p[:, 0:n])
                        for t in range(NT):
                            p = ps2[cnt[0] % 2]
                            cnt[0] += 1
                            for ci in range(2):
                                kb.I("pe", "matmul", [latT, wuvb], [p], p[:, 0:128], lhsT=latT[:, ci, t * 128:(t + 1) * 128],
                                     rhs=wuvb[:, ci, :], start=(ci == 0), stop=(ci == 1), inc=(ci == 1))
                            kb.copy("dve", [p], [vtok], vtok[:, t, 0:128], p[:, 0:128])
                    mixT = hT
                    with kb.scope():
                        HS = TP // 2
                        F = lambda nm: kb.sb(nm, [128, HS])
                        xr, rr, ii, aa, uu, hh, gl = F("xr"), F("rr"), F("ii"), F("aa"), F("uu"), F("hh"), F("gl")
                        xrb = kb.sb("xrb", [128, HS], BF16)
                        carry = kb.sb("carry", [128, 4])
                        gps = [kb.ps("gps%d" % i, [128, 512]) for i in range(4)]
                        gi = 0
                        for ch in range(4):
                            for hf in range(2):
                                o = hf * HS
                                x = xbT[:, ch, :]
                                kb.I("dve", "tensor_scalar", [xbT, vec], [xr], out=xr[:], in0=x[:, o:o + HS], scalar1=cw(3, ch), scalar2=cb(ch),
                                     op0=ALU.mult, op1=ALU.add)
                                for d in (1, 2, 3):
                                    lo = d if hf == 0 else 0
                                    kb.I("dve", "scalar_tensor_tensor", [xbT, vec, xr], [xr], out=xr[:, lo:HS], in0=x[:, o + lo - d:o + HS - d],
                                         scalar=cw(3 - d, ch), in1=xr[:, lo:HS], op0=ALU.mult, op1=ALU.add)
                                kb.copy("pool", [xr], [xrb], xrb[:], xr[:])
                                for (t0, n) in ((0, 512), (512, 512), (1024, 64)):
                                    pa = gps[gi % 4]
                                    px = gps[(gi + 1) % 4]
                                    gi += 2
                                    kb.I("pe", "matmul", [rgwb, xrb], [pa], pa[:, 0:n], lhsT=rgwb[:, ch, :], rhs=xrb[:, t0:t0 + n], start=True, stop=True)
                                    kb.I("pe", "matmul", [rgwb, xrb], [px], px[:, 0:n], lhsT=rgwb[:, 4 + ch, :], rhs=xrb[:, t0:t0 + n], start=True, stop=True)
                                    kb.I("act", "activation", [pa, vec], [rr], out=rr[:, t0:t0 + n], in_=pa[:, 0:n], func=AF.Sigmoid, bias=ba(ch))
                                    kb.I("act", "activation", [px, vec], [ii], out=ii[:, t0:t0 + n], in_=px[:, 0:n], func=AF.Sigmoid, bias=bx(ch))
                                kb.I("act", "activation", [rr, cc], [aa], out=aa[:], in_=rr[:], func=AF.Exp, scale=cc[:, ch:ch + 1])
                                kb.I("pool", "tensor_tensor", [aa], [uu], out=uu[:], in0=aa[:], in1=aa[:], op=ALU.mult)
                                kb.I("act", "activation", [uu], [uu], out=uu[:], in_=uu[:], func=AF.Sqrt, scale=-1.0, bias=1.0)
                                kb.I("pool", "tensor_tensor", [ii, xr], [ii], out=ii[:], in0=ii[:], in1=xr[:], op=ALU.mult)
                                kb.I("pool", "tensor_tensor", [uu, ii], [uu], out=uu[:], in0=uu[:], in1=ii[:], op=ALU.mult)
                                kb.I("dve", "tensor_tensor_scan", [aa, uu, carry], [hh], out=hh[:], data0=aa[:], data1=uu[:],
                                     initial=(0.0 if hf == 0 else carry[:, ch:ch + 1]), op0=ALU.mult, op1=ALU.add)
                                if hf == 0:
                                    kb.I("dve", "tensor_copy", [hh], [carry], out=carry[:, ch:ch + 1], in_=hh[:, HS - 1:HS])
                                g = gateT[:, ch, o:o + HS]
                                kb.I("act", "activation", [gateT], [gl], out=gl[:], in_=g, func=AF.Square)
                                kb.I("pool", "tensor_scalar", [gl], [gl], out=gl[:], in0=gl[:], scalar1=0.044715, scalar2=1.0, op0=ALU.mult, op1=ALU.add)
                                kb.I("pool", "tensor_tensor", [gl, gateT], [gl], out=gl[:], in0=gl[:], in1=g, op=ALU.mult)
                                kb.I("act", "activation", [gl], [gl], out=gl[:], in_=gl[:], func=AF.Sigmoid, scale=1.5957691216)
                                kb.I("pool", "tensor_tensor", [gl, gateT], [gl], out=gl[:], in0=gl[:], in1=g, op=ALU.mult)
                                kb.I("dve", "tensor_tensor", [hh, gl], [mixT], out=mixT[:, 4 + ch, o:o + HS], in0=hh[:], in1=gl[:], op=ALU.mult)
                with kb.scope():
                    score = [kb.sb("score%d" % i, [128, TP]) for i in range(2)]
                    mask = [kb.sb("mask%d" % i, [128, TP], BF16) for i in range(2)]
                    m8 = [kb.sb("m8_%d" % i, [128, 8]) for i in range(2)]
                    work = kb.sb("work", [128, TP])
                    thrc = kb.sb("thrc", [128, 1])
                    rlb = [kb.sb("rlb%d" % i, [128, 512], BF16) for i in range(4)]
                    dg = [kb.sb("dg%d" % i, [128, 8, 128], BF16) for i in range(1)]
                    lgt = [kb.sb("lgt%d" % i, [128, TP]) for i in range(2)]
                    ee = [kb.sb("ee%d" % i, [128, TP], BF16) for i in range(2)]
                    pT = [kb.sb("pT%d" % i, [128, NT, 128], BF16) for i in range(1)] * 2
                    sm = [kb.sb("sm%d" % i, [128, 4]) for i in range(2)]
                    on = [kb.sb("on%d" % i, [128, 128], BF16) for i in range(2)]
                    sps = [kb.ps("sps%d" % i, [128, 512]) for i in range(2)]
                    lps = [kb.ps("lps%d" % i, [128, 512]) for i in range(2)]
                    tps = [kb.ps("atp%d" % i, [128, 8, 128], BF16) for i in range(2)]
                    ops = kb.ps("ops", [128, 132])
                    scp = kb.ps("scp", [128, 512])
                    kb.I("dve", "memset", [], [thrc], thrc[:], -1.0e29)
                    cnts = {"si": 0, "ti": 0, "li": 0}

                    def indexer(i):
                        sc = score[i % 2]
                        dgt = dg[0]
                        nk = (i + 1) * 128
                        qs = slice(i * 128, (i + 1) * 128)
                        for h in range(8):
                            kb.I("pool", "tensor_scalar", [c["identb"], wq], [dgt], out=dgt[:, h, :], in0=c["identb"][:],
                                 scalar1=wq[:, i, h:h + 1], scalar2=None, op0=ALU.mult)
                        for t0 in range(0, nk, 512):
                            n = min(512, nk - t0)
                            last = (t0 + n == nk)
                            for hh in range(2):
                                for h in range(hh * 4, hh * 4 + 4):
                                    p = sps[cnts["si"] % 2]
                                    cnts["si"] += 1
                                    pr = slice((h % 2) * 64, (h % 2) * 64 + 64)
                                    kb.I("pe", "matmul", [qiT, kiT2], [p], p[:, 0:n], lhsT=qiT[pr, h // 2, qs], rhs=kiT2[pr, t0:t0 + n],
                                         start=True, stop=True)
                                    kb.I("act", "activation", [p], [rlb[h % 4]], out=rlb[h % 4][:, 0:n], in_=p[:, 0:n], func=AF.Relu)
                                for h in range(hh * 4, hh * 4 + 4):
                                    kb.I("pe", "matmul", [dgt, rlb[h % 4]], [scp], scp[:, 0:n], lhsT=dgt[:, h, :], rhs=rlb[h % 4][:, 0:n],
                                         start=(h == 0), stop=(h == 7 and not last), inc=(h % 4 == 3))
                            if last:
                                kb.I("pe", "matmul", [c["identb"], c["cmaskb"]], [scp], scp[:, n - 128:n], lhsT=c["identb"][:], rhs=c["cmaskb"][:],
                                     start=False, stop=True)
                            kb.copy("act", [scp], [sc], sc[:, t0:t0 + n], scp[:, 0:n])
                            yield

                    def topk(i):
                        sc, mk, m = score[i % 2], mask[i % 2], m8[i % 2]
                        nk = (i + 1) * 128
                        if i >= 2:
                            for it in range(32):
                                src = sc if it == 0 else work
                                kb.I("dve", "max", [src], [m], out=m[:], in_=src[:, 0:nk])
                                if it < 31:
                                    kb.I("dve", "match_replace", [m, src], [work], out=work[:, 0:nk], in_to_replace=m[:],
                                         in_values=src[:, 0:nk], imm_value=NEG)
                                if it % 8 == 7 and it < 31:
                                    yield
                            kb.I("dve", "tensor_scalar", [sc, m], [mk], out=mk[:, 0:nk], in0=sc[:, 0:nk], scalar1=m[:, 7:8],
                                 scalar2=None, op0=ALU.is_ge)
                        else:
                            kb.I("dve", "tensor_scalar", [sc, thrc], [mk], out=mk[:, 0:nk], in0=sc[:, 0:nk], scalar1=thrc[:, 0:1],
                                 scalar2=None, op0=ALU.is_ge)

                    def qk(i, h):
                        nk = (i + 1) * 128
                        qs = slice(i * 128, (i + 1) * 128)
                        lg = lgt[h % 2]
                        for t0 in range(0, nk, 512):
                            n = min(512, nk - t0)
                            p = lps[cnts["li"] % 2]
                            cnts["li"] += 1
                            kb.I("pe", "matmul", [qT, kT], [p], p[:, 0:n], lhsT=qT[:, h, qs], rhs=kT[:, t0:t0 + n], start=True, stop=True)
                            kb.I("act", "mul", [p], [lg], out=lg[:, t0:t0 + n], in_=p[:, 0:n], mul=128.0 ** -0.5)

                    def attention(i, gen, geni):
                        mk = mask[i % 2]
                        nk = (i + 1) * 128
                        qs = slice(i * 128, (i + 1) * 128)
                        qk(i, 0)
                        for h in range(4):
                            lg, e_, s_, pT_, on_ = lgt[h % 2], ee[h % 2], sm[h % 2], pT[h % 2], on[h % 2]
                            kb.I("dve", "tensor_reduce", [lg], [s_], out=s_[:, 0:1], in_=lg[:, 0:nk], axis=AX.X, op=ALU.max)
                            kb.I("dve", "tensor_scalar", [s_], [s_], out=s_[:, 1:2], in0=s_[:, 0:1], scalar1=-1.0, scalar2=None, op0=ALU.mult)
                            kb.I("act", "activation", [lg, s_], [e_], out=e_[:, 0:nk], in_=lg[:, 0:nk], func=AF.Exp, bias=s_[:, 1:2])
                            if h < 3:
                                qk(i, h + 1)
                            next(gen, None)
                            next(geni, None)
                            if h % 2:
                                next(geni, None)
                            kb.I("pool", "tensor_tensor", [e_, mk], [e_], out=e_[:, 0:nk], in0=e_[:, 0:nk], in1=mk[:, 0:nk], op=ALU.mult)
                            for j0 in range(0, i + 1, 8):
                                j1 = min(j0 + 8, i + 1)
                                tpp = tps[cnts["ti"] % 2]
                                cnts["ti"] += 1
                                for j in range(j0, j1):
                                    kb.I("pe", "transpose", [e_, c["identb"]], [tpp], out=tpp[:, j - j0, :], in_=e_[:, j * 128:(j + 1) * 128],
                                         identity=c["identb"][:], inc=(j == j1 - 1))
                                kb.copy("act", [tpp], [pT_], pT_[:, j0:j1, :], tpp[:, 0:j1 - j0, :])
                            for j in range(i + 1):
                                kb.I("pe", "matmul", [pT_, vtok], [ops], ops[:, 0:129], lhsT=pT_[:, j, :], rhs=vtok[:, j, 0:129], start=(j == 0), stop=(j == i),
                                     inc=(j == i))
                            kb.I("dve", "reciprocal", [ops], [s_], out=s_[:, 3:4], in_=ops[:, 128:129])
                            kb.I("dve", "tensor_scalar", [ops, s_], [on_], out=on_[:], in0=ops[:, 0:128], scalar1=s_[:, 3:4], scalar2=None, op0=ALU.mult)
                            otb = tps[cnts["ti"] % 2]
                            cnts["ti"] += 1
                            kb.I("pe", "transpose", [on_, c["identb"]], [otb], out=otb[:, 0, :], in_=on_[:], identity=c["identb"][:])
                            kb.copy("act", [otb], [mixT], mixT[:, h, qs], otb[:, 0, :])

                    for _ in indexer(0):
                        pass
                    for _ in indexer(1):
                        pass
                    for _ in topk(0):
                        pass
                    for i in range(NT):
                        gen = topk(i + 1) if i + 1 < NT else iter(())
                        geni = indexer(i + 2) if i + 2 < NT else iter(())
                        attention(i, gen, geni)
                        for _ in geni:
                            pass
                        for _ in gen:
                            pass
                with kb.scope():
                    woutb = kb.sb("woutb", [128, 8, D], BF16)
                    stg = [kb.sb("ostg%d" % i, [128, D]) for i in range(2)]
                    for k in range(8):
                        kb.dma("sp", stg[k % 2][:], wout_d[k * 128:(k + 1) * 128, :])
                        kb.copy("act" if k % 2 else "pool", [stg[k % 2]], [woutb], woutb[:, k, :], stg[k % 2][:])
                    hts = [kb.sb("oht%d" % i, [128, D]) for i in range(2)]
                    acc = [kb.sb("oacc%d" % i, [128, D]) for i in range(2)]
                    outt = [kb.sb("oout%d" % i, [128, D]) for i in range(2)]
                    st6 = kb.sb("st6", [128, 2, 6])
                    mv = kb.sb("mv", [128, 4])
                    ops2 = [kb.ps("opp%d" % i, [128, 512]) for i in range(4)]
                    for t in range(NT):
                        ht = hts[t % 2]
                        a = acc[t % 2]
                        kb.dma("sp", ht[:], h_d[r0 + t * 128: r0 + (t + 1) * 128, :])
                        for hf in range(2):
                            p = ops2[(2 * t + hf) % 4]
                            for k in range(8):
                                kb.I("pe", "matmul", [mixT, woutb], [p], p[:], lhsT=mixT[:, k, t * 128:(t + 1) * 128],
                                     rhs=woutb[:, k, hf * 512:(hf + 1) * 512], start=(k == 0), stop=(k == 7), inc=(k == 7))
                            kb.I("dve", "scalar_tensor_tensor", [ht, p], [a], out=a[:, hf * 512:(hf + 1) * 512], in0=ht[:, hf * 512:(hf + 1) * 512],
                                 scalar=ALPHA, in1=p[:], op0=ALU.mult, op1=ALU.add)
                        layer_norm_tile(kb, a, g_bc, b_bc, outt[t % 2], st6, mv)
                        kb.dma("sp", out_d[r0 + t * 128: r0 + (t + 1) * 128, :], outt[t % 2][:])


def prep_even(inp, i):
    f = lambda a: np.ascontiguousarray(a, dtype=np.float32)
    pc = lambda v, n: f(v.reshape(n, 128).T)
    vec = np.zeros((128, 40), np.float32)
    cwt = inp["even_conv_w"][i]
    for j in range(4):
        vec[:, j * 4:(j + 1) * 4] = pc(cwt[j], 4)
    vec[:, 16:20] = pc(inp["even_conv_b"][i], 4)
    vec[:, 20:24] = pc(inp["even_rg_ba"][i], 4)
    vec[:, 24:28] = pc(inp["even_rg_bx"][i], 4)
    vec[:, 28:32] = pc(inp["even_rg_lambda"][i], 4)
    vec[:, 32:34] = pc(inp["even_kv_norm"][i], 2)
    rgw = np.zeros((128, 8, 128), np.float32)
    for g, nm in enumerate(("even_rg_wa", "even_rg_wx")):
        w = inp[nm][i]
        for n in range(8):
            o = (n % 2) * 64
            rgw[o:o + 64, g * 4 + n // 2, o:o + 64] = w[n]
    return {
        "ev_win": f(inp["even_w_in"][i]), "ev_wout": f(inp["even_w_out"][i]),
        "ev_wuk": f(inp["even_w_uk"][i]), "ev_wuv": f(inp["even_w_uv"][i]),
        "ev_rgw": f(rgw.reshape(128, 1024)), "ev_vec": vec,
        "ev_lng": f(inp["ln_g"][2 * i, 0]), "ev_lnb": f(inp["ln_b"][2 * i, 0]),
    }


def build_gdn(kb, c, h_d, gw_d, wab_d, gconv_d, alog_d, dtb_d, onorm_d, wout_d, lng_d, lnb_d, out_d, o_d, nseq=SPC, stop=0):
    with kb.scope():
        g_bc = kb.sb("g_bc", [128, D])
        b_bc = kb.sb("b_bc", [128, D])
        kb.dma("sp", g_bc[:], lng_d.partition_broadcast(128))
        kb.dma("sp", b_bc[:], lnb_d.partition_broadcast(128))
        wabb = kb.sb("wabb", [128, 8, 32], BF16)
        gconv = kb.sb("gconv", [128, 8, 4, 4])
        nA = kb.sb("nA", [128, 16])
        dtb = kb.sb("dtb", [128, 16])
        onorm = kb.sb("onorm", [128, 1])
        eps6 = kb.sb("eps6", [128, 1])
        kb.I("pool", "memset", [], [eps6], eps6[:], 1e-6)
        kb.dma("sp", gconv[:], gconv_d)
        kb.dma("sp", nA[:], alog_d.partition_broadcast(128))
        kb.dma("sp", dtb[:], dtb_d.partition_broadcast(128))
        kb.dma("sp", onorm[:], onorm_d.rearrange("(p o) -> p o", o=1))
        kb.I("act", "activation", [nA], [nA], out=nA[:], in_=nA[:], func=AF.Exp)
        kb.I("dve", "tensor_scalar", [nA], [nA], out=nA[:], in0=nA[:], scalar1=-1.0, scalar2=None, op0=ALU.mult)
        ut = kb.sb("ut", [128, 128])
        slt = kb.sb("slt", [128, 128])
        lmask = kb.sb("lmask", [128, 128])
        smask = kb.sb("smask", [128, 128])
        kb.I("dve", "tensor_tensor", [c["sut"], c["ident"]], [ut], out=ut[:], in0=c["sut"][:], in1=c["ident"][:], op=ALU.add)
        kb.I("dve", "tensor_scalar", [ut], [slt], out=slt[:], in0=ut[:], scalar1=-1.0, scalar2=1.0, op0=ALU.mult, op1=ALU.add)
        kb.I("dve", "tensor_copy", [slt], [smask], out=smask[:], in_=slt[:])
        kb.I("dve", "tensor_tensor", [slt, c["ident"]], [lmask], out=lmask[:], in0=slt[:], in1=c["ident"][:], op=ALU.add)
        with kb.scope():
            st = kb.sb("abstg", [128, 8, 32])
            kb.dma("sp", st[:], wab_d.rearrange("(k p) e -> p k e", p=128))
            kb.I("dve", "tensor_copy", [st], [wabb], out=wabb[:], in_=st[:])

        for s in range(nseq):
            r0 = s * TP
            with kb.scope():
                hT = kb.sb("hT", [128, 8, TP], BF16)
                with kb.scope():
                    hts = [kb.sb("ght%d" % i, [128, D]) for i in range(2)]
                    tp = [kb.ps("gtp%d" % i, [128, 8, 128]) for i in range(2)]
                    for t in range(NT):
                        ht = hts[t % 2]
                        kb.dma("sp", ht[:], h_d[r0 + t * 128: r0 + (t + 1) * 128, :])
                        tpp = tp[t % 2]
                        for k in range(8):
                            kb.I("pe", "transpose", [ht, c["ident"]], [tpp], out=tpp[:, k, :],
                                 in_=ht[:, k * 128:(k + 1) * 128], identity=c["ident"][:], inc=(k == 7))
                        kb.copy("act" if t % 2 else "dve", [tpp], [hT], hT[:, :, t * 128:(t + 1) * 128], tpp[:])
                if stop == 1:
                    return
                S3 = lambda nm: kb.sb(nm, [128, NT, 16])
                gg, beta, gc, egc, ekd, egl, bege = S3("gg"), S3("beta"), S3("gc"), S3("egc"), S3("ekd"), S3("egl"), S3("bege")
                with kb.scope():
                    abp = [kb.ps("abp%d" % i, [128, 32]) for i in range(2)]
                    ab = kb.sb("ab", [128, NT, 32])
                    tmp = S3("tmpa")
                    for t in range(NT):
                        p = abp[t % 2]
                        for k in range(8):
                            kb.I("pe", "matmul", [hT, wabb], [p], p[:], lhsT=hT[:, k, t * 128:(t + 1) * 128], rhs=wabb[:, k, :],
                                 start=(k == 0), stop=(k == 7), inc=(k == 7))
                        kb.copy("act", [p], [ab], ab[:, t, :], p[:])
                    bcT = lambda a: a[:].unsqueeze(1).to_broadcast([128, NT, 16])
                    kb.I("dve", "tensor_tensor", [ab, dtb], [gg], out=gg[:], in0=ab[:, :, 0:16], in1=bcT(dtb), op=ALU.add)
                    kb.I("dve", "tensor_scalar", [gg], [tmp], out=tmp[:], in0=gg[:], scalar1=-1.0, scalar2=None, op0=ALU.mult)
                    kb.I("dve", "tensor_tensor", [gg, tmp], [tmp], out=tmp[:], in0=gg[:], in1=tmp[:], op=ALU.max)
                    kb.I("act", "activation", [tmp], [tmp], out=tmp[:], in_=tmp[:], func=AF.Exp, scale=-1.0)
                    kb.I("act", "activation", [tmp], [tmp], out=tmp[:], in_=tmp[:], func=AF.Ln, bias=1.0)
                    kb.I("dve", "tensor_scalar", [gg], [gg], out=gg[:], in0=gg[:], scalar1=0.0, scalar2=None, op0=ALU.max)
                    kb.I("dve", "tensor_tensor", [gg, tmp], [gg], out=gg[:], in0=gg[:], in1=tmp[:], op=ALU.add)
                    kb.I("dve", "tensor_tensor", [gg, nA], [gg], out=gg[:], in0=gg[:], in1=bcT(nA), op=ALU.mult)
                    kb.I("act", "activation", [ab], [beta], out=beta[:], in_=ab[:, :, 16:32], func=AF.Sigmoid)
                    for t in range(NT):
                        p = abp[t % 2]
                        kb.I("pe", "matmul", [ut, gg], [p], p[:, 0:16], lhsT=ut[:], rhs=gg[:, t, :], start=True, stop=True, inc=False)
                        kb.I("pe", "matmul", [c["ones"], gg], [p], p[:, 16:32], lhsT=c["ones"][:], rhs=gg[:, t, :], start=True, stop=True)
                        kb.copy("act", [p], [gc], gc[:, t, :], p[:, 0:16])
                        kb.copy("dve", [p], [egl], egl[:, t, :], p[:, 16:32])
                    kb.I("dve", "tensor_tensor", [egl, gc], [ekd], out=ekd[:], in0=egl[:], in1=gc[:], op=ALU.subtract)
                    kb.I("act", "activation", [ekd], [ekd], out=ekd[:], in_=ekd[:], func=AF.Exp)
                    kb.I("act", "activation", [egl], [egl], out=egl[:], in_=egl[:], func=AF.Exp)
                    kb.I("act", "activation", [gc], [egc], out=egc[:], in_=gc[:], func=AF.Exp)
                    kb.I("dve", "tensor_tensor", [beta, egc], [bege], out=bege[:], in0=beta[:], in1=egc[:], op=ALU.mult)

                if stop == 2:
                    return
                wsls = [kb.sb("wsl0", [128, 8, 768], BF16)] * 2
                gstg = [kb.sb("gstg%d" % i, [128, 768]) for i in range(2)]

                def load_wsl(kh_):
                    w_ = wsls[kh_ % 2]
                    for k in range(8):
                        kb.dma("sp", gstg[k % 2][:], gw_d[kh_, k * 128:(k + 1) * 128, :])
                        kb.copy("act" if k % 2 else "pool", [gstg[k % 2]], [w_], w_[:, k, :], gstg[k % 2][:])
                load_wsl(0)
                for kh in range(8):
                    with kb.scope():
                        wsl = wsls[kh % 2]
                        qkT = kb.sb("qkT", [128, 2, TP], BF16)
                        zsT = kb.sb("zsT", [128, 2, TP], BF16)
                        ktok = kb.sb("ktok", [128, NT, 128], BF16)
                        vtok = kb.sb("vtok", [128, NT, 256], BF16)
                        oTk = kb.sb("oTk", [128, 2, TP], BF16)
                        with kb.scope():
                            vT = kb.sb("vT", [128, 2, TP], BF16)
                            raws = [kb.sb("raw%d" % i, [128, TP]) for i in range(2)]
                            cvs = [kb.sb("cv%d" % i, [128, TP]) for i in range(2)]
                            sqs = [kb.sb("sq%d" % i, [128, 512], BF16) for i in range(3)]
                            rstds = [kb.sb("rstd%d" % i, [128, 512]) for i in range(3)]
                            ps2 = [kb.ps("gpp%d" % i, [128, 512]) for i in range(2)]
                            ps3 = [kb.ps("gpq%d" % i, [128, 512]) for i in range(2)]
                            tps = [kb.ps("gtq%d" % i, [128, 8, 128], BF16) for i in range(2)]
                            cnt = [0]
                            ti = 0
                            def do_proj(fi):
                                raw = raws[fi % 2]
                                def ev(n_, p, t0, n, fi=fi, raw=raw):
                                    if fi < 4:
                                        kb.copy("act", [p], [raw], raw[:, t0:t0 + n], p)
                                    else:
                                        kb.I("act", "activation", [p], [zsT], out=zsT[:, fi - 4, t0:t0 + n], in_=p, func=AF.Silu)
                                proj_fm(kb, ps2, cnt, wsl, fi * 128, hT, None, ev)

                            def do_post(fi):
                                nonlocal ti
                                raw, cv = raws[fi % 2], cvs[fi % 2]
                                cwc = lambda j: gconv[:, kh, fi, j:j + 1]
                                kb.I("dve", "tensor_scalar", [raw, gconv], [cv], out=cv[:], in0=raw[:], scalar1=cwc(3), scalar2=None, op0=ALU.mult)
                                for d in (1, 2, 3):
                                    kb.I("dve", "scalar_tensor_tensor", [raw, gconv, cv], [cv], out=cv[:, d:TP], in0=raw[:, 0:TP - d],
                                         scalar=cwc(3 - d), in1=cv[:, d:TP], op0=ALU.mult, op1=ALU.add)
                                if fi >= 2:
                                    kb.I("act", "activation", [cv], [vT], out=vT[:, fi - 2, :], in_=cv[:], func=AF.Silu)
                                    for j0 in range(0, NT, 8):
                                        j1 = min(j0 + 8, NT)
                                        tpp = tps[ti % 2]
                                        ti += 1
                                        for j in range(j0, j1):
                                            kb.I("pe", "transpose", [vT, c["identb"]], [tpp], out=tpp[:, j - j0, :],
                                                 in_=vT[:, fi - 2, j * 128:(j + 1) * 128], identity=c["identb"][:], inc=(j == j1 - 1))
                                        kb.copy("dve", [tpp], [vtok], vtok[:, j0:j1, (fi - 2) * 128:(fi - 1) * 128], tpp[:, 0:j1 - j0, :])
                                    return
                                kb.I("act", "activation", [cv], [cv], out=cv[:], in_=cv[:], func=AF.Silu)
                                for ci_, (t0, n) in enumerate(CHUNKS):
                                    sq, rstd = sqs[ci_ % 3], rstds[ci_ % 3]
                                    kb.I("pool", "tensor_tensor", [cv], [sq], out=sq[:, 0:n], in0=cv[:, t0:t0 + n], in1=cv[:, t0:t0 + n], op=ALU.mult)
                                    p = ps3[ci_ % 2]
                                    kb.I("pe", "matmul", [c["onesb"], sq], [p], p[:, 0:n], lhsT=c["onesb"][:], rhs=sq[:, 0:n], start=True, stop=True)
                                    kb.I("act", "activation", [p], [rstd], out=rstd[:, 0:n], in_=p[:, 0:n], func=AF.Ln, bias=eps6[:, 0:1])
                                    kb.I("act", "activation", [rstd], [rstd], out=rstd[:, 0:n], in_=rstd[:, 0:n], func=AF.Exp, scale=-0.5)
                                    kb.I("dve", "scalar_tensor_tensor", [cv, rstd], [qkT], out=qkT[:, fi, t0:t0 + n], in0=cv[:, t0:t0 + n],
                                         scalar=(128.0 ** -0.5 if fi == 0 else 1.0), in1=rstd[:, 0:n], op0=ALU.mult, op1=ALU.mult)
                                if fi == 1:
                                    for j0 in range(0, NT, 8):
                                        j1 = min(j0 + 8, NT)
                                        tpp = tps[ti % 2]
                                        ti += 1
                                        for j in range(j0, j1):
                                            kb.I("pe", "transpose", [qkT, c["identb"]], [tpp], out=tpp[:, j - j0, :],
                                                 in_=qkT[:, 1, j * 128:(j + 1) * 128], identity=c["identb"][:], inc=(j == j1 - 1))
                                        kb.copy("dve", [tpp], [ktok], ktok[:, j0:j1, :], tpp[:, 0:j1 - j0, :])

                            do_proj(0)
                            for fi in range(6):
                                if fi + 1 < 6:
                                    do_proj(fi + 1)
                                if fi < 4:
                                    do_post(fi)
                        if stop == 3:
                            return
                        if kh + 1 < 8:
                            load_wsl(kh + 1)
                        with kb.scope():
                            B = lambda nm: kb.sb(nm, [128, 128], BF16)
                            Fp = lambda nm: kb.sb(nm, [128, 128])
                            otok = [kb.sb("otok%d" % j, [128, NT, 128]) for j in range(2)]
                            us_all = [kb.sb("us_all%d" % j, [128, NT, 128]) for j in range(2)]
                            wT_all = [kb.sb("wT_all%d" % j, [128, NT, 128], BF16) for j in range(2)]
                            aT_all = [kb.sb("aT_all%d" % j, [128, NT, 128], BF16) for j in range(2)]
                            kd_all = [kb.sb("kd_all%d" % j, [128, NT, 128], BF16) for j in range(2)]
                            NCH = 6
                            X = [kb.ps("pX%d" % i, [128, 4, 128]) for i in range(NCH)]
                            TPb = kb.ps("TPb", [128, 8, 128], BF16)
                            CH = []
                            for ci in range(NCH):
                                CH.append(dict(
                                    Rm=Fp("Rm%d" % ci), Dm=Fp("Dm%d" % ci), attn=B("attn%d" % ci),
                                    MNA=[kb.sb("MNA%d_0" % ci, [128, 3, 128], BF16), kb.sb("MNA%d_1" % ci, [128, 3, 128], BF16)],
                                    vb=B("vb%d" % ci), kbg=B("kbg%d" % ci), X=X[ci]))
                            GsAs = [(Fp("Gs%d" % i), Fp("As%d" % i)) for i in range(NCH // 2)]
                            for j in range(2):
                                hd = 2 * kh + j
                                kb.I("pool", "tensor_tensor", [ktok, ekd], [kd_all[j]], out=kd_all[j][:], in0=ktok[:],
                                     in1=ekd[:, :, hd:hd + 1].to_broadcast([128, NT, 128]), op=ALU.mult)
                            Ssb = [Fp("S0"), Fp("S1")]
                            Sbf = [B("Sb0"), B("Sb1")]
                            vnew = [B("vnew0"), B("vnew1")]
                            o1 = [Fp("o1_0"), Fp("o1_1")]
                            P2b = kb.ps("pP2", [128, 4, 128])
                            for j in range(2):
                                kb.I("pool", "memset", [], [Ssb[j]], Ssb[j][:], 0.0)
                                kb.I("pool", "memset", [], [Sbf[j]], Sbf[j][:], 0.0)

                            def phase2(tlist):
                                for t in tlist:
                                    ts_ = slice(t * 128, (t + 1) * 128)
                                    for j in range(2):
                                        W = P2b
                                        kb.I("pe", "matmul", [wT_all[j], Sbf[j]], [W], W[:, 2 * j, :], lhsT=wT_all[j][:, t, :], rhs=Sbf[j][:], start=True, stop=True, inc=False)
                                        kb.I("pe", "matmul", [qkT, Sbf[j]], [W], W[:, 2 * j + 1, :], lhsT=qkT[:, 0, ts_], rhs=Sbf[j][:], start=True, stop=True)
                                    yield
                                    for j in range(2):
                                        W = P2b
                                        hd = 2 * kh + j
                                        kb.I("dve", "tensor_tensor", [us_all[j], W], [vnew[j]], out=vnew[j][:], in0=us_all[j][:, t, :], in1=W[:, 2 * j, :], op=ALU.subtract)
                                        kb.I("dve", "tensor_scalar", [W, egc], [o1[j]], out=o1[j][:], in0=W[:, 2 * j + 1, :], scalar1=egc[:, t, hd:hd + 1], scalar2=None, op0=ALU.mult)
                                    yield
                                    for j in range(2):
                                        V_ = P2b
                                        kb.I("pe", "matmul", [aT_all[j], vnew[j]], [V_], V_[:, 2 * j, :], lhsT=aT_all[j][:, t, :], rhs=vnew[j][:], start=True, stop=True, inc=False)
                                        kb.I("pe", "matmul", [kd_all[j], vnew[j]], [V_], V_[:, 2 * j + 1, :], lhsT=kd_all[j][:, t, :], rhs=vnew[j][:], start=True, stop=True)
                                    yield
                                    for j in range(2):
                                        V_ = P2b
                                        hd = 2 * kh + j
                                        kb.I("dve", "scalar_tensor_tensor", [Ssb[j], egl, V_], [Sbf[j]], out=Sbf[j][:], in0=Ssb[j][:], scalar=egl[:, t, hd:hd + 1],
                                             in1=V_[:, 2 * j + 1, :], op0=ALU.mult, op1=ALU.add)
                                        kb.I("dve", "scalar_tensor_tensor", [Ssb[j], egl, V_], [Ssb[j]], out=Ssb[j][:], in0=Ssb[j][:], scalar=egl[:, t, hd:hd + 1],
                                             in1=V_[:, 2 * j + 1, :], op0=ALU.mult, op1=ALU.add)
                                        kb.I("dve", "tensor_tensor", [o1[j], V_], [otok[j]], out=otok[j][:, t, :], in0=o1[j][:], in1=V_[:, 2 * j, :], op=ALU.add)
                                    yield
                            gen2 = iter(())
                            for t0 in range(0, NT, NCH // 2):
                                tl = [t for t in range(t0, t0 + NCH // 2) if t < NT]
                                chains = []
                                for ti_, t in enumerate(tl):
                                    ts_ = slice(t * 128, (t + 1) * 128)
                                    Gs, As = GsAs[ti_]
                                    XG, XA = X[2 * ti_], X[2 * ti_ + 1]
                                    kb.I("pe", "matmul", [qkT], [XG], XG[:, 3, :], lhsT=qkT[:, 1, ts_], rhs=qkT[:, 1, ts_], start=True, stop=True)
                                    kb.I("pe", "matmul", [qkT], [XA], XA[:, 3, :], lhsT=qkT[:, 0, ts_], rhs=qkT[:, 1, ts_], start=True, stop=True)
                                    kb.I("dve", "tensor_tensor", [XG, smask], [Gs], out=Gs[:], in0=XG[:, 3, :], in1=smask[:], op=ALU.mult)
                                    kb.I("dve", "tensor_tensor", [XA, lmask], [As], out=As[:], in0=XA[:, 3, :], in1=lmask[:], op=ALU.mult)
                                    for j in range(2):
                                        chn = dict(CH[ti_ * 2 + j])
                                        chn.update(t=t, j=j, hd=2 * kh + j, Gs=Gs, As=As, slot=ti_ * 2 + j)
                                        chains.append(chn)
                                col = lambda a, q: a[:, q["t"], q["hd"]:q["hd"] + 1]
                                for q in chains:
                                    kb.I("act", "mul", [ut, gg], [q["Rm"]], out=q["Rm"][:], in_=ut[:], mul=col(gg, q))
                                for q in chains:
                                    kb.I("pe", "matmul", [q["Rm"], slt], [q["X"]], q["X"][:, 0, :], lhsT=q["Rm"][:], rhs=slt[:], start=True, stop=True)
                                for q in chains:
                                    kb.I("act", "activation", [q["X"]], [q["Dm"]], out=q["Dm"][:], in_=q["X"][:, 0, :], func=AF.Exp)
                                for q in chains:
                                    kb.I("dve", "scalar_tensor_tensor", [q["Gs"], beta, q["Dm"]], [q["MNA"][0]], out=q["MNA"][0][:, 0, :], in0=q["Gs"][:],
                                         scalar=col(beta, q), in1=q["Dm"][:], op0=ALU.mult, op1=ALU.mult)
                                    kb.I("pool", "tensor_tensor", [q["As"], q["Dm"]], [q["attn"]], out=q["attn"][:], in0=q["As"][:], in1=q["Dm"][:], op=ALU.mult)
                                    kb.copy("act", [c["identb"]], [q["MNA"][0]], q["MNA"][0][:, 2, :], c["identb"][:])
                                for rr0 in range(0, len(chains), 4):
                                    for q in chains[rr0:rr0 + 4]:
                                        sl = q["slot"] - rr0
                                        kb.I("pe", "transpose", [q["MNA"][0], c["identb"]], [TPb], out=TPb[:, 2 * sl, :], in_=q["MNA"][0][:, 0, :], identity=c["identb"][:], inc=False)
                                        kb.I("pe", "transpose", [q["attn"], c["identb"]], [TPb], out=TPb[:, 2 * sl + 1, :], in_=q["attn"][:], identity=c["identb"][:])
                                    for q in chains[rr0:rr0 + 4]:
                                        sl = q["slot"] - rr0
                                        kb.copy("act", [TPb], [q["MNA"][0]], q["MNA"][0][:, 1, :], TPb[:, 2 * sl, :])
                                        kb.copy("act", [TPb], [aT_all[q["j"]]], aT_all[q["j"]][:, q["t"], :], TPb[:, 2 * sl + 1, :])
                                cur = 0
                                for st_ in range(7):
                                    nxt = 1 - cur
                                    next(gen2, None)
                                    for q in chains:
                                        Xq, T_ = q["X"], q["MNA"][cur]
                                        if st_ < 6:
                                            kb.I("pe", "matmul", [T_], [Xq], Xq[:, 0, :], lhsT=T_[:, 1, :], rhs=T_[:, 0, :], start=True, stop=True, inc=False)
                                            kb.I("pe", "matmul", [T_], [Xq], Xq[:, 1:3, :], lhsT=T_[:, 0, :], rhs=T_[:, 1:3, :], start=True, stop=True)
                                        else:
                                            kb.I("pe", "matmul", [T_], [Xq], Xq[:, 2, :], lhsT=T_[:, 0, :], rhs=T_[:, 2, :], start=True, stop=True)
                                    next(gen2, None)
                                    for q in chains:
                                        Xq, T_, Tn = q["X"], q["MNA"][cur], q["MNA"][nxt]
                                        if st_ < 6:
                                            kb.copy("act", [Xq], [Tn], Tn[:, 0:2, :], Xq[:, 0:2, :])
                                        kb.I("dve", "tensor_tensor", [T_, Xq], [Tn], out=Tn[:, 2, :], in0=T_[:, 2, :], in1=Xq[:, 2, :],
                                             op=(ALU.subtract if st_ == 0 else ALU.add))
                                    cur = nxt
                                for q in chains:
                                    j, t = q["j"], q["t"]
                                    kb.I("act", "mul", [vtok, beta], [q["vb"]], out=q["vb"][:], in_=vtok[:, t, j * 128:(j + 1) * 128], mul=col(beta, q))
                                    kb.I("pool", "tensor_scalar", [ktok, bege], [q["kbg"]], out=q["kbg"][:], in0=ktok[:, t, :], scalar1=col(bege, q),
                                         scalar2=None, op0=ALU.mult)
                                for q in chains:
                                    TT = q["MNA"][cur][:, 2, :]
                                    kb.I("pe", "matmul", [q["MNA"][cur], q["vb"]], [q["X"]], q["X"][:, 0, :], lhsT=TT, rhs=q["vb"][:], start=True, stop=True, inc=False)
                                    kb.I("pe", "matmul", [q["kbg"], q["MNA"][cur]], [q["X"]], q["X"][:, 1, :], lhsT=q["kbg"][:], rhs=TT, start=True, stop=True)
                                for q in chains:
                                    j, t = q["j"], q["t"]
                                    kb.copy("act", [q["X"]], [us_all[j]], us_all[j][:, t, :], q["X"][:, 0, :])
                                    kb.copy("dve", [q["X"]], [wT_all[j]], wT_all[j][:, t, :], q["X"][:, 1, :])
                                for _ in gen2:
                                    pass
                                gen2 = phase2(tl)
                            for _ in gen2:
                                pass
                            if stop == 4:
                                return
                            with kb.scope():
                                sqo = us_all[0]
                                ssq = kb.sb("ssq", [128, NT])
                                onb = wT_all[0]
                                tpo = [TPb, TPb]
                                ti = 0
                                for j in range(2):
                                    kb.I("pool", "tensor_tensor", [otok[j]], [sqo], out=sqo[:], in0=otok[j][:], in1=otok[j][:], op=ALU.mult)
                                    kb.I("dve", "tensor_reduce", [sqo], [ssq], out=ssq[:], in_=sqo[:], axis=AX.X, op=ALU.add)
                                    kb.I("dve", "tensor_scalar", [ssq], [ssq], out=ssq[:], in0=ssq[:], scalar1=1.0 / 128, scalar2=1e-6, op0=ALU.mult, op1=ALU.add)
                                    kb.I("act", "sqrt", [ssq], [ssq], out=ssq[:], in_=ssq[:])
                                    kb.I("dve", "reciprocal", [ssq], [ssq], out=ssq[:], in_=ssq[:])
                                    kb.I("dve", "tensor_tensor", [otok[j], ssq], [onb], out=onb[:], in0=otok[j][:],
                                         in1=ssq[:].unsqueeze(2).to_broadcast([128, NT, 128]), op=ALU.mult)
                                    for j0 in range(0, NT, 8):
                                        j1 = min(j0 + 8, NT)
                                        tpp = tpo[ti % 2]
                                        ti += 1
                                        for jj in range(j0, j1):
                                            kb.I("pe", "transpose", [onb, c["identb"]], [tpp], out=tpp[:, jj - j0, :], in_=onb[:, jj, :], identity=c["identb"][:], inc=(jj == j1 - 1))
                                        kb.I("dve", "scalar_tensor_tensor", [tpp, onorm, zsT], [oTk], out=oTk[:, j, j0 * 128:j1 * 128],
                                             in0=tpp[:, 0:j1 - j0, :].rearrange("p a b -> p (a b)"), scalar=onorm[:, 0:1], in1=zsT[:, j, j0 * 128:j1 * 128],
                                             op0=ALU.mult, op1=ALU.mult)
                                for j in range(2):
                                    kb.dma("sp", o_d[s, 2 * kh + j, :, :], oTk[:, j, :])
                if stop == 5:
                    return
                with kb.scope():
                    woutb = kb.sb("gwoutb", [128, 16, D], BF16)
                    stg = [kb.sb("gostg%d" % i, [128, D]) for i in range(2)]
                    for k in range(16):
                        kb.dma("sp", stg[k % 2][:], wout_d[k * 128:(k + 1) * 128, :])
                        kb.copy("act" if k % 2 else "pool", [stg[k % 2]], [woutb], woutb[:, k, :], stg[k % 2][:])
                    oTt = [kb.sb("oTt%d" % i, [128, 16, 128], BF16) for i in range(2)]
                    hts = [kb.sb("goht%d" % i, [128, D]) for i in range(2)]
                    acc = [kb.sb("goacc%d" % i, [128, D]) for i in range(2)]
                    outt = [kb.sb("goout%d" % i, [128, D]) for i in range(2)]
                    st6 = kb.sb("st6", [128, 2, 6])
                    mv = kb.sb("mv", [128, 4])
                    ops2 = [kb.ps("gopp%d" % i, [128, 512]) for i in range(4)]
                    for t in range(NT):
                        ht = hts[t % 2]
                        a = acc[t % 2]
                        ot = oTt[t % 2]
                        kb.dma("sp", ht[:], h_d[r0 + t * 128: r0 + (t + 1) * 128, :])
                        for hq in range(4):
                            kb.dma("act", ot[:, hq * 4:(hq + 1) * 4, :], o_d[s, hq * 4:(hq + 1) * 4, :, t * 128:(t + 1) * 128].rearrange("h p t -> p h t"),
                                   writes=[ot], group="oTt%d" % (t % 2))
                        for hf in range(2):
                            p = ops2[(2 * t + hf) % 4]
                            for k in range(16):
                                kb.I("pe", "matmul", [ot, woutb], [p], p[:], lhsT=ot[:, k, :], rhs=woutb[:, k, hf * 512:(hf + 1) * 512],
                                     start=(k == 0), stop=(k == 15), inc=(k == 15))
                            kb.I("dve", "scalar_tensor_tensor", [ht, p], [a], out=a[:, hf * 512:(hf + 1) * 512], in0=ht[:, hf * 512:(hf + 1) * 512],
                                 scalar=ALPHA, in1=p[:], op0=ALU.mult, op1=ALU.add)
                        layer_norm_tile(kb, a, g_bc, b_bc, outt[t % 2], st6, mv)
                        kb.dma("sp", out_d[r0 + t * 128: r0 + (t + 1) * 128, :], outt[t % 2][:])


def prep_gdn(inp, i):
    f = lambda a: np.ascontiguousarray(a, dtype=np.float32)
    w = inp["odd_w_in"][i]
    cwt = inp["odd_conv_w"][i]
    gw = np.zeros((8, 1024, 768), np.float32)
    gconv = np.zeros((128, 8, 4, 4), np.float32)
    for kh in range(8):
        cols = [np.arange(kh * 128, kh * 128 + 128), 1024 + np.arange(kh * 128, kh * 128 + 128),
                2048 + np.arange(2 * kh * 128, 2 * kh * 128 + 256)]
        zc = 4096 + np.arange(2 * kh * 128, 2 * kh * 128 + 256)
        gw[kh] = w[:, np.concatenate(cols + [zc])]
        cc_ = np.concatenate(cols)
        for fi in range(4):
            gconv[:, kh, fi, :] = cwt[:, cc_[fi * 128:(fi + 1) * 128]].T
    return {
        "gd_w": gw, "gd_wab": f(w[:, 6144:6176]), "gd_conv": gconv.reshape(128, 128).reshape(128, 8, 4, 4),
        "gd_alog": f(inp["odd_a_log"][i]), "gd_dtb": f(inp["odd_dt_bias"][i]), "gd_onorm": f(inp["odd_o_norm"][i]),
        "gd_wout": f(inp["odd_w_out"][i]), "gd_lng": f(inp["ln_g"][2 * i + 1, 0]), "gd_lnb": f(inp["ln_b"][2 * i + 1, 0]),
    }


def perm_expert(w, nk):
    E, R, F_ = w.shape
    return np.ascontiguousarray(w.reshape(E, nk, 128, F_).transpose(0, 2, 1, 3).reshape(E * 128, nk * F_), dtype=np.float32)


def prep_moe(inp, layer, pfx):
    f = lambda a: np.ascontiguousarray(a, dtype=np.float32)
    return {
        pfx + "wr": f(np.concatenate([inp["moe_group_w"][layer], inp["moe_expert_w"][layer]], axis=1)),
        pfx + "br": f(np.concatenate([inp["moe_group_b"][layer], inp["moe_expert_b"][layer]], axis=0)),
        pfx + "wg": perm_expert(inp["moe_w_gate"][layer], 8), pfx + "wu": perm_expert(inp["moe_w_up"][layer], 8),
        pfx + "wd": perm_expert(inp["moe_w_down"][layer], 4),
        pfx + "lng": f(inp["ln_g"][layer, 1]), pfx + "lnb": f(inp["ln_b"][layer, 1]),
    }


def build_program(shapes):
    nc = bass.Bass("TRN2", target_bir_lowering=False)
    kb = KB(nc)
    dd = {}
    for n, (shp, dt) in shapes.items():
        dd[n] = nc.dram_tensor(n, list(shp), dt, kind="ExternalInput").ap()
    out_d = nc.dram_tensor("out", [NTOK, D], F32, kind="ExternalOutput").ap()
    h1_d = kb.dram("h1", [NTOK, D])
    h2_d = kb.dram("h2", [NTOK, D])
    h3_d = kb.dram("h3", [NTOK, D])
    xs_d = kb.dram("xs", [NSLOT, D], BF16)
    ys_d = kb.dram("ys", [NSLOT, D], F32)
    o_d = kb.dram("o_scr", [SPC, 16, 128, TP], BF16)
    c = load_consts(kb, {k[2:]: v for k, v in dd.items() if k.startswith("c_")})
    build_even(kb, c, dd["h0"], dd["ev_win"], dd["ev_wout"], dd["ev_wuk"], dd["ev_wuv"], dd["ev_rgw"], dd["ev_vec"],
               dd["ev_lng"], dd["ev_lnb"], h1_d)
    build_moe(kb, c, h1_d, dd["m0_wr"], dd["m0_br"], dd["m0_wg"], dd["m0_wu"], dd["m0_wd"], dd["m0_lng"], dd["m0_lnb"],
              h2_d, xs_d, ys_d)
    build_gdn(kb, c, h2_d, dd["gd_w"], dd["gd_wab"], dd["gd_conv"], dd["gd_alog"], dd["gd_dtb"], dd["gd_onorm"],
              dd["gd_wout"], dd["gd_lng"], dd["gd_lnb"], h3_d, o_d)
    build_moe(kb, c, h3_d, dd["m1_wr"], dd["m1_br"], dd["m1_wg"], dd["m1_wu"], dd["m1_wd"], dd["m1_lng"], dd["m1_lnb"],
              out_d, xs_d, ys_d)
    kb.finish()
    return nc


def kernel(**inputs):
    import ml_dtypes
    inp = {k: np.asarray(v) for k, v in inputs.items()}
    x = inp["x"].astype(np.float32, copy=False)
    B = x.shape[0]
    hp = np.zeros((B, TP, D), np.float32)
    hp[:, :NMETA] = inp["meta_tokens"][None]
    hp[:, NMETA:T] = x
    shared = {}
    for k, v in make_consts().items():
        shared["c_" + k] = v
    shared.update(prep_even(inp, 0))
    shared.update(prep_gdn(inp, 0))
    shared.update(prep_moe(inp, 0, "m0_"))
    shared.update(prep_moe(inp, 1, "m1_"))
    shapes = {n: (v.shape, BF16 if v.dtype == ml_dtypes.bfloat16 else F32) for n, v in shared.items()}
    shapes["h0"] = ((NTOK, D), F32)
    nc = build_program(shapes)
    in_maps = []
    for ci in range(NCORES):
        m = dict(shared)
        m["h0"] = np.ascontiguousarray(hp[ci * SPC:(ci + 1) * SPC].reshape(NTOK, D))
        in_maps.append(m)
    res = run_bass_kernel_spmd(nc, in_maps, core_ids=list(range(NCORES)))
    outs = [r["out"].reshape(SPC, TP, D)[:, NMETA:T] for r in res.results]
    return np.ascontiguousarray(np.concatenate(outs, axis=0).astype(np.float32))
```

```python
import contextlib
import numpy as np
import concourse.bass as bass
import concourse.mybir as mybir
from concourse.bass_utils import run_bass_kernel_spmd

F32 = mybir.dt.float32
BF16 = mybir.dt.bfloat16
I32 = mybir.dt.int32
AF = mybir.ActivationFunctionType
ALU = mybir.AluOpType
AX = mybir.AxisListType

NCORES = 8
D = 1024
SEQ = 2048
NMETA = 16
T = SEQ + NMETA
TP = 17 * 128
NT = TP // 128
SPC = 4
NTOK = SPC * TP
NTILE = NTOK // 128
ALPHA = 4.0 ** 0.25
NEG = -1.0e30


class Res:
    __slots__ = ("w", "r", "dsem")

    def __init__(self):
        self.w = None
        self.r = {}
        self.dsem = None


class KB:
    def __init__(self, nc):
        self.nc = nc
        self.es = contextlib.ExitStack()
        self.stack = [self.es]
        self.eng = {"pe": nc.tensor, "act": nc.scalar, "dve": nc.vector, "pool": nc.gpsimd, "sp": nc.sync}
        self.sems = {}
        self.cnt = {}
        for k in self.eng:
            self.sems[k] = self.es.enter_context(nc.semaphore("sem_" + k))
            self.cnt[k] = 0
        self.seen = {k: {} for k in self.eng}
        self.res = {}
        self.dcount = {}
        self.free_dsems = []
        self.scope_res = [[]]
        self.nd = 0
        self.n_inst = 0
        self.uid = 0
        self.psum_names = set()

    def sb(self, name, shape, dtype=F32):
        self.uid += 1
        return self.stack[-1].enter_context(self.nc.sbuf_tensor("%s_%d" % (name, self.uid), list(shape), dtype))

    def ps(self, name, shape, dtype=F32):
        self.uid += 1
        nm = "%s_%d" % (name, self.uid)
        self.psum_names.add(nm)
        return self.stack[-1].enter_context(self.nc.psum_tensor(nm, list(shape), dtype))

    def dram(self, name, shape, dtype=F32, kind="Internal"):
        return self.nc.dram_tensor(name, list(shape), dtype, kind=kind).ap()

    @contextlib.contextmanager
    def scope(self):
        st = contextlib.ExitStack()
        self.stack.append(st)
        self.scope_res.append([])
        try:
            yield
        finally:
            self.barrier()
            for key in self.scope_res.pop():
                r = self.res.pop(key, None)
                if r is not None and r.dsem is not None:
                    self.free_dsems.append(r.dsem)
            self.stack.pop()
            st.close()

    def _res(self, ap):
        key = ap if isinstance(ap, str) else ap.name
        r = self.res.get(key)
        if r is None:
            r = self.res[key] = Res()
            self.scope_res[-1].append(key)
        return r

    def _wait(self, e, semkey, val):
        if semkey in self.dcount:
            val = max(val, self.dcount[semkey])
        if self.seen[e].get(semkey, 0) >= val:
            return
        if semkey == e and val > self.cnt[e]:
            return
        self.seen[e][semkey] = val
        self.eng[e].wait_ge(self.sems[semkey], val)

    def _deps(self, e, reads, writes):
        for a in reads:
            r = self._res(a)
            if r.w is not None:
                self._wait(e, *r.w)
        for a in writes:
            r = self._res(a)
            if r.w is not None:
                self._wait(e, *r.w)
            for sk, v in r.r.items():
                self._wait(e, sk, v)

    def _done(self, ev, reads, writes):
        for a in reads:
            r = self._res(a)
            if r.r.get(ev[0], 0) < ev[1]:
                r.r[ev[0]] = ev[1]
        for a in writes:
            r = self._res(a)
            r.w = ev
            r.r = {}

    def I(self, e, fn, reads, writes, *args, inc=True, **kw):
        writes = list(writes) + [a for a in reads if (not isinstance(a, str)) and a.name in self.psum_names]
        self._deps(e, reads, writes)
        ins = getattr(self.eng[e], fn)(*args, **kw)
        if inc:
            self.cnt[e] += 1
            ins.then_inc(self.sems[e], 1)
            self._done((e, self.cnt[e]), reads, writes)
        else:
            self._done((e, self.cnt[e] + 1), reads, writes)
        self.n_inst += 1
        return ins

    def dma(self, q, out, in_, reads=None, writes=None, group=None, indirect=None, **kw):
        reads = [in_] if reads is None else reads
        writes = [out] if writes is None else writes
        dst = self._res(group if group is not None else writes[0])
        if dst.dsem is None:
            if self.free_dsems:
                dst.dsem = self.free_dsems.pop()
            else:
                self.nd += 1
                dst.dsem = "d%d" % self.nd
                self.sems[dst.dsem] = self.es.enter_context(self.nc.semaphore(dst.dsem))
                self.dcount[dst.dsem] = 0
        if group is None:
            self._deps(q, reads, writes)
        else:
            self._deps(q, reads, [])
            for a in writes:
                r = self._res(a)
                if r.w is not None and r.w[0] != dst.dsem:
                    self._wait(q, *r.w)
                for sk, v in r.r.items():
                    self._wait(q, sk, v)
        if indirect is None:
            ins = self.eng[q].dma_start(out=out, in_=in_, **kw)
        else:
            ins = self.eng[q].indirect_dma_start(out=out, in_=in_, **indirect)
        self.dcount[dst.dsem] += 16
        ins.then_inc(self.sems[dst.dsem], 16)
        self._done((dst.dsem, self.dcount[dst.dsem]), reads, writes)
        self.n_inst += 1
        return ins

    def copy(self, e, reads, writes, out, in_):
        return self.I(e, "copy" if e == "act" else "tensor_copy", reads, writes, out=out, in_=in_)

    def barrier(self):
        for e in self.eng:
            for e2 in ("pe", "act", "dve", "pool"):
                if self.cnt[e2]:
                    self._wait(e, e2, self.cnt[e2])
            for sk, c in self.dcount.items():
                if c:
                    self._wait(e, sk, c)

    def finish(self):
        self.barrier()
        self.es.close()


def load_consts(kb, cd):
    c = {}
    for name, shape, dt in (("ident", [128, 128], F32), ("identb", [128, 128], BF16),
                            ("ones", [128, 128], F32), ("sut", [128, 128], F32),
                            ("ramp", [128, 128], F32), ("pidx", [128, 128], F32), ("cmask", [128, 128], F32),
                            ("onesb", [128, 128], BF16), ("cmaskb", [128, 128], BF16)):
        t = kb.sb("c_" + name, shape, dt)
        kb.dma("sp", t[:], cd[name])
        c[name] = t
    return c


def layer_norm_tile(kb, acc, g_bc, b_bc, outt, st6, mv, eng2="pool"):
    for j in range(2):
        kb.I("dve", "bn_stats", [acc], [st6], out=st6[:, j, :], in_=acc[:, j * 512:(j + 1) * 512])
    kb.I("dve", "bn_aggr", [st6], [mv], out=mv[:, 0:2], in_=st6[:].rearrange("p a b -> p (a b)"))
    kb.I("dve", "tensor_scalar", [mv], [mv], out=mv[:, 2:3], in0=mv[:, 1:2], scalar1=1e-5, scalar2=None, op0=ALU.add)
    kb.I("act", "sqrt", [mv], [mv], out=mv[:, 2:3], in_=mv[:, 2:3])
    kb.I("dve", "reciprocal", [mv], [mv], out=mv[:, 2:3], in_=mv[:, 2:3])
    kb.I("dve", "scalar_tensor_tensor", [mv], [mv], out=mv[:, 3:4], in0=mv[:, 0:1], scalar=-1.0, in1=mv[:, 2:3], op0=ALU.mult, op1=ALU.mult)
    kb.I("act", "activation", [acc, mv], [acc], out=acc[:], in_=acc[:], func=AF.Identity, scale=mv[:, 2:3], bias=mv[:, 3:4])
    kb.I("dve", "tensor_tensor", [acc, g_bc], [acc], out=acc[:], in0=acc[:], in1=g_bc[:], op=ALU.mult)
    kb.I(eng2, "tensor_tensor", [acc, b_bc], [outt], out=outt[:], in0=acc[:], in1=b_bc[:], op=ALU.add)


MOE_BS = 4
MOE_BLK = MOE_BS * 128
NBLK = -(-NTOK * 2 // MOE_BLK) + 32
NSLOT = NBLK * MOE_BLK


def build_moe(kb, c, h_d, wr_d, br_d, wg_d, wu_d, wd_d, lng_d, lnb_d, out_d, xs_d, ys_d, ntile=NTILE):
    nc = kb.nc
    nblk = -(-ntile * 128 * 2 // MOE_BLK) + 32
    BS, BLK = MOE_BS, MOE_BLK
    with kb.scope():
        s1i = kb.sb("s1i", [128, ntile], I32)
        s2i = kb.sb("s2i", [128, ntile], I32)
        g1 = kb.sb("g1", [128, ntile])
        g2 = kb.sb("g2", [128, ntile])
        idxe = kb.sb("idxe", [128, nblk], I32)
        g_bc = kb.sb("g_bc", [128, D])
        b_bc = kb.sb("b_bc", [128, D])
        kb.dma("sp", g_bc[:], lng_d.partition_broadcast(128))
        kb.dma("sp", b_bc[:], lnb_d.partition_broadcast(128))
        hts = [kb.sb("ht%d" % i, [128, D]) for i in range(2)]

        with kb.scope():
            wr = kb.sb("wr", [128, 8, 36])
            kb.dma("sp", wr[:], wr_d.rearrange("(k p) e -> p k e", p=128))
            br = kb.sb("br", [128, 36])
            kb.dma("sp", br[:], br_d.partition_broadcast(128))
            L = kb.sb("L", [128, ntile, 36])
            hT = [kb.sb("hT%d" % i, [128, 8, 128]) for i in range(2)]
            tp = [kb.ps("tp%d" % i, [128, 8, 128]) for i in range(2)]
            lg = [kb.ps("lg%d" % i, [128, 36]) for i in range(2)]
            for t in range(ntile):
                ht = hts[t % 2]
                kb.dma("sp", ht[:], h_d[t * 128:(t + 1) * 128, :])
                tpp = tp[t % 2]
                for k in range(8):
                    kb.I("pe", "transpose", [ht, c["ident"]], [tpp], out=tpp[:, k, :], in_=ht[:, k * 128:(k + 1) * 128],
                         identity=c["ident"][:], inc=(k == 7))
                hTt = hT[t % 2]
                kb.I("act", "copy", [tpp], [hTt], out=hTt[:], in_=tpp[:])
                lgp = lg[t % 2]
                for k in range(8):
                    kb.I("pe", "matmul", [hTt, wr], [lgp], lgp[:], lhsT=hTt[:, k, :], rhs=wr[:, k, :],
                         start=(k == 0), stop=(k == 7), inc=(k == 7))
                kb.I("dve", "tensor_tensor", [lgp, br], [L], out=L[:, t, :], in0=lgp[:], in1=br[:], op=ALU.add)

            NE = ntile * 32
            GL = L[:, :, 0:4]
            EL = L[:, :, 4:36]
            gmax = kb.sb("gmax", [128, ntile])
            t4 = kb.sb("t4", [128, ntile, 4])
            goh = kb.sb("goh", [128, ntile, 4])
            gg = kb.sb("gg", [128, ntile])
            ELm = kb.sb("ELm", [128, ntile, 32])
            EL2 = kb.sb("EL2", [128, ntile, 32])
            oh1 = kb.sb("oh1", [128, ntile, 32])
            oh2 = kb.sb("oh2", [128, ntile, 32])
            m1 = kb.sb("m1", [128, ntile])
            m2 = kb.sb("m2", [128, ntile])
            tmp = kb.sb("tmp", [128, ntile])
            V = "dve"
            kb.I(V, "tensor_reduce", [L], [gmax], out=gmax[:], in_=GL, axis=AX.X, op=ALU.max)
            bc4 = lambda a: a[:].unsqueeze(2).to_broadcast([128, ntile, 4])
            bc32 = lambda a: a[:].unsqueeze(2).to_broadcast([128, ntile, 32])
            kb.I(V, "tensor_tensor", [L, gmax], [goh], out=goh[:], in0=GL, in1=bc4(gmax), op=ALU.is_equal)
            kb.I(V, "tensor_tensor", [L, gmax], [t4], out=t4[:], in0=GL, in1=bc4(gmax), op=ALU.subtract)
            kb.I("act", "activation", [t4], [t4], out=t4[:], in_=t4[:], func=AF.Exp)
            kb.I(V, "tensor_reduce", [t4], [gg], out=gg[:], in_=t4[:], axis=AX.X, op=ALU.add)
            kb.I(V, "reciprocal", [gg], [gg], out=gg[:], in_=gg[:])
            kb.I(V, "tensor_scalar", [goh], [t4], out=t4[:], in0=goh[:], scalar1=-NEG, scalar2=NEG,
                 op0=ALU.mult, op1=ALU.add)
            kb.I(V, "tensor_tensor", [L, t4], [ELm], out=ELm[:].rearrange("p t (g e) -> p t g e", g=4),
                 in0=EL.rearrange("p t (g e) -> p t g e", g=4),
                 in1=t4[:].unsqueeze(3).to_broadcast([128, ntile, 4, 8]), op=ALU.add)
            kb.I(V, "tensor_reduce", [ELm], [m1], out=m1[:], in_=ELm[:], axis=AX.X, op=ALU.max)
            kb.I(V, "tensor_tensor", [ELm, m1], [oh1], out=oh1[:], in0=ELm[:], in1=bc32(m1), op=ALU.is_equal)
            kb.I(V, "scalar_tensor_tensor", [oh1, ELm], [EL2], out=EL2[:], in0=oh1[:], scalar=NEG, in1=ELm[:],
                 op0=ALU.mult, op1=ALU.add)
            kb.I(V, "tensor_reduce", [EL2], [m2], out=m2[:], in_=EL2[:], axis=AX.X, op=ALU.max)
            kb.I(V, "tensor_tensor", [EL2, m2], [oh2], out=oh2[:], in0=EL2[:], in1=bc32(m2), op=ALU.is_equal)
            kb.I(V, "tensor_tensor", [m1, m2], [tmp], out=tmp[:], in0=m2[:], in1=m1[:], op=ALU.subtract)
            kb.I("act", "activation", [tmp], [tmp], out=tmp[:], in_=tmp[:], func=AF.Exp)
            kb.I(V, "tensor_scalar", [tmp], [m1], out=m1[:], in0=tmp[:], scalar1=1.0, scalar2=None, op0=ALU.add)
            kb.I(V, "reciprocal", [m1], [m1], out=m1[:], in_=m1[:])
            kb.I(V, "tensor_tensor", [tmp, m1], [m2], out=m2[:], in0=tmp[:], in1=m1[:], op=ALU.mult)
            kb.I(V, "tensor_tensor", [m1, gg], [g1], out=g1[:], in0=m1[:], in1=gg[:], op=ALU.mult)
            kb.I(V, "tensor_tensor", [m2, gg], [g2], out=g2[:], in0=m2[:], in1=gg[:], op=ALU.mult)
            OH = ELm
            kb.I(V, "tensor_tensor", [oh1, oh2], [OH], out=OH[:], in0=oh1[:], in1=oh2[:], op=ALU.add)
            RK = kb.sb("RK", [128, NE])
            CT = kb.sb("CT", [128, ntile, 32])
            OHf = OH[:].rearrange("p t e -> p (t e)")
            CTf = CT[:].rearrange("p t e -> p (t e)")
            pr = [kb.ps("pr%d" % i, [128, 512]) for i in range(2)]
            ci = 0
            for dst, lhs in ((RK[:], c["sut"]), (CTf, c["ones"])):
                for o in range(0, NE, 512):
                    n = min(512, NE - o)
                    p = pr[ci % 2]
                    ci += 1
                    kb.I("pe", "matmul", [lhs, OH], [p], p[:, 0:n], lhsT=lhs[:], rhs=OHf[:, o:o + n],
                         start=True, stop=True)
                    kb.I("act", "copy", [p], [RK if dst is not CTf else CT], out=dst[:, o:o + n], in_=p[:, 0:n])
            base = kb.sb("base", [128, ntile, 32])
            kb.I(V, "memset", [], [base], base[:, 0, :], 0.0)
            for t in range(1, ntile):
                kb.I(V, "tensor_tensor", [base, CT], [base], out=base[:, t, :], in0=base[:, t - 1, :],
                     in1=CT[:, t - 1, :], op=ALU.add)
            tot = kb.sb("tot", [128, 32])
            pad = kb.sb("pad", [128, 32])
            pend = kb.sb("pend", [128, 32])
            one32 = kb.sb("one32", [128, 32])
            kb.I(V, "memset", [], [one32], one32[:], 1.0)
            kb.I(V, "tensor_tensor", [base, CT], [tot], out=tot[:], in0=base[:, ntile - 1, :], in1=CT[:, ntile - 1, :],
                 op=ALU.add)
            thr = kb.sb("thr", [128, nblk])
            kb.I(V, "tensor_scalar", [c["ramp"]], [thr], out=thr[:], in0=c["ramp"][:, 0:nblk], scalar1=float(BLK),
                 scalar2=None, op0=ALU.mult)
            cmp0 = kb.sb("cmp0", [128, 32, nblk])
            kb.I(V, "tensor_tensor", [tot, thr], [cmp0], out=cmp0[:],
                 in0=tot[:].unsqueeze(2).to_broadcast([128, 32, nblk]),
                 in1=thr[:].unsqueeze(1).to_broadcast([128, 32, nblk]), op=ALU.is_gt)
            kb.I(V, "tensor_reduce", [cmp0], [pad], out=pad[:], in_=cmp0[:], axis=AX.X, op=ALU.add)
            kb.I(V, "tensor_scalar", [pad], [pad], out=pad[:], in0=pad[:], scalar1=float(BLK), scalar2=None, op0=ALU.mult)
            kb.I(V, "tensor_tensor_scan", [one32, pad], [pend], out=pend[:], data0=one32[:], data1=pad[:], initial=0.0,
                 op0=ALU.mult, op1=ALU.add)
            kb.I(V, "tensor_tensor", [pend, pad], [pad], out=pad[:], in0=pend[:], in1=pad[:], op=ALU.subtract)
            cmp = kb.sb("cmp", [128, nblk, 32])
            bef = kb.sb("bef", [128, nblk])
            kb.I(V, "tensor_scalar", [c["ramp"]], [bef], out=bef[:], in0=c["ramp"][:, 0:nblk], scalar1=float(BLK),
                 scalar2=None, op0=ALU.mult)
            kb.I(V, "tensor_tensor", [pend, bef], [cmp], out=cmp[:],
                 in0=pend[:].unsqueeze(1).to_broadcast([128, nblk, 32]),
                 in1=bef[:].unsqueeze(2).to_broadcast([128, nblk, 32]), op=ALU.is_le)
            kb.I(V, "tensor_reduce", [cmp], [bef], out=bef[:], in_=cmp[:], axis=AX.X, op=ALU.add)
            kb.I(V, "tensor_scalar", [bef], [bef], out=bef[:], in0=bef[:], scalar1=31.0, scalar2=None, op0=ALU.min)
            ig = kb.sb("ig", [128, nblk])
            kb.I(V, "tensor_scalar", [bef, c["pidx"]], [ig], out=ig[:], in0=bef[:], scalar1=128.0, scalar2=c["pidx"][:, 0:1],
                 op0=ALU.mult, op1=ALU.add)
            usedf = kb.sb("usedf", [128, nblk])
            kb.I(V, "tensor_scalar", [thr, pend], [usedf], out=usedf[:], in0=thr[:], scalar1=pend[:, 31:32], scalar2=None, op0=ALU.is_lt)
            samef = kb.sb("samef", [128, nblk])
            kb.I(V, "memset", [], [samef], samef[:], 0.0)
            kb.I(V, "tensor_tensor", [bef], [samef], out=samef[:, 1:nblk], in0=bef[:, 1:nblk], in1=bef[:, 0:nblk - 1], op=ALU.is_equal)
            kb.I(V, "tensor_scalar", [samef], [samef], out=samef[:], in0=samef[:], scalar1=-1.0, scalar2=1.0, op0=ALU.mult, op1=ALU.add)
            kb.I(V, "tensor_tensor", [usedf, samef], [usedf], out=usedf[:], in0=usedf[:], in1=samef[:], op=ALU.mult)
            kb.I(V, "tensor_tensor", [ig, usedf], [ig], out=ig[:], in0=ig[:], in1=usedf[:], op=ALU.mult)
            kb.I(V, "tensor_scalar", [usedf], [usedf], out=usedf[:], in0=usedf[:], scalar1=-8192.0, scalar2=8192.0, op0=ALU.mult, op1=ALU.add)
            kb.I(V, "tensor_tensor", [ig, usedf], [ig], out=ig[:], in0=ig[:], in1=usedf[:], op=ALU.add)
            kb.I(V, "tensor_copy", [ig], [idxe], out=idxe[:], in_=ig[:])
            SL = EL2
            RK3 = RK[:].rearrange("p (t e) -> p t e", e=32)
            kb.I(V, "tensor_tensor", [RK, base], [SL], out=SL[:], in0=RK3, in1=base[:], op=ALU.add)
            kb.I(V, "tensor_tensor", [SL, pad], [SL], out=SL[:], in0=SL[:],
                 in1=pad[:].unsqueeze(1).to_broadcast([128, ntile, 32]), op=ALU.add)
            for oh, si in ((oh1, s1i), (oh2, s2i)):
                kb.I(V, "tensor_tensor", [SL, oh], [oh], out=oh[:], in0=SL[:], in1=oh[:], op=ALU.mult)
                kb.I(V, "tensor_reduce", [oh], [tmp], out=tmp[:], in_=oh[:], axis=AX.X, op=ALU.add)
                kb.I(V, "tensor_copy", [tmp], [si], out=si[:], in_=tmp[:])

        with kb.scope():
            hbs = [kb.sb("hb%d" % i, [128, D], BF16) for i in range(2)]
            for t in range(ntile):
                ht = hts[t % 2]
                hb = hbs[t % 2]
                kb.dma("sp", ht[:], h_d[t * 128:(t + 1) * 128, :])
                kb.I("act", "copy", [ht], [hb], out=hb[:], in_=ht[:])
                for si in (s1i, s2i):
                    kb.dma("pool", xs_d, hb[:], reads=[hb, si], writes=[xs_d], indirect=dict(
                        out_offset=bass.IndirectOffsetOnAxis(ap=si[:, t:t + 1], axis=0), in_offset=None))

        with kb.scope():
            xin = [[kb.sb("xin%d_%d" % (i, s), [128, D], BF16) for s in range(BS)] for i in range(2)]
            xT = [kb.sb("xT%d" % i, [128, 8, BLK], BF16) for i in range(2)]
            tps = [kb.ps("tps%d" % i, [128, 8, 128], BF16) for i in range(2)]
            stg = [kb.sb("stg%d" % i, [128, 4096]) for i in range(3)]
            wgb = [kb.sb("wgb%d" % i, [128, 8, 512], BF16) for i in range(2)]
            wub = [kb.sb("wub%d" % i, [128, 8, 512], BF16) for i in range(2)]
            wdb = [kb.sb("wdb%d" % i, [128, 4, 1024], BF16) for i in range(2)]
            hgp = [kb.ps("hgp%d" % i, [128, BLK]) for i in range(2)]
            hup = [kb.ps("hup%d" % i, [128, BLK]) for i in range(2)]
            yp = [kb.ps("yp%d" % i, [128, 512]) for i in range(2)]
            sg = [kb.sb("sg%d" % i, [128, BLK]) for i in range(2)]
            hTb = [kb.sb("hTb%d" % i, [128, 4, BLK], BF16) for i in range(2)]
            yb = [kb.sb("yb%d" % i, [128, D]) for i in range(2)]
            sti = 0
            cast_eng = ["dve", "act"]
            bc_reg = nc.gpsimd.alloc_register("moe_bc_%d" % kb.uid)
            nc.gpsimd.reg_mov(bc_reg, 4095)

            def load_weights(b):
                nonlocal sti
                par = b % 2
                for src, dstt in ((wg_d, wgb[par]), (wu_d, wub[par]), (wd_d, wdb[par])):
                    st = stg[sti % 3]
                    kb.dma("pool", st[:], src, reads=[idxe], writes=[st],
                           indirect=dict(out_offset=None, in_offset=bass.IndirectOffsetOnAxis(ap=idxe[:, b:b + 1], axis=0),
                                         bounds_check=bc_reg, oob_is_err=False))
                    dflat = dstt[:].rearrange("p a b -> p (a b)")
                    for half in range(2):
                        ce = cast_eng[(2 * sti + half) % 2]
                        kb.copy(ce, [st], [dstt], dflat[:, half * 2048:(half + 1) * 2048], st[:, half * 2048:(half + 1) * 2048])
                    sti += 1

            def fetch_tokens(b):
                par = b % 2
                for s in range(BS):
                    kb.dma("sp", xin[par][s][:], xs_d[b * BLK + s * 128: b * BLK + (s + 1) * 128, :])

            def load_tokens(b):
                par = b % 2
                for s in range(BS):
                    xi = xin[par][s]
                    tpp = tps[s % 2]
                    for k in range(8):
                        kb.I("pe", "transpose", [xi, c["identb"]], [tpp], out=tpp[:, k, :], in_=xi[:, k * 128:(k + 1) * 128],
                             identity=c["identb"][:], inc=(k == 7))
                    kb.I("dve", "tensor_copy", [tpp], [xT[par]], out=xT[par][:, :, s * 128:(s + 1) * 128], in_=tpp[:])

            def gate_up(b):
                par = b % 2
                for f in range(4):
                    hg = hgp[f % 2]
                    hu = hup[f % 2]
                    for k in range(8):
                        kb.I("pe", "matmul", [wgb[par], xT[par]], [hg], hg[:], lhsT=wgb[par][:, k, f * 128:(f + 1) * 128],
                             rhs=xT[par][:, k, :], start=(k == 0), stop=(k == 7), inc=(k == 7))
                    for k in range(8):
                        kb.I("pe", "matmul", [wub[par], xT[par]], [hu], hu[:], lhsT=wub[par][:, k, f * 128:(f + 1) * 128],
                             rhs=xT[par][:, k, :], start=(k == 0), stop=(k == 7), inc=(k == 7))
                    sgt = sg[f % 2]
                    kb.I("act", "activation", [hg], [sgt], out=sgt[:], in_=hg[:], func=AF.Silu)
                    kb.I("dve", "tensor_tensor", [sgt, hu], [hTb[par]], out=hTb[par][:, f, :], in0=sgt[:], in1=hu[:], op=ALU.mult)

            def down(b):
                par = b % 2
                for s in range(BS):
                    ybt = yb[s % 2]
                    for dh in range(2):
                        y = yp[dh]
                        for f in range(4):
                            kb.I("pe", "matmul", [hTb[par], wdb[par]], [y], y[:], lhsT=hTb[par][:, f, s * 128:(s + 1) * 128],
                                 rhs=wdb[par][:, f, dh * 512:(dh + 1) * 512], start=(f == 0), stop=(f == 3), inc=(f == 3))
                        kb.I("act" if dh == 0 else "dve", "tensor_copy" if dh else "copy", [y], [ybt],
                             out=ybt[:, dh * 512:(dh + 1) * 512], in_=y[:])
                    kb.dma("sp", ys_d[b * BLK + s * 128: b * BLK + (s + 1) * 128, :], ybt[:])

            load_weights(0)
            fetch_tokens(0)
            load_tokens(0)
            for b in range(nblk):
                if b + 1 < nblk:
                    fetch_tokens(b + 1)
                    load_weights(b + 1)
                gate_up(b)
                if b + 1 < nblk:
                    load_tokens(b + 1)
                down(b)

        with kb.scope():
            NS = 3
            y1 = [kb.sb("y1_%d" % i, [128, D]) for i in range(NS)]
            y2 = [kb.sb("y2_%d" % i, [128, D]) for i in range(NS)]
            hts5 = [kb.sb("ht5_%d" % i, [128, D]) for i in range(NS)]
            acc = [kb.sb("acc%d" % i, [128, D]) for i in range(2)]
            outt = [kb.sb("outt%d" % i, [128, D]) for i in range(2)]
            st6 = kb.sb("st6", [128, 2, 6])
            mv = kb.sb("mv", [128, 4])

            def issue(t):
                p = t % NS
                kb.dma("sp", hts5[p][:], h_d[t * 128:(t + 1) * 128, :])
                for yy, si in ((y1[p], s1i), (y2[p], s2i)):
                    kb.dma("pool", yy[:], ys_d, reads=[ys_d, si], writes=[yy], indirect=dict(
                        out_offset=None, in_offset=bass.IndirectOffsetOnAxis(ap=si[:, t:t + 1], axis=0)))
            for t in range(min(NS - 1, ntile)):
                issue(t)
            for t in range(ntile):
                if t + NS - 1 < ntile:
                    issue(t + NS - 1)
                p = t % NS
                ht = hts5[p]
                a = acc[t % 2]
                kb.I("act", "mul", [y1[p], g1], [a], out=a[:], in_=y1[p][:], mul=g1[:, t:t + 1])
                kb.I("dve", "scalar_tensor_tensor", [y2[p], g2, a], [a], out=a[:], in0=y2[p][:], scalar=g2[:, t:t + 1],
                     in1=a[:], op0=ALU.mult, op1=ALU.add)
                kb.I("dve", "scalar_tensor_tensor", [ht, a], [a], out=a[:], in0=ht[:], scalar=ALPHA, in1=a[:],
                     op0=ALU.mult, op1=ALU.add)
                layer_norm_tile(kb, a, g_bc, b_bc, outt[t % 2], st6, mv)
                kb.dma("sp", out_d[t * 128:(t + 1) * 128, :], outt[t % 2][:])

def make_consts():
    import ml_dtypes
    i = np.arange(128)
    return {
        "ident": np.eye(128, dtype=np.float32),
        "identb": np.eye(128, dtype=np.float32).astype(ml_dtypes.bfloat16),
        "ones": np.ones((128, 128), np.float32),
        "sut": (i[:, None] < i[None, :]).astype(np.float32),
        "ramp": np.broadcast_to(i[None, :].astype(np.float32), (128, 128)).copy(),
        "pidx": np.broadcast_to(i[:, None].astype(np.float32), (128, 128)).copy(),
        "cmask": np.where(i[None, :] > i[:, None], NEG, 0.0).astype(np.float32),
        "onesb": np.ones((128, 128), np.float32).astype(ml_dtypes.bfloat16),
        "cmaskb": np.where(i[None, :] > i[:, None], NEG, 0.0).astype(np.float32).astype(ml_dtypes.bfloat16),
    }


CHUNKS = [(0, 512), (512, 512), (1024, 512), (1536, 512), (2048, 128)]
EV_Q, EV_CKV, EV_QI, EV_KI, EV_WI, EV_GATE, EV_XB = 0, 512, 768, 1280, 1344, 1352, 1864


def proj_fm(kb, ps2, cnt, wb, col0, hT, dst_fn, evac):
    for (t0, n) in CHUNKS:
        p = ps2[cnt[0] % 2]
        for k in range(8):
            kb.I("pe", "matmul", [wb, hT], [p], p[:, 0:n], lhsT=wb[:, k, col0:col0 + 128], rhs=hT[:, k, t0:t0 + n],
                 start=(k == 0), stop=(k == 7), inc=(k == 7))
        evac(cnt[0], p[:, 0:n], t0, n)
        cnt[0] += 1


def build_even(kb, c, h_d, win_d, wout_d, wuk_d, wuv_d, rgw_d, vec_d, lng_d, lnb_d, out_d, nseq=SPC):
    with kb.scope():
        winb = kb.sb("winb", [128, 8, 2376], BF16)
        wk2 = kb.sb("wk2", [128, 8, 128], BF16)
        wukb = kb.sb("wukb", [128, 2, 128], BF16)
        wuvb = kb.sb("wuvb", [128, 2, 128], BF16)
        rgwb = kb.sb("rgwb", [128, 8, 128], BF16)
        vec = kb.sb("vec", [128, 40])
        cc = kb.sb("cc", [128, 4])
        g_bc = kb.sb("g_bc", [128, D])
        b_bc = kb.sb("b_bc", [128, D])
        kb.dma("sp", g_bc[:], lng_d.partition_broadcast(128))
        kb.dma("sp", b_bc[:], lnb_d.partition_broadcast(128))
        kb.dma("sp", vec[:], vec_d)
        with kb.scope():
            stg = [kb.sb("wstg%d" % i, [128, 2376]) for i in range(2)]
            for k in range(8):
                st = stg[k % 2]
                kb.dma("sp", st[:], win_d[k * 128:(k + 1) * 128, :])
                kb.copy("act" if k % 2 else "pool", [st], [winb], winb[:, k, :], st[:])
            for j in range(2):
                kb.I("dve", "tensor_copy", [winb], [wk2], out=wk2[:, :, j * 64:(j + 1) * 64], in_=winb[:, :, EV_KI:EV_KI + 64])
            st = stg[0]
            kb.dma("sp", st[:, 0:256], wuk_d.rearrange("(c p) d -> p c d", p=128))
            kb.I("dve", "tensor_copy", [st], [wukb], out=wukb[:], in_=st[:, 0:256].rearrange("p (c d) -> p c d", c=2))
            st = stg[1]
            kb.dma("sp", st[:, 0:256], wuv_d.rearrange("(c p) d -> p c d", p=128))
            kb.I("dve", "tensor_copy", [st], [wuvb], out=wuvb[:], in_=st[:, 0:256].rearrange("p (c d) -> p c d", c=2))
            st = stg[0]
            kb.dma("sp", st[:, 0:1024], rgw_d)
            kb.I("dve", "tensor_copy", [st], [rgwb], out=rgwb[:], in_=st[:, 0:1024].rearrange("p (c d) -> p c d", c=8))
            kb.I("act", "activation", [vec], [cc], out=cc[:], in_=vec[:, 28:32], func=AF.Exp, scale=-1.0)
            kb.I("act", "activation", [cc], [cc], out=cc[:], in_=cc[:], func=AF.Ln, bias=1.0)
            kb.I("dve", "tensor_scalar", [cc], [cc], out=cc[:], in0=cc[:], scalar1=-8.0, scalar2=None, op0=ALU.mult)
        cw = lambda j, ch: vec[:, j * 4 + ch: j * 4 + ch + 1]
        cb = lambda ch: vec[:, 16 + ch:17 + ch]
        ba = lambda ch: vec[:, 20 + ch:21 + ch]
        bx = lambda ch: vec[:, 24 + ch:25 + ch]
        kvn = lambda ch: vec[:, 32 + ch:33 + ch]

        for s in range(nseq):
            r0 = s * TP
            with kb.scope():
                hT = kb.sb("hT", [128, 8, TP], BF16)
                qT = kb.sb("qT", [128, 4, TP], BF16)
                qiT = kb.sb("qiT", [128, 4, TP], BF16)
                kiT2 = kb.sb("kiT2", [128, TP], BF16)
                kT = kb.sb("kT", [128, TP], BF16)
                vtok = kb.sb("vtok", [128, NT, 130], BF16)
                wq = kb.sb("wq", [128, NT, 8])
                kb.I("pool", "memset", [], [vtok], vtok[:, :, 128:130], 1.0)
                with kb.scope():
                    gateT = kb.sb("gateT", [128, 4, TP], BF16)
                    xbT = kb.sb("xbT", [128, 4, TP], BF16)
                    with kb.scope():
                        hts = [kb.sb("eht%d" % i, [128, D]) for i in range(2)]
                        tp = [kb.ps("etp%d" % i, [128, 8, 128]) for i in range(2)]
                        for t in range(NT):
                            ht = hts[t % 2]
                            kb.dma("sp", ht[:], h_d[r0 + t * 128: r0 + (t + 1) * 128, :])
                            tpp = tp[t % 2]
                            for k in range(8):
                                kb.I("pe", "transpose", [ht, c["ident"]], [tpp], out=tpp[:, k, :],
                                     in_=ht[:, k * 128:(k + 1) * 128], identity=c["ident"][:], inc=(k == 7))
                            kb.copy("act" if t % 2 else "dve", [tpp], [hT], hT[:, :, t * 128:(t + 1) * 128], tpp[:])
                    with kb.scope():
                        ckvT = kb.sb("ckvT", [128, 2, TP], BF16)
                        latT = kb.sb("latT", [128, 2, TP], BF16)
                        ps2 = [kb.ps("pp%d" % i, [128, 512]) for i in range(2)]
                        cnt = [0]

                        def mk_evac(dst, ci):
                            def ev(n_, p, t0, n):
                                kb.copy("act" if n_ % 2 else "dve", [p], [dst], dst[:, ci, t0:t0 + n] if ci is not None else dst[:, t0:t0 + n], p)
                            return ev
                        for ci in range(4):
                            proj_fm(kb, ps2, cnt, winb, EV_Q + ci * 128, hT, None, mk_evac(qT, ci))
                            proj_fm(kb, ps2, cnt, winb, EV_QI + ci * 128, hT, None, mk_evac(qiT, ci))
                            proj_fm(kb, ps2, cnt, winb, EV_GATE + ci * 128, hT, None, mk_evac(gateT, ci))
                            proj_fm(kb, ps2, cnt, winb, EV_XB + ci * 128, hT, None, mk_evac(xbT, ci))
                        for ci in range(2):
                            proj_fm(kb, ps2, cnt, winb, EV_CKV + ci * 128, hT, None, mk_evac(ckvT, ci))
                        proj_fm(kb, ps2, cnt, wk2, 0, hT, None, mk_evac(kiT2, None))
                        wps = [kb.ps("wps%d" % i, [128, 8]) for i in range(2)]
                        for t in range(NT):
                            p = wps[t % 2]
                            for k in range(8):
                                kb.I("pe", "matmul", [hT, winb], [p], p[:], lhsT=hT[:, k, t * 128:(t + 1) * 128],
                                     rhs=winb[:, k, EV_WI:EV_WI + 8], start=(k == 0), stop=(k == 7), inc=(k == 7))
                            kb.copy("act", [p], [wq], wq[:, t, :], p[:])
                        sq = kb.sb("sq", [128, 2, 512], BF16)
                        rstd = kb.sb("rstd", [128, 512])
                        for (t0, n) in CHUNKS:
                            kb.I("act", "activation", [ckvT], [sq], out=sq[:, :, 0:n], in_=ckvT[:, :, t0:t0 + n], func=AF.Square)
                            p = ps2[cnt[0] % 2]
                            cnt[0] += 1
                            for ci in range(2):
                                kb.I("pe", "matmul", [c["onesb"], sq], [p], p[:, 0:n], lhsT=c["onesb"][:], rhs=sq[:, ci, 0:n],
                                     start=(ci == 0), stop=(ci == 1), inc=(ci == 1))
                            kb.I("dve", "tensor_scalar", [p], [rstd], out=rstd[:, 0:n], in0=p[:, 0:n], scalar1=1.0 / 256, scalar2=1e-6,
                                 op0=ALU.mult, op1=ALU.add)
                            kb.I("act", "sqrt", [rstd], [rstd], out=rstd[:, 0:n], in_=rstd[:, 0:n])
                            kb.I("dve", "reciprocal", [rstd], [rstd], out=rstd[:, 0:n], in_=rstd[:, 0:n])
                            for ci in range(2):
                                kb.I("dve", "scalar_tensor_tensor", [ckvT, vec, rstd], [latT], out=latT[:, ci, t0:t0 + n],
                                     in0=ckvT[:, ci, t0:t0 + n], scalar=kvn(ci), in1=rstd[:, 0:n], op0=ALU.mult, op1=ALU.mult)
                            p = ps2[cnt[0] % 2]
                            cnt[0] += 1
                            for ci in range(2):
                                kb.I("pe", "matmul", [wukb, latT], [p], p[:, 0:n], lhsT=wukb[:, ci, :], rhs=latT[:, ci, t0:t0 + n],
                                     start=(ci == 0), stop=(ci == 1), inc=(ci == 1))
                            kb.copy("act", [p], [kT], kT[:, t0:t0 + n], p[:, 0:n])
                        for t in range(NT):
                            p = ps2[cnt[0] % 2]
                            cnt[0] += 1
                            for ci in range(2):
                                kb.I("pe", "matmul", [latT, wuvb], [p], p[:, 0:128], lhsT=latT[:, ci, t * 128:(t + 1) * 128],
                                     rhs=wuvb[:, ci, :], start=(ci == 0), stop=(ci == 1), inc=(ci == 1))
                            kb.copy("dve", [p], [vtok], vtok[:, t, 0:128], p[:, 0:128])
                    mixT = hT
                    with kb.scope():
                        HS = TP // 2
                        F = lambda nm: kb.sb(nm, [128, HS])
                        xr, rr, ii, aa, uu, hh, gl = F("xr"), F("rr"), F("ii"), F("aa"), F("uu"), F("hh"), F("gl")
                        xrb = kb.sb("xrb", [128, HS], BF16)
                        carry = kb.sb("carry", [128, 4])
                        gps = [kb.ps("gps%d" % i, [128, 512]) for i in range(4)]
                        gi = 0
                        for ch in range(4):
                            for hf in range(2):
                                o = hf * HS
                                x = xbT[:, ch, :]
                                kb.I("dve", "tensor_scalar", [xbT, vec], [xr], out=xr[:], in0=x[:, o:o + HS], scalar1=cw(3, ch), scalar2=cb(ch),
                                     op0=ALU.mult, op1=ALU.add)
                                for d in (1, 2, 3):
                                    lo = d if hf == 0 else 0
                                    kb.I("dve", "scalar_tensor_tensor", [xbT, vec, xr], [xr], out=xr[:, lo:HS], in0=x[:, o + lo - d:o + HS - d],
                                         scalar=cw(3 - d, ch), in1=xr[:, lo:HS], op0=ALU.mult, op1=ALU.add)
                                kb.copy("pool", [xr], [xrb], xrb[:], xr[:])
                                for (t0, n) in ((0, 512), (512, 512), (1024, 64)):
                                    pa = gps[gi % 4]
                                    px = gps[(gi + 1) % 4]
                                    gi += 2
                                    kb.I("pe", "matmul", [rgwb, xrb], [pa], pa[:, 0:n], lhsT=rgwb[:, ch, :], rhs=xrb[:, t0:t0 + n], start=True, stop=True)
                                    kb.I("pe", "matmul", [rgwb, xrb], [px], px[:, 0:n], lhsT=rgwb[:, 4 + ch, :], rhs=xrb[:, t0:t0 + n], start=True, stop=True)
                                    kb.I("act", "activation", [pa, vec], [rr], out=rr[:, t0:t0 + n], in_=pa[:, 0:n], func=AF.Sigmoid, bias=ba(ch))
                                    kb.I("act", "activation", [px, vec], [ii], out=ii[:, t0:t0 + n], in_=px[:, 0:n], func=AF.Sigmoid, bias=bx(ch))
                                kb.I("act", "activation", [rr, cc], [aa], out=aa[:], in_=rr[:], func=AF.Exp, scale=cc[:, ch:ch + 1])
                                kb.I("pool", "tensor_tensor", [aa], [uu], out=uu[:], in0=aa[:], in1=aa[:], op=ALU.mult)
                                kb.I("act", "activation", [uu], [uu], out=uu[:], in_=uu[:], func=AF.Sqrt, scale=-1.0, bias=1.0)
                                kb.I("pool", "tensor_tensor", [ii, xr], [ii], out=ii[:], in0=ii[:], in1=xr[:], op=ALU.mult)
                                kb.I("pool", "tensor_tensor", [uu, ii], [uu], out=uu[:], in0=uu[:], in1=ii[:], op=ALU.mult)
                                kb.I("dve", "tensor_tensor_scan", [aa, uu, carry], [hh], out=hh[:], data0=aa[:], data1=uu[:],
                                     initial=(0.0 if hf == 0 else carry[:, ch:ch + 1]), op0=ALU.mult, op1=ALU.add)
                                if hf == 0:
                                    kb.I("dve", "tensor_copy", [hh], [carry], out=carry[:, ch:ch + 1], in_=hh[:, HS - 1:HS])
                                g = gateT[:, ch, o:o + HS]
                                kb.I("act", "activation", [gateT], [gl], out=gl[:], in_=g, func=AF.Square)
                                kb.I("pool", "tensor_scalar", [gl], [gl], out=gl[:], in0=gl[:], scalar1=0.044715, scalar2=1.0, op0=ALU.mult, op1=ALU.add)
                                kb.I("pool", "tensor_tensor", [gl, gateT], [gl], out=gl[:], in0=gl[:], in1=g, op=ALU.mult)
                                kb.I("act", "activation", [gl], [gl], out=gl[:], in_=gl[:], func=AF.Sigmoid, scale=1.5957691216)
                                kb.I("pool", "tensor_tensor", [gl, gateT], [gl], out=gl[:], in0=gl[:], in1=g, op=ALU.mult)
                                kb.I("dve", "tensor_tensor", [hh, gl], [mixT], out=mixT[:, 4 + ch, o:o + HS], in0=hh[:], in1=gl[:], op=ALU.mult)
                with kb.scope():
                    score = [kb.sb("score%d" % i, [128, TP]) for i in range(2)]
                    mask = [kb.sb("mask%d" % i, [128, TP], BF16) for i in range(2)]
                    m8 = [kb.sb("m8_%d" % i, [128, 8]) for i in range(2)]
                    work = kb.sb("work", [128, TP])
                    thrc = kb.sb("thrc", [128, 1])
                    rlb = [kb.sb("rlb%d" % i, [128, 512], BF16) for i in range(4)]
                    dg = [kb.sb("dg%d" % i, [128, 8, 128], BF16) for i in range(1)]
                    lgt = [kb.sb("lgt%d" % i, [128, TP]) for i in range(2)]
                    ee = [kb.sb("ee%d" % i, [128, TP], BF16) for i in range(2)]
                    pT = [kb.sb("pT%d" % i, [128, NT, 128], BF16) for i in range(1)] * 2
                    sm = [kb.sb("sm%d" % i, [128, 4]) for i in range(2)]
                    on = [kb.sb("on%d" % i, [128, 128], BF16) for i in range(2)]
                    sps = [kb.ps("sps%d" % i, [128, 512]) for i in range(2)]
                    lps = [kb.ps("lps%d" % i, [128, 512]) for i in range(2)]
                    tps = [kb.ps("atp%d" % i, [128, 8, 128], BF16) for i in range(2)]
                    ops = kb.ps("ops", [128, 132])
                    scp = kb.ps("scp", [128, 512])
                    kb.I("dve", "memset", [], [thrc], thrc[:], -1.0e29)
                    cnts = {"si": 0, "ti": 0, "li": 0}

                    def indexer(i):
                        sc = score[i % 2]
                        dgt = dg[0]
                        nk = (i + 1) * 128
                        qs = slice(i * 128, (i + 1) * 128)
                        for h in range(8):
                            kb.I("pool", "tensor_scalar", [c["identb"], wq], [dgt], out=dgt[:, h, :], in0=c["identb"][:],
                                 scalar1=wq[:, i, h:h + 1], scalar2=None, op0=ALU.mult)
                        for t0 in range(0, nk, 512):
                            n = min(512, nk - t0)
                            last = (t0 + n == nk)
                            for hh in range(2):
                                for h in range(hh * 4, hh * 4 + 4):
                                    p = sps[cnts["si"] % 2]
                                    cnts["si"] += 1
                                    pr = slice((h % 2) * 64, (h % 2) * 64 + 64)
                                    kb.I("pe", "matmul", [qiT, kiT2], [p], p[:, 0:n], lhsT=qiT[pr, h // 2, qs], rhs=kiT2[pr, t0:t0 + n],
                                         start=True, stop=True)
                                    kb.I("act", "activation", [p], [rlb[h % 4]], out=rlb[h % 4][:, 0:n], in_=p[:, 0:n], func=AF.Relu)
                                for h in range(hh * 4, hh * 4 + 4):
                                    kb.I("pe", "matmul", [dgt, rlb[h % 4]], [scp], scp[:, 0:n], lhsT=dgt[:, h, :], rhs=rlb[h % 4][:, 0:n],
                                         start=(h == 0), stop=(h == 7 and not last), inc=(h % 4 == 3))
                            if last:
                                kb.I("pe", "matmul", [c["identb"], c["cmaskb"]], [scp], scp[:, n - 128:n], lhsT=c["identb"][:], rhs=c["cmaskb"][:],
                                     start=False, stop=True)
                            kb.copy("act", [scp], [sc], sc[:, t0:t0 + n], scp[:, 0:n])
                            yield

                    def topk(i):
                        sc, mk, m = score[i % 2], mask[i % 2], m8[i % 2]
                        nk = (i + 1) * 128
                        if i >= 2:
                            for it in range(32):
                                src = sc if it == 0 else work
                                kb.I("dve", "max", [src], [m], out=m[:], in_=src[:, 0:nk])
                                if it < 31:
                                    kb.I("dve", "match_replace", [m, src], [work], out=work[:, 0:nk], in_to_replace=m[:],
                                         in_values=src[:, 0:nk], imm_value=NEG)
                                if it % 8 == 7 and it < 31:
                                    yield
                            kb.I("dve", "tensor_scalar", [sc, m], [mk], out=mk[:, 0:nk], in0=sc[:, 0:nk], scalar1=m[:, 7:8],
                                 scalar2=None, op0=ALU.is_ge)
                        else:
                            kb.I("dve", "tensor_scalar", [sc, thrc], [mk], out=mk[:, 0:nk], in0=sc[:, 0:nk], scalar1=thrc[:, 0:1],
                                 scalar2=None, op0=ALU.is_ge)

                    def qk(i, h):
                        nk = (i + 1) * 128
                        qs = slice(i * 128, (i + 1) * 128)
                        lg = lgt[h % 2]
                        for t0 in range(0, nk, 512):
                            n = min(512, nk - t0)
                            p = lps[cnts["li"] % 2]
                            cnts["li"] += 1
                            kb.I("pe", "matmul", [qT, kT], [p], p[:, 0:n], lhsT=qT[:, h, qs], rhs=kT[:, t0:t0 + n], start=True, stop=True)
                            kb.I("act", "mul", [p], [lg], out=lg[:, t0:t0 + n], in_=p[:, 0:n], mul=128.0 ** -0.5)

                    def attention(i, gen, geni):
                        mk = mask[i % 2]
                        nk = (i + 1) * 128
                        qs = slice(i * 128, (i + 1) * 128)
                        qk(i, 0)
                        for h in range(4):
                            lg, e_, s_, pT_, on_ = lgt[h % 2], ee[h % 2], sm[h % 2], pT[h % 2], on[h % 2]
                            kb.I("dve", "tensor_reduce", [lg], [s_], out=s_[:, 0:1], in_=lg[:, 0:nk], axis=AX.X, op=ALU.max)
                            kb.I("dve", "tensor_scalar", [s_], [s_], out=s_[:, 1:2], in0=s_[:, 0:1], scalar1=-1.0, scalar2=None, op0=ALU.mult)
                            kb.I("act", "activation", [lg, s_], [e_], out=e_[:, 0:nk], in_=lg[:, 0:nk], func=AF.Exp, bias=s_[:, 1:2])
                            if h < 3:
                                qk(i, h + 1)
                            next(gen, None)
                            next(geni, None)
                            if h % 2:
                                next(geni, None)
                            kb.I("pool", "tensor_tensor", [e_, mk], [e_], out=e_[:, 0:nk], in0=e_[:, 0:nk], in1=mk[:, 0:nk], op=ALU.mult)
                            for j0 in range(0, i + 1, 8):
                                j1 = min(j0 + 8, i + 1)
                                tpp = tps[cnts["ti"] % 2]
                                cnts["ti"] += 1
                                for j in range(j0, j1):
                                    kb.I("pe", "transpose", [e_, c["identb"]], [tpp], out=tpp[:, j - j0, :], in_=e_[:, j * 128:(j + 1) * 128],
                                         identity=c["identb"][:], inc=(j == j1 - 1))
                                kb.copy("act", [tpp], [pT_], pT_[:, j0:j1, :], tpp[:, 0:j1 - j0, :])
                            for j in range(i + 1):
                                kb.I("pe", "matmul", [pT_, vtok], [ops], ops[:, 0:129], lhsT=pT_[:, j, :], rhs=vtok[:, j, 0:129], start=(j == 0), stop=(j == i),
                                     inc=(j == i))
                            kb.I("dve", "reciprocal", [ops], [s_], out=s_[:, 3:4], in_=ops[:, 128:129])
                            kb.I("dve", "tensor_scalar", [ops, s_], [on_], out=on_[:], in0=ops[:, 0:128], scalar1=s_[:, 3:4], scalar2=None, op0=ALU.mult)
                            otb = tps[cnts["ti"] % 2]
                            cnts["ti"] += 1
                            kb.I("pe", "transpose", [on_, c["identb"]], [otb], out=otb[:, 0, :], in_=on_[:], identity=c["identb"][:])
                            kb.copy("act", [otb], [mixT], mixT[:, h, qs], otb[:, 0, :])

                    for _ in indexer(0):
                        pass
                    for _ in indexer(1):
                        pass
                    for _ in topk(0):
                        pass
                    for i in range(NT):
                        gen = topk(i + 1) if i + 1 < NT else iter(())
                        geni = indexer(i + 2) if i + 2 < NT else iter(())
                        attention(i, gen, geni)
                        for _ in geni:
                            pass
                        for _ in gen:
                            pass
                with kb.scope():
                    woutb = kb.sb("woutb", [128, 8, D], BF16)
                    stg = [kb.sb("ostg%d" % i, [128, D]) for i in range(2)]
                    for k in range(8):
                        kb.dma("sp", stg[k % 2][:], wout_d[k * 128:(k + 1) * 128, :])
                        kb.copy("act" if k % 2 else "pool", [stg[k % 2]], [woutb], woutb[:, k, :], stg[k % 2][:])
                    hts = [kb.sb("oht%d" % i, [128, D]) for i in range(2)]
                    acc = [kb.sb("oacc%d" % i, [128, D]) for i in range(2)]
                    outt = [kb.sb("oout%d" % i, [128, D]) for i in range(2)]
                    st6 = kb.sb("st6", [128, 2, 6])
                    mv = kb.sb("mv", [128, 4])
                    ops2 = [kb.ps("opp%d" % i, [128, 512]) for i in range(4)]
                    kb.dma("sp", hts[0][:], h_d[r0: r0 + 128, :])
                    for t in range(NT):
                        ht = hts[t % 2]
                        a = acc[t % 2]
                        if t + 1 < NT:
                            kb.dma("sp", hts[(t + 1) % 2][:], h_d[r0 + (t + 1) * 128: r0 + (t + 2) * 128, :])
                        for hf in range(2):
                            p = ops2[(2 * t + hf) % 4]
                            for k in range(8):
                                kb.I("pe", "matmul", [mixT, woutb], [p], p[:], lhsT=mixT[:, k, t * 128:(t + 1) * 128],
                                     rhs=woutb[:, k, hf * 512:(hf + 1) * 512], start=(k == 0), stop=(k == 7), inc=(k == 7))
                            kb.I("dve", "scalar_tensor_tensor", [ht, p], [a], out=a[:, hf * 512:(hf + 1) * 512], in0=ht[:, hf * 512:(hf + 1) * 512],
                                 scalar=ALPHA, in1=p[:], op0=ALU.mult, op1=ALU.add)
                        layer_norm_tile(kb, a, g_bc, b_bc, outt[t % 2], st6, mv)
                        kb.dma("sp", out_d[r0 + t * 128: r0 + (t + 1) * 128, :], outt[t % 2][:])


def prep_even(inp, i):
    f = lambda a: np.ascontiguousarray(a, dtype=np.float32)
    pc = lambda v, n: f(v.reshape(n, 128).T)
    vec = np.zeros((128, 40), np.float32)
    cwt = inp["even_conv_w"][i]
    for j in range(4):
        vec[:, j * 4:(j + 1) * 4] = pc(cwt[j], 4)
    vec[:, 16:20] = pc(inp["even_conv_b"][i], 4)
    vec[:, 20:24] = pc(inp["even_rg_ba"][i], 4)
    vec[:, 24:28] = pc(inp["even_rg_bx"][i], 4)
    vec[:, 28:32] = pc(inp["even_rg_lambda"][i], 4)
    vec[:, 32:34] = pc(inp["even_kv_norm"][i], 2)
    rgw = np.zeros((128, 8, 128), np.float32)
    for g, nm in enumerate(("even_rg_wa", "even_rg_wx")):
        w = inp[nm][i]
        for n in range(8):
            o = (n % 2) * 64
            rgw[o:o + 64, g * 4 + n // 2, o:o + 64] = w[n]
    return {
        "ev_win": f(inp["even_w_in"][i]), "ev_wout": f(inp["even_w_out"][i]),
        "ev_wuk": f(inp["even_w_uk"][i]), "ev_wuv": f(inp["even_w_uv"][i]),
        "ev_rgw": f(rgw.reshape(128, 1024)), "ev_vec": vec,
        "ev_lng": f(inp["ln_g"][2 * i, 0]), "ev_lnb": f(inp["ln_b"][2 * i, 0]),
    }


def build_gdn(kb, c, h_d, gw_d, wab_d, gconv_d, alog_d, dtb_d, onorm_d, wout_d, lng_d, lnb_d, out_d, o_d, nseq=SPC, stop=0):
    with kb.scope():
        g_bc = kb.sb("g_bc", [128, D])
        b_bc = kb.sb("b_bc", [128, D])
        kb.dma("sp", g_bc[:], lng_d.partition_broadcast(128))
        kb.dma("sp", b_bc[:], lnb_d.partition_broadcast(128))
        wabb = kb.sb("wabb", [128, 8, 32], BF16)
        gconv = kb.sb("gconv", [128, 8, 4, 4])
        nA = kb.sb("nA", [128, 16])
        dtb = kb.sb("dtb", [128, 16])
        onorm = kb.sb("onorm", [128, 1])
        eps6 = kb.sb("eps6", [128, 1])
        kb.I("pool", "memset", [], [eps6], eps6[:], 1e-6)
        kb.dma("sp", gconv[:], gconv_d)
        kb.dma("sp", nA[:], alog_d.partition_broadcast(128))
        kb.dma("sp", dtb[:], dtb_d.partition_broadcast(128))
        kb.dma("sp", onorm[:], onorm_d.rearrange("(p o) -> p o", o=1))
        kb.I("act", "activation", [nA], [nA], out=nA[:], in_=nA[:], func=AF.Exp)
        kb.I("dve", "tensor_scalar", [nA], [nA], out=nA[:], in0=nA[:], scalar1=-1.0, scalar2=None, op0=ALU.mult)
        ut = kb.sb("ut", [128, 128])
        slt = kb.sb("slt", [128, 128])
        lmask = kb.sb("lmask", [128, 128])
        smask = kb.sb("smask", [128, 128])
        kb.I("dve", "tensor_tensor", [c["sut"], c["ident"]], [ut], out=ut[:], in0=c["sut"][:], in1=c["ident"][:], op=ALU.add)
        kb.I("dve", "tensor_scalar", [ut], [slt], out=slt[:], in0=ut[:], scalar1=-1.0, scalar2=1.0, op0=ALU.mult, op1=ALU.add)
        kb.I("dve", "tensor_copy", [slt], [smask], out=smask[:], in_=slt[:])
        kb.I("dve", "tensor_tensor", [slt, c["ident"]], [lmask], out=lmask[:], in0=slt[:], in1=c["ident"][:], op=ALU.add)
        with kb.scope():
            st = kb.sb("abstg", [128, 8, 32])
            kb.dma("sp", st[:], wab_d.rearrange("(k p) e -> p k e", p=128))
            kb.I("dve", "tensor_copy", [st], [wabb], out=wabb[:], in_=st[:])

        for s in range(nseq):
            r0 = s * TP
            with kb.scope():
                hT = kb.sb("hT", [128, 8, TP], BF16)
                with kb.scope():
                    hts = [kb.sb("ght%d" % i, [128, D]) for i in range(2)]
                    tp = [kb.ps("gtp%d" % i, [128, 8, 128]) for i in range(2)]
                    for t in range(NT):
                        ht = hts[t % 2]
                        kb.dma("sp", ht[:], h_d[r0 + t * 128: r0 + (t + 1) * 128, :])
                        tpp = tp[t % 2]
                        for k in range(8):
                            kb.I("pe", "transpose", [ht, c["ident"]], [tpp], out=tpp[:, k, :],
                                 in_=ht[:, k * 128:(k + 1) * 128], identity=c["ident"][:], inc=(k == 7))
                        kb.copy("act" if t % 2 else "dve", [tpp], [hT], hT[:, :, t * 128:(t + 1) * 128], tpp[:])
                if stop == 1:
                    return
                S3 = lambda nm: kb.sb(nm, [128, NT, 16])
                gg, beta, gc, egc, ekd, egl, bege = S3("gg"), S3("beta"), S3("gc"), S3("egc"), S3("ekd"), S3("egl"), S3("bege")
                with kb.scope():
                    abp = [kb.ps("abp%d" % i, [128, 32]) for i in range(2)]
                    ab = kb.sb("ab", [128, NT, 32])
                    tmp = S3("tmpa")
                    for t in range(NT):
                        p = abp[t % 2]
                        for k in range(8):
                            kb.I("pe", "matmul", [hT, wabb], [p], p[:], lhsT=hT[:, k, t * 128:(t + 1) * 128], rhs=wabb[:, k, :],
                                 start=(k == 0), stop=(k == 7), inc=(k == 7))
                        kb.copy("act", [p], [ab], ab[:, t, :], p[:])
                    bcT = lambda a: a[:].unsqueeze(1).to_broadcast([128, NT, 16])
                    kb.I("dve", "tensor_tensor", [ab, dtb], [gg], out=gg[:], in0=ab[:, :, 0:16], in1=bcT(dtb), op=ALU.add)
                    kb.I("dve", "tensor_scalar", [gg], [tmp], out=tmp[:], in0=gg[:], scalar1=-1.0, scalar2=None, op0=ALU.mult)
                    kb.I("dve", "tensor_tensor", [gg, tmp], [tmp], out=tmp[:], in0=gg[:], in1=tmp[:], op=ALU.max)
                    kb.I("act", "activation", [tmp], [tmp], out=tmp[:], in_=tmp[:], func=AF.Exp, scale=-1.0)
                    kb.I("act", "activation", [tmp], [tmp], out=tmp[:], in_=tmp[:], func=AF.Ln, bias=1.0)
                    kb.I("dve", "tensor_scalar", [gg], [gg], out=gg[:], in0=gg[:], scalar1=0.0, scalar2=None, op0=ALU.max)
                    kb.I("dve", "tensor_tensor", [gg, tmp], [gg], out=gg[:], in0=gg[:], in1=tmp[:], op=ALU.add)
                    kb.I("dve", "tensor_tensor", [gg, nA], [gg], out=gg[:], in0=gg[:], in1=bcT(nA), op=ALU.mult)
                    kb.I("act", "activation", [ab], [beta], out=beta[:], in_=ab[:, :, 16:32], func=AF.Sigmoid)
                    for t in range(NT):
                        p = abp[t % 2]
                        kb.I("pe", "matmul", [ut, gg], [p], p[:, 0:16], lhsT=ut[:], rhs=gg[:, t, :], start=True, stop=True, inc=False)
                        kb.I("pe", "matmul", [c["ones"], gg], [p], p[:, 16:32], lhsT=c["ones"][:], rhs=gg[:, t, :], start=True, stop=True)
                        kb.copy("act", [p], [gc], gc[:, t, :], p[:, 0:16])
                        kb.copy("dve", [p], [egl], egl[:, t, :], p[:, 16:32])
                    kb.I("dve", "tensor_tensor", [egl, gc], [ekd], out=ekd[:], in0=egl[:], in1=gc[:], op=ALU.subtract)
                    kb.I("act", "activation", [ekd], [ekd], out=ekd[:], in_=ekd[:], func=AF.Exp)
                    kb.I("act", "activation", [egl], [egl], out=egl[:], in_=egl[:], func=AF.Exp)
                    kb.I("act", "activation", [gc], [egc], out=egc[:], in_=gc[:], func=AF.Exp)
                    kb.I("dve", "tensor_tensor", [beta, egc], [bege], out=bege[:], in0=beta[:], in1=egc[:], op=ALU.mult)

                if stop == 2:
                    return
                wsls = [kb.sb("wsl0", [128, 8, 768], BF16)] * 2
                gstg = [kb.sb("gstg%d" % i, [128, 768]) for i in range(2)]

                def load_wsl(kh_):
                    w_ = wsls[kh_ % 2]
                    for k in range(8):
                        kb.dma("sp", gstg[k % 2][:], gw_d[kh_, k * 128:(k + 1) * 128, :])
                        kb.copy("act" if k % 2 else "pool", [gstg[k % 2]], [w_], w_[:, k, :], gstg[k % 2][:])
                load_wsl(0)
                for kh in range(8):
                    with kb.scope():
                        wsl = wsls[kh % 2]
                        qkT = kb.sb("qkT", [128, 2, TP], BF16)
                        zsT = kb.sb("zsT", [128, 2, TP], BF16)
                        ktok = kb.sb("ktok", [128, NT, 128], BF16)
                        vtok = kb.sb("vtok", [128, NT, 256], BF16)
                        oTk = kb.sb("oTk", [128, 2, TP], BF16)
                        with kb.scope():
                            vT = kb.sb("vT", [128, 2, TP], BF16)
                            raws = [kb.sb("raw%d" % i, [128, TP]) for i in range(2)]
                            cvs = [kb.sb("cv%d" % i, [128, TP]) for i in range(2)]
                            sqs = [kb.sb("sq%d" % i, [128, 512], BF16) for i in range(3)]
                            rstds = [kb.sb("rstd%d" % i, [128, 512]) for i in range(3)]
                            ps2 = [kb.ps("gpp%d" % i, [128, 512]) for i in range(2)]
                            ps3 = [kb.ps("gpq%d" % i, [128, 512]) for i in range(2)]
                            tps = [kb.ps("gtq%d" % i, [128, 8, 128], BF16) for i in range(2)]
                            cnt = [0]
                            ti = 0
                            def do_proj(fi):
                                raw = raws[fi % 2]
                                def ev(n_, p, t0, n, fi=fi, raw=raw):
                                    if fi < 4:
                                        kb.copy("act", [p], [raw], raw[:, t0:t0 + n], p)
                                    else:
                                        kb.I("act", "activation", [p], [zsT], out=zsT[:, fi - 4, t0:t0 + n], in_=p, func=AF.Silu)
                                proj_fm(kb, ps2, cnt, wsl, fi * 128, hT, None, ev)

                            def do_post(fi):
                                nonlocal ti
                                raw, cv = raws[fi % 2], cvs[fi % 2]
                                cwc = lambda j: gconv[:, kh, fi, j:j + 1]
                                kb.I("dve", "tensor_scalar", [raw, gconv], [cv], out=cv[:], in0=raw[:], scalar1=cwc(3), scalar2=None, op0=ALU.mult)
                                for d in (1, 2, 3):
                                    kb.I("dve", "scalar_tensor_tensor", [raw, gconv, cv], [cv], out=cv[:, d:TP], in0=raw[:, 0:TP - d],
                                         scalar=cwc(3 - d), in1=cv[:, d:TP], op0=ALU.mult, op1=ALU.add)
                                if fi >= 2:
                                    kb.I("act", "activation", [cv], [vT], out=vT[:, fi - 2, :], in_=cv[:], func=AF.Silu)
                                    for j0 in range(0, NT, 8):
                                        j1 = min(j0 + 8, NT)
                                        tpp = tps[ti % 2]
                                        ti += 1
                                        for j in range(j0, j1):
                                            kb.I("pe", "transpose", [vT, c["identb"]], [tpp], out=tpp[:, j - j0, :],
                                                 in_=vT[:, fi - 2, j * 128:(j + 1) * 128], identity=c["identb"][:], inc=(j == j1 - 1))
                                        kb.copy("dve", [tpp], [vtok], vtok[:, j0:j1, (fi - 2) * 128:(fi - 1) * 128], tpp[:, 0:j1 - j0, :])
                                    return
                                kb.I("act", "activation", [cv], [cv], out=cv[:], in_=cv[:], func=AF.Silu)
                                for ci_, (t0, n) in enumerate(CHUNKS):
                                    sq, rstd = sqs[ci_ % 3], rstds[ci_ % 3]
                                    kb.I("pool", "tensor_tensor", [cv], [sq], out=sq[:, 0:n], in0=cv[:, t0:t0 + n], in1=cv[:, t0:t0 + n], op=ALU.mult)
                                    p = ps3[ci_ % 2]
                                    kb.I("pe", "matmul", [c["onesb"], sq], [p], p[:, 0:n], lhsT=c["onesb"][:], rhs=sq[:, 0:n], start=True, stop=True)
                                    kb.I("act", "activation", [p], [rstd], out=rstd[:, 0:n], in_=p[:, 0:n], func=AF.Ln, bias=eps6[:, 0:1])
                                    kb.I("act", "activation", [rstd], [rstd], out=rstd[:, 0:n], in_=rstd[:, 0:n], func=AF.Exp, scale=-0.5)
                                    kb.I("dve", "scalar_tensor_tensor", [cv, rstd], [qkT], out=qkT[:, fi, t0:t0 + n], in0=cv[:, t0:t0 + n],
                                         scalar=(128.0 ** -0.5 if fi == 0 else 1.0), in1=rstd[:, 0:n], op0=ALU.mult, op1=ALU.mult)
                                if fi == 1:
                                    for j0 in range(0, NT, 8):
                                        j1 = min(j0 + 8, NT)
                                        tpp = tps[ti % 2]
                                        ti += 1
                                        for j in range(j0, j1):
                                            kb.I("pe", "transpose", [qkT, c["identb"]], [tpp], out=tpp[:, j - j0, :],
                                                 in_=qkT[:, 1, j * 128:(j + 1) * 128], identity=c["identb"][:], inc=(j == j1 - 1))
                                        kb.copy("dve", [tpp], [ktok], ktok[:, j0:j1, :], tpp[:, 0:j1 - j0, :])

                            do_proj(0)
                            for fi in range(6):
                                if fi + 1 < 6:
                                    do_proj(fi + 1)
                                if fi < 4:
                                    do_post(fi)
                        if stop == 3:
                            return
                        if kh + 1 < 8:
                            load_wsl(kh + 1)
                        with kb.scope():
                            B = lambda nm: kb.sb(nm, [128, 128], BF16)
                            Fp = lambda nm: kb.sb(nm, [128, 128])
                            otok = [kb.sb("otok%d" % j, [128, NT, 128]) for j in range(2)]
                            us_all = [kb.sb("us_all%d" % j, [128, NT, 128]) for j in range(2)]
                            wT_all = [kb.sb("wT_all%d" % j, [128, NT, 128], BF16) for j in range(2)]
                            aT_all = [kb.sb("aT_all%d" % j, [128, NT, 128], BF16) for j in range(2)]
                            kd_all = [kb.sb("kd_all%d" % j, [128, NT, 128], BF16) for j in range(2)]
                            NCH = 6
                            X = [kb.ps("pX%d" % i, [128, 4, 128]) for i in range(NCH)]
                            TPb = kb.ps("TPb", [128, 8, 128], BF16)
                            CH = []
                            for ci in range(NCH):
                                CH.append(dict(
                                    Rm=Fp("Rm%d" % ci), Dm=Fp("Dm%d" % ci), attn=B("attn%d" % ci),
                                    MNA=[kb.sb("MNA%d_0" % ci, [128, 3, 128], BF16), kb.sb("MNA%d_1" % ci, [128, 3, 128], BF16)],
                                    vb=B("vb%d" % ci), kbg=B("kbg%d" % ci), X=X[ci]))
                            GsAs = [(Fp("Gs%d" % i), Fp("As%d" % i)) for i in range(NCH // 2)]
                            for j in range(2):
                                hd = 2 * kh + j
                                kb.I("pool", "tensor_tensor", [ktok, ekd], [kd_all[j]], out=kd_all[j][:], in0=ktok[:],
                                     in1=ekd[:, :, hd:hd + 1].to_broadcast([128, NT, 128]), op=ALU.mult)
                            Ssb = [Fp("S0"), Fp("S1")]
                            Sbf = [B("Sb0"), B("Sb1")]
                            vnew = [B("vnew0"), B("vnew1")]
                            o1 = [Fp("o1_0"), Fp("o1_1")]
                            P2b = kb.ps("pP2", [128, 4, 128])
                            for j in range(2):
                                kb.I("pool", "memset", [], [Ssb[j]], Ssb[j][:], 0.0)
                                kb.I("pool", "memset", [], [Sbf[j]], Sbf[j][:], 0.0)

                            def phase2(tlist):
                                for t in tlist:
                                    ts_ = slice(t * 128, (t + 1) * 128)
                                    for j in range(2):
                                        W = P2b
                                        kb.I("pe", "matmul", [wT_all[j], Sbf[j]], [W], W[:, 2 * j, :], lhsT=wT_all[j][:, t, :], rhs=Sbf[j][:], start=True, stop=True, inc=False)
                                        kb.I("pe", "matmul", [qkT, Sbf[j]], [W], W[:, 2 * j + 1, :], lhsT=qkT[:, 0, ts_], rhs=Sbf[j][:], start=True, stop=True)
                                    yield
                                    for j in range(2):
                                        W = P2b
                                        hd = 2 * kh + j
                                        kb.I("dve", "tensor_tensor", [us_all[j], W], [vnew[j]], out=vnew[j][:], in0=us_all[j][:, t, :], in1=W[:, 2 * j, :], op=ALU.subtract)
                                        kb.I("dve", "tensor_scalar", [W, egc], [o1[j]], out=o1[j][:], in0=W[:, 2 * j + 1, :], scalar1=egc[:, t, hd:hd + 1], scalar2=None, op0=ALU.mult)
                                    yield
                                    for j in range(2):
                                        V_ = P2b
                                        kb.I("pe", "matmul", [aT_all[j], vnew[j]], [V_], V_[:, 2 * j, :], lhsT=aT_all[j][:, t, :], rhs=vnew[j][:], start=True, stop=True, inc=False)
                                        kb.I("pe", "matmul", [kd_all[j], vnew[j]], [V_], V_[:, 2 * j + 1, :], lhsT=kd_all[j][:, t, :], rhs=vnew[j][:], start=True, stop=True)
                                    yield
                                    for j in range(2):
                                        V_ = P2b
                                        hd = 2 * kh + j
                                        kb.I("dve", "scalar_tensor_tensor", [Ssb[j], egl, V_], [Sbf[j]], out=Sbf[j][:], in0=Ssb[j][:], scalar=egl[:, t, hd:hd + 1],
                                             in1=V_[:, 2 * j + 1, :], op0=ALU.mult, op1=ALU.add)
                                        kb.I("dve", "scalar_tensor_tensor", [Ssb[j], egl, V_], [Ssb[j]], out=Ssb[j][:], in0=Ssb[j][:], scalar=egl[:, t, hd:hd + 1],
                                             in1=V_[:, 2 * j + 1, :], op0=ALU.mult, op1=ALU.add)
                                        kb.I("dve", "tensor_tensor", [o1[j], V_], [otok[j]], out=otok[j][:, t, :], in0=o1[j][:], in1=V_[:, 2 * j, :], op=ALU.add)
                                    yield
                            gen2 = iter(())
                            for t0 in range(0, NT, NCH // 2):
                                tl = [t for t in range(t0, t0 + NCH // 2) if t < NT]
                                chains = []
                                for ti_, t in enumerate(tl):
                                    ts_ = slice(t * 128, (t + 1) * 128)
                                    Gs, As = GsAs[ti_]
                                    XG, XA = X[2 * ti_], X[2 * ti_ + 1]
                                    kb.I("pe", "matmul", [qkT], [XG], XG[:, 3, :], lhsT=qkT[:, 1, ts_], rhs=qkT[:, 1, ts_], start=True, stop=True)
                                    kb.I("pe", "matmul", [qkT], [XA], XA[:, 3, :], lhsT=qkT[:, 0, ts_], rhs=qkT[:, 1, ts_], start=True, stop=True)
                                    kb.I("dve", "tensor_tensor", [XG, smask], [Gs], out=Gs[:], in0=XG[:, 3, :], in1=smask[:], op=ALU.mult)
                                    kb.I("dve", "tensor_tensor", [XA, lmask], [As], out=As[:], in0=XA[:, 3, :], in1=lmask[:], op=ALU.mult)
                                    for j in range(2):
                                        chn = dict(CH[ti_ * 2 + j])
                                        chn.update(t=t, j=j, hd=2 * kh + j, Gs=Gs, As=As, slot=ti_ * 2 + j)
                                        chains.append(chn)
                                col = lambda a, q: a[:, q["t"], q["hd"]:q["hd"] + 1]
                                for q in chains:
                                    kb.I("act", "mul", [ut, gg], [q["Rm"]], out=q["Rm"][:], in_=ut[:], mul=col(gg, q))
                                for q in chains:
                                    kb.I("pe", "matmul", [q["Rm"], slt], [q["X"]], q["X"][:, 0, :], lhsT=q["Rm"][:], rhs=slt[:], start=True, stop=True)
                                for q in chains:
                                    kb.I("act", "activation", [q["X"]], [q["Dm"]], out=q["Dm"][:], in_=q["X"][:, 0, :], func=AF.Exp)
                                for q in chains:
                                    kb.I("dve", "scalar_tensor_tensor", [q["Gs"], beta, q["Dm"]], [q["MNA"][0]], out=q["MNA"][0][:, 0, :], in0=q["Gs"][:],
                                         scalar=col(beta, q), in1=q["Dm"][:], op0=ALU.mult, op1=ALU.mult)
                                    kb.I("pool", "tensor_tensor", [q["As"], q["Dm"]], [q["attn"]], out=q["attn"][:], in0=q["As"][:], in1=q["Dm"][:], op=ALU.mult)
                                    kb.copy("act", [c["identb"]], [q["MNA"][0]], q["MNA"][0][:, 2, :], c["identb"][:])
                                for rr0 in range(0, len(chains), 4):
                                    for q in chains[rr0:rr0 + 4]:
                                        sl = q["slot"] - rr0
                                        kb.I("pe", "transpose", [q["MNA"][0], c["identb"]], [TPb], out=TPb[:, 2 * sl, :], in_=q["MNA"][0][:, 0, :], identity=c["identb"][:], inc=False)
                                        kb.I("pe", "transpose", [q["attn"], c["identb"]], [TPb], out=TPb[:, 2 * sl + 1, :], in_=q["attn"][:], identity=c["identb"][:])
                                    for q in chains[rr0:rr0 + 4]:
                                        sl = q["slot"] - rr0
                                        kb.copy("act", [TPb], [q["MNA"][0]], q["MNA"][0][:, 1, :], TPb[:, 2 * sl, :])
                                        kb.copy("act", [TPb], [aT_all[q["j"]]], aT_all[q["j"]][:, q["t"], :], TPb[:, 2 * sl + 1, :])
                                cur = 0
                                for st_ in range(7):
                                    nxt = 1 - cur
                                    next(gen2, None)
                                    for q in chains:
                                        Xq, T_ = q["X"], q["MNA"][cur]
                                        if st_ < 6:
                                            kb.I("pe", "matmul", [T_], [Xq], Xq[:, 0, :], lhsT=T_[:, 1, :], rhs=T_[:, 0, :], start=True, stop=True, inc=False)
                                            kb.I("pe", "matmul", [T_], [Xq], Xq[:, 1:3, :], lhsT=T_[:, 0, :], rhs=T_[:, 1:3, :], start=True, stop=True)
                                        else:
                                            kb.I("pe", "matmul", [T_], [Xq], Xq[:, 2, :], lhsT=T_[:, 0, :], rhs=T_[:, 2, :], start=True, stop=True)
                                    next(gen2, None)
                                    for q in chains:
                                        Xq, T_, Tn = q["X"], q["MNA"][cur], q["MNA"][nxt]
                                        if st_ < 6:
                                            kb.copy("act", [Xq], [Tn], Tn[:, 0:2, :], Xq[:, 0:2, :])
                                        kb.I("dve", "tensor_tensor", [T_, Xq], [Tn], out=Tn[:, 2, :], in0=T_[:, 2, :], in1=Xq[:, 2, :],
                                             op=(ALU.subtract if st_ == 0 else ALU.add))
                                    cur = nxt
                                for q in chains:
                                    j, t = q["j"], q["t"]
                                    kb.I("act", "mul", [vtok, beta], [q["vb"]], out=q["vb"][:], in_=vtok[:, t, j * 128:(j + 1) * 128], mul=col(beta, q))
                                    kb.I("pool", "tensor_scalar", [ktok, bege], [q["kbg"]], out=q["kbg"][:], in0=ktok[:, t, :], scalar1=col(bege, q),
                                         scalar2=None, op0=ALU.mult)
                                for q in chains:
                                    TT = q["MNA"][cur][:, 2, :]
                                    kb.I("pe", "matmul", [q["MNA"][cur], q["vb"]], [q["X"]], q["X"][:, 0, :], lhsT=TT, rhs=q["vb"][:], start=True, stop=True, inc=False)
                                    kb.I("pe", "matmul", [q["kbg"], q["MNA"][cur]], [q["X"]], q["X"][:, 1, :], lhsT=q["kbg"][:], rhs=TT, start=True, stop=True)
                                for q in chains:
                                    j, t = q["j"], q["t"]
                                    kb.copy("act", [q["X"]], [us_all[j]], us_all[j][:, t, :], q["X"][:, 0, :])
                                    kb.copy("dve", [q["X"]], [wT_all[j]], wT_all[j][:, t, :], q["X"][:, 1, :])
                                for _ in gen2:
                                    pass
                                gen2 = phase2(tl)
                            for _ in gen2:
                                pass
                            if stop == 4:
                                return
                            with kb.scope():
                                sqo = us_all[0]
                                ssq = kb.sb("ssq", [128, NT])
                                onb = wT_all[0]
                                tpo = [TPb, TPb]
                                ti = 0
                                for j in range(2):
                                    kb.I("pool", "tensor_tensor", [otok[j]], [sqo], out=sqo[:], in0=otok[j][:], in1=otok[j][:], op=ALU.mult)
                                    kb.I("dve", "tensor_reduce", [sqo], [ssq], out=ssq[:], in_=sqo[:], axis=AX.X, op=ALU.add)
                                    kb.I("dve", "tensor_scalar", [ssq], [ssq], out=ssq[:], in0=ssq[:], scalar1=1.0 / 128, scalar2=1e-6, op0=ALU.mult, op1=ALU.add)
                                    kb.I("act", "sqrt", [ssq], [ssq], out=ssq[:], in_=ssq[:])
                                    kb.I("dve", "reciprocal", [ssq], [ssq], out=ssq[:], in_=ssq[:])
                                    kb.I("dve", "tensor_tensor", [otok[j], ssq], [onb], out=onb[:], in0=otok[j][:],
                                         in1=ssq[:].unsqueeze(2).to_broadcast([128, NT, 128]), op=ALU.mult)
                                    for j0 in range(0, NT, 8):
                                        j1 = min(j0 + 8, NT)
                                        tpp = tpo[ti % 2]
                                        ti += 1
                                        for jj in range(j0, j1):
                                            kb.I("pe", "transpose", [onb, c["identb"]], [tpp], out=tpp[:, jj - j0, :], in_=onb[:, jj, :], identity=c["identb"][:], inc=(jj == j1 - 1))
                                        kb.I("dve", "scalar_tensor_tensor", [tpp, onorm, zsT], [oTk], out=oTk[:, j, j0 * 128:j1 * 128],
                                             in0=tpp[:, 0:j1 - j0, :].rearrange("p a b -> p (a b)"), scalar=onorm[:, 0:1], in1=zsT[:, j, j0 * 128:j1 * 128],
                                             op0=ALU.mult, op1=ALU.mult)
                                for j in range(2):
                                    kb.dma("sp", o_d[s, 2 * kh + j, :, :], oTk[:, j, :])
                if stop == 5:
                    return
                with kb.scope():
                    woutb = kb.sb("gwoutb", [128, 16, D], BF16)
                    stg = [kb.sb("gostg%d" % i, [128, D]) for i in range(2)]
                    for k in range(16):
                        kb.dma("sp", stg[k % 2][:], wout_d[k * 128:(k + 1) * 128, :])
                        kb.copy("act" if k % 2 else "pool", [stg[k % 2]], [woutb], woutb[:, k, :], stg[k % 2][:])
                    oTt = [kb.sb("oTt%d" % i, [128, 16, 128], BF16) for i in range(2)]
                    hts = [kb.sb("goht%d" % i, [128, D]) for i in range(2)]
                    acc = [kb.sb("goacc%d" % i, [128, D]) for i in range(2)]
                    outt = [kb.sb("goout%d" % i, [128, D]) for i in range(2)]
                    st6 = kb.sb("st6", [128, 2, 6])
                    mv = kb.sb("mv", [128, 4])
                    ops2 = [kb.ps("gopp%d" % i, [128, 512]) for i in range(4)]
                    def fetch_tile(t_):
                        kb.dma("sp", hts[t_ % 2][:], h_d[r0 + t_ * 128: r0 + (t_ + 1) * 128, :])
                        for hq in range(4):
                            kb.dma("sp", oTt[t_ % 2][:, hq * 4:(hq + 1) * 4, :],
                                   o_d[s, hq * 4:(hq + 1) * 4, :, t_ * 128:(t_ + 1) * 128].rearrange("h p t -> p h t"),
                                   writes=[oTt[t_ % 2]], group="oTt%d" % (t_ % 2))
                    fetch_tile(0)
                    for t in range(NT):
                        ht = hts[t % 2]
                        a = acc[t % 2]
                        ot = oTt[t % 2]
                        if t + 1 < NT:
                            fetch_tile(t + 1)
                        for hf in range(2):
                            p = ops2[(2 * t + hf) % 4]
                            for k in range(16):
                                kb.I("pe", "matmul", [ot, woutb], [p], p[:], lhsT=ot[:, k, :], rhs=woutb[:, k, hf * 512:(hf + 1) * 512],
                                     start=(k == 0), stop=(k == 15), inc=(k == 15))
                            kb.I("dve", "scalar_tensor_tensor", [ht, p], [a], out=a[:, hf * 512:(hf + 1) * 512], in0=ht[:, hf * 512:(hf + 1) * 512],
                                 scalar=ALPHA, in1=p[:], op0=ALU.mult, op1=ALU.add)
                        layer_norm_tile(kb, a, g_bc, b_bc, outt[t % 2], st6, mv)
                        kb.dma("sp", out_d[r0 + t * 128: r0 + (t + 1) * 128, :], outt[t % 2][:])


def prep_gdn(inp, i):
    f = lambda a: np.ascontiguousarray(a, dtype=np.float32)
    w = inp["odd_w_in"][i]
    cwt = inp["odd_conv_w"][i]
    gw = np.zeros((8, 1024, 768), np.float32)
    gconv = np.zeros((128, 8, 4, 4), np.float32)
    for kh in range(8):
        cols = [np.arange(kh * 128, kh * 128 + 128), 1024 + np.arange(kh * 128, kh * 128 + 128),
                2048 + np.arange(2 * kh * 128, 2 * kh * 128 + 256)]
        zc = 4096 + np.arange(2 * kh * 128, 2 * kh * 128 + 256)
        gw[kh] = w[:, np.concatenate(cols + [zc])]
        cc_ = np.concatenate(cols)
        for fi in range(4):
            gconv[:, kh, fi, :] = cwt[:, cc_[fi * 128:(fi + 1) * 128]].T
    return {
        "gd_w": gw, "gd_wab": f(w[:, 6144:6176]), "gd_conv": gconv.reshape(128, 128).reshape(128, 8, 4, 4),
        "gd_alog": f(inp["odd_a_log"][i]), "gd_dtb": f(inp["odd_dt_bias"][i]), "gd_onorm": f(inp["odd_o_norm"][i]),
        "gd_wout": f(inp["odd_w_out"][i]), "gd_lng": f(inp["ln_g"][2 * i + 1, 0]), "gd_lnb": f(inp["ln_b"][2 * i + 1, 0]),
    }


def perm_expert(w, nk):
    E, R, F_ = w.shape
    return np.ascontiguousarray(w.reshape(E, nk, 128, F_).transpose(0, 2, 1, 3).reshape(E * 128, nk * F_), dtype=np.float32)


def prep_moe(inp, layer, pfx):
    f = lambda a: np.ascontiguousarray(a, dtype=np.float32)
    return {
        pfx + "wr": f(np.concatenate([inp["moe_group_w"][layer], inp["moe_expert_w"][layer]], axis=1)),
        pfx + "br": f(np.concatenate([inp["moe_group_b"][layer], inp["moe_expert_b"][layer]], axis=0)),
        pfx + "wg": perm_expert(inp["moe_w_gate"][layer], 8), pfx + "wu": perm_expert(inp["moe_w_up"][layer], 8),
        pfx + "wd": perm_expert(inp["moe_w_down"][layer], 4),
        pfx + "lng": f(inp["ln_g"][layer, 1]), pfx + "lnb": f(inp["ln_b"][layer, 1]),
    }


def build_program(shapes):
    nc = bass.Bass("TRN2", target_bir_lowering=False)
    kb = KB(nc)
    dd = {}
    for n, (shp, dt) in shapes.items():
        dd[n] = nc.dram_tensor(n, list(shp), dt, kind="ExternalInput").ap()
    out_d = nc.dram_tensor("out", [NTOK, D], F32, kind="ExternalOutput").ap()
    h1_d = kb.dram("h1", [NTOK, D])
    h2_d = kb.dram("h2", [NTOK, D])
    h3_d = kb.dram("h3", [NTOK, D])
    xs_d = kb.dram("xs", [NSLOT, D], BF16)
    ys_d = kb.dram("ys", [NSLOT, D], F32)
    o_d = kb.dram("o_scr", [SPC, 16, 128, TP], BF16)
    c = load_consts(kb, {k[2:]: v for k, v in dd.items() if k.startswith("c_")})
    build_even(kb, c, dd["h0"], dd["ev_win"], dd["ev_wout"], dd["ev_wuk"], dd["ev_wuv"], dd["ev_rgw"], dd["ev_vec"],
               dd["ev_lng"], dd["ev_lnb"], h1_d)
    build_moe(kb, c, h1_d, dd["m0_wr"], dd["m0_br"], dd["m0_wg"], dd["m0_wu"], dd["m0_wd"], dd["m0_lng"], dd["m0_lnb"],
              h2_d, xs_d, ys_d)
    build_gdn(kb, c, h2_d, dd["gd_w"], dd["gd_wab"], dd["gd_conv"], dd["gd_alog"], dd["gd_dtb"], dd["gd_onorm"],
              dd["gd_wout"], dd["gd_lng"], dd["gd_lnb"], h3_d, o_d)
    build_moe(kb, c, h3_d, dd["m1_wr"], dd["m1_br"], dd["m1_wg"], dd["m1_wu"], dd["m1_wd"], dd["m1_lng"], dd["m1_lnb"],
              out_d, xs_d, ys_d)
    kb.finish()
    return nc


def kernel(**inputs):
    import ml_dtypes
    inp = {k: np.asarray(v) for k, v in inputs.items()}
    x = inp["x"].astype(np.float32, copy=False)
    B = x.shape[0]
    hp = np.zeros((B, TP, D), np.float32)
    hp[:, :NMETA] = inp["meta_tokens"][None]
    hp[:, NMETA:T] = x
    shared = {}
    for k, v in make_consts().items():
        shared["c_" + k] = v
    shared.update(prep_even(inp, 0))
    shared.update(prep_gdn(inp, 0))
    shared.update(prep_moe(inp, 0, "m0_"))
    shared.update(prep_moe(inp, 1, "m1_"))
    shapes = {n: (v.shape, BF16 if v.dtype == ml_dtypes.bfloat16 else F32) for n, v in shared.items()}
    shapes["h0"] = ((NTOK, D), F32)
    nc = build_program(shapes)
    in_maps = []
    for ci in range(NCORES):
        m = dict(shared)
        m["h0"] = np.ascontiguousarray(hp[ci * SPC:(ci + 1) * SPC].reshape(NTOK, D))
        in_maps.append(m)
    res = run_bass_kernel_spmd(nc, in_maps, core_ids=list(range(NCORES)))
    outs = [r["out"].reshape(SPC, TP, D)[:, NMETA:T] for r in res.results]
    return np.ascontiguousarray(np.concatenate(outs, axis=0).astype(np.float32))
```
